# Optimizing a Trainium2 kernel written in Bass

```python
import math
import jax, jax.numpy as jnp
from jax import lax
import numpy as np


D_MODEL = 1024
BATCH = 2
SEQ = 8192
DEPTH = 2

HEAD_DIM = 64
RET_DIM = D_MODEL // 4
RET_HEADS = RET_DIM // HEAD_DIM
RET_CHUNK = 128
SSD_DIM = D_MODEL // 2
SSD_HEADS = SSD_DIM // HEAD_DIM
SSD_GROUPS = 2
SSD_STATE = 64
SSD_CONV = 4
SSD_CHUNK = 128
SSD_CONV_DIM = SSD_DIM + 2 * SSD_GROUPS * SSD_STATE
SWA_DIM = D_MODEL // 4
SWA_HEADS = SWA_DIM // HEAD_DIM
SWA_KV_HEADS = 2
SWA_KV_DIM = SWA_KV_HEADS * HEAD_DIM
WINDOW = 128
REL_BUCKETS = 32
REL_MAX_DIST = WINDOW
MIX_DIM = RET_DIM + SSD_DIM + SWA_DIM
IN_SIZES = (RET_DIM, RET_DIM, RET_DIM, RET_DIM,
            SSD_DIM, SSD_CONV_DIM, SSD_HEADS,
            SWA_DIM, SWA_KV_DIM, SWA_KV_DIM)
IN_DIM = sum(IN_SIZES)
D_FF = 2816
N_EXPERTS = 8
TOP_K = 2
D_FF_EXPERT = 3584
N_DENSE = (DEPTH + 1) // 2
N_MOE = DEPTH // 2
DEEPNORM_ALPHA = (2 * DEPTH) ** 0.25
DEEPNORM_BETA = (8 * DEPTH) ** -0.25
LN_EPS = 1e-5

kernel_name = 'hybrid_ret_ssd_swa_moe_deepnorm_adaln'


def _layer_norm(x, g, b):
    xf = x.astype(jnp.float32)
    mu = jnp.mean(xf, -1, keepdims=True)
    var = jnp.mean(jnp.square(xf - mu), -1, keepdims=True)
    return ((xf - mu) * lax.rsqrt(var + LN_EPS) * g + b).astype(x.dtype)


def _rotary(t, pos):
    half = t.shape[-1] // 2
    inv = jnp.exp(-math.log(10000.0) * jnp.arange(half, dtype=jnp.float32) / half)
    ang = pos.astype(jnp.float32)[..., None] * inv
    cos = jnp.cos(ang)[:, :, None, :]
    sin = jnp.sin(ang)[:, :, None, :]
    t1, t2 = t[..., :half], t[..., half:]
    return jnp.concatenate([t1 * cos - t2 * sin, t1 * sin + t2 * cos], -1).astype(t.dtype)


def _retention(q, k, v, g, pos):
    Bn, L, _ = q.shape
    H, d, C = RET_HEADS, HEAD_DIM, RET_CHUNK
    N = L // C
    dt = q.dtype
    q = _rotary(q.reshape(Bn, L, H, d), pos)
    k = _rotary(k.reshape(Bn, L, H, d), pos) * (d ** -0.5)
    v = v.reshape(Bn, L, H, d)
    log_gamma = jnp.log(1.0 - 2.0 ** (-5.0 - jnp.arange(H, dtype=jnp.float32)))
    idx = jnp.arange(C, dtype=jnp.float32)
    diff = idx[:, None] - idx[None, :]
    decay_in = jnp.where(diff >= 0, jnp.exp(log_gamma[:, None, None] * jnp.maximum(diff, 0.0)), 0.0).astype(dt)
    decay_q = jnp.exp(log_gamma[:, None] * (idx + 1.0)).astype(dt)
    decay_k = jnp.exp(log_gamma[:, None] * (C - 1.0 - idx)).astype(dt)
    decay_chunk = jnp.exp(log_gamma * C).astype(dt)
    qc = q.reshape(Bn, N, C, H, d)
    kc = k.reshape(Bn, N, C, H, d)
    vc = v.reshape(Bn, N, C, H, d)
    scores = jnp.einsum('bnihd,bnjhd->bnhij', qc, kc) * decay_in
    inner = jnp.einsum('bnhij,bnjhe->bnihe', scores, vc)
    chunk_kv = jnp.einsum('bnjhd,hj,bnjhe->nbhde', kc, decay_k, vc)

    def step(state, kv):
        return decay_chunk[None, :, None, None] * state + kv, state

    _, prev = lax.scan(step, jnp.zeros(chunk_kv.shape[1:], chunk_kv.dtype), chunk_kv)
    cross = jnp.einsum('bnihd,hi,nbhde->bnihe', qc, decay_q, prev)
    o = (inner + cross).reshape(Bn, L, H, d).astype(jnp.float32)
    mu = jnp.mean(o, -1, keepdims=True)
    var = jnp.mean(jnp.square(o - mu), -1, keepdims=True)
    o = ((o - mu) * lax.rsqrt(var + LN_EPS)).astype(dt).reshape(Bn, L, RET_DIM)
    return jax.nn.silu(g) * o


def _ssd(z, xbc, dt_raw, conv_w, conv_b, dt_bias, a_log, d_skip, norm_w):
    Bn, L, _ = z.shape
    G, R, P, NS, C = SSD_GROUPS, SSD_HEADS // SSD_GROUPS, HEAD_DIM, SSD_STATE, SSD_CHUNK
    NC = L // C
    xbc = lax.conv_general_dilated(xbc, conv_w[:, None, :], window_strides=(1,),
                                   padding=[(SSD_CONV - 1, 0)],
                                   dimension_numbers=('NWC', 'WIO', 'NWC'),
                                   feature_group_count=SSD_CONV_DIM) + conv_b
    xbc = jax.nn.silu(xbc)
    xs, Bm, Cm = jnp.split(xbc, [SSD_DIM, SSD_DIM + G * NS], -1)
    dt = jax.nn.softplus(dt_raw.astype(jnp.float32) + dt_bias)
    a = -jnp.exp(a_log.astype(jnp.float32)) * dt
    x = xs.reshape(Bn, NC, C, G, R, P)
    xdt = x * dt.reshape(Bn, NC, C, G, R, 1).astype(x.dtype)
    Bm = Bm.reshape(Bn, NC, C, G, NS)
    Cm = Cm.reshape(Bn, NC, C, G, NS)
    a_cs = jnp.cumsum(a.reshape(Bn, NC, C, G, R), axis=2).transpose(0, 1, 3, 4, 2)
    causal = jnp.tril(jnp.ones((C, C), dtype=bool))
    seg = a_cs[..., :, None] - a_cs[..., None, :]
    lmat = jnp.exp(jnp.where(causal, seg, -jnp.inf))
    cb = jnp.einsum('bclgn,bcsgn->bcgls', Cm, Bm)
    y_diag = jnp.einsum('bcgls,bcgrls,bcsgrp->bclgrp', cb, lmat, xdt)
    decay_states = jnp.exp(a_cs[..., -1:] - a_cs)
    states = jnp.einsum('bclgn,bcgrl,bclgrp->cbgrpn', Bm, decay_states, xdt).astype(jnp.float32)
    chunk_decay = jnp.exp(a_cs[..., -1]).transpose(1, 0, 2, 3)

    def step(S, inp):
        dec, st = inp
        return dec[..., None, None] * S + st, S

    _, prev = lax.scan(step, jnp.zeros(states.shape[1:], jnp.float32), (chunk_decay, states))
    y_off = jnp.einsum('bclgn,cbgrpn,bcgrl->bclgrp', Cm, prev, jnp.exp(a_cs))
    y = y_diag + y_off + x * d_skip.reshape(G, R, 1)
    y = y.reshape(Bn, L, SSD_DIM).astype(z.dtype)
    h = (y * jax.nn.silu(z)).astype(jnp.float32).reshape(Bn, L, G, SSD_DIM // G)
    h = h * lax.rsqrt(jnp.mean(jnp.square(h), -1, keepdims=True) + LN_EPS)
    return (h.reshape(Bn, L, SSD_DIM) * norm_w).astype(z.dtype)


def _t5_bucket(dist):
    exact = REL_BUCKETS // 2
    df = jnp.maximum(dist, 1).astype(jnp.float32)
    large = exact + (jnp.log(df / exact) / math.log(REL_MAX_DIST / exact) * (REL_BUCKETS - exact)).astype(jnp.int32)
    large = jnp.minimum(large, REL_BUCKETS - 1)
    return jnp.where(dist < exact, dist, large)


def _swa(q, k, v, rel_bias, sinks):
    Bn, L, _ = q.shape
    W, HK, GQ, d = WINDOW, SWA_KV_HEADS, SWA_HEADS // SWA_KV_HEADS, HEAD_DIM
    NB = L // W
    qb = q.reshape(Bn, NB, W, HK, GQ, d)
    kb = k.reshape(Bn, NB, W, HK, d)
    vb = v.reshape(Bn, NB, W, HK, d)

    def band(t):
        prev = jnp.concatenate([jnp.zeros_like(t[:, :1]), t[:, :-1]], axis=1)
        return jnp.concatenate([prev, t], axis=2)

    kband, vband = band(kb), band(vb)
    qi = jnp.arange(W)[:, None]
    kj = jnp.arange(2 * W)[None, :]
    dist = qi + W - kj
    in_band = (dist >= 0) & (dist < W)
    kpos = jnp.arange(NB)[:, None, None] * W - W + kj[None]
    valid = in_band[None] & (kpos >= 0)
    bias = rel_bias[_t5_bucket(jnp.clip(dist, 0, W - 1))]
    bias = bias.transpose(2, 0, 1).reshape(HK, GQ, W, 2 * W).astype(jnp.float32)
    logits = jnp.einsum('bnikgd,bnjkd->bnkgij', qb, kband).astype(jnp.float32) * (d ** -0.5) + bias
    logits = jnp.where(valid[None, :, None, None], logits, -jnp.inf)
    sink = sinks.astype(jnp.float32).reshape(HK, GQ, 1)
    m = jnp.maximum(jnp.max(logits, -1), sink)
    p = jnp.exp(logits - m[..., None])
    denom = jnp.sum(p, -1) + jnp.exp(sink - m)
    o = jnp.einsum('bnkgij,bnjkd->bnikgd', (p / denom[..., None]).astype(v.dtype), vband)
    return o.reshape(Bn, L, SWA_DIM)


def _swiglu(h, wg, wu, wd):
    return (jax.nn.silu(h @ wg) * (h @ wu)) @ wd


def _moe(h, w_router, b_router, wg, wu, wd):
    Bn, L, D = h.shape
    t = h.reshape(-1, D)
    logits = (t @ w_router).astype(jnp.float32) + b_router
    top_v, top_i = lax.top_k(logits, TOP_K)
    top_w = jax.nn.softmax(top_v, -1)
    gates = jnp.sum(jax.nn.one_hot(top_i, N_EXPERTS, dtype=jnp.float32) * top_w[..., None], axis=1)
    out = jnp.zeros_like(t)
    for e in range(N_EXPERTS):
        out = out + gates[:, e:e + 1].astype(t.dtype) * _swiglu(t, wg[e], wu[e], wd[e])
    return out.reshape(Bn, L, D)


def setup_inputs(seed: int = 0) -> dict:
    key = jax.random.key(seed)
    ks = jax.random.split(key, 32)
    f32 = jnp.float32
    D = D_MODEL

    def nrm(k, shape, scale):
        return jax.random.normal(k, shape, f32) * scale

    x = nrm(ks[0], (BATCH, SEQ, D), 1.0)
    c = nrm(ks[1], (BATCH, D), 1.0)
    offsets = jax.random.randint(ks[2], (BATCH, 1), 0, 1024, dtype=jnp.int32)
    positions = offsets + jnp.arange(SEQ, dtype=jnp.int32)[None, :]
    rel_bias = nrm(ks[3], (REL_BUCKETS, SWA_HEADS), 0.5)
    w_ada = nrm(ks[4], (DEPTH, D, 6 * D), 0.1 * D ** -0.5)
    b_ada = nrm(ks[5], (DEPTH, 6 * D), 0.01)
    w_in = nrm(ks[6], (DEPTH, D, IN_DIM), D ** -0.5)
    w_out = nrm(ks[7], (DEPTH, MIX_DIM, D), DEEPNORM_BETA * MIX_DIM ** -0.5)
    conv_w = nrm(ks[8], (DEPTH, SSD_CONV, SSD_CONV_DIM), SSD_CONV ** -0.5)
    conv_b = nrm(ks[9], (DEPTH, SSD_CONV_DIM), 0.01)
    u = jax.random.uniform(ks[10], (DEPTH, SSD_HEADS), f32)
    dt0 = jnp.exp(u * (math.log(0.1) - math.log(0.001)) + math.log(0.001))
    dt_bias = dt0 + jnp.log(-jnp.expm1(-dt0))
    a_log = jnp.log(jax.random.uniform(ks[11], (DEPTH, SSD_HEADS), f32, 1.0, 16.0))
    d_skip = 1.0 + nrm(ks[12], (DEPTH, SSD_HEADS), 0.1)
    ssd_norm_w = 1.0 + nrm(ks[13], (DEPTH, SSD_DIM), 0.02)
    sinks = nrm(ks[14], (DEPTH, SWA_HEADS), 0.5)
    ln_g = 1.0 + nrm(ks[15], (DEPTH, 2, D), 0.02)
    ln_b = nrm(ks[16], (DEPTH, 2, D), 0.02)
    ffn_w_gate = nrm(ks[17], (N_DENSE, D, D_FF), D ** -0.5)
    ffn_w_up = nrm(ks[18], (N_DENSE, D, D_FF), D ** -0.5)
    ffn_w_down = nrm(ks[19], (N_DENSE, D_FF, D), DEEPNORM_BETA * D_FF ** -0.5)
    router_w = nrm(ks[20], (N_MOE, D, N_EXPERTS), D ** -0.5)
    router_b = nrm(ks[21], (N_MOE, N_EXPERTS), 0.01)
    expert_w_gate = nrm(ks[22], (N_MOE, N_EXPERTS, D, D_FF_EXPERT), D ** -0.5)
    expert_w_up = nrm(ks[23], (N_MOE, N_EXPERTS, D, D_FF_EXPERT), D ** -0.5)
    expert_w_down = nrm(ks[24], (N_MOE, N_EXPERTS, D_FF_EXPERT, D), DEEPNORM_BETA * D_FF_EXPERT ** -0.5)
    return {'x': x, 'c': c, 'positions': positions, 'rel_bias': rel_bias,
            'w_ada': w_ada, 'b_ada': b_ada, 'w_in': w_in, 'w_out': w_out,
            'conv_w': conv_w, 'conv_b': conv_b, 'dt_bias': dt_bias, 'a_log': a_log,
            'd_skip': d_skip, 'ssd_norm_w': ssd_norm_w, 'sinks': sinks,
            'ln_g': ln_g, 'ln_b': ln_b,
            'ffn_w_gate': ffn_w_gate, 'ffn_w_up': ffn_w_up, 'ffn_w_down': ffn_w_down,
            'router_w': router_w, 'router_b': router_b,
            'expert_w_gate': expert_w_gate, 'expert_w_up': expert_w_up, 'expert_w_down': expert_w_down}


def reference(x, c, positions, rel_bias, w_ada, b_ada, w_in, w_out, conv_w, conv_b,
              dt_bias, a_log, d_skip, ssd_norm_w, sinks, ln_g, ln_b,
              ffn_w_gate, ffn_w_up, ffn_w_down, router_w, router_b,
              expert_w_gate, expert_w_up, expert_w_down):
    split_idx = np.cumsum(IN_SIZES)[:-1].tolist()
    for layer in range(DEPTH):
        mod = c @ w_ada[layer] + b_ada[layer]
        sh_a, sc_a, g_a, sh_f, sc_f, g_f = [m[:, None, :] for m in jnp.split(mod, 6, -1)]
        h = x * (1.0 + sc_a) + sh_a
        u = h @ w_in[layer]
        rq, rk, rv, rg, sz, sxbc, sdt, aq, ak, av = jnp.split(u, split_idx, -1)
        y_ret = _retention(rq, rk, rv, rg, positions)
        y_ssd = _ssd(sz, sxbc, sdt, conv_w[layer], conv_b[layer], dt_bias[layer],
                     a_log[layer], d_skip[layer], ssd_norm_w[layer])
        y_swa = _swa(aq, ak, av, rel_bias, sinks[layer])
        mix = jnp.concatenate([y_ret, y_ssd, y_swa], -1) @ w_out[layer]
        x = _layer_norm(DEEPNORM_ALPHA * x + (1.0 + g_a) * mix, ln_g[layer, 0], ln_b[layer, 0])
        h = x * (1.0 + sc_f) + sh_f
        if layer % 2 == 0:
            i = layer // 2
            f = _swiglu(h, ffn_w_gate[i], ffn_w_up[i], ffn_w_down[i])
        else:
            i = layer // 2
            f = _moe(h, router_w[i], router_b[i], expert_w_gate[i], expert_w_up[i], expert_w_down[i])
        x = _layer_norm(DEEPNORM_ALPHA * x + (1.0 + g_f) * f, ln_g[layer, 1], ln_b[layer, 1])
    return x
```

```python
import math
import contextlib
import numpy as np
import concourse.bass as bass
import concourse.mybir as mybir
from concourse.bass_utils import run_bass_kernel_spmd

F32 = mybir.dt.float32
BF16 = mybir.dt.bfloat16
I32 = mybir.dt.int32
ALU = mybir.AluOpType
AF = mybir.ActivationFunctionType
AX = mybir.AxisListType

D = 1024
DEPTH = 2
NTOK = 2048
NCH = 16
IN_DIM = 2824
D_FF = 2816
NEXP = 8
D_FFE = 3584
ALPHA = (2 * DEPTH) ** 0.25
EPS = 1e-5
NEG = -30000.0
STW = 392

ENGS = ("pe", "act", "dve", "pool", "sp")
ND = 12
SAME_ENGINE_SYNC = True


SEM_CAP = 3000
CHAIN_EPOCHS = 3


class Prog:
    def __init__(self):
        self.nc = bass.Bass("TRN2", target_bir_lowering=False)
        self.ops = {e: [] for e in ENGS}
        self.lastw = {}
        self.readers = {}
        self.seen = {e: {} for e in ENGS}
        self.dma_cnt = [0] * (ND + 1)
        self.dma_last_tok = [None] * (ND + 1)
        self.dma_next = 0
        self.out_tokens = []
        self.pending = {e: [] for e in ENGS}
        self.st = contextlib.ExitStack()
        self.chain = {e: {"last": None, "count": 0, "sems": None, "epoch": 0} for e in ("act", "dve", "pool")}

    def sbuf(self, name, shape, dtype):
        return self.st.enter_context(self.nc.sbuf_tensor(name, list(shape), dtype))

    def psum(self, name, shape, dtype):
        return self.st.enter_context(self.nc.psum_tensor(name, list(shape), dtype))

    def barrier(self):
        toks = []
        for e in ENGS:
            for i in range(len(self.ops[e]) - 1, -1, -1):
                if self.ops[e][i]["dma"] is None:
                    toks.append(("e", e, i))
                    break
        for t in self.dma_last_tok:
            if t is not None:
                toks.append(t)
        for e in ENGS:
            self.pending[e] = list(toks)

    def _need(self, eng, tok, waits):
        if tok is None:
            return
        if tok[0] == "e":
            _, src, seq = tok
            if src == eng and (not SAME_ENGINE_SYNC or eng == "pe"):
                return
            if self.seen[eng].get(src, -1) >= seq:
                return
            cur = waits.get(src)
            if cur is None or cur[2] < seq:
                waits[src] = tok
        else:
            _, idx, cnt = tok
            key = ("d", idx)
            if self.seen[eng].get(key, -1) >= cnt:
                return
            cur = waits.get(key)
            if cur is None or cur[2] < cnt:
                waits[key] = tok

    def _deps(self, eng, reads, writes):
        waits = {}
        for k in reads:
            self._need(eng, self.lastw.get(k), waits)
        for k in writes:
            self._need(eng, self.lastw.get(k), waits)
            for tok in self.readers.get(k, {}).values():
                self._need(eng, tok, waits)
        if self.pending[eng]:
            for tok in self.pending[eng]:
                self._need(eng, tok, waits)
            self.pending[eng] = []
        for key, tok in waits.items():
            self.seen[eng][key] = tok[2]
            if tok[0] == "e":
                self.ops[tok[1]][tok[2]]["sig"] = True
        return list(waits.values())

    def _commit(self, tok, reads, writes, rkey):
        for k in writes:
            self.lastw[k] = tok
            self.readers[k] = {}
        for k in reads:
            self.readers.setdefault(k, {})[rkey] = tok

    def op(self, eng, emit, R=(), W=()):
        waits = self._deps(eng, R, W)
        seq = len(self.ops[eng])
        self.ops[eng].append({"waits": waits, "emit": emit, "sig": False, "dma": None})
        self._commit(("e", eng, seq), R, W, eng)

    def dma(self, eng, out, in_, R=(), W=(), is_output=False, **kw):
        if "_coll" in kw:
            idx = ND
        else:
            idx = self.dma_next
            self.dma_next = (self.dma_next + 1) % ND
        waits = self._deps(eng, R, W)
        prev = self.dma_last_tok[idx]
        if prev is not None:
            w = {}
            self._need(eng, prev, w)
            for key, tok in w.items():
                self.seen[eng][key] = tok[2]
                waits.append(tok)
        self.dma_cnt[idx] += (1 if idx == ND else 16)
        tok = ("d", idx, self.dma_cnt[idx])
        self.dma_last_tok[idx] = tok
        self.ops[eng].append({"waits": waits, "emit": None, "sig": False,
                              "dma": (out, in_, idx, kw)})
        self._commit(tok, R, W, ("d", idx))
        if is_output:
            self.out_tokens.append(tok)
        return tok

    def coll(self, kind, out, in_, groups, R=(), W=()):
        import os
        if os.environ.get("NOCOLL"):
            return self.dma("sp", out[0:128, :], in_, R=R, W=W)
        return self.dma("pool", out, in_, R=R, W=W, _coll=(kind, groups))

    def emit(self):
        nc = self.nc
        fin = {}
        for tok in self.out_tokens:
            self._need("sp", tok, fin)
        fin_waits = list(fin.values())
        pref = {}
        for e in ENGS:
            c = 0
            arr = []
            for o in self.ops[e]:
                if o["sig"]:
                    c += 1
                arr.append(c)
            pref[e] = arr
        with self.st as st:
            esem = {e: [st.enter_context(nc.semaphore("s_%s%d" % (e, k_))) for k_ in range(max(1, -(-(pref[e][-1] if pref[e] else 0) // SEM_CAP)))]
                    for e in ENGS}
            dsem = [st.enter_context(nc.semaphore("d%d" % i)) for i in range(ND + 1)]
            for e in self.chain:
                self.chain[e]["sems"] = [st.enter_context(nc.semaphore("c_%s%d" % (e, k_))) for k_ in range(CHAIN_EPOCHS)]
            block = st.enter_context(nc.Block())

            def do_wait(E, tok):
                if tok[0] == "e":
                    c_ = pref[tok[1]][tok[2]]
                    E.wait_ge(esem[tok[1]][(c_ - 1) // SEM_CAP], (c_ - 1) % SEM_CAP + 1)
                else:
                    E.wait_ge(dsem[tok[1]], tok[2])

            def run(e, E):
                for oi, o in enumerate(self.ops[e]):
                    for tok in o["waits"]:
                        do_wait(E, tok)
                    if o["dma"] is not None:
                        out, in_, idx, kw = o["dma"]
                        if "_coll" in kw:
                            kind, groups = kw["_coll"]
                            nc.gpsimd.collective_compute(kind, ALU.bypass, replica_groups=groups,
                                                         ins=[in_], outs=[out]).then_inc(dsem[idx], 1)
                        else:
                            E.dma_start(out=out, in_=in_, **kw).then_inc(dsem[idx], 16)
                    else:
                        if e in self.chain:
                            self.chain[e]["last"] = None
                        ins = o["emit"]()
                        if o["sig"]:
                            c_ = pref[e][oi]
                            ins.then_inc(esem[e][(c_ - 1) // SEM_CAP], 1)
                if e == "sp":
                    for tok in fin_waits:
                        do_wait(E, tok)

            @block.tensor
            def _(E):
                run("pe", E)

            @block.scalar
            def _(E):
                run("act", E)

            @block.vector
            def _(E):
                run("dve", E)

            @block.gpsimd
            def _(E):
                run("pool", E)

            @block.sync
            def _(E):
                run("sp", E)
        return nc


class EngProxy:
    def __init__(self, prog, name, eng):
        self._p, self._n, self._e = prog, name, eng

    def __getattr__(self, attr):
        fn = getattr(self._e, attr)
        if attr in ("wait_ge", "dma_start"):
            return fn
        st = self._p.chain[self._n]

        def w(*a, **k):
            if st["last"] is not None:
                if st["count"] >= SEM_CAP:
                    st["epoch"] += 1
                    st["count"] = 0
                sem_ = st["sems"][st["epoch"]]
                st["last"].then_inc(sem_, 1)
                st["count"] += 1
                self._e.wait_ge(sem_, st["count"])
            ins = fn(*a, **k)
            st["last"] = ins
            return ins
        return w


class Arena:
    def __init__(self, t, nwords):
        self.t = t
        self.n = nwords
        self.off = 0

    def alloc(self, shape, dtype=F32):
        n = 1
        for s in shape[1:]:
            n *= s
        words = n if dtype in (F32, I32) else (n + 1) // 2
        assert self.off + words <= self.n, ("arena overflow", self.off, words, self.n)
        v = self.t[:, self.off:self.off + words]
        self.off += words
        if dtype == BF16:
            v = v.bitcast(BF16)[:, 0:n]
        elif dtype == I32:
            v = v.bitcast(I32)
        if len(shape) > 2:
            names = ["a%d" % i for i in range(len(shape) - 1)]
            pat = "p (" + " ".join(names) + ") -> p " + " ".join(names)
            v = v.rearrange(pat, **{nm: s for nm, s in zip(names, shape[1:])})
        return v


def _bc(ap, shape):
    return ap.to_broadcast(list(shape))


STOP = [99]
DBG = set()
DBG_OUT = {}
_P1ONLY = [False]
SUB = [99]


def build(stage, L):
    P = Prog()
    nc = P.nc
    V, S, G, T = EngProxy(P, "dve", nc.vector), EngProxy(P, "act", nc.scalar), EngProxy(P, "pool", nc.gpsimd), nc.tensor
    full = stage in ("main", "ffn")
    ffn_only = stage == "ffn"
    moe = (L % 2 == 1)
    last = (L == DEPTH - 1)

    def din(name, shape, dt=F32):
        return nc.dram_tensor(name, list(shape), dt, kind="ExternalInput").ap()

    def dout(name, shape, dt=F32):
        return nc.dram_tensor(name, list(shape), dt, kind="ExternalOutput").ap()

    def dbg(name, ap, keys):
        if name not in DBG:
            return
        shp = list(ap.shape)
        d_ = dout("dbg_" + name, shp, ap.dtype)
        P.dma("sp", d_, ap, R=list(keys), is_output=True)

    x_d = din("xin", [NTOK + 128, D])
    pos_d = din("pos", [128, NCH], I32)
    cst_d = din("cst", [128, CW])
    misc_d = din("misc", [128, MW])
    rowp_d = din("rowp", [RW])
    w_in_d = din("w_in", [D, IN_DIM])
    if full:
        w_out_d = din("w_out", [D, D])
        eoh_d = din("eoh", [128, 256 * 32])
        st_in_d = din("st_in", [3, 128, STW])
        if moe:
            wg_d = din("wg", [NEXP, D, D_FFE])
            wu_d = din("wu", [NEXP, D, D_FFE])
            wd_d = din("wd", [NEXP, D_FFE, D])
            rw_d = din("rw", [128, 8, 8])
        else:
            wg_d = din("wg", [1, D, D_FF])
            wu_d = din("wu", [1, D, D_FF])
            wd_d = din("wd", [1, D_FF, D])
        xo_d = dout("xout", [NTOK, D])
    else:
        st_out_d = dout("st_out", [128, STW])

    xT = P.sbuf("xT", [128, 8, NTOK], F32)
    cst = P.sbuf("cst_sb", [128, CW], F32)
    misc = P.sbuf("misc_sb", [128, MW], F32)
    rowp = P.sbuf("rowp_sb", [128, RW], F32)
    ident_b = P.sbuf("ident_b", [128, 128], BF16)
    AW = 33700
    arena_t = P.sbuf("arena", [128, AW], F32)
    TAIL = AW - 2048
    cosT = arena_t[:, TAIL:TAIL + 512].rearrange("p (n f) -> p n f", f=32)
    sinT = arena_t[:, TAIL + 512:TAIL + 1024].rearrange("p (n f) -> p n f", f=32)
    biasw = arena_t[:, TAIL + 1024:TAIL + 2048].rearrange("p (h j) -> p h j", h=4)
    ps = [P.psum("ps%d" % i, [128, 512], F32) for i in range(8)]
    psk = ["ps%d" % i for i in range(8)]
    pctr = [0]

    def pb():
        i = pctr[0] % 8
        pctr[0] += 1
        return ps[i], psk[i]

    ident = cst[:, C_ID:C_ID + 128]
    tri = cst[:, C_TRI:C_TRI + 128]
    mgt = cst[:, C_MGT:C_MGT + 128]
    ones = cst[:, C_ONE:C_ONE + 128]
    dq = cst[:, C_DQ:C_DQ + 4]
    dk = cst[:, C_DK:C_DK + 4]
    gC = cst[:, C_GC:C_GC + 2]
    invf = cst[:, C_INV:C_INV + 32]
    madd = cst[:, C_MADD:C_MADD + 256]
    wret = cst[:, C_WRET:C_WRET + 6]
    m0 = cst[:, C_M0:C_M0 + 1]
    m1 = cst[:, C_M0 + 1:C_M0 + 2]
    sel8 = cst[:, C_SEL:C_SEL + 8 * 128]

    def mcol(o, n):
        return misc[:, o:o + n]
    A_in, B_in = mcol(M_AIN, 8), mcol(M_BIN, 8)
    g1a, g1f = mcol(M_G1A, 8), mcol(M_G1F, 8)
    convw = mcol(M_CW, 24)
    convb = mcol(M_CB, 6)
    halovalid = mcol(M_HV, 1)
    halomask = mcol(M_HM, 1)
    modT = mcol(M_MOD, 96)
    lncol = mcol(M_LN, 32)
    ccol = mcol(M_C, 8)
    badaT = mcol(M_BADA, 96)
    dcol = mcol(M_DER, 64)
    rbias = mcol(M_RB, 8)

    dtb = rowp[:, R_DTB:R_DTB + 8]
    alog = rowp[:, R_ALOG:R_ALOG + 8]
    dskip = rowp[:, R_DSK:R_DSK + 8]
    normw = rowp[:, R_NW:R_NW + 512]
    sinks = rowp[:, R_SINK:R_SINK + 4]
    relb = rowp[:, R_RB:R_RB + 128]

    sp = "sp"
    P.dma(sp, cst[:], cst_d, W=["cst"])
    P.dma(sp, misc[:], misc_d, W=["misc"])
    P.dma(sp, rowp[:], rowp_d.partition_broadcast(128), W=["rowp"])
    P.op("dve", lambda: V.tensor_copy(out=ident_b[:], in_=ident), R=["cst"], W=["ident_b"])

    ar = Arena(arena_t, TAIL)
    wada_d = din("w_ada", [1, D, 6 * D])
    wada_sb = [ar.alloc([128, 8, 512]) for _ in range(2)]
    bank_mod, kmod = pb()
    nl = 1
    j = 0
    for li in range(nl):
        for cg in range(12):
            buf = wada_sb[j % 2]
            bk = "wada%d" % (j % 2)
            P.dma(sp, buf, wada_d[li].rearrange("(c p) n -> p c n", p=128)[:, :, cg * 512:(cg + 1) * 512], W=[bk])
            for cc in range(4):
                col = li * 48 + cg * 4 + cc

                def mm(buf=buf, cc=cc, col=col):
                    r = None
                    for kc in range(8):
                        r = T.matmul(bank_mod[:, col:col + 1], lhsT=buf[:, kc, cc * 128:(cc + 1) * 128],
                                     rhs=ccol[:, kc:kc + 1], start=(kc == 0), stop=(kc == 7))
                    return r
                P.op("pe", mm, R=[bk, "misc"], W=[kmod])
            j += 1
    P.op("dve", lambda: V.tensor_tensor(out=modT[:, 0:48 * nl], in0=bank_mod[:, 0:48 * nl], in1=badaT[:, 0:48 * nl], op=ALU.add),
         R=[kmod, "misc"], W=["mod"])
    lg1, lb1, lg2, lb2 = lncol[:, 0:8], lncol[:, 8:16], lncol[:, 16:24], lncol[:, 24:32]
    GA1, BA1 = dcol[:, 0:8], dcol[:, 8:16]
    A2, B2 = dcol[:, 16:24], dcol[:, 24:32]
    GA2, BA2 = dcol[:, 32:40], dcol[:, 40:48]
    tmpc = dcol[:, 48:56]

    def der():
        V.tensor_scalar(out=A_in, in0=modT[:, 8:16], scalar1=1.0, scalar2=1.0 / ALPHA, op0=ALU.add, op1=ALU.mult)
        V.tensor_copy(out=B_in, in_=modT[:, 0:8])
        V.tensor_scalar(out=g1a, in0=modT[:, 16:24], scalar1=1.0, scalar2=None, op0=ALU.add)
        V.tensor_scalar(out=g1f, in0=modT[:, 40:48], scalar1=1.0, scalar2=None, op0=ALU.add)
        V.tensor_scalar(out=GA1, in0=lg1, scalar1=ALPHA, scalar2=None, op0=ALU.mult)
        V.tensor_scalar(out=BA1, in0=lb1, scalar1=ALPHA, scalar2=None, op0=ALU.mult)
        V.tensor_scalar(out=A2, in0=modT[:, 32:40], scalar1=1.0, scalar2=1.0 / ALPHA, op0=ALU.add, op1=ALU.mult)
        V.tensor_copy(out=B2, in_=modT[:, 24:32])
        sc = 1.0
        V.tensor_scalar(out=GA2, in0=lg2, scalar1=sc, scalar2=None, op0=ALU.mult)
        return V.tensor_scalar(out=BA2, in0=lb2, scalar1=sc, scalar2=None, op0=ALU.mult)
    P.op("dve", der, R=["mod", "misc"], W=["der"])
    P.barrier()

    ar = Arena(arena_t, TAIL)
    posi = ar.alloc([128, NCH], I32)
    posf = ar.alloc([128, NCH])
    ang = ar.alloc([128, NCH, 32])
    ang2 = ar.alloc([128, NCH, 32])
    ti = ar.alloc([128, NCH, 32], I32)
    tf = ar.alloc([128, NCH, 32])
    P.dma(sp, posi, pos_d, W=["posi"])

    def rot_tables():
        V.tensor_copy(out=posf, in_=posi)
        V.tensor_tensor(out=ang, in0=_bc(posf.unsqueeze(2), [128, NCH, 32]), in1=_bc(invf.unsqueeze(1), [128, NCH, 32]), op=ALU.mult)
        V.tensor_scalar(out=ang, in0=ang, scalar1=float(1.0 / (2 * np.pi)), scalar2=None, op0=ALU.mult)
        V.tensor_scalar(out=ang2, in0=ang, scalar1=0.25, scalar2=None, op0=ALU.add)
        r = None
        for a in (ang, ang2):
            V.tensor_copy(out=ti, in_=a)
            V.tensor_copy(out=tf, in_=ti)
            V.tensor_tensor(out=a, in0=a, in1=tf, op=ALU.subtract)
            V.tensor_scalar(out=tf, in0=a, scalar1=0.5, scalar2=None, op0=ALU.is_gt)
            V.tensor_tensor(out=a, in0=a, in1=tf, op=ALU.subtract)
            V.tensor_scalar(out=tf, in0=a, scalar1=-0.5, scalar2=None, op0=ALU.is_lt)
            r = V.tensor_tensor(out=a, in0=a, in1=tf, op=ALU.add)
        return r
    P.op("dve", rot_tables, R=["posi", "cst"], W=["ang"])

    def rot_sin():
        S.activation(out=sinT, in_=ang, func=AF.Sin, scale=float(2 * np.pi))
        return S.activation(out=cosT, in_=ang2, func=AF.Sin, scale=float(2 * np.pi))
    P.op("act", rot_sin, R=["ang"], W=["rot"])

    P.op("act", lambda: S.activation(out=alog, in_=alog, func=AF.Exp), R=["rowp"], W=["rowp"])
    P.op("dve", lambda: V.tensor_scalar(out=alog, in0=alog, scalar1=-1.0, scalar2=None, op0=ALU.mult), R=["rowp"], W=["rowp"])
    negA = alog
    dbg("rowp", rowp[:, 0:32], ["rowp"])
    dbg("modT", modT[:, 0:48], ["mod"])

    if full:
        eoh = ar.alloc([128, 256, 32])
        etmp = ar.alloc([128, 256, 32])
        P.dma(sp, eoh.rearrange("p a b -> p (a b)"), eoh_d, W=["eoh"])
        rb3 = relb.rearrange("p (b h) -> p b h", h=4)
        for h in range(4):
            P.op("pool", lambda h=h: G.tensor_tensor(out=etmp, in0=eoh, in1=_bc(rb3[:, :, h].unsqueeze(1), [128, 256, 32]), op=ALU.mult),
                 R=["eoh", "rowp"], W=["etmp"])

            def red(h=h):
                V.tensor_reduce(out=biasw[:, h, :], in_=etmp, axis=AX.X, op=ALU.add)
                return V.tensor_tensor(out=biasw[:, h, :], in0=biasw[:, h, :], in1=madd, op=ALU.add)
            P.op("dve", red, R=["etmp", "cst"], W=["biasw"])
    P.barrier()

    ar = Arena(arena_t, TAIL)
    hT_halo = ar.alloc([128, 8, 128], BF16)
    _mark = ar.off
    xtok = [ar.alloc([128, D]) for _ in range(2)]
    for n in range(-1, NCH):
        buf = xtok[n % 2]
        bk = "xtok%d" % (n % 2)
        P.dma(sp, buf, x_d[(n + 1) * 128:(n + 2) * 128, :], W=[bk])
        for half in range(2):
            bank, bkey = pb()

            def tr(buf=buf, half=half, bank=bank):
                r = None
                for q in range(4):
                    fc = half * 4 + q
                    r = T.transpose(bank[:, q * 128:(q + 1) * 128], buf[:, fc * 128:(fc + 1) * 128], ident)
                return r
            P.op("pe", tr, R=[bk, "cst"], W=[bkey])
            if n >= 0:
                P.op("act", lambda n=n, half=half, bank=bank: S.mul(out=xT[:, half * 4:half * 4 + 4, n * 128:(n + 1) * 128],
                                                                   in_=bank[:].rearrange("p (q t) -> p q t", q=4), mul=(1.0 if ffn_only else ALPHA)),
                     R=[bkey], W=[("xT", n, half)])
            else:
                def hh(half=half, bank=bank):
                    r = None
                    for q in range(4):
                        fc = half * 4 + q
                        r = V.tensor_scalar(out=hT_halo[:, fc, :], in0=bank[:, q * 128:(q + 1) * 128],
                                            scalar1=A_in[:, fc:fc + 1], scalar2=B_in[:, fc:fc + 1], op0=ALU.mult, op1=ALU.add)
                    return r
                def hh2(half=half, bank=bank):
                    r = None
                    for q in range(4):
                        fc = half * 4 + q
                        V.tensor_scalar(out=tmpc[:, 0:1], in0=A_in[:, fc:fc + 1], scalar1=ALPHA, scalar2=None, op0=ALU.mult)
                        r = V.tensor_scalar(out=hT_halo[:, fc, :], in0=bank[:, q * 128:(q + 1) * 128],
                                            scalar1=tmpc[:, 0:1], scalar2=B_in[:, fc:fc + 1], op0=ALU.mult, op1=ALU.add)
                    return r
                P.op("dve", hh2, R=[bkey, "misc", "der"], W=["hT_halo", "der"])

    P.barrier()
    ar.off = _mark
    w_in = ar.alloc([128, 8, IN_DIM], BF16)
    wcols = "w_in_d"
    wi_src = w_in_d.rearrange("(c p) n -> p c n", p=128)
    for a, b in ((0, 1024), (1024, 2048), (2048, 2312), (2568, 2824)):
        P.dma("pool", w_in[:, :, a:b], wi_src[:, :, a:b], W=["w_in"])
    for slot, h in enumerate((0, 2, 1, 3)):
        P.dma("pool", w_in[:, :, 2312 + slot * 64:2312 + (slot + 1) * 64], wi_src[:, :, 2312 + h * 64:2312 + (h + 1) * 64], W=["w_in"])
    if full:
        w_out = ar.alloc([128, 8, D], BF16)
        P.dma("pool", w_out, w_out_d.rearrange("(c p) n -> p c n", p=128), W=["w_out"])

    Sret = ar.alloc([128, 2, 64])
    Sret_b = ar.alloc([128, 2, 64], BF16)
    Sssd = ar.alloc([128, 256])
    Sssd_b = ar.alloc([128, 256], BF16)
    totacc = ar.alloc([128, 8])
    if full:
        stin = ar.alloc([128, 3, STW])
        P.dma(sp, stin, st_in_d.rearrange("s p w -> p s w"), W=["stin"])
        wss = ar.alloc([128, 3, 4])

        def comb():
            V.tensor_tensor(out=Sret, in0=stin[:, 0, 0:128].rearrange("p (t e) -> p t e", t=2),
                            in1=_bc(wret[:, 0:2].unsqueeze(2), [128, 2, 64]), op=ALU.mult)
            for s_ in (1, 2):
                V.tensor_tensor(out=Sssd[:, 0:128].rearrange("p (t e) -> p t e", t=2), in0=stin[:, s_, 0:128].rearrange("p (t e) -> p t e", t=2),
                                in1=_bc(wret[:, 2 * s_:2 * s_ + 2].unsqueeze(2), [128, 2, 64]), op=ALU.mult)
                V.tensor_tensor(out=Sret, in0=Sret, in1=Sssd[:, 0:128].rearrange("p (t e) -> p t e", t=2), op=ALU.add)
            for g in range(2):
                pr = slice(g * 64, (g + 1) * 64)
                V.tensor_copy(out=wss[pr, 1, :], in_=stin[pr, 0, 384 + g * 4:384 + g * 4 + 4])
                V.tensor_tensor(out=wss[pr, 2, :], in0=stin[pr, 0, 384 + g * 4:384 + g * 4 + 4],
                                in1=stin[pr, 1, 384 + g * 4:384 + g * 4 + 4], op=ALU.add)
            return V.memset(wss[:, 0, :], 0.0)
        P.op("dve", comb, R=["stin", "cst"], W=["Sret", "Sssd", "wss"])
        P.op("act", lambda: S.activation(out=wss, in_=wss, func=AF.Exp), R=["wss"], W=["wss"])

        def comb2():
            V.tensor_tensor(out=Sssd.rearrange("p (r e) -> p r e", r=4), in0=stin[:, 0, 128:384].rearrange("p (r e) -> p r e", r=4),
                            in1=_bc(wss[:, 0, :].unsqueeze(2), [128, 4, 64]), op=ALU.mult)
            for s_ in (1, 2):
                V.tensor_tensor(out=stin[:, s_, 128:384].rearrange("p (r e) -> p r e", r=4), in0=stin[:, s_, 128:384].rearrange("p (r e) -> p r e", r=4),
                                in1=_bc(wss[:, s_, :].unsqueeze(2), [128, 4, 64]), op=ALU.mult)
                V.tensor_tensor(out=Sssd, in0=Sssd, in1=stin[:, s_, 128:384], op=ALU.add)
            V.tensor_copy(out=Sssd_b, in_=Sssd)
            return V.tensor_copy(out=Sret_b, in_=Sret)
        P.op("dve", comb2, R=["stin", "wss", "Sret", "Sssd"], W=["Sret", "Sssd", "stin"])
    else:
        def zinit():
            V.memset(Sret, 0.0)
            V.memset(Sssd, 0.0)
            return V.memset(totacc, 0.0)
        P.op("dve", zinit, W=["Sret", "Sssd", "totacc"])

    hTc = [ar.alloc([128, 8, 128], BF16) for _ in range(2)]
    qk_sb = ar.alloc([128, 512])
    qr = ar.alloc([128, 4, 2, 32])
    kr = ar.alloc([128, 4, 2, 32])
    rt = [ar.alloc([128, 4, 32]) for _ in range(4)]
    q2b = ar.alloc([128, 256], BF16)
    k2b = ar.alloc([128, 256], BF16)
    v_b = ar.alloc([128, 256], BF16)
    sg = ar.alloc([128, 256])
    qkT = ar.alloc([128, 512], BF16)
    qm = ar.alloc([128, 4, 128], BF16)
    BCm = ar.alloc([128, 4, 128], BF16)
    sTm = ar.alloc([128, 512], BF16)
    osq = qr.rearrange("p h two f -> p h (two f)")
    onr = kr.rearrange("p h two f -> p h (two f)")
    gst = ar.alloc([128, 16])
    xr = ar.alloc([128, 6, 131])
    acc = ar.alloc([128, 6, 128])
    bc_b = ar.alloc([128, 2, 128], BF16)
    Bm_b = ar.alloc([128, 128], BF16)
    amask = ar.alloc([128, 8, 128])
    eseg = ar.alloc([128, 8, 128], BF16)
    mT = ar.alloc([128, 8, 128], BF16)
    cbm = ar.alloc([128, 2, 128])
    xdt_b = ar.alloc([128, 8, 64], BF16)
    xdd_b = ar.alloc([128, 8, 64], BF16)
    xskip = ar.alloc([128, 8, 64])
    t1 = ar.alloc([128, 8, 64])
    szs = ar.alloc([128, 512])
    hsq = ar.alloc([128, 512])
    sm = ar.alloc([128, 64])
    qT_s = ar.alloc([128, 4, 128], BF16)
    kT_pp = [ar.alloc([128, 128], BF16) for _ in range(2)]
    v_pp = [ar.alloc([128, 128], BF16) for _ in range(2)]
    sl = amask.rearrange("p h l -> p (h l)").rearrange("p (h j) -> p h j", h=4)
    p_b = ar.alloc([128, 4, 256], BF16)
    pT_b = ar.alloc([128, 8, 128], BF16)
    ssw = ar.alloc([128, 32])
    mix_tok = ar.alloc([128, D], BF16)
    mixT = ar.alloc([128, 8, 128], BF16)
    sq = [ar.alloc([128, 128]) for _ in range(2)]
    lnst = t1.rearrange("p h d -> p (h d)").rearrange("p (a b) -> p a b", a=4)
    lnk = ["t1"]
    mixer_arena_end = ar.off

    def zmask():
        V.memset(qm, 0.0)
        V.memset(BCm, 0.0)
        return V.memset(qT_s, 0.0)
    P.op("dve", zmask, W=["qm", "BCm", "qT_s"])

    dtv, av_, acs_tot, ed, eaed, cdec, dd = (sm[:, 0:8], sm[:, 8:16], sm[:, 16:32], sm[:, 32:48], sm[:, 48:64], None, None)

    sm2 = ar.alloc([128, 32])
    cdec = sm2[:, 0:8]
    dd = sm2[:, 8:16]
    rr = sm2[:, 16:24]

    def ln_inplace(n_cols, xs_keyR, xview, GA, BA, nfree):
        bank, bkey = pb()
        bank2, bkey2 = (bank[:, nfree:2 * nfree], bkey) if nfree <= 256 else pb()
        if nfree > 256:
            bank2 = bank2[:, 0:nfree]
        for fc in range(8):
            sqb = sq[fc % 2]
            sk = "sq%d" % (fc % 2)
            P.op("pool", lambda fc=fc, sqb=sqb: G.tensor_tensor(out=sqb[:, 0:nfree], in0=xview[:, fc, :], in1=xview[:, fc, :], op=ALU.mult),
                 R=xs_keyR, W=[sk])

            def mm(fc=fc, sqb=sqb, bank=bank):
                T.matmul(bank[:, 0:nfree], lhsT=ones, rhs=xview[:, fc, :], start=(fc == 0), stop=(fc == 7))
                return T.matmul(bank2, lhsT=ones, rhs=sqb[:, 0:nfree], start=(fc == 0), stop=(fc == 7))
            P.op("pe", mm, R=xs_keyR + [sk, "cst"], W=[bkey, bkey2])
        mean, msq, var, rstd = lnst[:, 0, 0:nfree], lnst[:, 1, 0:nfree], lnst[:, 2, 0:nfree], lnst[:, 3, 0:nfree]

        def st(bank=bank):
            V.tensor_scalar(out=mean, in0=bank[:, 0:nfree], scalar1=1.0 / D, scalar2=None, op0=ALU.mult)
            V.tensor_tensor(out=msq, in0=mean, in1=mean, op=ALU.mult)
            V.scalar_tensor_tensor(out=var, in0=bank2, scalar=1.0 / D, in1=msq, op0=ALU.mult, op1=ALU.subtract)
            return V.tensor_scalar(out=var, in0=var, scalar1=EPS, scalar2=None, op0=ALU.add)
        P.op("dve", st, R=[bkey, bkey2], W=[lnk[0]])
        P.op("act", lambda: S.sqrt(out=rstd, in_=var), R=[lnk[0]], W=[lnk[0]])

        def nrm():
            V.reciprocal(out=rstd, in_=rstd)
            V.tensor_tensor(out=xview, in0=xview, in1=_bc(mean.unsqueeze(1), [128, 8, nfree]), op=ALU.subtract)
            return V.tensor_tensor(out=xview, in0=xview, in1=_bc(rstd.unsqueeze(1), [128, 8, nfree]), op=ALU.mult)
        P.op("dve", nrm, R=[lnk[0]] + xs_keyR, W=xs_keyR + [lnk[0]])

        def aff():
            G.tensor_tensor(out=xview, in0=xview, in1=_bc(GA.unsqueeze(2), [128, 8, nfree]), op=ALU.mult)
            return G.tensor_tensor(out=xview, in0=xview, in1=_bc(BA.unsqueeze(2), [128, 8, nfree]), op=ALU.add)
        P.op("pool", aff, R=xs_keyR + ["der", "misc"], W=xs_keyR)

    def chunk(n):
        halo = n < 0
        cur, prv = (n % 2), ((n + 1) % 2)
        if halo:
            hc = hT_halo
            hk = "hT_halo"
        else:
            hc = hTc[n % 2]
            hk = "hTc%d" % (n % 2)
            xk = [("xT", n, 0), ("xT", n, 1)]
            Tn = slice(n * 128, (n + 1) * 128)

            def mkh2():
                r = None
                for fc in range(8):
                    r = G.tensor_scalar(out=hc[:, fc, :], in0=xT[:, fc, Tn], scalar1=A_in[:, fc:fc + 1], scalar2=B_in[:, fc:fc + 1],
                                        op0=ALU.mult, op1=ALU.add)
                return r
            P.op("pool", mkh2, R=xk + ["misc", "der"], W=[hk])

        def proj_tok(bank, c0, c1, o0=0):
            def f():
                r = None
                for kc in range(8):
                    r = T.matmul(bank[:, o0:o0 + (c1 - c0)], lhsT=hc[:, kc, :], rhs=w_in[:, kc, c0:c1], start=(kc == 0), stop=(kc == 7))
                return r
            return f

        def proj_feat(bank, c0, o0):
            def f():
                r = None
                for kc in range(8):
                    r = T.matmul(bank[:, o0:o0 + 128], lhsT=w_in[:, kc, c0:c0 + 128], rhs=hc[:, kc, :], start=(kc == 0), stop=(kc == 7))
                return r
            return f

        bD, kD = pb()
        bE, kE = pb()
        bF, kF = pb()
        if full:
            P.op("pe", proj_tok(bD, 2696, 2824, 8), R=[hk, "w_in"], W=[kD])
            P.op("pe", proj_feat(bD, 2568, 256), R=[hk, "w_in"], W=[kD])
        for c in range(4):
            P.op("pe", proj_feat(bE, 1536 + c * 128, c * 128), R=[hk, "w_in"], W=[kE])
        P.op("pe", proj_feat(bF, 2048, 0), R=[hk, "w_in"], W=[kF])
        P.op("pe", proj_feat(bF, 2176, 128), R=[hk, "w_in"], W=[kF])
        if halo:
            def tail():
                V.tensor_scalar(out=xr[:, 0:4, 128:131], in0=bE[:].rearrange("p (c t) -> p c t", c=4)[:, :, 125:128],
                                scalar1=halovalid, scalar2=None, op0=ALU.mult)
                return V.tensor_scalar(out=xr[:, 4:6, 128:131], in0=bF[:, 0:256].rearrange("p (c t) -> p c t", c=2)[:, :, 125:128],
                                       scalar1=halovalid, scalar2=None, op0=ALU.mult)
            P.op("dve", tail, R=[kE, kF, "misc"], W=["xr"])
            if full:
                def kv():
                    S.copy(out=kT_pp[cur], in_=bD[:, 256:384])
                    return S.copy(out=v_pp[cur], in_=bD[:, 8:136])
                P.op("act", kv, R=[kD], W=["kT_pp%d" % cur, "v_pp%d" % cur])
            return
        P.op("pe", proj_tok(bD, 2304, 2312, 0), R=[hk, "w_in"], W=[kD])
        bA, kA = pb()
        bB, kB = pb()
        if full:
            P.op("pe", proj_tok(bA, 0, 512), R=[hk, "w_in"], W=[kA])
            P.op("pe", proj_tok(bB, 512, 1024), R=[hk, "w_in"], W=[kB])
            bC, kC = pb()
            P.op("pe", proj_tok(bC, 1024, 1536), R=[hk, "w_in"], W=[kC])
            P.op("pe", proj_feat(bF, 2312, 256), R=[hk, "w_in"], W=[kF])
            P.op("pe", proj_feat(bF, 2440, 384), R=[hk, "w_in"], W=[kF])
        else:
            P.op("pe", proj_tok(bA, 256, 512, 256), R=[hk, "w_in"], W=[kA])
            P.op("pe", proj_tok(bB, 512, 768, 0), R=[hk, "w_in"], W=[kB])

        P.op("dve", lambda: V.tensor_tensor(out=dtv, in0=bD[:, 0:8], in1=dtb, op=ALU.add), R=[kD, "rowp"], W=["dtv"])
        P.op("pool", lambda: G.tensor_copy(out=xr[:, :, 0:3], in_=xr[:, :, 128:131]), R=["xr"], W=["xr"])

        def xrcp():
            S.copy(out=xr[:, 0:4, 3:131], in_=bE[:].rearrange("p (c t) -> p c t", c=4))
            return S.copy(out=xr[:, 4:6, 3:131], in_=bF[:, 0:256].rearrange("p (c t) -> p c t", c=2))
        P.op("act", xrcp, R=[kE, kF], W=["xr"])
        P.op("act", lambda: S.copy(out=qk_sb[:, (0 if full else 256):512], in_=bA[:, (0 if full else 256):512]), R=[kA], W=["qk_sb"])
        P.op("act", lambda: S.copy(out=v_b, in_=bB[:, 0:256]), R=[kB], W=["v_b"])
        if full:
            def swc():
                for h_ in range(4):
                    kvh_, gq_ = h_ // 2, h_ % 2
                    pr_ = slice(kvh_ * 64, kvh_ * 64 + 64)
                    S.copy(out=qT_s[pr_, h_, :], in_=bF[pr_, 256 + gq_ * 128:256 + (gq_ + 1) * 128])
                S.copy(out=kT_pp[cur], in_=bD[:, 256:384])
                return S.copy(out=v_pp[cur], in_=bD[:, 8:136])
            P.op("act", swc, R=[kF, kD], W=["qT_s", "kT_pp%d" % cur, "v_pp%d" % cur])
            P.op("act", lambda: S.activation(out=sg, in_=bB[:, 256:512], func=AF.Silu), R=[kB], W=["sg"])
            P.op("act", lambda: S.activation(out=szs, in_=bC[:], func=AF.Silu), R=[kC], W=["szs"])
        if SUB[0] < 1:
            return
        cosb = _bc(cosT[:, n, :].unsqueeze(1), [128, 4, 32])
        sinb = _bc(sinT[:, n, :].unsqueeze(1), [128, 4, 32])

        def rotary(E, src, dst, ta, tb):
            X = src.rearrange("p (h two f) -> p h two f", h=4, two=2)
            x1, x2 = X[:, :, 0, :], X[:, :, 1, :]
            E.tensor_tensor(out=ta, in0=x1, in1=cosb, op=ALU.mult)
            E.tensor_tensor(out=tb, in0=x2, in1=sinb, op=ALU.mult)
            E.tensor_tensor(out=dst[:, :, 0, :], in0=ta, in1=tb, op=ALU.subtract)
            E.tensor_tensor(out=ta, in0=x1, in1=sinb, op=ALU.mult)
            E.tensor_tensor(out=tb, in0=x2, in1=cosb, op=ALU.mult)
            return E.tensor_tensor(out=dst[:, :, 1, :], in0=ta, in1=tb, op=ALU.add)

        def krot():
            rotary(V, qk_sb[:, 256:512], kr, rt[2], rt[3])
            return V.tensor_tensor(out=k2b[:].rearrange("p (h d) -> p h d", h=4), in0=_bc(dk.unsqueeze(2), [128, 4, 64]),
                                   in1=kr.rearrange("p h two f -> p h (two f)"), op=ALU.mult)
        P.op("dve", krot, R=["qk_sb", "rot", "cst"], W=["kr", "k2b"])
        if full:
            def qrot():
                rotary(V, qk_sb[:, 0:256], qr, rt[0], rt[1])
                return V.tensor_tensor(out=q2b[:].rearrange("p (h d) -> p h d", h=4), in0=_bc(dq.unsqueeze(2), [128, 4, 64]),
                                       in1=qr.rearrange("p h two f -> p h (two f)"), op=ALU.mult)
            P.op("dve", qrot, R=["qk_sb", "rot", "cst"], W=["qr", "q2b"])
            if SUB[0] < 1.05:
                return
            bT, kT_ = pb()
            bTb = bT[:].bitcast(BF16)

            def trqk():
                r = None
                for t in range(2):
                    T.transpose(bTb[:, t * 128:(t + 1) * 128], q2b[:, t * 128:(t + 1) * 128], ident_b[:])
                    r = T.transpose(bTb[:, 256 + t * 128:256 + (t + 1) * 128], k2b[:, t * 128:(t + 1) * 128], ident_b[:])
                return r
            P.op("pe", trqk, R=["q2b", "k2b", "ident_b"], W=[kT_])
            def qkcp():
                for h_ in range(4):
                    t_, hf2 = h_ // 2, h_ % 2
                    pr_ = slice(hf2 * 64, hf2 * 64 + 64)
                    S.copy(out=qm[pr_, h_, :], in_=bTb[pr_, t_ * 128:(t_ + 1) * 128])
                return S.copy(out=qkT[:, 256:512], in_=bTb[:, 256:512])
            P.op("act", qkcp, R=[kT_], W=["qkT", "qm"])
            if SUB[0] < 1.1:
                return
            bS, kS = pb()

            def scores():
                r = None
                import os
                for h in [int(c_) for c_ in os.environ.get("SUBH", "0123")]:
                    t, hf_ = h // 2, h % 2
                    pr = slice(hf_ * 64, hf_ * 64 + 64)
                    r = T.matmul(bS[:, h * 128:(h + 1) * 128], lhsT=qkT[:, 256 + t * 128:256 + (t + 1) * 128],
                                 rhs=qm[:, h, :], start=True, stop=True)
                return r
            P.op("pe", scores, R=["qkT", "qm"], W=[kS])
            if SUB[0] < 1.15:
                return
            P.op("dve", lambda: V.tensor_tensor(out=sTm[:].rearrange("p (h i) -> p h i", h=4), in0=_bc(tri.unsqueeze(1), [128, 4, 128]),
                                                in1=bS[:].rearrange("p (h i) -> p h i", h=4), op=ALU.mult), R=[kS, "cst"], W=["sTm"])
            if SUB[0] < 1.2:
                return
            bO, kO = pb()

            def oret():
                r = None
                for h in range(4):
                    t, hf_ = h // 2, h % 2
                    pr = slice(hf_ * 64, hf_ * 64 + 64)
                    T.matmul(bO[:, h * 64:(h + 1) * 64], lhsT=sTm[:, h * 128:(h + 1) * 128], rhs=v_b[:, h * 64:(h + 1) * 64], start=True, stop=False)
                    r = T.matmul(bO[:, h * 64:(h + 1) * 64], lhsT=qm[:, h, :], rhs=Sret_b[:, t, :], start=False, stop=True)
                return r
            P.op("pe", oret, R=["sTm", "v_b", "qm", "Sret_b"], W=[kO])
        if SUB[0] < 1.3:
            return
        bK, kK = pb()

        def kvm():
            r = None
            for t in range(2):
                r = T.matmul(bK[:, t * 128:(t + 1) * 128], lhsT=k2b[:, t * 128:(t + 1) * 128], rhs=v_b[:, t * 128:(t + 1) * 128], start=True, stop=True)
            return r
        P.op("pe", kvm, R=["k2b", "v_b"], W=[kK])

        if SUB[0] < 1.6:
            return

        def supd():
            K4 = bK[:, 0:256].rearrange("p (t hf e) -> p t hf e", t=2, hf=2)
            V.scalar_tensor_tensor(out=Sret, in0=K4[:, :, 0, :], scalar=m0, in1=Sret, op0=ALU.mult, op1=ALU.add)
            V.scalar_tensor_tensor(out=Sret, in0=K4[:, :, 1, :], scalar=m1, in1=Sret, op0=ALU.mult, op1=ALU.add)
            V.tensor_tensor(out=Sret, in0=Sret, in1=_bc(gC.unsqueeze(2), [128, 2, 64]), op=ALU.mult)
            return V.tensor_copy(out=Sret_b, in_=Sret)
        P.op("dve", supd, R=[kK, "Sret", "cst"], W=["Sret", "Sret_b"])
        if full:
            P.op("act", lambda: S.activation(out=osq, in_=bO[:, 0:256].rearrange("p (h d) -> p h d", h=4), func=AF.Square), R=[kO], W=["qr"])

            def gn1():
                V.tensor_reduce(out=gst[:, 0:4], in_=bO[:, 0:256].rearrange("p (h d) -> p h d", h=4), axis=AX.X, op=ALU.add)
                V.tensor_reduce(out=gst[:, 4:8], in_=osq, axis=AX.X, op=ALU.add)
                V.tensor_scalar(out=gst[:, 0:4], in0=gst[:, 0:4], scalar1=1.0 / 64, scalar2=None, op0=ALU.mult)
                V.tensor_tensor(out=gst[:, 8:12], in0=gst[:, 0:4], in1=gst[:, 0:4], op=ALU.mult)
                V.scalar_tensor_tensor(out=gst[:, 4:8], in0=gst[:, 4:8], scalar=1.0 / 64, in1=gst[:, 8:12], op0=ALU.mult, op1=ALU.subtract)
                return V.tensor_scalar(out=gst[:, 4:8], in0=gst[:, 4:8], scalar1=EPS, scalar2=None, op0=ALU.add)
            P.op("dve", gn1, R=[kO, "qr"], W=["gst"])
            P.op("act", lambda: S.sqrt(out=gst[:, 4:8], in_=gst[:, 4:8]), R=["gst"], W=["gst"])

            def gn2():
                V.reciprocal(out=gst[:, 4:8], in_=gst[:, 4:8])
                V.tensor_tensor(out=onr, in0=bO[:, 0:256].rearrange("p (h d) -> p h d", h=4), in1=_bc(gst[:, 0:4].unsqueeze(2), [128, 4, 64]), op=ALU.subtract)
                V.tensor_tensor(out=onr, in0=onr, in1=_bc(gst[:, 4:8].unsqueeze(2), [128, 4, 64]), op=ALU.mult)
                return V.tensor_tensor(out=mix_tok[:, 0:256], in0=onr.rearrange("p h d -> p (h d)"), in1=sg, op=ALU.mult)
            P.op("dve", gn2, R=[kO, "gst", "sg"], W=["kr", "gst", "mix_ret"])

        if SUB[0] < 2:
            return

        def conv(E, cs):
            def f():
                r = None
                for c in cs:
                    E.tensor_scalar(out=acc[:, c, :], in0=xr[:, c, 0:128], scalar1=convw[:, c * 4:c * 4 + 1], scalar2=convb[:, c:c + 1],
                                    op0=ALU.mult, op1=ALU.add)
                    for w in range(1, 4):
                        r = E.scalar_tensor_tensor(out=acc[:, c, :], in0=xr[:, c, w:w + 128], scalar=convw[:, c * 4 + w:c * 4 + w + 1],
                                                   in1=acc[:, c, :], op0=ALU.mult, op1=ALU.add)
                return r
            return f
        P.op("dve", conv(V, (0, 1, 4)), R=["xr", "misc"], W=["accA"])
        P.op("dve", conv(V, (2, 3, 5)), R=["xr", "misc"], W=["accB"])

        def sil():
            S.activation(out=acc[:, 0:4, :], in_=acc[:, 0:4, :], func=AF.Silu)
            return S.activation(out=bc_b, in_=acc[:, 4:6, :], func=AF.Silu)
        P.op("act", sil, R=["accA", "accB"], W=["accA", "accB", "bc_b"])
        if full:
            def bcm():
                r = None
                for g_ in range(2):
                    pr_ = slice(g_ * 64, g_ * 64 + 64)
                    G.tensor_copy(out=BCm[pr_, g_, :], in_=bc_b[pr_, 0, :])
                    r = G.tensor_copy(out=BCm[pr_, 2 + g_, :], in_=bc_b[pr_, 1, :])
                return r
            P.op("pool", bcm, R=["bc_b"], W=["BCm"])
        bX, kX = pb()

        def trx():
            r = None
            for c in range(4):
                r = T.transpose(bX[:, c * 128:(c + 1) * 128], acc[:, c, :], ident)
            return r
        P.op("pe", trx, R=["accA", "accB", "cst"], W=[kX])
        bBm, kBm = pb()
        bBmb = bBm[:].bitcast(BF16)
        P.op("pe", lambda: T.transpose(bBmb[:, 0:128], bc_b[:, 0, :], ident_b[:]), R=["bc_b", "ident_b"], W=[kBm])
        P.op("act", lambda: S.copy(out=Bm_b, in_=bBmb[:, 0:128]), R=[kBm], W=["Bm_b"])
        if SUB[0] < 3:
            return
        def sp_():
            S.activation(out=dtv, in_=dtv, func=AF.Exp)
            return S.activation(out=dtv, in_=dtv, func=AF.Ln, bias=1.0)
        if n == 0:
            dbg("dtv_pre", dtv, ["dtv"])
        P.op("act", sp_, R=["dtv"], W=["dtv"])
        if n == 0:
            dbg("dtv", dtv, ["dtv"])
        P.op("dve", lambda: V.tensor_tensor(out=av_, in0=dtv, in1=negA, op=ALU.mult), R=["dtv", "rowp"], W=["av"])
        bY, kY = pb()

        def acsm():
            T.matmul(bY[:, 0:8], lhsT=tri, rhs=av_, start=True, stop=True)
            return T.matmul(bY[:, 8:16], lhsT=ones, rhs=av_, start=True, stop=True)
        P.op("pe", acsm, R=["av", "cst"], W=[kY])
        P.op("act", lambda: S.copy(out=acs_tot, in_=bY[:, 0:16]), R=[kY], W=["acs_tot"])
        if n == 0:
            dbg("av", av_, ["av"])
            dbg("acs_tot", acs_tot, ["acs_tot"])

        def edf():
            V.tensor_copy(out=ed[:, 0:8], in_=acs_tot[:, 0:8])
            return V.tensor_tensor(out=ed[:, 8:16], in0=acs_tot[:, 8:16], in1=acs_tot[:, 0:8], op=ALU.subtract)
        P.op("dve", edf, R=["acs_tot"], W=["ed"])

        def exps():
            S.activation(out=eaed, in_=ed, func=AF.Exp)
            return S.activation(out=cdec, in_=acs_tot[:, 8:16], func=AF.Exp)
        P.op("act", exps, R=["ed", "acs_tot"], W=["eaed", "cdec"])
        if not full:
            P.op("pool", lambda: G.tensor_tensor(out=totacc, in0=totacc, in1=acs_tot[:, 8:16], op=ALU.add), R=["acs_tot", "totacc"], W=["totacc"])
        P.op("dve", lambda: V.tensor_tensor(out=dd, in0=dtv, in1=eaed[:, 8:16], op=ALU.mult), R=["dtv", "eaed"], W=["dd"])
        X3 = bX[:].rearrange("p (h d) -> p h d", h=8)
        P.op("dve", lambda: V.tensor_tensor(out=xdd_b, in0=_bc(dd.unsqueeze(2), [128, 8, 64]), in1=X3, op=ALU.mult), R=[kX, "dd"], W=["xdd_b"])
        if full:
            def xd():
                V.tensor_tensor(out=xdt_b, in0=_bc(dtv.unsqueeze(2), [128, 8, 64]), in1=X3, op=ALU.mult)
                return V.tensor_tensor(out=xskip, in0=X3, in1=_bc(dskip.unsqueeze(2), [128, 8, 64]), op=ALU.mult)
            P.op("dve", xd, R=[kX, "dtv", "rowp"], W=["xdt_b", "xskip"])
            P.op("pool", lambda: G.tensor_tensor(out=amask, in0=_bc(mgt.unsqueeze(1), [128, 8, 128]), in1=_bc(av_.unsqueeze(2), [128, 8, 128]), op=ALU.mult),
                 R=["av", "cst"], W=["amask"])
            bCB, kCB = pb()

            def cbm_():
                r = None
                for g in range(2):
                    pr = slice(g * 64, g * 64 + 64)
                    r = T.matmul(bCB[:, g * 128:(g + 1) * 128], lhsT=BCm[:, g, :], rhs=bc_b[:, 1, :], start=True, stop=True)
                return r
            P.op("pe", cbm_, R=["bc_b", "BCm"], W=[kCB])
            P.op("dve", lambda: V.tensor_tensor(out=cbm, in0=bCB[:, 0:256].rearrange("p (g l) -> p g l", g=2), in1=_bc(tri.unsqueeze(1), [128, 2, 128]), op=ALU.mult),
                 R=[kCB, "cst"], W=["cbm"])
            for g in range(2):
                bSg, kSg = pb()

                def segm(g=g, bSg=bSg):
                    r = None
                    for r_ in range(4):
                        r = T.matmul(bSg[:, r_ * 128:(r_ + 1) * 128], lhsT=amask[:, g * 4 + r_, :], rhs=tri, start=True, stop=True)
                    return r
                P.op("pe", segm, R=["amask", "cst"], W=[kSg])
                P.op("act", lambda g=g, bSg=bSg: S.activation(out=eseg[:, g * 4:g * 4 + 4, :], in_=bSg[:].rearrange("p (r l) -> p r l", r=4), func=AF.Exp),
                     R=[kSg], W=["eseg%d" % g])
                P.op("dve", lambda g=g: V.tensor_tensor(out=mT[:, g * 4:g * 4 + 4, :], in0=_bc(cbm[:, g, :].unsqueeze(1), [128, 4, 128]),
                                                        in1=eseg[:, g * 4:g * 4 + 4, :], op=ALU.mult),
                     R=["eseg%d" % g, "cbm"], W=["mT%d" % g])
            bYD, kYD = pb()

            def ydm():
                r = None
                for h in range(8):
                    r = T.matmul(bYD[:, h * 64:(h + 1) * 64], lhsT=mT[:, h, :], rhs=xdt_b[:, h, :], start=True, stop=True)
                return r
            P.op("pe", ydm, R=["mT0", "mT1", "xdt_b"], W=[kYD])
            bYO, kYO = pb()

            def yom():
                r = None
                for g in range(2):
                    pr = slice(g * 64, g * 64 + 64)
                    r = T.matmul(bYO[:, g * 256:(g + 1) * 256], lhsT=BCm[:, 2 + g, :], rhs=Sssd_b, start=True, stop=True)
                return r
            P.op("pe", yom, R=["BCm", "Sssd_b"], W=[kYO])
        if SUB[0] < 4:
            return
        bST, kST = pb()
        P.op("pe", lambda: T.matmul(bST[:, 0:512], lhsT=Bm_b, rhs=xdd_b[:].rearrange("p h d -> p (h d)"), start=True, stop=True),
             R=["Bm_b", "xdd_b"], W=[kST])

        def sssd():
            r = None
            for g in range(2):
                pr = slice(g * 64, g * 64 + 64)
                V.tensor_tensor(out=Sssd[pr, :].rearrange("p (r e) -> p r e", r=4), in0=Sssd[pr, :].rearrange("p (r e) -> p r e", r=4),
                                in1=_bc(cdec[pr, g * 4:g * 4 + 4].unsqueeze(2), [64, 4, 64]), op=ALU.mult)
                r = V.tensor_tensor(out=Sssd[pr, :], in0=Sssd[pr, :], in1=bST[pr, g * 256:(g + 1) * 256], op=ALU.add)
            return r
        P.op("dve", sssd, R=[kST, "Sssd", "cdec"], W=["Sssd"])
        if not full:
            return
        P.op("act", lambda: S.copy(out=Sssd_b, in_=Sssd), R=["Sssd"], W=["Sssd_b"])

        def ycomb():
            V.tensor_tensor(out=t1, in0=bYO[:].rearrange("p (h d) -> p h d", h=8), in1=_bc(eaed[:, 0:8].unsqueeze(2), [128, 8, 64]), op=ALU.mult)
            return V.tensor_tensor(out=t1, in0=t1, in1=bYD[:].rearrange("p (h d) -> p h d", h=8), op=ALU.add)
        P.op("dve", ycomb, R=[kYO, kYD, "eaed"], W=["t1"])
        t1f = t1.rearrange("p h d -> p (h d)")

        def yg():
            G.tensor_tensor(out=t1f, in0=t1f, in1=xskip.rearrange("p h d -> p (h d)"), op=ALU.add)
            G.tensor_tensor(out=t1f, in0=t1f, in1=szs, op=ALU.mult)
            return G.tensor_tensor(out=hsq, in0=t1f, in1=t1f, op=ALU.mult)
        P.op("pool", yg, R=["t1", "xskip", "szs"], W=["t1", "hsq"])

        def rms1():
            V.tensor_reduce(out=rr[:, 0:2], in_=hsq.rearrange("p (g e) -> p g e", g=2), axis=AX.X, op=ALU.add)
            return V.tensor_scalar(out=rr[:, 0:2], in0=rr[:, 0:2], scalar1=1.0 / 256, scalar2=EPS, op0=ALU.mult, op1=ALU.add)
        P.op("dve", rms1, R=["hsq"], W=["rr"])
        P.op("act", lambda: S.sqrt(out=rr[:, 0:2], in_=rr[:, 0:2]), R=["rr"], W=["rr"])

        def rms2():
            V.reciprocal(out=rr[:, 0:2], in_=rr[:, 0:2])
            V.tensor_tensor(out=hsq.rearrange("p (g e) -> p g e", g=2), in0=t1f.rearrange("p (g e) -> p g e", g=2),
                            in1=_bc(rr[:, 0:2].unsqueeze(2), [128, 2, 256]), op=ALU.mult)
            return V.tensor_tensor(out=mix_tok[:, 256:768], in0=hsq, in1=normw, op=ALU.mult)
        P.op("dve", rms2, R=["rr", "t1", "hsq", "rowp"], W=["hsq", "mix_ssd", "rr"])

        bL = [pb(), pb()]

        def lgm():
            r = None
            for h in range(4):
                kvh, gq = h // 2, h % 2
                pr = slice(kvh * 64, kvh * 64 + 64)
                bank = bL[h // 2][0]
                for part, buf in ((0, kT_pp[prv]), (1, kT_pp[cur])):
                    o = (h % 2) * 256 + part * 128
                    r = T.matmul(bank[:, o:o + 128], lhsT=qT_s[:, h, :], rhs=buf, start=True, stop=True)
            return r
        P.op("pe", lgm, R=["qT_s", "kT_pp0", "kT_pp1"], W=[bL[0][1], bL[1][1]])

        def sls():
            r = None
            for hb in range(2):
                r = V.scalar_tensor_tensor(out=sl[:, hb * 2:hb * 2 + 2, :], in0=bL[hb][0][:].rearrange("p (h j) -> p h j", h=2), scalar=0.125,
                                           in1=biasw[:, hb * 2:hb * 2 + 2, :], op0=ALU.mult, op1=ALU.add)
            if n == 0:
                r = V.tensor_scalar(out=sl[:, :, 0:128], in0=sl[:, :, 0:128], scalar1=halomask, scalar2=None, op0=ALU.add)
            V.tensor_reduce(out=ssw[:, 0:4], in_=sl, axis=AX.X, op=ALU.max)
            V.tensor_tensor(out=ssw[:, 0:4], in0=ssw[:, 0:4], in1=sinks, op=ALU.max)
            V.tensor_scalar(out=ssw[:, 4:8], in0=ssw[:, 0:4], scalar1=-1.0, scalar2=None, op0=ALU.mult)
            return V.tensor_tensor(out=ssw[:, 8:12], in0=sinks, in1=ssw[:, 4:8], op=ALU.add)
        P.op("dve", sls, R=[bL[0][1], bL[1][1], "biasw", "misc", "rowp"], W=["amask", "ssw"])

        def pex():
            r = None
            for h in range(4):
                r = S.activation(out=p_b[:, h, :], in_=sl[:, h, :], func=AF.Exp, bias=ssw[:, 4 + h:5 + h], scale=1.0)
            return S.activation(out=ssw[:, 12:16], in_=ssw[:, 8:12], func=AF.Exp)
        P.op("act", pex, R=["amask", "ssw"], W=["p_b", "ssw2"])

        def den():
            V.tensor_reduce(out=ssw[:, 16:20], in_=p_b, axis=AX.X, op=ALU.add)
            V.tensor_tensor(out=ssw[:, 16:20], in0=ssw[:, 16:20], in1=ssw[:, 12:16], op=ALU.add)
            return V.reciprocal(out=ssw[:, 20:24], in_=ssw[:, 16:20])
        P.op("dve", den, R=["p_b", "ssw2"], W=["ssw3"])
        bPT, kPT = pb()
        bPTb = bPT[:].bitcast(BF16)

        def ptr():
            r = None
            for h in range(4):
                for part in range(2):
                    j_ = h * 2 + part
                    r = T.transpose(bPTb[:, j_ * 128:(j_ + 1) * 128], p_b[:, h, part * 128:(part + 1) * 128], ident_b[:])
            return r
        P.op("pe", ptr, R=["p_b", "ident_b"], W=[kPT])
        P.op("act", lambda: S.copy(out=pT_b[:, 0:4, :], in_=bPTb[:, 0:512].rearrange("p (j i) -> p j i", j=4)), R=[kPT], W=["pT_b0"])
        P.op("dve", lambda: V.tensor_copy(out=pT_b[:, 4:8, :], in_=bPTb[:, 512:1024].rearrange("p (j i) -> p j i", j=4)), R=[kPT], W=["pT_b1"])
        bOS, kOS = pb()

        def osw():
            r = None
            for h in range(4):
                kvh = h // 2
                for part, buf in ((0, v_pp[prv]), (1, v_pp[cur])):
                    r = T.matmul(bOS[:, h * 64:(h + 1) * 64], lhsT=pT_b[:, h * 2 + part, :], rhs=buf[:, kvh * 64:(kvh + 1) * 64],
                                 start=(part == 0), stop=(part == 1))
            return r
        P.op("pe", osw, R=["pT_b0", "pT_b1", "v_pp0", "v_pp1"], W=[kOS])
        P.op("dve", lambda: V.tensor_tensor(out=mix_tok[:, 768:1024].rearrange("p (h d) -> p h d", h=4), in0=_bc(ssw[:, 20:24].unsqueeze(2), [128, 4, 64]),
                                            in1=bOS[:, 0:256].rearrange("p (h d) -> p h d", h=4), op=ALU.mult), R=[kOS, "ssw3"], W=["mix_swa"])

        bMT, kMT = pb()
        bMTb = bMT[:].bitcast(BF16)

        def mtr():
            r = None
            for kc in range(8):
                r = T.transpose(bMTb[:, kc * 128:(kc + 1) * 128], mix_tok[:, kc * 128:(kc + 1) * 128], ident_b[:])
            return r
        P.op("pe", mtr, R=["mix_ret", "mix_ssd", "mix_swa", "ident_b"], W=[kMT])
        P.op("act", lambda: S.copy(out=mixT, in_=bMTb[:, 0:1024].rearrange("p (k t) -> p k t", k=8)), R=[kMT], W=["mixT"])
        for half in range(2):
            bW, kW = pb()

            def wo(half=half, bW=bW):
                r = None
                for q in range(4):
                    fc = half * 4 + q
                    for kc in range(8):
                        r = T.matmul(bW[:, q * 128:(q + 1) * 128], lhsT=w_out[:, kc, fc * 128:(fc + 1) * 128], rhs=mixT[:, kc, :],
                                     start=(kc == 0), stop=(kc == 7))
                return r
            P.op("pe", wo, R=["w_out", "mixT"], W=[kW])

            def res(half=half, bW=bW):
                r = None
                for q in range(4):
                    fc = half * 4 + q
                    r = V.scalar_tensor_tensor(out=xT[:, fc, Tn], in0=bW[:, q * 128:(q + 1) * 128], scalar=g1a[:, fc:fc + 1], in1=xT[:, fc, Tn],
                                               op0=ALU.mult, op1=ALU.add)
                return r
            P.op("dve", res, R=[kW, "der", ("xT", n, half)], W=[("xT", n, half)])
        ln_inplace(128, xk, xT[:, :, Tn], GA1, BA1, 128)

    for n in range(-1, NCH):
        if STOP[0] >= 4 + (n + 1) and not ffn_only:
            chunk(n)

    if not full:
        sto = ar.alloc([128, STW])

        def pk():
            V.tensor_copy(out=sto[:, 0:128], in_=Sret.rearrange("p t e -> p (t e)"))
            V.tensor_copy(out=sto[:, 128:384], in_=Sssd)
            return V.tensor_copy(out=sto[:, 384:392], in_=totacc)
        P.op("dve", pk, R=["Sret", "Sssd", "totacc"], W=["sto"])
        P.dma(sp, st_out_d, sto, R=["sto"], is_output=True)
        return P.emit()

    P.barrier()
    ar = Arena(arena_t, AW)
    hT = ar.alloc([128, 8, NTOK], BF16)
    aT = [ar.alloc([128, 4, 1024], BF16) for _ in range(2)]
    wgu = [ar.alloc([128, 8, 1024], BF16) for _ in range(2)]
    wdb = [ar.alloc([128, 4, D], BF16) for _ in range(2)]
    sgt = [ar.alloc([128, 512]) for _ in range(2)]
    evt = [ar.alloc([128, 512]) for _ in range(2)]
    if moe:
        gbc = [ar.alloc([128, 1024]) for _ in range(2)]
        rw_sb = ar.alloc([128, 8, 8])
        lgT = ar.alloc([128, 512])
        gatesT = ar.alloc([128, NTOK])
        lg = ar.alloc([128, NCH, 8])
        gts = ar.alloc([128, NCH, 8])
        e1 = ar.alloc([128, NCH, 8])
        e2 = ar.alloc([128, NCH, 8])
        l2 = ar.alloc([128, NCH, 8])
        tk = ar.alloc([128, 6, NCH])
        h2f = [ar.alloc([128, 512]) for _ in range(2)]
        P.dma(sp, rw_sb, rw_d, W=["rw_sb"])

    if moe:
        bG, kG = pb()
    for tg in range(4):
        Tg = slice(tg * 512, (tg + 1) * 512)
        xkeys = [("xT", n, hf_) for n in range(tg * 4, tg * 4 + 4) for hf_ in range(2)]
        if moe:
            bR, kR = pb()
        for fc in range(8):
            P.op("act", lambda fc=fc, Tg=Tg: S.activation(out=hT[:, fc, Tg], in_=xT[:, fc, Tg], func=AF.Identity, bias=B2[:, fc:fc + 1], scale=A2[:, fc:fc + 1]),
                 R=xkeys + ["der"], W=[("hT", tg)])
            if moe:
                hb = h2f[fc % 2]
                hbk = "h2f%d" % (fc % 2)
                P.op("dve", lambda fc=fc, Tg=Tg, hb=hb: V.tensor_scalar(out=hb, in0=xT[:, fc, Tg], scalar1=A2[:, fc:fc + 1], scalar2=B2[:, fc:fc + 1], op0=ALU.mult, op1=ALU.add),
                     R=xkeys + ["der"], W=[hbk])
                P.op("pe", lambda fc=fc, hb=hb, bR=bR: T.matmul(bR[0:8, 0:512], lhsT=rw_sb[:, fc, :], rhs=hb, start=(fc == 0), stop=(fc == 7)),
                     R=[hbk, "rw_sb"], W=[kR])
        if moe:
            P.op("act", lambda bR=bR: S.activation(out=lgT[0:8, 0:512], in_=bR[0:8, 0:512], func=AF.Identity, bias=rbias[0:8, 0:1], scale=1.0),
                 R=[kR, "misc"], W=["lgT"])

            def ltr(tg=tg):
                r = None
                for q in range(4):
                    n = tg * 4 + q
                    r = T.transpose(bG[:, n * 8:(n + 1) * 8], lgT[0:8, q * 128:(q + 1) * 128], ident[0:8, 0:8])
                return r
            P.op("pe", ltr, R=["lgT", "cst"], W=[kG])
    if moe:

        def top2():
            V.tensor_copy(out=lg, in_=bG[:, 0:128].rearrange("p (n e) -> p n e", e=8))
            m1_, m2_, dlt, w1_, w2_ = tk[:, 0, :], tk[:, 1, :], tk[:, 2, :], tk[:, 3, :], tk[:, 4, :]
            V.tensor_reduce(out=m1_, in_=lg, axis=AX.X, op=ALU.max)
            V.tensor_tensor(out=e1, in0=lg, in1=_bc(m1_.unsqueeze(2), [128, NCH, 8]), op=ALU.is_equal)
            V.scalar_tensor_tensor(out=l2, in0=e1, scalar=-1e30, in1=lg, op0=ALU.mult, op1=ALU.add)
            V.tensor_reduce(out=m2_, in_=l2, axis=AX.X, op=ALU.max)
            V.tensor_tensor(out=e2, in0=l2, in1=_bc(m2_.unsqueeze(2), [128, NCH, 8]), op=ALU.is_equal)
            return V.tensor_tensor(out=dlt, in0=m2_, in1=m1_, op=ALU.subtract)
        P.op("dve", top2, R=[kG], W=["tk", "lg"])
        P.op("act", lambda: S.activation(out=tk[:, 2, :], in_=tk[:, 2, :], func=AF.Exp), R=["tk"], W=["tk"])

        def top2b():
            dlt, w1_, w2_ = tk[:, 2, :], tk[:, 3, :], tk[:, 4, :]
            V.tensor_scalar(out=w1_, in0=dlt, scalar1=1.0, scalar2=None, op0=ALU.add)
            V.reciprocal(out=w1_, in_=w1_)
            V.tensor_tensor(out=w2_, in0=dlt, in1=w1_, op=ALU.mult)
            V.tensor_tensor(out=e1, in0=e1, in1=_bc(w1_.unsqueeze(2), [128, NCH, 8]), op=ALU.mult)
            V.tensor_tensor(out=e2, in0=e2, in1=_bc(w2_.unsqueeze(2), [128, NCH, 8]), op=ALU.mult)
            return V.tensor_tensor(out=gts, in0=e1, in1=e2, op=ALU.add)
        P.op("dve", top2b, R=["tk", "lg"], W=["gts", "tk", "lg"])
        for tg in range(4):
            bG2, kG2 = pb()

            def gtr(tg=tg, bG2=bG2):
                r = None
                for q in range(4):
                    n = tg * 4 + q
                    r = T.transpose(bG2[0:8, q * 128:(q + 1) * 128], gts[:, n, :], ident)
                return r
            P.op("pe", gtr, R=["gts", "cst"], W=[kG2])
            P.op("act", lambda tg=tg, bG2=bG2: S.copy(out=gatesT[0:8, tg * 512:(tg + 1) * 512], in_=bG2[0:8, 0:512]), R=[kG2], W=["gatesT"])

    nexp = NEXP if moe else 1
    dff = D_FFE if moe else D_FF
    pieces = []
    o = 0
    while o < dff:
        w = min(512, dff - o)
        pieces.append((o, w))
        o += w
    pi = 0
    for e in range(nexp):
        if moe:
            for half in range(2):
                for q in range(2):
                    bg_, kg_ = pb()
                    P.op("pe", lambda e=e, half=half, q=q, bg_=bg_: T.matmul(bg_[:, 0:512], lhsT=sel8[0:8, e * 128:(e + 1) * 128],
                                                                              rhs=gatesT[0:8, half * 1024 + q * 512:half * 1024 + (q + 1) * 512], start=True, stop=True),
                         R=["gatesT", "cst"], W=[kg_])
                    P.op("act", lambda half=half, q=q, bg_=bg_: S.copy(out=gbc[half][:, q * 512:(q + 1) * 512], in_=bg_[:, 0:512]), R=[kg_], W=["gbc%d" % half])
        for (o, w) in pieces:
            nb = w // 128
            wb = pi % 2
            kwg, kwd = "wgu%d" % wb, "wd%d" % wb
            P.dma("pool", wgu[wb][:, :, 0:w], wg_d[e].rearrange("(c p) n -> p c n", p=128)[:, :, o:o + w], W=[kwg])
            P.dma("pool", wgu[wb][:, :, 512:512 + w], wu_d[e].rearrange("(c p) n -> p c n", p=128)[:, :, o:o + w], W=[kwg])
            P.dma("pool", wdb[wb][:, 0:nb, :], wd_d[e][o:o + w, :].rearrange("(c p) n -> p c n", p=128), W=[kwd])
            for half in range(2):
                for blk in range(nb):
                    for q in range(2):
                        tg = half * 2 + q
                        Tg = slice(tg * 512, (tg + 1) * 512)
                        bg_, kg_ = pb()
                        bu_, ku_ = pb()

                        def gu(blk=blk, Tg=Tg, bg_=bg_, bu_=bu_, wb=wb):
                            r = None
                            for kc in range(8):
                                T.matmul(bg_[:, 0:512], lhsT=wgu[wb][:, kc, blk * 128:(blk + 1) * 128], rhs=hT[:, kc, Tg], start=(kc == 0), stop=(kc == 7))
                            for kc in range(8):
                                r = T.matmul(bu_[:, 0:512], lhsT=wgu[wb][:, kc, 512 + blk * 128:512 + (blk + 1) * 128], rhs=hT[:, kc, Tg], start=(kc == 0), stop=(kc == 7))
                            return r
                        P.op("pe", gu, R=[kwg, ("hT", tg)], W=[kg_, ku_])
                        sb_ = sgt[(blk * 2 + q) % 2]
                        sk_ = "sgt%d" % ((blk * 2 + q) % 2)
                        P.op("act", lambda bg_=bg_, sb_=sb_: S.activation(out=sb_, in_=bg_[:, 0:512], func=AF.Silu), R=[kg_], W=[sk_])
                        P.op("dve", lambda half=half, blk=blk, q=q, bu_=bu_, sb_=sb_: V.tensor_tensor(out=aT[half][:, blk, q * 512:(q + 1) * 512], in0=bu_[:, 0:512], in1=sb_, op=ALU.mult),
                             R=[ku_, sk_], W=[("aT", half, blk, q)])
            for half in range(2):
                for fc in range(8):
                    for q in range(2):
                        tg = half * 2 + q
                        Tg = slice(tg * 512, (tg + 1) * 512)
                        bo_, ko_ = pb()

                        def dn(half=half, fc=fc, q=q, bo_=bo_, wb=wb, nb=nb):
                            r = None
                            for blk in range(nb):
                                r = T.matmul(bo_[:, 0:512], lhsT=wdb[wb][:, blk, fc * 128:(fc + 1) * 128], rhs=aT[half][:, blk, q * 512:(q + 1) * 512],
                                             start=(blk == 0), stop=(blk == nb - 1))
                            return r
                        P.op("pe", dn, R=[kwd] + [("aT", half, blk, q) for blk in range(nb)], W=[ko_])
                        eb = evt[(fc * 2 + q) % 2]
                        ek = "evt%d" % ((fc * 2 + q) % 2)
                        if moe:
                            P.op("dve", lambda fc=fc, half=half, q=q, bo_=bo_, eb=eb: V.scalar_tensor_tensor(out=eb, in0=bo_[:, 0:512], scalar=g1f[:, fc:fc + 1],
                                                                                                          in1=gbc[half][:, q * 512:(q + 1) * 512], op0=ALU.mult, op1=ALU.mult),
                                 R=[ko_, "der", "gbc%d" % half], W=[ek])
                        else:
                            P.op("dve", lambda fc=fc, bo_=bo_, eb=eb: V.tensor_scalar(out=eb, in0=bo_[:, 0:512], scalar1=g1f[:, fc:fc + 1], scalar2=None, op0=ALU.mult),
                                 R=[ko_, "der"], W=[ek])
                        xkeys = [("xT", n, fc // 4) for n in range(tg * 4, tg * 4 + 4)]
                        P.op("pool", lambda fc=fc, Tg=Tg, eb=eb: G.tensor_tensor(out=xT[:, fc, Tg], in0=xT[:, fc, Tg], in1=eb, op=ALU.add),
                             R=[ek] + xkeys, W=xkeys)
            pi += 1

    P.barrier()
    ar = Arena(arena_t, AW)
    sq = [ar.alloc([128, 512]) for _ in range(2)]
    lnst = ar.alloc([128, 4, 512])
    lnk = ["lnst"]
    for tg in range(4):
        Tg = slice(tg * 512, (tg + 1) * 512)
        xkeys = [("xT", n, hf_) for n in range(tg * 4, tg * 4 + 4) for hf_ in range(2)]
        ln_inplace(512, xkeys, xT[:, :, Tg], GA2, BA2, 512)

    xo = [ar.alloc([128, D]) for _ in range(2)]
    for n in range(NCH):
        buf = xo[n % 2]
        bk = "xo%d" % (n % 2)
        for half in range(2):
            bank, bkey = pb()

            def tr2(n=n, half=half, bank=bank):
                r = None
                for q in range(4):
                    fc = half * 4 + q
                    r = T.transpose(bank[:, q * 128:(q + 1) * 128], xT[:, fc, n * 128:(n + 1) * 128], ident)
                return r
            P.op("pe", tr2, R=[("xT", n, 0), ("xT", n, 1), "cst"], W=[bkey])
            P.op("act", lambda half=half, bank=bank, buf=buf: S.copy(out=buf[:, half * 512:(half + 1) * 512], in_=bank[:, 0:512]), R=[bkey], W=[bk + "_%d" % half])
        P.dma(sp, xo_d[n * 128:(n + 1) * 128, :], buf, R=[bk + "_0", bk + "_1"], is_output=True)
    return P.emit()


FSTOP = [99]
NR = 4
AONLY = [True]
NEXPRUN = [99]
MAINCH = [99]
MSUB = [99]
DECL_IN = set()


def build_fused():
    P = Prog()
    nc = P.nc
    V, S, G, T = EngProxy(P, "dve", nc.vector), EngProxy(P, "act", nc.scalar), EngProxy(P, "pool", nc.gpsimd), nc.tensor

    def din(name, shape, dt=F32):
        DECL_IN.add(name)
        return nc.dram_tensor(name, list(shape), dt, kind="ExternalInput").ap()

    def dout(name, shape, dt=F32):
        return nc.dram_tensor(name, list(shape), dt, kind="ExternalOutput").ap()

    def dbg(name, ap, keys):
        if name not in DBG:
            return
        shp = list(ap.shape)
        d_ = dout("dbg_" + name, shp, ap.dtype)
        P.dma("sp", d_, ap, R=list(keys), is_output=True)

    x_d = din("xin", [NTOK + 128, D])
    pos_d = din("pos", [128, NCH], I32)
    cst_d = din("cst", [128, CW])
    misc_all = din("misc", [DEPTH, 128, MW])
    rowp_all = din("rowp", [DEPTH, RW])
    relb_d = din("relb", [128])
    selw_d = din("selw", [128, 32])
    w_in_all = din("w_in", [DEPTH, D, IN_DIM])
    w_out_all = din("w_out", [DEPTH, D, D])
    wada_all = din("w_ada", [DEPTH, D, 6 * D])
    eoh_d = din("eoh", [128, 256 * 32])
    wg0_d = din("wg0", [1, D, D_FF])
    wu0_d = din("wu0", [1, D, D_FF])
    wd0_d = din("wd0", [1, D_FF, D])
    if FSTOP[0] >= 14:
        wg1_d = din("wg1", [NEXP, D, D_FFE])
        wu1_d = din("wu1", [NEXP, D, D_FFE])
        wd1_d = din("wd1", [NEXP, D_FFE, D])
        rw_d = din("rw", [128, 8, 8])
    else:
        wg1_d = wu1_d = wd1_d = rw_d = None
    xo_d = dout("xout", [NTOK, D])
    bounce_s = [nc.dram_tensor("bounce_s%d" % i, [128, STW], F32).ap() for i in range(DEPTH)]
    gath_s = [nc.dram_tensor("gath_s%d" % i, [NR * 128, STW], F32).ap() for i in range(DEPTH)]
    bounce_h = nc.dram_tensor("bounce_h", [128, D], F32).ap()
    gath_h = nc.dram_tensor("gath_h", [NR * 128, D], F32).ap()
    ALLC = [[0, 1, 2, 3], [4, 5, 6, 7]]

    xT = P.sbuf("xT", [128, 8, NTOK], F32)
    cst = P.sbuf("cst_sb", [128, CW], F32)
    misc = P.sbuf("misc_sb", [128, MW], F32)
    rowp = P.sbuf("rowp_sb", [128, RW], F32)
    ident_b = P.sbuf("ident_b", [128, 128], BF16)
    AW = 33700
    arena_t = P.sbuf("arena", [128, AW], F32)
    TAIL = AW - 2048
    cosT = arena_t[:, TAIL:TAIL + 512].rearrange("p (n f) -> p n f", f=32)
    sinT = arena_t[:, TAIL + 512:TAIL + 1024].rearrange("p (n f) -> p n f", f=32)
    biasw = arena_t[:, TAIL + 1024:TAIL + 2048].rearrange("p (h j) -> p h j", h=4)
    ps = [P.psum("ps%d" % i, [128, 512], F32) for i in range(8)]
    psk = ["ps%d" % i for i in range(8)]
    pctr = [0]

    def pb():
        i = pctr[0] % 8
        pctr[0] += 1
        return ps[i], psk[i]

    ident = cst[:, C_ID:C_ID + 128]
    tri = cst[:, C_TRI:C_TRI + 128]
    mgt = cst[:, C_MGT:C_MGT + 128]
    ones = cst[:, C_ONE:C_ONE + 128]
    dq = cst[:, C_DQ:C_DQ + 4]
    dk = cst[:, C_DK:C_DK + 4]
    gC = cst[:, C_GC:C_GC + 2]
    invf = cst[:, C_INV:C_INV + 32]
    madd = cst[:, C_MADD:C_MADD + 256]
    wret = cst[:, C_WRET:C_WRET + 6]
    m0 = cst[:, C_M0:C_M0 + 1]
    m1 = cst[:, C_M0 + 1:C_M0 + 2]
    sel8 = cst[:, C_SEL:C_SEL + 8 * 128]

    def mcol(o, n):
        return misc[:, o:o + n]
    A_in, B_in = mcol(M_AIN, 8), mcol(M_BIN, 8)
    g1a, g1f = mcol(M_G1A, 8), mcol(M_G1F, 8)
    convw = mcol(M_CW, 24)
    convb = mcol(M_CB, 6)
    halovalid = mcol(M_HV, 1)
    halomask = mcol(M_HM, 1)
    modT = mcol(M_MOD, 96)
    lncol = mcol(M_LN, 32)
    ccol = mcol(M_C, 8)
    badaT = mcol(M_BADA, 96)
    dcol = mcol(M_DER, 64)
    rbias = mcol(M_RB, 8)

    dtb = rowp[:, R_DTB:R_DTB + 8]
    alog = rowp[:, R_ALOG:R_ALOG + 8]
    dskip = rowp[:, R_DSK:R_DSK + 8]
    normw = rowp[:, R_NW:R_NW + 512]
    sinks = rowp[:, R_SINK:R_SINK + 4]

    selw = P.sbuf("selw_sb", [128, 32], F32)
    relbt = P.sbuf("relb_sb", [128, 128], F32)
    relb = relbt[:, 0:128]
    sp = "sp"
    P.dma(sp, cst[:], cst_d, W=["cst"])
    P.dma(sp, selw[:], selw_d, W=["selw"])
    P.dma(sp, relbt[:], relb_d.partition_broadcast(128), W=["relb"])
    P.op("dve", lambda: V.tensor_copy(out=ident_b[:], in_=ident), R=["cst"], W=["ident_b"])

    ar = Arena(arena_t, TAIL)
    posi = ar.alloc([128, NCH], I32)
    posf = ar.alloc([128, NCH])
    ang = ar.alloc([128, NCH, 32])
    ang2 = ar.alloc([128, NCH, 32])
    ti = ar.alloc([128, NCH, 32], I32)
    tf = ar.alloc([128, NCH, 32])
    P.dma(sp, posi, pos_d, W=["posi"])

    def rot_tables():
        V.tensor_copy(out=posf, in_=posi)
        V.tensor_tensor(out=ang, in0=_bc(posf.unsqueeze(2), [128, NCH, 32]), in1=_bc(invf.unsqueeze(1), [128, NCH, 32]), op=ALU.mult)
        V.tensor_scalar(out=ang, in0=ang, scalar1=float(1.0 / (2 * np.pi)), scalar2=None, op0=ALU.mult)
        V.tensor_scalar(out=ang2, in0=ang, scalar1=0.25, scalar2=None, op0=ALU.add)
        r = None
        for a in (ang, ang2):
            V.tensor_copy(out=ti, in_=a)
            V.tensor_copy(out=tf, in_=ti)
            V.tensor_tensor(out=a, in0=a, in1=tf, op=ALU.subtract)
            V.tensor_scalar(out=tf, in0=a, scalar1=0.5, scalar2=None, op0=ALU.is_gt)
            V.tensor_tensor(out=a, in0=a, in1=tf, op=ALU.subtract)
            V.tensor_scalar(out=tf, in0=a, scalar1=-0.5, scalar2=None, op0=ALU.is_lt)
            r = V.tensor_tensor(out=a, in0=a, in1=tf, op=ALU.add)
        return r
    P.op("dve", rot_tables, R=["posi", "cst"], W=["ang"])

    def rot_sin():
        S.activation(out=sinT, in_=ang, func=AF.Sin, scale=float(2 * np.pi))
        return S.activation(out=cosT, in_=ang2, func=AF.Sin, scale=float(2 * np.pi))
    P.op("act", rot_sin, R=["ang"], W=["rot"])

    if True:
        eoh = ar.alloc([128, 256, 32])
        etmp = ar.alloc([128, 256, 32])
        P.dma(sp, eoh.rearrange("p a b -> p (a b)"), eoh_d, W=["eoh"])
        rb3 = relb.rearrange("p (b h) -> p b h", h=4)
        for h in range(4):
            P.op("pool", lambda h=h: G.tensor_tensor(out=etmp, in0=eoh, in1=_bc(rb3[:, :, h].unsqueeze(1), [128, 256, 32]), op=ALU.mult),
                 R=["eoh", "relb"], W=["etmp"])

            def red(h=h):
                V.tensor_reduce(out=biasw[:, h, :], in_=etmp, axis=AX.X, op=ALU.add)
                return V.tensor_tensor(out=biasw[:, h, :], in0=biasw[:, h, :], in1=madd, op=ALU.add)
            P.op("dve", red, R=["etmp", "cst"], W=["biasw"])
    P.barrier()

    def layer(L):
        moe = (L % 2 == 1)
        last = (L == DEPTH - 1)
        w_in_d, w_out_d = w_in_all[L], w_out_all[L]
        wg_d, wu_d, wd_d = (wg1_d, wu1_d, wd1_d) if moe else (wg0_d, wu0_d, wd0_d)
        LIM = AW if moe else TAIL
        P.barrier()
        P.dma(sp, misc[:], misc_all[L], W=["misc", "der", "mod"])
        P.dma(sp, rowp[:], rowp_all[L].partition_broadcast(128), W=["rowp"])

        P.op("act", lambda: S.activation(out=alog, in_=alog, func=AF.Exp), R=["rowp"], W=["rowp"])
        P.op("dve", lambda: V.tensor_scalar(out=alog, in0=alog, scalar1=-1.0, scalar2=None, op0=ALU.mult), R=["rowp"], W=["rowp"])
        negA = alog
        dbg("rowp", rowp[:, 0:32], ["rowp"])
        dbg("modT", modT[:, 0:48], ["mod"])

        ar = Arena(arena_t, TAIL)
        wada_d = wada_all[L:L + 1]
        wada_sb = [ar.alloc([128, 8, 512]) for _ in range(2)]
        bank_mod, kmod = pb()
        nl = 1
        j = 0
        for li in range(nl):
            for cg in range(12):
                buf = wada_sb[j % 2]
                bk = "wada%d" % (j % 2)
                P.dma(sp, buf, wada_d[li].rearrange("(c p) n -> p c n", p=128)[:, :, cg * 512:(cg + 1) * 512], W=[bk])
                for cc in range(4):
                    col = li * 48 + cg * 4 + cc

                    def mm(buf=buf, cc=cc, col=col):
                        r = None
                        for kc in range(8):
                            r = T.matmul(bank_mod[:, col:col + 1], lhsT=buf[:, kc, cc * 128:(cc + 1) * 128],
                                         rhs=ccol[:, kc:kc + 1], start=(kc == 0), stop=(kc == 7))
                        return r
                    P.op("pe", mm, R=[bk, "misc"], W=[kmod])
                j += 1
        P.op("dve", lambda: V.tensor_tensor(out=modT[:, 0:48 * nl], in0=bank_mod[:, 0:48 * nl], in1=badaT[:, 0:48 * nl], op=ALU.add),
             R=[kmod, "misc"], W=["mod"])
        lg1, lb1, lg2, lb2 = lncol[:, 0:8], lncol[:, 8:16], lncol[:, 16:24], lncol[:, 24:32]
        GA1, BA1 = dcol[:, 0:8], dcol[:, 8:16]
        A2, B2 = dcol[:, 16:24], dcol[:, 24:32]
        GA2, BA2 = dcol[:, 32:40], dcol[:, 40:48]
        tmpc = dcol[:, 48:56]

        def der():
            V.tensor_scalar(out=A_in, in0=modT[:, 8:16], scalar1=1.0, scalar2=1.0 / ALPHA, op0=ALU.add, op1=ALU.mult)
            V.tensor_copy(out=B_in, in_=modT[:, 0:8])
            V.tensor_scalar(out=g1a, in0=modT[:, 16:24], scalar1=1.0, scalar2=None, op0=ALU.add)
            V.tensor_scalar(out=g1f, in0=modT[:, 40:48], scalar1=1.0, scalar2=None, op0=ALU.add)
            V.tensor_scalar(out=GA1, in0=lg1, scalar1=ALPHA, scalar2=None, op0=ALU.mult)
            V.tensor_scalar(out=BA1, in0=lb1, scalar1=ALPHA, scalar2=None, op0=ALU.mult)
            V.tensor_scalar(out=A2, in0=modT[:, 32:40], scalar1=1.0, scalar2=1.0 / ALPHA, op0=ALU.add, op1=ALU.mult)
            V.tensor_copy(out=B2, in_=modT[:, 24:32])
            sc = 1.0 if last else ALPHA
            V.tensor_scalar(out=GA2, in0=lg2, scalar1=sc, scalar2=None, op0=ALU.mult)
            return V.tensor_scalar(out=BA2, in0=lb2, scalar1=sc, scalar2=None, op0=ALU.mult)
        P.op("dve", der, R=["mod", "misc"], W=["der"])
        P.barrier()

        if L == 0:
            ar = Arena(arena_t, TAIL)
            hT_halo = ar.alloc([128, 8, 128], BF16)
            _mark = ar.off
            xtok = [ar.alloc([128, D]) for _ in range(2)]
            for n in range(-1, NCH):
                buf = xtok[n % 2]
                bk = "xtok%d" % (n % 2)
                P.dma(sp, buf, x_d[(n + 1) * 128:(n + 2) * 128, :], W=[bk])
                for half in range(2):
                    bank, bkey = pb()

                    def tr(buf=buf, half=half, bank=bank):
                        r = None
                        for q in range(4):
                            fc = half * 4 + q
                            r = T.transpose(bank[:, q * 128:(q + 1) * 128], buf[:, fc * 128:(fc + 1) * 128], ident)
                        return r
                    P.op("pe", tr, R=[bk, "cst"], W=[bkey])
                    if n >= 0:
                        P.op("act", lambda n=n, half=half, bank=bank: S.mul(out=xT[:, half * 4:half * 4 + 4, n * 128:(n + 1) * 128],
                                                                           in_=bank[:].rearrange("p (q t) -> p q t", q=4), mul=ALPHA),
                             R=[bkey], W=[("xT", n, half)])
                    else:
                        def hh(half=half, bank=bank):
                            r = None
                            for q in range(4):
                                fc = half * 4 + q
                                r = V.tensor_scalar(out=hT_halo[:, fc, :], in0=bank[:, q * 128:(q + 1) * 128],
                                                    scalar1=A_in[:, fc:fc + 1], scalar2=B_in[:, fc:fc + 1], op0=ALU.mult, op1=ALU.add)
                            return r
                        def hh2(half=half, bank=bank):
                            r = None
                            for q in range(4):
                                fc = half * 4 + q
                                V.tensor_scalar(out=tmpc[:, 0:1], in0=A_in[:, fc:fc + 1], scalar1=ALPHA, scalar2=None, op0=ALU.mult)
                                r = V.tensor_scalar(out=hT_halo[:, fc, :], in0=bank[:, q * 128:(q + 1) * 128],
                                                    scalar1=tmpc[:, 0:1], scalar2=B_in[:, fc:fc + 1], op0=ALU.mult, op1=ALU.add)
                            return r
                        P.op("dve", hh2, R=[bkey, "misc", "der"], W=["hT_halo", "der"])

        else:
            ar = Arena(arena_t, TAIL)
            hT_halo = ar.alloc([128, 8, 128], BF16)
            _mark = ar.off
            halo_g = ar.alloc([128, NR, D])
            hacc = ar.alloc([128, 8, 128])
            P.dma(sp, bounce_h.rearrange("p (c t) -> p c t", c=8), xT[:, :, NTOK - 128:NTOK], R=[("xT", NCH - 1, 0), ("xT", NCH - 1, 1)], W=["bounce_h"])
            P.coll("AllGather", gath_h, bounce_h, ALLC, R=["bounce_h"], W=["gath_h"])
            P.dma(sp, halo_g, gath_h.rearrange("(r p) w -> p r w", p=128), R=["gath_h"], W=["halo_g"])

            def hsel():
                hf_ = hacc.rearrange("p c t -> p (c t)")
                V.tensor_scalar(out=hf_, in0=halo_g[:, 0, :], scalar1=selw[:, 0:1], scalar2=None, op0=ALU.mult)
                for r_ in range(1, NR):
                    V.scalar_tensor_tensor(out=hf_, in0=halo_g[:, r_, :], scalar=selw[:, r_:r_ + 1], in1=hf_, op0=ALU.mult, op1=ALU.add)
                r = None
                for fc in range(8):
                    r = V.tensor_scalar(out=hT_halo[:, fc, :], in0=hacc[:, fc, :], scalar1=A_in[:, fc:fc + 1], scalar2=B_in[:, fc:fc + 1],
                                        op0=ALU.mult, op1=ALU.add)
                return r
            P.op("dve", hsel, R=["halo_g", "selw", "misc", "der"], W=["hT_halo", "hacc"])

        P.barrier()
        ar.off = _mark
        w_in = ar.alloc([128, 8, IN_DIM], BF16)
        wcols = "w_in_d"
        wi_src = w_in_d.rearrange("(c p) n -> p c n", p=128)
        for a, b in ((0, 1024), (1024, 2048), (2048, 2312), (2568, 2824)):
            P.dma("pool", w_in[:, :, a:b], wi_src[:, :, a:b], W=["w_in"])
        for slot, h in enumerate((0, 2, 1, 3)):
            P.dma("pool", w_in[:, :, 2312 + slot * 64:2312 + (slot + 1) * 64], wi_src[:, :, 2312 + h * 64:2312 + (h + 1) * 64], W=["w_in"])
        if True:
            w_out = ar.alloc([128, 8, D], BF16)
            P.dma("pool", w_out, w_out_d.rearrange("(c p) n -> p c n", p=128), W=["w_out"])

        Sret = ar.alloc([128, 2, 64])
        Sret_b = ar.alloc([128, 2, 64], BF16)
        Sssd = ar.alloc([128, 256])
        Sssd_b = ar.alloc([128, 256], BF16)
        totacc = ar.alloc([128, 8])
        def zinit():
            V.memset(Sret, 0.0)
            V.memset(Sssd, 0.0)
            return V.memset(totacc, 0.0)
        P.op("dve", zinit, W=["Sret", "Sssd", "totacc"])

        _off_tmp = ar.off
        hTc = [ar.alloc([128, 8, 128], BF16) for _ in range(2)]
        qk_sb = ar.alloc([128, 512])
        qr = ar.alloc([128, 4, 2, 32])
        kr = ar.alloc([128, 4, 2, 32])
        rt = [ar.alloc([128, 4, 32]) for _ in range(4)]
        q2b = ar.alloc([128, 256], BF16)
        k2b = ar.alloc([128, 256], BF16)
        v_b = ar.alloc([128, 256], BF16)
        sg = ar.alloc([128, 256])
        qkT = ar.alloc([128, 512], BF16)
        qm = ar.alloc([128, 4, 128], BF16)
        BCm = ar.alloc([128, 4, 128], BF16)
        sTm = ar.alloc([128, 512], BF16)
        osq = qr.rearrange("p h two f -> p h (two f)")
        onr = kr.rearrange("p h two f -> p h (two f)")
        gst = ar.alloc([128, 16])
        xr = ar.alloc([128, 6, 131])
        acc = ar.alloc([128, 6, 128])
        bc_b = ar.alloc([128, 2, 128], BF16)
        Bm_b = ar.alloc([128, 128], BF16)
        amask = ar.alloc([128, 8, 128])
        eseg = ar.alloc([128, 8, 128], BF16)
        mT = ar.alloc([128, 8, 128], BF16)
        cbm = ar.alloc([128, 2, 128])
        xdt_b = ar.alloc([128, 8, 64], BF16)
        xdd_b = ar.alloc([128, 8, 64], BF16)
        xskip = ar.alloc([128, 8, 64])
        t1 = ar.alloc([128, 8, 64])
        szs = ar.alloc([128, 512])
        hsq = ar.alloc([128, 512])
        sm = ar.alloc([128, 64])
        qT_s = ar.alloc([128, 4, 128], BF16)
        kT_pp = [ar.alloc([128, 128], BF16) for _ in range(2)]
        v_pp = [ar.alloc([128, 128], BF16) for _ in range(2)]
        sl = amask.rearrange("p h l -> p (h l)").rearrange("p (h j) -> p h j", h=4)
        p_b = ar.alloc([128, 4, 256], BF16)
        pT_b = ar.alloc([128, 8, 128], BF16)
        ssw = ar.alloc([128, 32])
        mix_tok = ar.alloc([128, D], BF16)
        mixT = ar.alloc([128, 8, 128], BF16)
        sq = [ar.alloc([128, 128]) for _ in range(2)]
        lnst = t1.rearrange("p h d -> p (h d)").rearrange("p (a b) -> p a b", a=4)
        lnk = ["t1"]
        mixer_arena_end = ar.off

        def zmask():
            V.memset(qm, 0.0)
            V.memset(BCm, 0.0)
            return V.memset(qT_s, 0.0)


        dtv, av_, acs_tot, ed, eaed, cdec, dd = (sm[:, 0:8], sm[:, 8:16], sm[:, 16:32], sm[:, 32:48], sm[:, 48:64], None, None)

        sm2 = ar.alloc([128, 32])
        cdec = sm2[:, 0:8]
        dd = sm2[:, 8:16]
        rr = sm2[:, 16:24]

        def ln_inplace(n_cols, xs_keyR, xview, GA, BA, nfree):
            bank, bkey = pb()
            bank2, bkey2 = (bank[:, nfree:2 * nfree], bkey) if nfree <= 256 else pb()
            if nfree > 256:
                bank2 = bank2[:, 0:nfree]
            for fc in range(8):
                sqb = sq[fc % 2]
                sk = "sq%d" % (fc % 2)
                P.op("pool", lambda fc=fc, sqb=sqb: G.tensor_tensor(out=sqb[:, 0:nfree], in0=xview[:, fc, :], in1=xview[:, fc, :], op=ALU.mult),
                     R=xs_keyR, W=[sk])

                def mm(fc=fc, sqb=sqb, bank=bank):
                    T.matmul(bank[:, 0:nfree], lhsT=ones, rhs=xview[:, fc, :], start=(fc == 0), stop=(fc == 7))
                    return T.matmul(bank2, lhsT=ones, rhs=sqb[:, 0:nfree], start=(fc == 0), stop=(fc == 7))
                P.op("pe", mm, R=xs_keyR + [sk, "cst"], W=[bkey, bkey2])
            mean, msq, var, rstd = lnst[:, 0, 0:nfree], lnst[:, 1, 0:nfree], lnst[:, 2, 0:nfree], lnst[:, 3, 0:nfree]

            def st(bank=bank):
                V.tensor_scalar(out=mean, in0=bank[:, 0:nfree], scalar1=1.0 / D, scalar2=None, op0=ALU.mult)
                V.tensor_tensor(out=msq, in0=mean, in1=mean, op=ALU.mult)
                V.scalar_tensor_tensor(out=var, in0=bank2, scalar=1.0 / D, in1=msq, op0=ALU.mult, op1=ALU.subtract)
                return V.tensor_scalar(out=var, in0=var, scalar1=EPS, scalar2=None, op0=ALU.add)
            P.op("dve", st, R=[bkey, bkey2], W=[lnk[0]])
            P.op("act", lambda: S.sqrt(out=rstd, in_=var), R=[lnk[0]], W=[lnk[0]])

            def nrm():
                V.reciprocal(out=rstd, in_=rstd)
                V.tensor_tensor(out=xview, in0=xview, in1=_bc(mean.unsqueeze(1), [128, 8, nfree]), op=ALU.subtract)
                return V.tensor_tensor(out=xview, in0=xview, in1=_bc(rstd.unsqueeze(1), [128, 8, nfree]), op=ALU.mult)
            P.op("dve", nrm, R=[lnk[0]] + xs_keyR, W=xs_keyR + [lnk[0]])

            def aff():
                G.tensor_tensor(out=xview, in0=xview, in1=_bc(GA.unsqueeze(2), [128, 8, nfree]), op=ALU.mult)
                return G.tensor_tensor(out=xview, in0=xview, in1=_bc(BA.unsqueeze(2), [128, 8, nfree]), op=ALU.add)
            P.op("pool", aff, R=xs_keyR + ["der", "misc"], W=xs_keyR)

        def chunk(n):
            halo = n < 0
            cur, prv = (n % 2), ((n + 1) % 2)
            if halo:
                hc = hT_halo
                hk = "hT_halo"
            else:
                hc = hTc[n % 2]
                hk = "hTc%d" % (n % 2)
                xk = [("xT", n, 0), ("xT", n, 1)]
                Tn = slice(n * 128, (n + 1) * 128)

                def mkh2():
                    r = None
                    for fc in range(8):
                        r = G.tensor_scalar(out=hc[:, fc, :], in0=xT[:, fc, Tn], scalar1=A_in[:, fc:fc + 1], scalar2=B_in[:, fc:fc + 1],
                                            op0=ALU.mult, op1=ALU.add)
                    return r
                P.op("pool", mkh2, R=xk + ["misc", "der"], W=[hk])

            def proj_tok(bank, c0, c1, o0=0):
                def f():
                    r = None
                    for kc in range(8):
                        r = T.matmul(bank[:, o0:o0 + (c1 - c0)], lhsT=hc[:, kc, :], rhs=w_in[:, kc, c0:c1], start=(kc == 0), stop=(kc == 7))
                    return r
                return f

            def proj_feat(bank, c0, o0):
                def f():
                    r = None
                    for kc in range(8):
                        r = T.matmul(bank[:, o0:o0 + 128], lhsT=w_in[:, kc, c0:c0 + 128], rhs=hc[:, kc, :], start=(kc == 0), stop=(kc == 7))
                    return r
                return f

            bD, kD = pb()
            bE, kE = pb()
            bF, kF = pb()
            if full:
                P.op("pe", proj_tok(bD, 2696, 2824, 8), R=[hk, "w_in"], W=[kD])
                P.op("pe", proj_feat(bD, 2568, 256), R=[hk, "w_in"], W=[kD])
            for c in range(4):
                P.op("pe", proj_feat(bE, 1536 + c * 128, c * 128), R=[hk, "w_in"], W=[kE])
            P.op("pe", proj_feat(bF, 2048, 0), R=[hk, "w_in"], W=[kF])
            P.op("pe", proj_feat(bF, 2176, 128), R=[hk, "w_in"], W=[kF])
            if halo:
                def tail():
                    V.tensor_scalar(out=xr[:, 0:4, 128:131], in0=bE[:].rearrange("p (c t) -> p c t", c=4)[:, :, 125:128],
                                    scalar1=halovalid, scalar2=None, op0=ALU.mult)
                    return V.tensor_scalar(out=xr[:, 4:6, 128:131], in0=bF[:, 0:256].rearrange("p (c t) -> p c t", c=2)[:, :, 125:128],
                                           scalar1=halovalid, scalar2=None, op0=ALU.mult)
                P.op("dve", tail, R=[kE, kF, "misc"], W=["xr"])
                if full:
                    def kv():
                        S.copy(out=kT_pp[cur], in_=bD[:, 256:384])
                        return S.copy(out=v_pp[cur], in_=bD[:, 8:136])
                    P.op("act", kv, R=[kD], W=["kT_pp%d" % cur, "v_pp%d" % cur])
                return
            P.op("pe", proj_tok(bD, 2304, 2312, 0), R=[hk, "w_in"], W=[kD])
            bA, kA = pb()
            bB, kB = pb()
            if full:
                P.op("pe", proj_tok(bA, 0, 512), R=[hk, "w_in"], W=[kA])
                P.op("pe", proj_tok(bB, 512, 1024), R=[hk, "w_in"], W=[kB])
                bC, kC = pb()
                P.op("pe", proj_tok(bC, 1024, 1536), R=[hk, "w_in"], W=[kC])
                P.op("pe", proj_feat(bF, 2312, 256), R=[hk, "w_in"], W=[kF])
                P.op("pe", proj_feat(bF, 2440, 384), R=[hk, "w_in"], W=[kF])
            else:
                P.op("pe", proj_tok(bA, 256, 512, 256), R=[hk, "w_in"], W=[kA])
                P.op("pe", proj_tok(bB, 512, 768, 0), R=[hk, "w_in"], W=[kB])

            P.op("dve", lambda: V.tensor_tensor(out=dtv, in0=bD[:, 0:8], in1=dtb, op=ALU.add), R=[kD, "rowp"], W=["dtv"])
            P.op("pool", lambda: G.tensor_copy(out=xr[:, :, 0:3], in_=xr[:, :, 128:131]), R=["xr"], W=["xr"])

            def xrcp():
                S.copy(out=xr[:, 0:4, 3:131], in_=bE[:].rearrange("p (c t) -> p c t", c=4))
                return S.copy(out=xr[:, 4:6, 3:131], in_=bF[:, 0:256].rearrange("p (c t) -> p c t", c=2))
            P.op("act", xrcp, R=[kE, kF], W=["xr"])
            P.op("act", lambda: S.copy(out=qk_sb[:, (0 if full else 256):512], in_=bA[:, (0 if full else 256):512]), R=[kA], W=["qk_sb"])
            P.op("act", lambda: S.copy(out=v_b, in_=bB[:, 0:256]), R=[kB], W=["v_b"])
            if full:
                def swc():
                    for h_ in range(4):
                        kvh_, gq_ = h_ // 2, h_ % 2
                        pr_ = slice(kvh_ * 64, kvh_ * 64 + 64)
                        S.copy(out=qT_s[pr_, h_, :], in_=bF[pr_, 256 + gq_ * 128:256 + (gq_ + 1) * 128])
                    S.copy(out=kT_pp[cur], in_=bD[:, 256:384])
                    return S.copy(out=v_pp[cur], in_=bD[:, 8:136])
                P.op("act", swc, R=[kF, kD], W=["qT_s", "kT_pp%d" % cur, "v_pp%d" % cur])
                P.op("act", lambda: S.activation(out=sg, in_=bB[:, 256:512], func=AF.Silu), R=[kB], W=["sg"])
                P.op("act", lambda: S.activation(out=szs, in_=bC[:], func=AF.Silu), R=[kC], W=["szs"])
            if SUB[0] < 1:
                return
            cosb = _bc(cosT[:, n, :].unsqueeze(1), [128, 4, 32])
            sinb = _bc(sinT[:, n, :].unsqueeze(1), [128, 4, 32])

            def rotary(E, src, dst, ta, tb):
                X = src.rearrange("p (h two f) -> p h two f", h=4, two=2)
                x1, x2 = X[:, :, 0, :], X[:, :, 1, :]
                E.tensor_tensor(out=ta, in0=x1, in1=cosb, op=ALU.mult)
                E.tensor_tensor(out=tb, in0=x2, in1=sinb, op=ALU.mult)
                E.tensor_tensor(out=dst[:, :, 0, :], in0=ta, in1=tb, op=ALU.subtract)
                E.tensor_tensor(out=ta, in0=x1, in1=sinb, op=ALU.mult)
                E.tensor_tensor(out=tb, in0=x2, in1=cosb, op=ALU.mult)
                return E.tensor_tensor(out=dst[:, :, 1, :], in0=ta, in1=tb, op=ALU.add)

            def krot():
                rotary(V, qk_sb[:, 256:512], kr, rt[2], rt[3])
                return V.tensor_tensor(out=k2b[:].rearrange("p (h d) -> p h d", h=4), in0=_bc(dk.unsqueeze(2), [128, 4, 64]),
                                       in1=kr.rearrange("p h two f -> p h (two f)"), op=ALU.mult)
            P.op("dve", krot, R=["qk_sb", "rot", "cst"], W=["kr", "k2b"])
            if full:
                def qrot():
                    rotary(V, qk_sb[:, 0:256], qr, rt[0], rt[1])
                    return V.tensor_tensor(out=q2b[:].rearrange("p (h d) -> p h d", h=4), in0=_bc(dq.unsqueeze(2), [128, 4, 64]),
                                           in1=qr.rearrange("p h two f -> p h (two f)"), op=ALU.mult)
                P.op("dve", qrot, R=["qk_sb", "rot", "cst"], W=["qr", "q2b"])
                if SUB[0] < 1.05:
                    return
                bT, kT_ = pb()
                bTb = bT[:].bitcast(BF16)

                def trqk():
                    r = None
                    for t in range(2):
                        T.transpose(bTb[:, t * 128:(t + 1) * 128], q2b[:, t * 128:(t + 1) * 128], ident_b[:])
                        r = T.transpose(bTb[:, 256 + t * 128:256 + (t + 1) * 128], k2b[:, t * 128:(t + 1) * 128], ident_b[:])
                    return r
                P.op("pe", trqk, R=["q2b", "k2b", "ident_b"], W=[kT_])
                def qkcp():
                    for h_ in range(4):
                        t_, hf2 = h_ // 2, h_ % 2
                        pr_ = slice(hf2 * 64, hf2 * 64 + 64)
                        S.copy(out=qm[pr_, h_, :], in_=bTb[pr_, t_ * 128:(t_ + 1) * 128])
                    return S.copy(out=qkT[:, 256:512], in_=bTb[:, 256:512])
                P.op("act", qkcp, R=[kT_], W=["qkT", "qm"])
                if SUB[0] < 1.1:
                    return
                bS, kS = pb()

                def scores():
                    r = None
                    import os
                    for h in [int(c_) for c_ in os.environ.get("SUBH", "0123")]:
                        t, hf_ = h // 2, h % 2
                        pr = slice(hf_ * 64, hf_ * 64 + 64)
                        r = T.matmul(bS[:, h * 128:(h + 1) * 128], lhsT=qkT[:, 256 + t * 128:256 + (t + 1) * 128],
                                     rhs=qm[:, h, :], start=True, stop=True)
                    return r
                P.op("pe", scores, R=["qkT", "qm"], W=[kS])
                if SUB[0] < 1.15:
                    return
                P.op("dve", lambda: V.tensor_tensor(out=sTm[:].rearrange("p (h i) -> p h i", h=4), in0=_bc(tri.unsqueeze(1), [128, 4, 128]),
                                                    in1=bS[:].rearrange("p (h i) -> p h i", h=4), op=ALU.mult), R=[kS, "cst"], W=["sTm"])
                if SUB[0] < 1.2:
                    return
                bO, kO = pb()

                def oret():
                    r = None
                    for h in range(4):
                        t, hf_ = h // 2, h % 2
                        pr = slice(hf_ * 64, hf_ * 64 + 64)
                        T.matmul(bO[:, h * 64:(h + 1) * 64], lhsT=sTm[:, h * 128:(h + 1) * 128], rhs=v_b[:, h * 64:(h + 1) * 64], start=True, stop=False)
                        r = T.matmul(bO[:, h * 64:(h + 1) * 64], lhsT=qm[:, h, :], rhs=Sret_b[:, t, :], start=False, stop=True)
                    return r
                P.op("pe", oret, R=["sTm", "v_b", "qm", "Sret_b"], W=[kO])
            if SUB[0] < 1.3:
                return
            bK, kK = pb()

            def kvm():
                r = None
                for t in range(2):
                    r = T.matmul(bK[:, t * 128:(t + 1) * 128], lhsT=k2b[:, t * 128:(t + 1) * 128], rhs=v_b[:, t * 128:(t + 1) * 128], start=True, stop=True)
                return r
            P.op("pe", kvm, R=["k2b", "v_b"], W=[kK])

            if SUB[0] < 1.6:
                return

            def supd():
                K4 = bK[:, 0:256].rearrange("p (t hf e) -> p t hf e", t=2, hf=2)
                V.scalar_tensor_tensor(out=Sret, in0=K4[:, :, 0, :], scalar=m0, in1=Sret, op0=ALU.mult, op1=ALU.add)
                V.scalar_tensor_tensor(out=Sret, in0=K4[:, :, 1, :], scalar=m1, in1=Sret, op0=ALU.mult, op1=ALU.add)
                V.tensor_tensor(out=Sret, in0=Sret, in1=_bc(gC.unsqueeze(2), [128, 2, 64]), op=ALU.mult)
                return V.tensor_copy(out=Sret_b, in_=Sret)
            P.op("dve", supd, R=[kK, "Sret", "cst"], W=["Sret", "Sret_b"])
            if full:
                P.op("act", lambda: S.activation(out=osq, in_=bO[:, 0:256].rearrange("p (h d) -> p h d", h=4), func=AF.Square), R=[kO], W=["qr"])

                def gn1():
                    V.tensor_reduce(out=gst[:, 0:4], in_=bO[:, 0:256].rearrange("p (h d) -> p h d", h=4), axis=AX.X, op=ALU.add)
                    V.tensor_reduce(out=gst[:, 4:8], in_=osq, axis=AX.X, op=ALU.add)
                    V.tensor_scalar(out=gst[:, 0:4], in0=gst[:, 0:4], scalar1=1.0 / 64, scalar2=None, op0=ALU.mult)
                    V.tensor_tensor(out=gst[:, 8:12], in0=gst[:, 0:4], in1=gst[:, 0:4], op=ALU.mult)
                    V.scalar_tensor_tensor(out=gst[:, 4:8], in0=gst[:, 4:8], scalar=1.0 / 64, in1=gst[:, 8:12], op0=ALU.mult, op1=ALU.subtract)
                    return V.tensor_scalar(out=gst[:, 4:8], in0=gst[:, 4:8], scalar1=EPS, scalar2=None, op0=ALU.add)
                P.op("dve", gn1, R=[kO, "qr"], W=["gst"])
                P.op("act", lambda: S.sqrt(out=gst[:, 4:8], in_=gst[:, 4:8]), R=["gst"], W=["gst"])

                def gn2():
                    V.reciprocal(out=gst[:, 4:8], in_=gst[:, 4:8])
                    V.tensor_tensor(out=onr, in0=bO[:, 0:256].rearrange("p (h d) -> p h d", h=4), in1=_bc(gst[:, 0:4].unsqueeze(2), [128, 4, 64]), op=ALU.subtract)
                    V.tensor_tensor(out=onr, in0=onr, in1=_bc(gst[:, 4:8].unsqueeze(2), [128, 4, 64]), op=ALU.mult)
                    return V.tensor_tensor(out=mix_tok[:, 0:256], in0=onr.rearrange("p h d -> p (h d)"), in1=sg, op=ALU.mult)
                P.op("dve", gn2, R=[kO, "gst", "sg"], W=["kr", "gst", "mix_ret"])

            if SUB[0] < 2:
                return

            def conv(E, cs):
                def f():
                    r = None
                    for c in cs:
                        E.tensor_scalar(out=acc[:, c, :], in0=xr[:, c, 0:128], scalar1=convw[:, c * 4:c * 4 + 1], scalar2=convb[:, c:c + 1],
                                        op0=ALU.mult, op1=ALU.add)
                        for w in range(1, 4):
                            r = E.scalar_tensor_tensor(out=acc[:, c, :], in0=xr[:, c, w:w + 128], scalar=convw[:, c * 4 + w:c * 4 + w + 1],
                                                       in1=acc[:, c, :], op0=ALU.mult, op1=ALU.add)
                    return r
                return f
            P.op("dve", conv(V, (0, 1, 4)), R=["xr", "misc"], W=["accA"])
            P.op("dve", conv(V, (2, 3, 5)), R=["xr", "misc"], W=["accB"])

            def sil():
                S.activation(out=acc[:, 0:4, :], in_=acc[:, 0:4, :], func=AF.Silu)
                return S.activation(out=bc_b, in_=acc[:, 4:6, :], func=AF.Silu)
            P.op("act", sil, R=["accA", "accB"], W=["accA", "accB", "bc_b"])
            if full:
                def bcm():
                    r = None
                    for g_ in range(2):
                        pr_ = slice(g_ * 64, g_ * 64 + 64)
                        G.tensor_copy(out=BCm[pr_, g_, :], in_=bc_b[pr_, 0, :])
                        r = G.tensor_copy(out=BCm[pr_, 2 + g_, :], in_=bc_b[pr_, 1, :])
                    return r
                P.op("pool", bcm, R=["bc_b"], W=["BCm"])
            bX, kX = pb()

            def trx():
                r = None
                for c in range(4):
                    r = T.transpose(bX[:, c * 128:(c + 1) * 128], acc[:, c, :], ident)
                return r
            P.op("pe", trx, R=["accA", "accB", "cst"], W=[kX])
            bBm, kBm = pb()
            bBmb = bBm[:].bitcast(BF16)
            P.op("pe", lambda: T.transpose(bBmb[:, 0:128], bc_b[:, 0, :], ident_b[:]), R=["bc_b", "ident_b"], W=[kBm])
            P.op("act", lambda: S.copy(out=Bm_b, in_=bBmb[:, 0:128]), R=[kBm], W=["Bm_b"])
            if SUB[0] < 3:
                return
            def sp_():
                S.activation(out=dtv, in_=dtv, func=AF.Exp)
                return S.activation(out=dtv, in_=dtv, func=AF.Ln, bias=1.0)
            if n == 0:
                dbg("dtv_pre", dtv, ["dtv"])
            P.op("act", sp_, R=["dtv"], W=["dtv"])
            if n == 0:
                dbg("dtv", dtv, ["dtv"])
            P.op("dve", lambda: V.tensor_tensor(out=av_, in0=dtv, in1=negA, op=ALU.mult), R=["dtv", "rowp"], W=["av"])
            bY, kY = pb()

            def acsm():
                T.matmul(bY[:, 0:8], lhsT=tri, rhs=av_, start=True, stop=True)
                return T.matmul(bY[:, 8:16], lhsT=ones, rhs=av_, start=True, stop=True)
            P.op("pe", acsm, R=["av", "cst"], W=[kY])
            P.op("act", lambda: S.copy(out=acs_tot, in_=bY[:, 0:16]), R=[kY], W=["acs_tot"])
            if n == 0:
                dbg("av", av_, ["av"])
                dbg("acs_tot", acs_tot, ["acs_tot"])

            def edf():
                V.tensor_copy(out=ed[:, 0:8], in_=acs_tot[:, 0:8])
                return V.tensor_tensor(out=ed[:, 8:16], in0=acs_tot[:, 8:16], in1=acs_tot[:, 0:8], op=ALU.subtract)
            P.op("dve", edf, R=["acs_tot"], W=["ed"])

            def exps():
                S.activation(out=eaed, in_=ed, func=AF.Exp)
                return S.activation(out=cdec, in_=acs_tot[:, 8:16], func=AF.Exp)
            P.op("act", exps, R=["ed", "acs_tot"], W=["eaed", "cdec"])
            if not full:
                P.op("pool", lambda: G.tensor_tensor(out=totacc, in0=totacc, in1=acs_tot[:, 8:16], op=ALU.add), R=["acs_tot", "totacc"], W=["totacc"])
            P.op("dve", lambda: V.tensor_tensor(out=dd, in0=dtv, in1=eaed[:, 8:16], op=ALU.mult), R=["dtv", "eaed"], W=["dd"])
            X3 = bX[:].rearrange("p (h d) -> p h d", h=8)
            P.op("dve", lambda: V.tensor_tensor(out=xdd_b, in0=_bc(dd.unsqueeze(2), [128, 8, 64]), in1=X3, op=ALU.mult), R=[kX, "dd"], W=["xdd_b"])
            if full:
                def xd():
                    V.tensor_tensor(out=xdt_b, in0=_bc(dtv.unsqueeze(2), [128, 8, 64]), in1=X3, op=ALU.mult)
                    return V.tensor_tensor(out=xskip, in0=X3, in1=_bc(dskip.unsqueeze(2), [128, 8, 64]), op=ALU.mult)
                P.op("dve", xd, R=[kX, "dtv", "rowp"], W=["xdt_b", "xskip"])
                P.op("pool", lambda: G.tensor_tensor(out=amask, in0=_bc(mgt.unsqueeze(1), [128, 8, 128]), in1=_bc(av_.unsqueeze(2), [128, 8, 128]), op=ALU.mult),
                     R=["av", "cst"], W=["amask"])
                bCB, kCB = pb()

                def cbm_():
                    r = None
                    for g in range(2):
                        pr = slice(g * 64, g * 64 + 64)
                        r = T.matmul(bCB[:, g * 128:(g + 1) * 128], lhsT=BCm[:, g, :], rhs=bc_b[:, 1, :], start=True, stop=True)
                    return r
                P.op("pe", cbm_, R=["bc_b", "BCm"], W=[kCB])
                P.op("dve", lambda: V.tensor_tensor(out=cbm, in0=bCB[:, 0:256].rearrange("p (g l) -> p g l", g=2), in1=_bc(tri.unsqueeze(1), [128, 2, 128]), op=ALU.mult),
                     R=[kCB, "cst"], W=["cbm"])
                for g in range(2):
                    bSg, kSg = pb()

                    def segm(g=g, bSg=bSg):
                        r = None
                        for r_ in range(4):
                            r = T.matmul(bSg[:, r_ * 128:(r_ + 1) * 128], lhsT=amask[:, g * 4 + r_, :], rhs=tri, start=True, stop=True)
                        return r
                    P.op("pe", segm, R=["amask", "cst"], W=[kSg])
                    P.op("act", lambda g=g, bSg=bSg: S.activation(out=eseg[:, g * 4:g * 4 + 4, :], in_=bSg[:].rearrange("p (r l) -> p r l", r=4), func=AF.Exp),
                         R=[kSg], W=["eseg%d" % g])
                    P.op("dve", lambda g=g: V.tensor_tensor(out=mT[:, g * 4:g * 4 + 4, :], in0=_bc(cbm[:, g, :].unsqueeze(1), [128, 4, 128]),
                                                            in1=eseg[:, g * 4:g * 4 + 4, :], op=ALU.mult),
                         R=["eseg%d" % g, "cbm"], W=["mT%d" % g])
                bYD, kYD = pb()

                def ydm():
                    r = None
                    for h in range(8):
                        r = T.matmul(bYD[:, h * 64:(h + 1) * 64], lhsT=mT[:, h, :], rhs=xdt_b[:, h, :], start=True, stop=True)
                    return r
                P.op("pe", ydm, R=["mT0", "mT1", "xdt_b"], W=[kYD])
                bYO, kYO = pb()

                def yom():
                    r = None
                    for g in range(2):
                        pr = slice(g * 64, g * 64 + 64)
                        r = T.matmul(bYO[:, g * 256:(g + 1) * 256], lhsT=BCm[:, 2 + g, :], rhs=Sssd_b, start=True, stop=True)
                    return r
                P.op("pe", yom, R=["BCm", "Sssd_b"], W=[kYO])
            if SUB[0] < 4:
                return
            bST, kST = pb()
            P.op("pe", lambda: T.matmul(bST[:, 0:512], lhsT=Bm_b, rhs=xdd_b[:].rearrange("p h d -> p (h d)"), start=True, stop=True),
                 R=["Bm_b", "xdd_b"], W=[kST])

            def sssd():
                r = None
                for g in range(2):
                    pr = slice(g * 64, g * 64 + 64)
                    V.tensor_tensor(out=Sssd[pr, :].rearrange("p (r e) -> p r e", r=4), in0=Sssd[pr, :].rearrange("p (r e) -> p r e", r=4),
                                    in1=_bc(cdec[pr, g * 4:g * 4 + 4].unsqueeze(2), [64, 4, 64]), op=ALU.mult)
                    r = V.tensor_tensor(out=Sssd[pr, :], in0=Sssd[pr, :], in1=bST[pr, g * 256:(g + 1) * 256], op=ALU.add)
                return r
            P.op("dve", sssd, R=[kST, "Sssd", "cdec"], W=["Sssd"])
            if not full:
                return
            P.op("act", lambda: S.copy(out=Sssd_b, in_=Sssd), R=["Sssd"], W=["Sssd_b"])

            def ycomb():
                V.tensor_tensor(out=t1, in0=bYO[:].rearrange("p (h d) -> p h d", h=8), in1=_bc(eaed[:, 0:8].unsqueeze(2), [128, 8, 64]), op=ALU.mult)
                return V.tensor_tensor(out=t1, in0=t1, in1=bYD[:].rearrange("p (h d) -> p h d", h=8), op=ALU.add)
            P.op("dve", ycomb, R=[kYO, kYD, "eaed"], W=["t1"])
            t1f = t1.rearrange("p h d -> p (h d)")

            def yg():
                G.tensor_tensor(out=t1f, in0=t1f, in1=xskip.rearrange("p h d -> p (h d)"), op=ALU.add)
                G.tensor_tensor(out=t1f, in0=t1f, in1=szs, op=ALU.mult)
                return G.tensor_tensor(out=hsq, in0=t1f, in1=t1f, op=ALU.mult)
            P.op("pool", yg, R=["t1", "xskip", "szs"], W=["t1", "hsq"])

            def rms1():
                V.tensor_reduce(out=rr[:, 0:2], in_=hsq.rearrange("p (g e) -> p g e", g=2), axis=AX.X, op=ALU.add)
                return V.tensor_scalar(out=rr[:, 0:2], in0=rr[:, 0:2], scalar1=1.0 / 256, scalar2=EPS, op0=ALU.mult, op1=ALU.add)
            P.op("dve", rms1, R=["hsq"], W=["rr"])
            P.op("act", lambda: S.sqrt(out=rr[:, 0:2], in_=rr[:, 0:2]), R=["rr"], W=["rr"])

            def rms2():
                V.reciprocal(out=rr[:, 0:2], in_=rr[:, 0:2])
                V.tensor_tensor(out=hsq.rearrange("p (g e) -> p g e", g=2), in0=t1f.rearrange("p (g e) -> p g e", g=2),
                                in1=_bc(rr[:, 0:2].unsqueeze(2), [128, 2, 256]), op=ALU.mult)
                return V.tensor_tensor(out=mix_tok[:, 256:768], in0=hsq, in1=normw, op=ALU.mult)
            P.op("dve", rms2, R=["rr", "t1", "hsq", "rowp"], W=["hsq", "mix_ssd", "rr"])

            bL = [pb(), pb()]

            def lgm():
                r = None
                for h in range(4):
                    kvh, gq = h // 2, h % 2
                    pr = slice(kvh * 64, kvh * 64 + 64)
                    bank = bL[h // 2][0]
                    for part, buf in ((0, kT_pp[prv]), (1, kT_pp[cur])):
                        o = (h % 2) * 256 + part * 128
                        r = T.matmul(bank[:, o:o + 128], lhsT=qT_s[:, h, :], rhs=buf, start=True, stop=True)
                return r
            P.op("pe", lgm, R=["qT_s", "kT_pp0", "kT_pp1"], W=[bL[0][1], bL[1][1]])

            def sls():
                r = None
                for hb in range(2):
                    r = V.scalar_tensor_tensor(out=sl[:, hb * 2:hb * 2 + 2, :], in0=bL[hb][0][:].rearrange("p (h j) -> p h j", h=2), scalar=0.125,
                                               in1=biasw[:, hb * 2:hb * 2 + 2, :], op0=ALU.mult, op1=ALU.add)
                if n == 0:
                    r = V.tensor_scalar(out=sl[:, :, 0:128], in0=sl[:, :, 0:128], scalar1=halomask, scalar2=None, op0=ALU.add)
                V.tensor_reduce(out=ssw[:, 0:4], in_=sl, axis=AX.X, op=ALU.max)
                V.tensor_tensor(out=ssw[:, 0:4], in0=ssw[:, 0:4], in1=sinks, op=ALU.max)
                V.tensor_scalar(out=ssw[:, 4:8], in0=ssw[:, 0:4], scalar1=-1.0, scalar2=None, op0=ALU.mult)
                return V.tensor_tensor(out=ssw[:, 8:12], in0=sinks, in1=ssw[:, 4:8], op=ALU.add)
            P.op("dve", sls, R=[bL[0][1], bL[1][1], "biasw", "misc", "rowp"], W=["amask", "ssw"])

            def pex():
                r = None
                for h in range(4):
                    r = S.activation(out=p_b[:, h, :], in_=sl[:, h, :], func=AF.Exp, bias=ssw[:, 4 + h:5 + h], scale=1.0)
                return S.activation(out=ssw[:, 12:16], in_=ssw[:, 8:12], func=AF.Exp)
            P.op("act", pex, R=["amask", "ssw"], W=["p_b", "ssw2"])

            def den():
                V.tensor_reduce(out=ssw[:, 16:20], in_=p_b, axis=AX.X, op=ALU.add)
                V.tensor_tensor(out=ssw[:, 16:20], in0=ssw[:, 16:20], in1=ssw[:, 12:16], op=ALU.add)
                return V.reciprocal(out=ssw[:, 20:24], in_=ssw[:, 16:20])
            P.op("dve", den, R=["p_b", "ssw2"], W=["ssw3"])
            bPT, kPT = pb()
            bPTb = bPT[:].bitcast(BF16)

            def ptr():
                r = None
                for h in range(4):
                    for part in range(2):
                        j_ = h * 2 + part
                        r = T.transpose(bPTb[:, j_ * 128:(j_ + 1) * 128], p_b[:, h, part * 128:(part + 1) * 128], ident_b[:])
                return r
            P.op("pe", ptr, R=["p_b", "ident_b"], W=[kPT])
            P.op("act", lambda: S.copy(out=pT_b[:, 0:4, :], in_=bPTb[:, 0:512].rearrange("p (j i) -> p j i", j=4)), R=[kPT], W=["pT_b0"])
            P.op("dve", lambda: V.tensor_copy(out=pT_b[:, 4:8, :], in_=bPTb[:, 512:1024].rearrange("p (j i) -> p j i", j=4)), R=[kPT], W=["pT_b1"])
            bOS, kOS = pb()

            def osw():
                r = None
                for h in range(4):
                    kvh = h // 2
                    for part, buf in ((0, v_pp[prv]), (1, v_pp[cur])):
                        r = T.matmul(bOS[:, h * 64:(h + 1) * 64], lhsT=pT_b[:, h * 2 + part, :], rhs=buf[:, kvh * 64:(kvh + 1) * 64],
                                     start=(part == 0), stop=(part == 1))
                return r
            P.op("pe", osw, R=["pT_b0", "pT_b1", "v_pp0", "v_pp1"], W=[kOS])
            P.op("dve", lambda: V.tensor_tensor(out=mix_tok[:, 768:1024].rearrange("p (h d) -> p h d", h=4), in0=_bc(ssw[:, 20:24].unsqueeze(2), [128, 4, 64]),
                                                in1=bOS[:, 0:256].rearrange("p (h d) -> p h d", h=4), op=ALU.mult), R=[kOS, "ssw3"], W=["mix_swa"])

            bMT, kMT = pb()
            bMTb = bMT[:].bitcast(BF16)

            def mtr():
                r = None
                for kc in range(8):
                    r = T.transpose(bMTb[:, kc * 128:(kc + 1) * 128], mix_tok[:, kc * 128:(kc + 1) * 128], ident_b[:])
                return r
            P.op("pe", mtr, R=["mix_ret", "mix_ssd", "mix_swa", "ident_b"], W=[kMT])
            P.op("act", lambda: S.copy(out=mixT, in_=bMTb[:, 0:1024].rearrange("p (k t) -> p k t", k=8)), R=[kMT], W=["mixT"])
            for half in range(2):
                bW, kW = pb()

                def wo(half=half, bW=bW):
                    r = None
                    for q in range(4):
                        fc = half * 4 + q
                        for kc in range(8):
                            r = T.matmul(bW[:, q * 128:(q + 1) * 128], lhsT=w_out[:, kc, fc * 128:(fc + 1) * 128], rhs=mixT[:, kc, :],
                                         start=(kc == 0), stop=(kc == 7))
                    return r
                P.op("pe", wo, R=["w_out", "mixT"], W=[kW])

                def res(half=half, bW=bW):
                    r = None
                    for q in range(4):
                        fc = half * 4 + q
                        r = V.scalar_tensor_tensor(out=xT[:, fc, Tn], in0=bW[:, q * 128:(q + 1) * 128], scalar=g1a[:, fc:fc + 1], in1=xT[:, fc, Tn],
                                                   op0=ALU.mult, op1=ALU.add)
                    return r
                P.op("dve", res, R=[kW, "der", ("xT", n, half)], W=[("xT", n, half)])
            ln_inplace(128, xk, xT[:, :, Tn], GA1, BA1, 128)

        if FSTOP[0] < L * 10 + 1:
            return
        full = False
        for n in range(-1, NCH):
            chunk(n)
        P.barrier()
        if FSTOP[0] < L * 10 + 2:
            return
        ex = Arena(arena_t, TAIL)
        ex.off = _off_tmp
        sto = ex.alloc([128, STW])
        g8 = ex.alloc([128, NR, STW])
        stin = ex.alloc([128, 3, STW])
        wss = ex.alloc([128, 3, 4])

        def pk():
            V.tensor_copy(out=sto[:, 0:128], in_=Sret.rearrange("p t e -> p (t e)"))
            V.tensor_copy(out=sto[:, 128:384], in_=Sssd)
            return V.tensor_copy(out=sto[:, 384:392], in_=totacc)
        P.op("dve", pk, R=["Sret", "Sssd", "totacc"], W=["sto"])
        P.dma(sp, bounce_s[L], sto, R=["sto"], W=["bounce_s%d" % L])
        P.coll("AllGather", gath_s[L], bounce_s[L], ALLC, R=["bounce_s%d" % L], W=["gath_s%d" % L])
        P.dma(sp, g8, gath_s[L].rearrange("(r p) w -> p r w", p=128), R=["gath_s%d" % L], W=["g8"])

        def ssel():
            r = None
            for s_ in range(3):
                V.tensor_scalar(out=stin[:, s_, :], in0=g8[:, 0, :], scalar1=selw[:, s_ * 8:s_ * 8 + 1], scalar2=None, op0=ALU.mult)
                for r_ in range(1, NR):
                    r = V.scalar_tensor_tensor(out=stin[:, s_, :], in0=g8[:, r_, :], scalar=selw[:, s_ * 8 + r_:s_ * 8 + r_ + 1], in1=stin[:, s_, :],
                                               op0=ALU.mult, op1=ALU.add)
            return r
        P.op("dve", ssel, R=["g8", "selw"], W=["stin"])


        def comb():
            V.tensor_tensor(out=Sret, in0=stin[:, 0, 0:128].rearrange("p (t e) -> p t e", t=2),
                            in1=_bc(wret[:, 0:2].unsqueeze(2), [128, 2, 64]), op=ALU.mult)
            for s_ in (1, 2):
                V.tensor_tensor(out=Sssd[:, 0:128].rearrange("p (t e) -> p t e", t=2), in0=stin[:, s_, 0:128].rearrange("p (t e) -> p t e", t=2),
                                in1=_bc(wret[:, 2 * s_:2 * s_ + 2].unsqueeze(2), [128, 2, 64]), op=ALU.mult)
                V.tensor_tensor(out=Sret, in0=Sret, in1=Sssd[:, 0:128].rearrange("p (t e) -> p t e", t=2), op=ALU.add)
            for g in range(2):
                pr = slice(g * 64, (g + 1) * 64)
                V.tensor_copy(out=wss[pr, 1, :], in_=stin[pr, 0, 384 + g * 4:384 + g * 4 + 4])
                V.tensor_tensor(out=wss[pr, 2, :], in0=stin[pr, 0, 384 + g * 4:384 + g * 4 + 4],
                                in1=stin[pr, 1, 384 + g * 4:384 + g * 4 + 4], op=ALU.add)
            return V.memset(wss[:, 0, :], 0.0)
        P.op("dve", comb, R=["stin", "cst"], W=["Sret", "Sssd", "wss"])
        P.op("act", lambda: S.activation(out=wss, in_=wss, func=AF.Exp), R=["wss"], W=["wss"])

        def comb2():
            V.tensor_tensor(out=Sssd.rearrange("p (r e) -> p r e", r=4), in0=stin[:, 0, 128:384].rearrange("p (r e) -> p r e", r=4),
                            in1=_bc(wss[:, 0, :].unsqueeze(2), [128, 4, 64]), op=ALU.mult)
            for s_ in (1, 2):
                V.tensor_tensor(out=stin[:, s_, 128:384].rearrange("p (r e) -> p r e", r=4), in0=stin[:, s_, 128:384].rearrange("p (r e) -> p r e", r=4),
                                in1=_bc(wss[:, s_, :].unsqueeze(2), [128, 4, 64]), op=ALU.mult)
                V.tensor_tensor(out=Sssd, in0=Sssd, in1=stin[:, s_, 128:384], op=ALU.add)
            V.tensor_copy(out=Sssd_b, in_=Sssd)
            return V.tensor_copy(out=Sret_b, in_=Sret)
        P.op("dve", comb2, R=["stin", "wss", "Sret", "Sssd"], W=["Sret", "Sssd", "stin"])
        P.barrier()
        if FSTOP[0] < L * 10 + 3:
            return
        P.op("dve", zmask, W=["qm", "BCm", "qT_s"])
        full = True
        SUB[0] = MSUB[0]
        for n in range(-1, min(NCH, MAINCH[0])):
            chunk(n)
        SUB[0] = 99
        if FSTOP[0] < L * 10 + 4:
            return

        P.barrier()
        ar = Arena(arena_t, LIM)
        hT = ar.alloc([128, 8, NTOK], BF16)
        aT = [ar.alloc([128, 4, 1024], BF16) for _ in range(2)]
        wgu = [ar.alloc([128, 8, 1024], BF16) for _ in range(2)]
        wdb = [ar.alloc([128, 4, D], BF16) for _ in range(2)]
        sgt = [ar.alloc([128, 512]) for _ in range(2)]
        evt = [ar.alloc([128, 512]) for _ in range(2)]
        if moe:
            gbc = [ar.alloc([128, 1024]) for _ in range(2)]
            rw_sb = ar.alloc([128, 8, 8])
            lgT = ar.alloc([128, 512])
            gatesT = ar.alloc([128, NTOK])
            lg = ar.alloc([128, NCH, 8])
            gts = ar.alloc([128, NCH, 8])
            e1 = ar.alloc([128, NCH, 8])
            e2 = ar.alloc([128, NCH, 8])
            l2 = ar.alloc([128, NCH, 8])
            tk = ar.alloc([128, 6, NCH])
            h2f = [ar.alloc([128, 512]) for _ in range(2)]
            P.dma(sp, rw_sb, rw_d, W=["rw_sb"])

        if moe:
            bG, kG = pb()
        for tg in range(4):
            Tg = slice(tg * 512, (tg + 1) * 512)
            xkeys = [("xT", n, hf_) for n in range(tg * 4, tg * 4 + 4) for hf_ in range(2)]
            if moe:
                bR, kR = pb()
            for fc in range(8):
                P.op("act", lambda fc=fc, Tg=Tg: S.activation(out=hT[:, fc, Tg], in_=xT[:, fc, Tg], func=AF.Identity, bias=B2[:, fc:fc + 1], scale=A2[:, fc:fc + 1]),
                     R=xkeys + ["der"], W=[("hT", tg)])
                if moe:
                    hb = h2f[fc % 2]
                    hbk = "h2f%d" % (fc % 2)
                    P.op("dve", lambda fc=fc, Tg=Tg, hb=hb: V.tensor_scalar(out=hb, in0=xT[:, fc, Tg], scalar1=A2[:, fc:fc + 1], scalar2=B2[:, fc:fc + 1], op0=ALU.mult, op1=ALU.add),
                         R=xkeys + ["der"], W=[hbk])
                    P.op("pe", lambda fc=fc, hb=hb, bR=bR: T.matmul(bR[0:8, 0:512], lhsT=rw_sb[:, fc, :], rhs=hb, start=(fc == 0), stop=(fc == 7)),
                         R=[hbk, "rw_sb"], W=[kR])
            if moe:
                P.op("act", lambda bR=bR: S.activation(out=lgT[0:8, 0:512], in_=bR[0:8, 0:512], func=AF.Identity, bias=rbias[0:8, 0:1], scale=1.0),
                     R=[kR, "misc"], W=["lgT"])

                def ltr(tg=tg):
                    r = None
                    for q in range(4):
                        n = tg * 4 + q
                        r = T.transpose(bG[:, n * 8:(n + 1) * 8], lgT[0:8, q * 128:(q + 1) * 128], ident[0:8, 0:8])
                    return r
                P.op("pe", ltr, R=["lgT", "cst"], W=[kG])
        if moe:

            def top2():
                V.tensor_copy(out=lg, in_=bG[:, 0:128].rearrange("p (n e) -> p n e", e=8))
                m1_, m2_, dlt, w1_, w2_ = tk[:, 0, :], tk[:, 1, :], tk[:, 2, :], tk[:, 3, :], tk[:, 4, :]
                V.tensor_reduce(out=m1_, in_=lg, axis=AX.X, op=ALU.max)
                V.tensor_tensor(out=e1, in0=lg, in1=_bc(m1_.unsqueeze(2), [128, NCH, 8]), op=ALU.is_equal)
                V.scalar_tensor_tensor(out=l2, in0=e1, scalar=-1e30, in1=lg, op0=ALU.mult, op1=ALU.add)
                V.tensor_reduce(out=m2_, in_=l2, axis=AX.X, op=ALU.max)
                V.tensor_tensor(out=e2, in0=l2, in1=_bc(m2_.unsqueeze(2), [128, NCH, 8]), op=ALU.is_equal)
                return V.tensor_tensor(out=dlt, in0=m2_, in1=m1_, op=ALU.subtract)
            P.op("dve", top2, R=[kG], W=["tk", "lg"])
            P.op("act", lambda: S.activation(out=tk[:, 2, :], in_=tk[:, 2, :], func=AF.Exp), R=["tk"], W=["tk"])

            def top2b():
                dlt, w1_, w2_ = tk[:, 2, :], tk[:, 3, :], tk[:, 4, :]
                V.tensor_scalar(out=w1_, in0=dlt, scalar1=1.0, scalar2=None, op0=ALU.add)
                V.reciprocal(out=w1_, in_=w1_)
                V.tensor_tensor(out=w2_, in0=dlt, in1=w1_, op=ALU.mult)
                V.tensor_tensor(out=e1, in0=e1, in1=_bc(w1_.unsqueeze(2), [128, NCH, 8]), op=ALU.mult)
                V.tensor_tensor(out=e2, in0=e2, in1=_bc(w2_.unsqueeze(2), [128, NCH, 8]), op=ALU.mult)
                return V.tensor_tensor(out=gts, in0=e1, in1=e2, op=ALU.add)
            P.op("dve", top2b, R=["tk", "lg"], W=["gts", "tk", "lg"])
            for tg in range(4):
                bG2, kG2 = pb()

                def gtr(tg=tg, bG2=bG2):
                    r = None
                    for q in range(4):
                        n = tg * 4 + q
                        r = T.transpose(bG2[0:8, q * 128:(q + 1) * 128], gts[:, n, :], ident)
                    return r
                P.op("pe", gtr, R=["gts", "cst"], W=[kG2])
                P.op("act", lambda tg=tg, bG2=bG2: S.copy(out=gatesT[0:8, tg * 512:(tg + 1) * 512], in_=bG2[0:8, 0:512]), R=[kG2], W=["gatesT"])

        nexp = NEXP if moe else 1
        dff = D_FFE if moe else D_FF
        pieces = []
        o = 0
        while o < dff:
            w = min(512, dff - o)
            pieces.append((o, w))
            o += w
        pi = 0
        for e in range(min(nexp, NEXPRUN[0])):
            if moe:
                for half in range(2):
                    for q in range(2):
                        bg_, kg_ = pb()
                        P.op("pe", lambda e=e, half=half, q=q, bg_=bg_: T.matmul(bg_[:, 0:512], lhsT=sel8[0:8, e * 128:(e + 1) * 128],
                                                                                  rhs=gatesT[0:8, half * 1024 + q * 512:half * 1024 + (q + 1) * 512], start=True, stop=True),
                             R=["gatesT", "cst"], W=[kg_])
                        P.op("act", lambda half=half, q=q, bg_=bg_: S.copy(out=gbc[half][:, q * 512:(q + 1) * 512], in_=bg_[:, 0:512]), R=[kg_], W=["gbc%d" % half])
            for (o, w) in pieces:
                nb = w // 128
                wb = pi % 2
                kwg, kwd = "wgu%d" % wb, "wd%d" % wb
                P.dma("pool", wgu[wb][:, :, 0:w], wg_d[e].rearrange("(c p) n -> p c n", p=128)[:, :, o:o + w], W=[kwg])
                P.dma("pool", wgu[wb][:, :, 512:512 + w], wu_d[e].rearrange("(c p) n -> p c n", p=128)[:, :, o:o + w], W=[kwg])
                P.dma("pool", wdb[wb][:, 0:nb, :], wd_d[e][o:o + w, :].rearrange("(c p) n -> p c n", p=128), W=[kwd])
                for half in range(2):
                    for blk in range(nb):
                        for q in range(2):
                            tg = half * 2 + q
                            Tg = slice(tg * 512, (tg + 1) * 512)
                            bg_, kg_ = pb()
                            bu_, ku_ = pb()

                            def gu(blk=blk, Tg=Tg, bg_=bg_, bu_=bu_, wb=wb):
                                r = None
                                for kc in range(8):
                                    T.matmul(bg_[:, 0:512], lhsT=wgu[wb][:, kc, blk * 128:(blk + 1) * 128], rhs=hT[:, kc, Tg], start=(kc == 0), stop=(kc == 7))
                                for kc in range(8):
                                    r = T.matmul(bu_[:, 0:512], lhsT=wgu[wb][:, kc, 512 + blk * 128:512 + (blk + 1) * 128], rhs=hT[:, kc, Tg], start=(kc == 0), stop=(kc == 7))
                                return r
                            P.op("pe", gu, R=[kwg, ("hT", tg)], W=[kg_, ku_])
                            sb_ = sgt[(blk * 2 + q) % 2]
                            sk_ = "sgt%d" % ((blk * 2 + q) % 2)
                            P.op("act", lambda bg_=bg_, sb_=sb_: S.activation(out=sb_, in_=bg_[:, 0:512], func=AF.Silu), R=[kg_], W=[sk_])
                            P.op("dve", lambda half=half, blk=blk, q=q, bu_=bu_, sb_=sb_: V.tensor_tensor(out=aT[half][:, blk, q * 512:(q + 1) * 512], in0=bu_[:, 0:512], in1=sb_, op=ALU.mult),
                                 R=[ku_, sk_], W=[("aT", half, blk, q)])
                for half in range(2):
                    for fc in range(8):
                        for q in range(2):
                            tg = half * 2 + q
                            Tg = slice(tg * 512, (tg + 1) * 512)
                            bo_, ko_ = pb()

                            def dn(half=half, fc=fc, q=q, bo_=bo_, wb=wb, nb=nb):
                                r = None
                                for blk in range(nb):
                                    r = T.matmul(bo_[:, 0:512], lhsT=wdb[wb][:, blk, fc * 128:(fc + 1) * 128], rhs=aT[half][:, blk, q * 512:(q + 1) * 512],
                                                 start=(blk == 0), stop=(blk == nb - 1))
                                return r
                            P.op("pe", dn, R=[kwd] + [("aT", half, blk, q) for blk in range(nb)], W=[ko_])
                            eb = evt[(fc * 2 + q) % 2]
                            ek = "evt%d" % ((fc * 2 + q) % 2)
                            if moe:
                                P.op("dve", lambda fc=fc, half=half, q=q, bo_=bo_, eb=eb: V.scalar_tensor_tensor(out=eb, in0=bo_[:, 0:512], scalar=g1f[:, fc:fc + 1],
                                                                                                              in1=gbc[half][:, q * 512:(q + 1) * 512], op0=ALU.mult, op1=ALU.mult),
                                     R=[ko_, "der", "gbc%d" % half], W=[ek])
                            else:
                                P.op("dve", lambda fc=fc, bo_=bo_, eb=eb: V.tensor_scalar(out=eb, in0=bo_[:, 0:512], scalar1=g1f[:, fc:fc + 1], scalar2=None, op0=ALU.mult),
                                     R=[ko_, "der"], W=[ek])
                            xkeys = [("xT", n, fc // 4) for n in range(tg * 4, tg * 4 + 4)]
                            P.op("pool", lambda fc=fc, Tg=Tg, eb=eb: G.tensor_tensor(out=xT[:, fc, Tg], in0=xT[:, fc, Tg], in1=eb, op=ALU.add),
                                 R=[ek] + xkeys, W=xkeys)
                pi += 1

        P.barrier()
        ar = Arena(arena_t, LIM)
        sq = [ar.alloc([128, 512]) for _ in range(2)]
        lnst = ar.alloc([128, 4, 512])
        lnk = ["lnst"]
        for tg in range(4):
            Tg = slice(tg * 512, (tg + 1) * 512)
            xkeys = [("xT", n, hf_) for n in range(tg * 4, tg * 4 + 4) for hf_ in range(2)]
            ln_inplace(512, xkeys, xT[:, :, Tg], GA2, BA2, 512)

    for L_ in range(DEPTH):
        layer(L_)
    ar = Arena(arena_t, TAIL)
    ar.off = 3072
    xo = [ar.alloc([128, D]) for _ in range(2)]
    for n in range(NCH):
        buf = xo[n % 2]
        bk = "xo%d" % (n % 2)
        for half in range(2):
            bank, bkey = pb()

            def tr2(n=n, half=half, bank=bank):
                r = None
                for q in range(4):
                    fc = half * 4 + q
                    r = T.transpose(bank[:, q * 128:(q + 1) * 128], xT[:, fc, n * 128:(n + 1) * 128], ident)
                return r
            P.op("pe", tr2, R=[("xT", n, 0), ("xT", n, 1), "cst"], W=[bkey])
            P.op("act", lambda half=half, bank=bank, buf=buf: S.copy(out=buf[:, half * 512:(half + 1) * 512], in_=bank[:, 0:512]), R=[bkey], W=[bk + "_%d" % half])
        P.dma(sp, xo_d[n * 128:(n + 1) * 128, :], buf, R=[bk + "_0", bk + "_1"], is_output=True)
    return P.emit()

C_ID, C_TRI, C_MGT, C_ONE = 0, 128, 256, 384
C_DQ, C_DK, C_GC, C_INV = 512, 516, 520, 522
C_MADD = 554
C_WRET = 810
C_M0 = 816
C_SEL = 818
CW = C_SEL + 8 * 128

M_AIN, M_BIN, M_G1A, M_G1F = 0, 8, 16, 24
M_CW, M_CB, M_HV, M_HM = 32, 56, 62, 63
M_MOD, M_LN, M_C, M_BADA, M_DER, M_RB = 64, 160, 192, 200, 296, 360
MW = 368

R_DTB, R_ALOG, R_DSK, R_NW, R_SINK, R_RB = 0, 8, 16, 24, 536, 540
RW = 668


def _t5_bucket(dist):
    exact = 16
    df = np.maximum(dist, 1).astype(np.float32)
    large = exact + (np.log(df / exact) / math.log(128 / exact) * (32 - exact)).astype(np.int32)
    large = np.minimum(large, 31)
    return np.where(dist < exact, dist, large)


def make_consts():
    c = np.zeros((128, CW), np.float32)
    i = np.arange(128)
    c[:, C_ID:C_ID + 128] = np.eye(128, dtype=np.float32)
    c[:, C_TRI:C_TRI + 128] = (i[:, None] <= i[None, :]).astype(np.float32)
    c[:, C_MGT:C_MGT + 128] = (i[:, None] > i[None, :]).astype(np.float32)
    c[:, C_ONE:C_ONE + 128] = 1.0
    lg = np.log(1.0 - 2.0 ** (-5.0 - np.arange(4, dtype=np.float64)))
    c[:, C_DQ:C_DQ + 4] = np.exp(lg[None, :] * (i[:, None] + 1.0))
    c[:, C_DK:C_DK + 4] = np.exp(-lg[None, :] * (i[:, None] + 1.0)) * (64 ** -0.5)
    for t in range(2):
        for hf in range(2):
            c[hf * 64:(hf + 1) * 64, C_GC + t] = np.exp(lg[2 * t + hf] * 128.0)
    c[:, C_INV:C_INV + 32] = np.exp(-math.log(10000.0) * np.arange(32, dtype=np.float32) / 32)[None, :]
    jj = np.arange(256)
    dist = i[:, None] + 128 - jj[None, :]
    valid = (dist >= 0) & (dist < 128)
    c[:, C_MADD:C_MADD + 256] = np.where(valid, 0.0, NEG)
    for s in range(3):
        for t in range(2):
            for hf in range(2):
                c[hf * 64:(hf + 1) * 64, C_WRET + s * 2 + t] = np.exp(lg[2 * t + hf] * 2048.0 * s)
    c[0:64, C_M0] = 1.0
    c[64:128, C_M0 + 1] = 1.0
    for e in range(8):
        c[e, C_SEL + e * 128:C_SEL + (e + 1) * 128] = 1.0
    bk = _t5_bucket(np.clip(dist, 0, 127))
    eoh = np.zeros((128, 256, 32), np.float32)
    ii, jj2 = np.meshgrid(i, jj, indexing="ij")
    eoh[ii, jj2, bk] = 1.0
    return c, eoh.reshape(128, 256 * 32)


def col(v):
    v = np.asarray(v, np.float32)
    return np.ascontiguousarray(v.reshape(-1, 128).T)


_PROG_CACHE = {}


def kernel(x, c, positions, rel_bias, w_ada, b_ada, w_in, w_out, conv_w, conv_b,
           dt_bias, a_log, d_skip, ssd_norm_w, sinks, ln_g, ln_b,
           ffn_w_gate, ffn_w_up, ffn_w_down, router_w, router_b,
           expert_w_gate, expert_w_up, expert_w_down):
    f = lambda a: np.ascontiguousarray(np.asarray(a, dtype=np.float32))
    x = f(x)
    cst, eoh = make_consts()
    positions = np.asarray(positions)
    shared = {
        "cst": cst, "eoh": eoh, "relb": f(rel_bias).reshape(-1),
        "w_in": f(w_in), "w_out": f(w_out), "w_ada": f(w_ada),
        "wg0": f(ffn_w_gate), "wu0": f(ffn_w_up), "wd0": f(ffn_w_down),
        "wg1": f(expert_w_gate[0]), "wu1": f(expert_w_up[0]), "wd1": f(expert_w_down[0]),
        "rw": np.ascontiguousarray(f(router_w[0]).reshape(8, 128, 8).transpose(1, 0, 2)),
    }
    rowp = np.zeros((DEPTH, RW), np.float32)
    for L in range(DEPTH):
        rowp[L, R_DTB:R_DTB + 8] = f(dt_bias[L])
        rowp[L, R_ALOG:R_ALOG + 8] = f(a_log[L])
        rowp[L, R_DSK:R_DSK + 8] = f(d_skip[L])
        rowp[L, R_NW:R_NW + 512] = f(ssd_norm_w[L])
        rowp[L, R_SINK:R_SINK + 4] = f(sinks[L])
    maps = []
    for core in range(8):
        b, sq_ = core // 4, core % 4
        t0 = sq_ * NTOK
        xin = np.zeros((NTOK + 128, D), np.float32)
        xin[128:] = x[b, t0:t0 + NTOK]
        if sq_ > 0:
            xin[:128] = x[b, t0 - 128:t0]
        misc = np.zeros((DEPTH, 128, MW), np.float32)
        for L in range(DEPTH):
            m = misc[L]
            m[:, M_CW:M_CW + 24] = np.ascontiguousarray(f(conv_w[L]).reshape(4, 6, 128).transpose(2, 1, 0)).reshape(128, 24)
            m[:, M_CB:M_CB + 6] = col(conv_b[L])
            m[:, M_HV] = 1.0 if sq_ > 0 else 0.0
            m[:, M_HM] = 0.0 if sq_ > 0 else NEG
            m[:, M_LN:M_LN + 8] = col(ln_g[L, 0])
            m[:, M_LN + 8:M_LN + 16] = col(ln_b[L, 0])
            m[:, M_LN + 16:M_LN + 24] = col(ln_g[L, 1])
            m[:, M_LN + 24:M_LN + 32] = col(ln_b[L, 1])
            m[:, M_C:M_C + 8] = col(c[b])
            m[:, M_BADA:M_BADA + 48] = col(b_ada[L])
            if L % 2 == 1:
                m[0:8, M_RB] = f(router_b[L // 2])
        selw = np.zeros((128, 32), np.float32)
        for s in range(3):
            if sq_ - 1 - s >= 0:
                selw[:, s * 8 + (sq_ - 1 - s)] = 1.0
        pos = np.ascontiguousarray(positions[b, t0:t0 + NTOK].astype(np.int32).reshape(NCH, 128).T)
        mm = dict(shared)
        mm.update({"xin": xin, "pos": pos, "misc": misc, "rowp": rowp, "selw": selw})
        maps.append(mm)
    FSTOP[0] = 99 if AONLY[0] else 13.5
    if "f" not in _PROG_CACHE:
        _PROG_CACHE["f"] = build_fused()
    mapsA = [{k_: v_ for k_, v_ in m_.items() if k_ in DECL_IN} for m_ in maps]
    res = run_bass_kernel_spmd(_PROG_CACHE["f"], mapsA, core_ids=list(range(8)))
    if AONLY[0]:
        out = np.zeros_like(x)
        for core in range(8):
            b, sq_ = core // 4, core % 4
            out[b, sq_ * NTOK:(sq_ + 1) * NTOK] = np.asarray(res.results[core]["xout"], np.float32)
        return out
    L = DEPTH - 1
    i = L // 2
    wada_l = f(w_ada[L:L + 1])
    w_in_l, w_out_l = f(w_in[L]), f(w_out[L])
    wg_l, wu_l, wd_l = f(expert_w_gate[i]), f(expert_w_up[i]), f(expert_w_down[i])
    rw_l = np.ascontiguousarray(f(router_w[i]).reshape(8, 128, 8).transpose(1, 0, 2))
    rowp1 = np.zeros((RW,), np.float32)
    rowp1[:] = rowp[L]
    rowp1[R_RB:R_RB + 128] = f(rel_bias).reshape(-1)
    st_in = np.zeros((3, 128, STW), np.float32)
    maps2 = []
    for core in range(8):
        xin = np.zeros((NTOK + 128, D), np.float32)
        xin[128:] = np.asarray(res.results[core]["xout"], np.float32)
        mA = maps[core]
        maps2.append({"xin": xin, "pos": mA["pos"], "cst": cst, "misc": np.ascontiguousarray(mA["misc"][L]), "rowp": rowp1,
                      "w_in": w_in_l, "w_ada": wada_l, "st_in": st_in, "eoh": eoh, "w_out": w_out_l,
                      "wg": wg_l, "wu": wu_l, "wd": wd_l, "rw": rw_l})
    if "b" not in _PROG_CACHE:
        _PROG_CACHE["b"] = build("ffn", L)
    res2 = run_bass_kernel_spmd(_PROG_CACHE["b"], maps2, core_ids=list(range(8)))
    out = np.zeros_like(x)
    for core in range(8):
        b, sq_ = core // 4, core % 4
        out[b, sq_ * NTOK:(sq_ + 1) * NTOK] = np.asarray(res2.results[core]["xout"], np.float32)
    return out
```

```python
import math
import contextlib
import numpy as np
import concourse.bass as bass
import concourse.mybir as mybir
from concourse.bass_utils import run_bass_kernel_spmd

F32 = mybir.dt.float32
BF16 = mybir.dt.bfloat16
I32 = mybir.dt.int32
ALU = mybir.AluOpType
AF = mybir.ActivationFunctionType
AX = mybir.AxisListType

D = 1024
DEPTH = 2
NTOK = 2048
NCH = 16
IN_DIM = 2824
D_FF = 2816
NEXP = 8
D_FFE = 3584
ALPHA = (2 * DEPTH) ** 0.25
EPS = 1e-5
NEG = -30000.0
STW = 392

ENGS = ("pe", "act", "dve", "pool", "sp")
ND = 12
SAME_ENGINE_SYNC = False


SEM_CAP = 3000
CHAIN_EPOCHS = 3


class Prog:
    def __init__(self):
        self.nc = bass.Bass("TRN2", target_bir_lowering=False)
        self.ops = {e: [] for e in ENGS}
        self.lastw = {}
        self.readers = {}
        self.seen = {e: {} for e in ENGS}
        self.dma_cnt = [0] * (ND + 1)
        self.dma_last_tok = [None] * (ND + 1)
        self.dma_next = 0
        self.out_tokens = []
        self.pending = {e: [] for e in ENGS}
        self.st = contextlib.ExitStack()
        self.chain = {e: {"last": None, "count": 0, "sems": None, "epoch": 0} for e in ("act", "dve", "pool")}

    def sbuf(self, name, shape, dtype):
        return self.st.enter_context(self.nc.sbuf_tensor(name, list(shape), dtype))

    def psum(self, name, shape, dtype):
        return self.st.enter_context(self.nc.psum_tensor(name, list(shape), dtype))

    def barrier(self):
        toks = []
        for e in ENGS:
            for i in range(len(self.ops[e]) - 1, -1, -1):
                if self.ops[e][i]["dma"] is None:
                    toks.append(("e", e, i))
                    break
        for t in self.dma_last_tok:
            if t is not None:
                toks.append(t)
        for e in ENGS:
            self.pending[e] = list(toks)

    def _need(self, eng, tok, waits):
        if tok is None:
            return
        if tok[0] == "e":
            _, src, seq = tok
            if src == eng and (not SAME_ENGINE_SYNC or eng == "pe"):
                return
            if self.seen[eng].get(src, -1) >= seq:
                return
            cur = waits.get(src)
            if cur is None or cur[2] < seq:
                waits[src] = tok
        else:
            _, idx, cnt = tok
            key = ("d", idx)
            if self.seen[eng].get(key, -1) >= cnt:
                return
            cur = waits.get(key)
            if cur is None or cur[2] < cnt:
                waits[key] = tok

    def _deps(self, eng, reads, writes):
        waits = {}
        for k in reads:
            self._need(eng, self.lastw.get(k), waits)
        for k in writes:
            self._need(eng, self.lastw.get(k), waits)
            for tok in self.readers.get(k, {}).values():
                self._need(eng, tok, waits)
        if self.pending[eng]:
            for tok in self.pending[eng]:
                self._need(eng, tok, waits)
            self.pending[eng] = []
        for key, tok in waits.items():
            self.seen[eng][key] = tok[2]
            if tok[0] == "e":
                self.ops[tok[1]][tok[2]]["sig"] = True
        return list(waits.values())

    def _commit(self, tok, reads, writes, rkey):
        for k in writes:
            self.lastw[k] = tok
            self.readers[k] = {}
        for k in reads:
            self.readers.setdefault(k, {})[rkey] = tok

    def op(self, eng, emit, R=(), W=()):
        waits = self._deps(eng, R, W)
        seq = len(self.ops[eng])
        self.ops[eng].append({"waits": waits, "emit": emit, "sig": False, "dma": None})
        self._commit(("e", eng, seq), R, W, eng)

    def dma(self, eng, out, in_, R=(), W=(), is_output=False, **kw):
        if "_coll" in kw:
            idx = ND
        else:
            idx = self.dma_next
            self.dma_next = (self.dma_next + 1) % ND
        waits = self._deps(eng, R, W)
        prev = self.dma_last_tok[idx]
        if prev is not None:
            w = {}
            self._need(eng, prev, w)
            for key, tok in w.items():
                self.seen[eng][key] = tok[2]
                waits.append(tok)
        self.dma_cnt[idx] += (1 if idx == ND else 16)
        tok = ("d", idx, self.dma_cnt[idx])
        self.dma_last_tok[idx] = tok
        self.ops[eng].append({"waits": waits, "emit": None, "sig": False,
                              "dma": (out, in_, idx, kw)})
        self._commit(tok, R, W, ("d", idx))
        if is_output:
            self.out_tokens.append(tok)
        return tok

    def coll(self, kind, out, in_, groups, R=(), W=()):
        import os
        if os.environ.get("NOCOLL"):
            return self.dma("sp", out[0:128, :], in_, R=R, W=W)
        return self.dma("pool", out, in_, R=R, W=W, _coll=(kind, groups))

    def emit(self):
        nc = self.nc
        fin = {}
        for tok in self.out_tokens:
            self._need("sp", tok, fin)
        fin_waits = list(fin.values())
        pref = {}
        for e in ENGS:
            c = 0
            arr = []
            for o in self.ops[e]:
                if o["sig"]:
                    c += 1
                arr.append(c)
            pref[e] = arr
        with self.st as st:
            esem = {e: [st.enter_context(nc.semaphore("s_%s%d" % (e, k_))) for k_ in range(max(1, -(-(pref[e][-1] if pref[e] else 0) // SEM_CAP)))]
                    for e in ENGS}
            dsem = [st.enter_context(nc.semaphore("d%d" % i)) for i in range(ND + 1)]
            for e in self.chain:
                self.chain[e]["sems"] = [st.enter_context(nc.semaphore("c_%s%d" % (e, k_))) for k_ in range(CHAIN_EPOCHS)]
            block = st.enter_context(nc.Block())

            def do_wait(E, tok):
                if tok[0] == "e":
                    c_ = pref[tok[1]][tok[2]]
                    E.wait_ge(esem[tok[1]][(c_ - 1) // SEM_CAP], (c_ - 1) % SEM_CAP + 1)
                else:
                    E.wait_ge(dsem[tok[1]], tok[2])

            def run(e, E):
                for oi, o in enumerate(self.ops[e]):
                    for tok in o["waits"]:
                        do_wait(E, tok)
                    if o["dma"] is not None:
                        out, in_, idx, kw = o["dma"]
                        if "_coll" in kw:
                            kind, groups = kw["_coll"]
                            nc.gpsimd.collective_compute(kind, ALU.bypass, replica_groups=groups,
                                                         ins=[in_], outs=[out]).then_inc(dsem[idx], 1)
                        else:
                            E.dma_start(out=out, in_=in_, **kw).then_inc(dsem[idx], 16)
                    else:
                        if e in self.chain:
                            self.chain[e]["last"] = None
                        ins = o["emit"]()
                        if o["sig"]:
                            c_ = pref[e][oi]
                            ins.then_inc(esem[e][(c_ - 1) // SEM_CAP], 1)
                if e == "sp":
                    for tok in fin_waits:
                        do_wait(E, tok)

            @block.tensor
            def _(E):
                run("pe", E)

            @block.scalar
            def _(E):
                run("act", E)

            @block.vector
            def _(E):
                run("dve", E)

            @block.gpsimd
            def _(E):
                run("pool", E)

            @block.sync
            def _(E):
                run("sp", E)
        return nc


class EngProxy:
    def __init__(self, prog, name, eng):
        self._p, self._n, self._e = prog, name, eng

    def __getattr__(self, attr):
        fn = getattr(self._e, attr)
        if attr in ("wait_ge", "dma_start"):
            return fn
        st = self._p.chain[self._n]

        def w(*a, **k):
            if st["last"] is not None:
                if st["count"] >= SEM_CAP:
                    st["epoch"] += 1
                    st["count"] = 0
                sem_ = st["sems"][st["epoch"]]
                st["last"].then_inc(sem_, 1)
                st["count"] += 1
                self._e.wait_ge(sem_, st["count"])
            ins = fn(*a, **k)
            st["last"] = ins
            return ins
        return w


class Arena:
    def __init__(self, t, nwords):
        self.t = t
        self.n = nwords
        self.off = 0

    def alloc(self, shape, dtype=F32):
        n = 1
        for s in shape[1:]:
            n *= s
        words = n if dtype in (F32, I32) else (n + 1) // 2
        assert self.off + words <= self.n, ("arena overflow", self.off, words, self.n)
        v = self.t[:, self.off:self.off + words]
        self.off += words
        if dtype == BF16:
            v = v.bitcast(BF16)[:, 0:n]
        elif dtype == I32:
            v = v.bitcast(I32)
        if len(shape) > 2:
            names = ["a%d" % i for i in range(len(shape) - 1)]
            pat = "p (" + " ".join(names) + ") -> p " + " ".join(names)
            v = v.rearrange(pat, **{nm: s for nm, s in zip(names, shape[1:])})
        return v


def _bc(ap, shape):
    return ap.to_broadcast(list(shape))


STOP = [99]
DBG = set()
DBG_OUT = {}
_P1ONLY = [False]
SUB = [99]


def build(stage, L):
    P = Prog()
    nc = P.nc
    V, S, G, T = EngProxy(P, "dve", nc.vector), EngProxy(P, "act", nc.scalar), EngProxy(P, "pool", nc.gpsimd), nc.tensor
    full = stage in ("main", "ffn")
    ffn_only = stage == "ffn"
    moe = (L % 2 == 1)
    last = (L == DEPTH - 1)

    def din(name, shape, dt=F32):
        return nc.dram_tensor(name, list(shape), dt, kind="ExternalInput").ap()

    def dout(name, shape, dt=F32):
        return nc.dram_tensor(name, list(shape), dt, kind="ExternalOutput").ap()

    def dbg(name, ap, keys):
        if name not in DBG:
            return
        shp = list(ap.shape)
        d_ = dout("dbg_" + name, shp, ap.dtype)
        P.dma("sp", d_, ap, R=list(keys), is_output=True)

    x_d = din("xin", [NTOK + 128, D])
    pos_d = din("pos", [128, NCH], I32)
    cst_d = din("cst", [128, CW])
    misc_d = din("misc", [128, MW])
    rowp_d = din("rowp", [RW])
    w_in_d = din("w_in", [D, IN_DIM])
    if full:
        w_out_d = din("w_out", [D, D])
        eoh_d = din("eoh", [128, 256 * 32])
        st_in_d = din("st_in", [3, 128, STW])
        if moe:
            wg_d = din("wg", [NEXP, D, D_FFE])
            wu_d = din("wu", [NEXP, D, D_FFE])
            wd_d = din("wd", [NEXP, D_FFE, D])
            rw_d = din("rw", [128, 8, 8])
        else:
            wg_d = din("wg", [1, D, D_FF])
            wu_d = din("wu", [1, D, D_FF])
            wd_d = din("wd", [1, D_FF, D])
        xo_d = dout("xout", [NTOK, D])
    else:
        st_out_d = dout("st_out", [128, STW])

    xT = P.sbuf("xT", [128, 8, NTOK], F32)
    cst = P.sbuf("cst_sb", [128, CW], F32)
    misc = P.sbuf("misc_sb", [128, MW], F32)
    rowp = P.sbuf("rowp_sb", [128, RW], F32)
    ident_b = P.sbuf("ident_b", [128, 128], BF16)
    AW = 33700
    arena_t = P.sbuf("arena", [128, AW], F32)
    TAIL = AW - 2048
    cosT = arena_t[:, TAIL:TAIL + 512].rearrange("p (n f) -> p n f", f=32)
    sinT = arena_t[:, TAIL + 512:TAIL + 1024].rearrange("p (n f) -> p n f", f=32)
    biasw = arena_t[:, TAIL + 1024:TAIL + 2048].rearrange("p (h j) -> p h j", h=4)
    ps = [P.psum("ps%d" % i, [128, 512], F32) for i in range(8)]
    psk = ["ps%d" % i for i in range(8)]
    pctr = [0]

    def pb():
        i = pctr[0] % 8
        pctr[0] += 1
        return ps[i], psk[i]

    ident = cst[:, C_ID:C_ID + 128]
    tri = cst[:, C_TRI:C_TRI + 128]
    mgt = cst[:, C_MGT:C_MGT + 128]
    ones = cst[:, C_ONE:C_ONE + 128]
    dq = cst[:, C_DQ:C_DQ + 4]
    dk = cst[:, C_DK:C_DK + 4]
    gC = cst[:, C_GC:C_GC + 2]
    invf = cst[:, C_INV:C_INV + 32]
    madd = cst[:, C_MADD:C_MADD + 256]
    wret = cst[:, C_WRET:C_WRET + 6]
    m0 = cst[:, C_M0:C_M0 + 1]
    m1 = cst[:, C_M0 + 1:C_M0 + 2]
    sel8 = cst[:, C_SEL:C_SEL + 8 * 128]

    def mcol(o, n):
        return misc[:, o:o + n]
    A_in, B_in = mcol(M_AIN, 8), mcol(M_BIN, 8)
    g1a, g1f = mcol(M_G1A, 8), mcol(M_G1F, 8)
    convw = mcol(M_CW, 24)
    convb = mcol(M_CB, 6)
    halovalid = mcol(M_HV, 1)
    halomask = mcol(M_HM, 1)
    modT = mcol(M_MOD, 96)
    lncol = mcol(M_LN, 32)
    ccol = mcol(M_C, 8)
    badaT = mcol(M_BADA, 96)
    dcol = mcol(M_DER, 64)
    rbias = mcol(M_RB, 8)

    dtb = rowp[:, R_DTB:R_DTB + 8]
    alog = rowp[:, R_ALOG:R_ALOG + 8]
    dskip = rowp[:, R_DSK:R_DSK + 8]
    normw = rowp[:, R_NW:R_NW + 512]
    sinks = rowp[:, R_SINK:R_SINK + 4]
    relb = rowp[:, R_RB:R_RB + 128]

    sp = "sp"
    P.dma(sp, cst[:], cst_d, W=["cst"])
    P.dma(sp, misc[:], misc_d, W=["misc"])
    P.dma(sp, rowp[:], rowp_d.partition_broadcast(128), W=["rowp"])
    P.op("dve", lambda: V.tensor_copy(out=ident_b[:], in_=ident), R=["cst"], W=["ident_b"])

    ar = Arena(arena_t, TAIL)
    wada_d = din("w_ada", [1, D, 6 * D])
    wada_sb = [ar.alloc([128, 8, 512]) for _ in range(2)]
    bank_mod, kmod = pb()
    nl = 1
    j = 0
    for li in range(nl):
        for cg in range(12):
            buf = wada_sb[j % 2]
            bk = "wada%d" % (j % 2)
            P.dma(sp, buf, wada_d[li].rearrange("(c p) n -> p c n", p=128)[:, :, cg * 512:(cg + 1) * 512], W=[bk])
            for cc in range(4):
                col = li * 48 + cg * 4 + cc

                def mm(buf=buf, cc=cc, col=col):
                    r = None
                    for kc in range(8):
                        r = T.matmul(bank_mod[:, col:col + 1], lhsT=buf[:, kc, cc * 128:(cc + 1) * 128],
                                     rhs=ccol[:, kc:kc + 1], start=(kc == 0), stop=(kc == 7))
                    return r
                P.op("pe", mm, R=[bk, "misc"], W=[kmod])
            j += 1
    P.op("dve", lambda: V.tensor_tensor(out=modT[:, 0:48 * nl], in0=bank_mod[:, 0:48 * nl], in1=badaT[:, 0:48 * nl], op=ALU.add),
         R=[kmod, "misc"], W=["mod"])
    lg1, lb1, lg2, lb2 = lncol[:, 0:8], lncol[:, 8:16], lncol[:, 16:24], lncol[:, 24:32]
    GA1, BA1 = dcol[:, 0:8], dcol[:, 8:16]
    A2, B2 = dcol[:, 16:24], dcol[:, 24:32]
    GA2, BA2 = dcol[:, 32:40], dcol[:, 40:48]
    tmpc = dcol[:, 48:56]

    def der():
        V.tensor_scalar(out=A_in, in0=modT[:, 8:16], scalar1=1.0, scalar2=1.0 / ALPHA, op0=ALU.add, op1=ALU.mult)
        V.tensor_copy(out=B_in, in_=modT[:, 0:8])
        V.tensor_scalar(out=g1a, in0=modT[:, 16:24], scalar1=1.0, scalar2=None, op0=ALU.add)
        V.tensor_scalar(out=g1f, in0=modT[:, 40:48], scalar1=1.0, scalar2=None, op0=ALU.add)
        V.tensor_scalar(out=GA1, in0=lg1, scalar1=ALPHA, scalar2=None, op0=ALU.mult)
        V.tensor_scalar(out=BA1, in0=lb1, scalar1=ALPHA, scalar2=None, op0=ALU.mult)
        V.tensor_scalar(out=A2, in0=modT[:, 32:40], scalar1=1.0, scalar2=1.0 / ALPHA, op0=ALU.add, op1=ALU.mult)
        V.tensor_copy(out=B2, in_=modT[:, 24:32])
        sc = 1.0
        V.tensor_scalar(out=GA2, in0=lg2, scalar1=sc, scalar2=None, op0=ALU.mult)
        return V.tensor_scalar(out=BA2, in0=lb2, scalar1=sc, scalar2=None, op0=ALU.mult)
    P.op("dve", der, R=["mod", "misc"], W=["der"])
    P.barrier()

    ar = Arena(arena_t, TAIL)
    posi = ar.alloc([128, NCH], I32)
    posf = ar.alloc([128, NCH])
    ang = ar.alloc([128, NCH, 32])
    ang2 = ar.alloc([128, NCH, 32])
    ti = ar.alloc([128, NCH, 32], I32)
    tf = ar.alloc([128, NCH, 32])
    P.dma(sp, posi, pos_d, W=["posi"])

    def rot_tables():
        V.tensor_copy(out=posf, in_=posi)
        V.tensor_tensor(out=ang, in0=_bc(posf.unsqueeze(2), [128, NCH, 32]), in1=_bc(invf.unsqueeze(1), [128, NCH, 32]), op=ALU.mult)
        V.tensor_scalar(out=ang, in0=ang, scalar1=float(1.0 / (2 * np.pi)), scalar2=None, op0=ALU.mult)
        V.tensor_scalar(out=ang2, in0=ang, scalar1=0.25, scalar2=None, op0=ALU.add)
        r = None
        for a in (ang, ang2):
            V.tensor_copy(out=ti, in_=a)
            V.tensor_copy(out=tf, in_=ti)
            V.tensor_tensor(out=a, in0=a, in1=tf, op=ALU.subtract)
            V.tensor_scalar(out=tf, in0=a, scalar1=0.5, scalar2=None, op0=ALU.is_gt)
            V.tensor_tensor(out=a, in0=a, in1=tf, op=ALU.subtract)
            V.tensor_scalar(out=tf, in0=a, scalar1=-0.5, scalar2=None, op0=ALU.is_lt)
            r = V.tensor_tensor(out=a, in0=a, in1=tf, op=ALU.add)
        return r
    P.op("dve", rot_tables, R=["posi", "cst"], W=["ang"])

    def rot_sin():
        S.activation(out=sinT, in_=ang, func=AF.Sin, scale=float(2 * np.pi))
        return S.activation(out=cosT, in_=ang2, func=AF.Sin, scale=float(2 * np.pi))
    P.op("act", rot_sin, R=["ang"], W=["rot"])

    P.op("act", lambda: S.activation(out=alog, in_=alog, func=AF.Exp), R=["rowp"], W=["rowp"])
    P.op("dve", lambda: V.tensor_scalar(out=alog, in0=alog, scalar1=-1.0, scalar2=None, op0=ALU.mult), R=["rowp"], W=["rowp"])
    negA = alog
    dbg("rowp", rowp[:, 0:32], ["rowp"])
    dbg("modT", modT[:, 0:48], ["mod"])

    if full:
        eoh = ar.alloc([128, 256, 32])
        etmp = ar.alloc([128, 256, 32])
        P.dma(sp, eoh.rearrange("p a b -> p (a b)"), eoh_d, W=["eoh"])
        rb3 = relb.rearrange("p (b h) -> p b h", h=4)
        for h in range(4):
            P.op("pool", lambda h=h: G.tensor_tensor(out=etmp, in0=eoh, in1=_bc(rb3[:, :, h].unsqueeze(1), [128, 256, 32]), op=ALU.mult),
                 R=["eoh", "rowp"], W=["etmp"])

            def red(h=h):
                V.tensor_reduce(out=biasw[:, h, :], in_=etmp, axis=AX.X, op=ALU.add)
                return V.tensor_tensor(out=biasw[:, h, :], in0=biasw[:, h, :], in1=madd, op=ALU.add)
            P.op("dve", red, R=["etmp", "cst"], W=["biasw"])
    P.barrier()

    ar = Arena(arena_t, TAIL)
    hT_halo = ar.alloc([128, 8, 128], BF16)
    _mark = ar.off
    xtok = [ar.alloc([128, D]) for _ in range(2)]
    for n in range(-1, NCH):
        buf = xtok[n % 2]
        bk = "xtok%d" % (n % 2)
        P.dma(sp, buf, x_d[(n + 1) * 128:(n + 2) * 128, :], W=[bk])
        for half in range(2):
            bank, bkey = pb()

            def tr(buf=buf, half=half, bank=bank):
                r = None
                for q in range(4):
                    fc = half * 4 + q
                    r = T.transpose(bank[:, q * 128:(q + 1) * 128], buf[:, fc * 128:(fc + 1) * 128], ident)
                return r
            P.op("pe", tr, R=[bk, "cst"], W=[bkey])
            if n >= 0:
                P.op("act", lambda n=n, half=half, bank=bank: S.mul(out=xT[:, half * 4:half * 4 + 4, n * 128:(n + 1) * 128],
                                                                   in_=bank[:].rearrange("p (q t) -> p q t", q=4), mul=(1.0 if ffn_only else ALPHA)),
                     R=[bkey], W=[("xT", n, half)])
            else:
                def hh(half=half, bank=bank):
                    r = None
                    for q in range(4):
                        fc = half * 4 + q
                        r = V.tensor_scalar(out=hT_halo[:, fc, :], in0=bank[:, q * 128:(q + 1) * 128],
                                            scalar1=A_in[:, fc:fc + 1], scalar2=B_in[:, fc:fc + 1], op0=ALU.mult, op1=ALU.add)
                    return r
                def hh2(half=half, bank=bank):
                    r = None
                    for q in range(4):
                        fc = half * 4 + q
                        V.tensor_scalar(out=tmpc[:, 0:1], in0=A_in[:, fc:fc + 1], scalar1=ALPHA, scalar2=None, op0=ALU.mult)
                        r = V.tensor_scalar(out=hT_halo[:, fc, :], in0=bank[:, q * 128:(q + 1) * 128],
                                            scalar1=tmpc[:, 0:1], scalar2=B_in[:, fc:fc + 1], op0=ALU.mult, op1=ALU.add)
                    return r
                P.op("dve", hh2, R=[bkey, "misc", "der"], W=["hT_halo", "der"])

    P.barrier()
    ar.off = _mark
    w_in = ar.alloc([128, 8, IN_DIM], BF16)
    wcols = "w_in_d"
    wi_src = w_in_d.rearrange("(c p) n -> p c n", p=128)
    for a, b in ((0, 1024), (1024, 2048), (2048, 2312), (2568, 2824)):
        P.dma("pool", w_in[:, :, a:b], wi_src[:, :, a:b], W=["w_in"])
    for slot, h in enumerate((0, 2, 1, 3)):
        P.dma("pool", w_in[:, :, 2312 + slot * 64:2312 + (slot + 1) * 64], wi_src[:, :, 2312 + h * 64:2312 + (h + 1) * 64], W=["w_in"])
    if full:
        w_out = ar.alloc([128, 8, D], BF16)
        P.dma("pool", w_out, w_out_d.rearrange("(c p) n -> p c n", p=128), W=["w_out"])

    Sret = ar.alloc([128, 2, 64])
    Sret_b = ar.alloc([128, 2, 64], BF16)
    Sssd = ar.alloc([128, 256])
    Sssd_b = ar.alloc([128, 256], BF16)
    totacc = ar.alloc([128, 8])
    if full:
        stin = ar.alloc([128, 3, STW])
        P.dma(sp, stin, st_in_d.rearrange("s p w -> p s w"), W=["stin"])
        wss = ar.alloc([128, 3, 4])

        def comb():
            V.tensor_tensor(out=Sret, in0=stin[:, 0, 0:128].rearrange("p (t e) -> p t e", t=2),
                            in1=_bc(wret[:, 0:2].unsqueeze(2), [128, 2, 64]), op=ALU.mult)
            for s_ in (1, 2):
                V.tensor_tensor(out=Sssd[:, 0:128].rearrange("p (t e) -> p t e", t=2), in0=stin[:, s_, 0:128].rearrange("p (t e) -> p t e", t=2),
                                in1=_bc(wret[:, 2 * s_:2 * s_ + 2].unsqueeze(2), [128, 2, 64]), op=ALU.mult)
                V.tensor_tensor(out=Sret, in0=Sret, in1=Sssd[:, 0:128].rearrange("p (t e) -> p t e", t=2), op=ALU.add)
            for g in range(2):
                pr = slice(g * 64, (g + 1) * 64)
                V.tensor_copy(out=wss[pr, 1, :], in_=stin[pr, 0, 384 + g * 4:384 + g * 4 + 4])
                V.tensor_tensor(out=wss[pr, 2, :], in0=stin[pr, 0, 384 + g * 4:384 + g * 4 + 4],
                                in1=stin[pr, 1, 384 + g * 4:384 + g * 4 + 4], op=ALU.add)
            return V.memset(wss[:, 0, :], 0.0)
        P.op("dve", comb, R=["stin", "cst"], W=["Sret", "Sssd", "wss"])
        P.op("act", lambda: S.activation(out=wss, in_=wss, func=AF.Exp), R=["wss"], W=["wss"])

        def comb2():
            V.tensor_tensor(out=Sssd.rearrange("p (r e) -> p r e", r=4), in0=stin[:, 0, 128:384].rearrange("p (r e) -> p r e", r=4),
                            in1=_bc(wss[:, 0, :].unsqueeze(2), [128, 4, 64]), op=ALU.mult)
            for s_ in (1, 2):
                V.tensor_tensor(out=stin[:, s_, 128:384].rearrange("p (r e) -> p r e", r=4), in0=stin[:, s_, 128:384].rearrange("p (r e) -> p r e", r=4),
                                in1=_bc(wss[:, s_, :].unsqueeze(2), [128, 4, 64]), op=ALU.mult)
                V.tensor_tensor(out=Sssd, in0=Sssd, in1=stin[:, s_, 128:384], op=ALU.add)
            V.tensor_copy(out=Sssd_b, in_=Sssd)
            return V.tensor_copy(out=Sret_b, in_=Sret)
        P.op("dve", comb2, R=["stin", "wss", "Sret", "Sssd"], W=["Sret", "Sssd", "stin"])
    else:
        def zinit():
            V.memset(Sret, 0.0)
            V.memset(Sssd, 0.0)
            return V.memset(totacc, 0.0)
        P.op("dve", zinit, W=["Sret", "Sssd", "totacc"])

    hTc = [ar.alloc([128, 8, 128], BF16) for _ in range(2)]
    qk_sb = ar.alloc([128, 512])
    qr = ar.alloc([128, 4, 2, 32])
    kr = ar.alloc([128, 4, 2, 32])
    rt = [ar.alloc([128, 4, 32]) for _ in range(4)]
    q2b = ar.alloc([128, 256], BF16)
    k2b = ar.alloc([128, 256], BF16)
    v_b = ar.alloc([128, 256], BF16)
    sg = ar.alloc([128, 256])
    qkT = ar.alloc([128, 512], BF16)
    qm = ar.alloc([128, 4, 128], BF16)
    BCm = ar.alloc([128, 4, 128], BF16)
    sTm = ar.alloc([128, 512], BF16)
    osq = qr.rearrange("p h two f -> p h (two f)")
    onr = kr.rearrange("p h two f -> p h (two f)")
    gst = ar.alloc([128, 16])
    xr = ar.alloc([128, 6, 131])
    acc = ar.alloc([128, 6, 128])
    bc_b = ar.alloc([128, 2, 128], BF16)
    Bm_b = ar.alloc([128, 128], BF16)
    amask = ar.alloc([128, 8, 128])
    eseg = ar.alloc([128, 8, 128], BF16)
    mT = ar.alloc([128, 8, 128], BF16)
    cbm = ar.alloc([128, 2, 128])
    xdt_b = ar.alloc([128, 8, 64], BF16)
    xdd_b = ar.alloc([128, 8, 64], BF16)
    xskip = ar.alloc([128, 8, 64])
    t1 = ar.alloc([128, 8, 64])
    szs = ar.alloc([128, 512])
    hsq = ar.alloc([128, 512])
    sm = ar.alloc([128, 64])
    qT_s = ar.alloc([128, 4, 128], BF16)
    kT_pp = [ar.alloc([128, 128], BF16) for _ in range(2)]
    v_pp = [ar.alloc([128, 128], BF16) for _ in range(2)]
    sl = amask.rearrange("p h l -> p (h l)").rearrange("p (h j) -> p h j", h=4)
    p_b = ar.alloc([128, 4, 256], BF16)
    pT_b = ar.alloc([128, 8, 128], BF16)
    ssw = ar.alloc([128, 32])
    mix_tok = ar.alloc([128, D], BF16)
    mixT = ar.alloc([128, 8, 128], BF16)
    sq = [ar.alloc([128, 128]) for _ in range(2)]
    lnst = t1.rearrange("p h d -> p (h d)").rearrange("p (a b) -> p a b", a=4)
    lnk = ["t1"]
    mixer_arena_end = ar.off

    def zmask():
        V.memset(qm, 0.0)
        V.memset(BCm, 0.0)
        return V.memset(qT_s, 0.0)
    P.op("dve", zmask, W=["qm", "BCm", "qT_s"])

    dtv, av_, acs_tot, ed, eaed, cdec, dd = (sm[:, 0:8], sm[:, 8:16], sm[:, 16:32], sm[:, 32:48], sm[:, 48:64], None, None)

    sm2 = ar.alloc([128, 32])
    cdec = sm2[:, 0:8]
    dd = sm2[:, 8:16]
    rr = sm2[:, 16:24]

    def ln_inplace(n_cols, xs_keyR, xview, GA, BA, nfree):
        bank, bkey = pb()
        bank2, bkey2 = (bank[:, nfree:2 * nfree], bkey) if nfree <= 256 else pb()
        if nfree > 256:
            bank2 = bank2[:, 0:nfree]
        for fc in range(8):
            sqb = sq[fc % 2]
            sk = "sq%d" % (fc % 2)
            P.op("pool", lambda fc=fc, sqb=sqb: G.tensor_tensor(out=sqb[:, 0:nfree], in0=xview[:, fc, :], in1=xview[:, fc, :], op=ALU.mult),
                 R=xs_keyR, W=[sk])

            def mm(fc=fc, sqb=sqb, bank=bank):
                T.matmul(bank[:, 0:nfree], lhsT=ones, rhs=xview[:, fc, :], start=(fc == 0), stop=(fc == 7))
                return T.matmul(bank2, lhsT=ones, rhs=sqb[:, 0:nfree], start=(fc == 0), stop=(fc == 7))
            P.op("pe", mm, R=xs_keyR + [sk, "cst"], W=[bkey, bkey2])
        mean, msq, var, rstd = lnst[:, 0, 0:nfree], lnst[:, 1, 0:nfree], lnst[:, 2, 0:nfree], lnst[:, 3, 0:nfree]

        def st(bank=bank):
            V.tensor_scalar(out=mean, in0=bank[:, 0:nfree], scalar1=1.0 / D, scalar2=None, op0=ALU.mult)
            V.tensor_tensor(out=msq, in0=mean, in1=mean, op=ALU.mult)
            V.scalar_tensor_tensor(out=var, in0=bank2, scalar=1.0 / D, in1=msq, op0=ALU.mult, op1=ALU.subtract)
            return V.tensor_scalar(out=var, in0=var, scalar1=EPS, scalar2=None, op0=ALU.add)
        P.op("dve", st, R=[bkey, bkey2], W=[lnk[0]])
        P.op("act", lambda: S.sqrt(out=rstd, in_=var), R=[lnk[0]], W=[lnk[0]])

        def nrm():
            V.reciprocal(out=rstd, in_=rstd)
            V.tensor_tensor(out=xview, in0=xview, in1=_bc(mean.unsqueeze(1), [128, 8, nfree]), op=ALU.subtract)
            return V.tensor_tensor(out=xview, in0=xview, in1=_bc(rstd.unsqueeze(1), [128, 8, nfree]), op=ALU.mult)
        P.op("dve", nrm, R=[lnk[0]] + xs_keyR, W=xs_keyR + [lnk[0]])

        def aff():
            G.tensor_tensor(out=xview, in0=xview, in1=_bc(GA.unsqueeze(2), [128, 8, nfree]), op=ALU.mult)
            return G.tensor_tensor(out=xview, in0=xview, in1=_bc(BA.unsqueeze(2), [128, 8, nfree]), op=ALU.add)
        P.op("pool", aff, R=xs_keyR + ["der", "misc"], W=xs_keyR)

    def chunk(n):
        halo = n < 0
        cur, prv = (n % 2), ((n + 1) % 2)
        if halo:
            hc = hT_halo
            hk = "hT_halo"
        else:
            hc = hTc[n % 2]
            hk = "hTc%d" % (n % 2)
            xk = [("xT", n, 0), ("xT", n, 1)]
            Tn = slice(n * 128, (n + 1) * 128)

            def mkh2():
                r = None
                for fc in range(8):
                    r = G.tensor_scalar(out=hc[:, fc, :], in0=xT[:, fc, Tn], scalar1=A_in[:, fc:fc + 1], scalar2=B_in[:, fc:fc + 1],
                                        op0=ALU.mult, op1=ALU.add)
                return r
            P.op("pool", mkh2, R=xk + ["misc", "der"], W=[hk])

        def proj_tok(bank, c0, c1, o0=0):
            def f():
                r = None
                for kc in range(8):
                    r = T.matmul(bank[:, o0:o0 + (c1 - c0)], lhsT=hc[:, kc, :], rhs=w_in[:, kc, c0:c1], start=(kc == 0), stop=(kc == 7))
                return r
            return f

        def proj_feat(bank, c0, o0):
            def f():
                r = None
                for kc in range(8):
                    r = T.matmul(bank[:, o0:o0 + 128], lhsT=w_in[:, kc, c0:c0 + 128], rhs=hc[:, kc, :], start=(kc == 0), stop=(kc == 7))
                return r
            return f

        bD, kD = pb()
        bE, kE = pb()
        bF, kF = pb()
        if full:
            P.op("pe", proj_tok(bD, 2696, 2824, 8), R=[hk, "w_in"], W=[kD])
            P.op("pe", proj_feat(bD, 2568, 256), R=[hk, "w_in"], W=[kD])
        for c in range(4):
            P.op("pe", proj_feat(bE, 1536 + c * 128, c * 128), R=[hk, "w_in"], W=[kE])
        P.op("pe", proj_feat(bF, 2048, 0), R=[hk, "w_in"], W=[kF])
        P.op("pe", proj_feat(bF, 2176, 128), R=[hk, "w_in"], W=[kF])
        if halo:
            def tail():
                V.tensor_scalar(out=xr[:, 0:4, 128:131], in0=bE[:].rearrange("p (c t) -> p c t", c=4)[:, :, 125:128],
                                scalar1=halovalid, scalar2=None, op0=ALU.mult)
                return V.tensor_scalar(out=xr[:, 4:6, 128:131], in0=bF[:, 0:256].rearrange("p (c t) -> p c t", c=2)[:, :, 125:128],
                                       scalar1=halovalid, scalar2=None, op0=ALU.mult)
            P.op("dve", tail, R=[kE, kF, "misc"], W=["xr"])
            if full:
                def kv():
                    S.copy(out=kT_pp[cur], in_=bD[:, 256:384])
                    return S.copy(out=v_pp[cur], in_=bD[:, 8:136])
                P.op("act", kv, R=[kD], W=["kT_pp%d" % cur, "v_pp%d" % cur])
            return
        P.op("pe", proj_tok(bD, 2304, 2312, 0), R=[hk, "w_in"], W=[kD])
        bA, kA = pb()
        bB, kB = pb()
        if full:
            P.op("pe", proj_tok(bA, 0, 512), R=[hk, "w_in"], W=[kA])
            P.op("pe", proj_tok(bB, 512, 1024), R=[hk, "w_in"], W=[kB])
            bC, kC = pb()
            P.op("pe", proj_tok(bC, 1024, 1536), R=[hk, "w_in"], W=[kC])
            P.op("pe", proj_feat(bF, 2312, 256), R=[hk, "w_in"], W=[kF])
            P.op("pe", proj_feat(bF, 2440, 384), R=[hk, "w_in"], W=[kF])
        else:
            P.op("pe", proj_tok(bA, 256, 512, 256), R=[hk, "w_in"], W=[kA])
            P.op("pe", proj_tok(bB, 512, 768, 0), R=[hk, "w_in"], W=[kB])

        P.op("dve", lambda: V.tensor_tensor(out=dtv, in0=bD[:, 0:8], in1=dtb, op=ALU.add), R=[kD, "rowp"], W=["dtv"])
        P.op("pool", lambda: G.tensor_copy(out=xr[:, :, 0:3], in_=xr[:, :, 128:131]), R=["xr"], W=["xr"])

        def xrcp():
            S.copy(out=xr[:, 0:4, 3:131], in_=bE[:].rearrange("p (c t) -> p c t", c=4))
            return S.copy(out=xr[:, 4:6, 3:131], in_=bF[:, 0:256].rearrange("p (c t) -> p c t", c=2))
        P.op("act", xrcp, R=[kE, kF], W=["xr"])
        P.op("act", lambda: S.copy(out=qk_sb[:, (0 if full else 256):512], in_=bA[:, (0 if full else 256):512]), R=[kA], W=["qk_sb"])
        P.op("act", lambda: S.copy(out=v_b, in_=bB[:, 0:256]), R=[kB], W=["v_b"])
        if full:
            def swc():
                for h_ in range(4):
                    kvh_, gq_ = h_ // 2, h_ % 2
                    pr_ = slice(kvh_ * 64, kvh_ * 64 + 64)
                    S.copy(out=qT_s[pr_, h_, :], in_=bF[pr_, 256 + gq_ * 128:256 + (gq_ + 1) * 128])
                S.copy(out=kT_pp[cur], in_=bD[:, 256:384])
                return S.copy(out=v_pp[cur], in_=bD[:, 8:136])
            P.op("act", swc, R=[kF, kD], W=["qT_s", "kT_pp%d" % cur, "v_pp%d" % cur])
            P.op("act", lambda: S.activation(out=sg, in_=bB[:, 256:512], func=AF.Silu), R=[kB], W=["sg"])
            P.op("act", lambda: S.activation(out=szs, in_=bC[:], func=AF.Silu), R=[kC], W=["szs"])
        if SUB[0] < 1:
            return
        cosb = _bc(cosT[:, n, :].unsqueeze(1), [128, 4, 32])
        sinb = _bc(sinT[:, n, :].unsqueeze(1), [128, 4, 32])

        def rotary(E, src, dst, ta, tb):
            X = src.rearrange("p (h two f) -> p h two f", h=4, two=2)
            x1, x2 = X[:, :, 0, :], X[:, :, 1, :]
            E.tensor_tensor(out=ta, in0=x1, in1=cosb, op=ALU.mult)
            E.tensor_tensor(out=tb, in0=x2, in1=sinb, op=ALU.mult)
            E.tensor_tensor(out=dst[:, :, 0, :], in0=ta, in1=tb, op=ALU.subtract)
            E.tensor_tensor(out=ta, in0=x1, in1=sinb, op=ALU.mult)
            E.tensor_tensor(out=tb, in0=x2, in1=cosb, op=ALU.mult)
            return E.tensor_tensor(out=dst[:, :, 1, :], in0=ta, in1=tb, op=ALU.add)

        def krot():
            rotary(V, qk_sb[:, 256:512], kr, rt[2], rt[3])
            return V.tensor_tensor(out=k2b[:].rearrange("p (h d) -> p h d", h=4), in0=_bc(dk.unsqueeze(2), [128, 4, 64]),
                                   in1=kr.rearrange("p h two f -> p h (two f)"), op=ALU.mult)
        P.op("dve", krot, R=["qk_sb", "rot", "cst"], W=["kr", "k2b"])
        if full:
            def qrot():
                rotary(V, qk_sb[:, 0:256], qr, rt[0], rt[1])
                return V.tensor_tensor(out=q2b[:].rearrange("p (h d) -> p h d", h=4), in0=_bc(dq.unsqueeze(2), [128, 4, 64]),
                                       in1=qr.rearrange("p h two f -> p h (two f)"), op=ALU.mult)
            P.op("dve", qrot, R=["qk_sb", "rot", "cst"], W=["qr", "q2b"])
            if SUB[0] < 1.05:
                return
            bT, kT_ = pb()
            bTb = bT[:].bitcast(BF16)

            def trqk():
                r = None
                for t in range(2):
                    T.transpose(bTb[:, t * 128:(t + 1) * 128], q2b[:, t * 128:(t + 1) * 128], ident_b[:])
                    r = T.transpose(bTb[:, 256 + t * 128:256 + (t + 1) * 128], k2b[:, t * 128:(t + 1) * 128], ident_b[:])
                return r
            P.op("pe", trqk, R=["q2b", "k2b", "ident_b"], W=[kT_])
            def qkcp():
                for h_ in range(4):
                    t_, hf2 = h_ // 2, h_ % 2
                    pr_ = slice(hf2 * 64, hf2 * 64 + 64)
                    S.copy(out=qm[pr_, h_, :], in_=bTb[pr_, t_ * 128:(t_ + 1) * 128])
                return S.copy(out=qkT[:, 256:512], in_=bTb[:, 256:512])
            P.op("act", qkcp, R=[kT_], W=["qkT", "qm"])
            if SUB[0] < 1.1:
                return
            bS, kS = pb()

            def scores():
                r = None
                import os
                for h in [int(c_) for c_ in os.environ.get("SUBH", "0123")]:
                    t, hf_ = h // 2, h % 2
                    pr = slice(hf_ * 64, hf_ * 64 + 64)
                    r = T.matmul(bS[:, h * 128:(h + 1) * 128], lhsT=qkT[:, 256 + t * 128:256 + (t + 1) * 128],
                                 rhs=qm[:, h, :], start=True, stop=True)
                return r
            P.op("pe", scores, R=["qkT", "qm"], W=[kS])
            if SUB[0] < 1.15:
                return
            P.op("dve", lambda: V.tensor_tensor(out=sTm[:].rearrange("p (h i) -> p h i", h=4), in0=_bc(tri.unsqueeze(1), [128, 4, 128]),
                                                in1=bS[:].rearrange("p (h i) -> p h i", h=4), op=ALU.mult), R=[kS, "cst"], W=["sTm"])
            if SUB[0] < 1.2:
                return
            bO, kO = pb()

            def oret():
                r = None
                for h in range(4):
                    t, hf_ = h // 2, h % 2
                    pr = slice(hf_ * 64, hf_ * 64 + 64)
                    T.matmul(bO[:, h * 64:(h + 1) * 64], lhsT=sTm[:, h * 128:(h + 1) * 128], rhs=v_b[:, h * 64:(h + 1) * 64], start=True, stop=False)
                    r = T.matmul(bO[:, h * 64:(h + 1) * 64], lhsT=qm[:, h, :], rhs=Sret_b[:, t, :], start=False, stop=True)
                return r
            P.op("pe", oret, R=["sTm", "v_b", "qm", "Sret_b"], W=[kO])
        if SUB[0] < 1.3:
            return
        bK, kK = pb()

        def kvm():
            r = None
            for t in range(2):
                r = T.matmul(bK[:, t * 128:(t + 1) * 128], lhsT=k2b[:, t * 128:(t + 1) * 128], rhs=v_b[:, t * 128:(t + 1) * 128], start=True, stop=True)
            return r
        P.op("pe", kvm, R=["k2b", "v_b"], W=[kK])

        if SUB[0] < 1.6:
            return

        def supd():
            K4 = bK[:, 0:256].rearrange("p (t hf e) -> p t hf e", t=2, hf=2)
            V.scalar_tensor_tensor(out=Sret, in0=K4[:, :, 0, :], scalar=m0, in1=Sret, op0=ALU.mult, op1=ALU.add)
            V.scalar_tensor_tensor(out=Sret, in0=K4[:, :, 1, :], scalar=m1, in1=Sret, op0=ALU.mult, op1=ALU.add)
            V.tensor_tensor(out=Sret, in0=Sret, in1=_bc(gC.unsqueeze(2), [128, 2, 64]), op=ALU.mult)
            return V.tensor_copy(out=Sret_b, in_=Sret)
        P.op("dve", supd, R=[kK, "Sret", "cst"], W=["Sret", "Sret_b"])
        if full:
            P.op("act", lambda: S.activation(out=osq, in_=bO[:, 0:256].rearrange("p (h d) -> p h d", h=4), func=AF.Square), R=[kO], W=["qr"])

            def gn1():
                V.tensor_reduce(out=gst[:, 0:4], in_=bO[:, 0:256].rearrange("p (h d) -> p h d", h=4), axis=AX.X, op=ALU.add)
                V.tensor_reduce(out=gst[:, 4:8], in_=osq, axis=AX.X, op=ALU.add)
                V.tensor_scalar(out=gst[:, 0:4], in0=gst[:, 0:4], scalar1=1.0 / 64, scalar2=None, op0=ALU.mult)
                V.tensor_tensor(out=gst[:, 8:12], in0=gst[:, 0:4], in1=gst[:, 0:4], op=ALU.mult)
                V.scalar_tensor_tensor(out=gst[:, 4:8], in0=gst[:, 4:8], scalar=1.0 / 64, in1=gst[:, 8:12], op0=ALU.mult, op1=ALU.subtract)
                return V.tensor_scalar(out=gst[:, 4:8], in0=gst[:, 4:8], scalar1=EPS, scalar2=None, op0=ALU.add)
            P.op("dve", gn1, R=[kO, "qr"], W=["gst"])
            P.op("act", lambda: S.sqrt(out=gst[:, 4:8], in_=gst[:, 4:8]), R=["gst"], W=["gst"])

            def gn2():
                V.reciprocal(out=gst[:, 4:8], in_=gst[:, 4:8])
                V.tensor_tensor(out=onr, in0=bO[:, 0:256].rearrange("p (h d) -> p h d", h=4), in1=_bc(gst[:, 0:4].unsqueeze(2), [128, 4, 64]), op=ALU.subtract)
                V.tensor_tensor(out=onr, in0=onr, in1=_bc(gst[:, 4:8].unsqueeze(2), [128, 4, 64]), op=ALU.mult)
                return V.tensor_tensor(out=mix_tok[:, 0:256], in0=onr.rearrange("p h d -> p (h d)"), in1=sg, op=ALU.mult)
            P.op("dve", gn2, R=[kO, "gst", "sg"], W=["kr", "gst", "mix_ret"])

        if SUB[0] < 2:
            return

        def conv(E, cs):
            def f():
                r = None
                for c in cs:
                    E.tensor_scalar(out=acc[:, c, :], in0=xr[:, c, 0:128], scalar1=convw[:, c * 4:c * 4 + 1], scalar2=convb[:, c:c + 1],
                                    op0=ALU.mult, op1=ALU.add)
                    for w in range(1, 4):
                        r = E.scalar_tensor_tensor(out=acc[:, c, :], in0=xr[:, c, w:w + 128], scalar=convw[:, c * 4 + w:c * 4 + w + 1],
                                                   in1=acc[:, c, :], op0=ALU.mult, op1=ALU.add)
                return r
            return f
        P.op("dve", conv(V, (0, 1, 4)), R=["xr", "misc"], W=["accA"])
        P.op("dve", conv(V, (2, 3, 5)), R=["xr", "misc"], W=["accB"])

        def sil():
            S.activation(out=acc[:, 0:4, :], in_=acc[:, 0:4, :], func=AF.Silu)
            return S.activation(out=bc_b, in_=acc[:, 4:6, :], func=AF.Silu)
        P.op("act", sil, R=["accA", "accB"], W=["accA", "accB", "bc_b"])
        if full:
            def bcm():
                r = None
                for g_ in range(2):
                    pr_ = slice(g_ * 64, g_ * 64 + 64)
                    G.tensor_copy(out=BCm[pr_, g_, :], in_=bc_b[pr_, 0, :])
                    r = G.tensor_copy(out=BCm[pr_, 2 + g_, :], in_=bc_b[pr_, 1, :])
                return r
            P.op("pool", bcm, R=["bc_b"], W=["BCm"])
        bX, kX = pb()

        def trx():
            r = None
            for c in range(4):
                r = T.transpose(bX[:, c * 128:(c + 1) * 128], acc[:, c, :], ident)
            return r
        P.op("pe", trx, R=["accA", "accB", "cst"], W=[kX])
        bBm, kBm = pb()
        bBmb = bBm[:].bitcast(BF16)
        P.op("pe", lambda: T.transpose(bBmb[:, 0:128], bc_b[:, 0, :], ident_b[:]), R=["bc_b", "ident_b"], W=[kBm])
        P.op("act", lambda: S.copy(out=Bm_b, in_=bBmb[:, 0:128]), R=[kBm], W=["Bm_b"])
        if SUB[0] < 3:
            return
        def sp_():
            S.activation(out=dtv, in_=dtv, func=AF.Exp)
            return S.activation(out=dtv, in_=dtv, func=AF.Ln, bias=1.0)
        if n == 0:
            dbg("dtv_pre", dtv, ["dtv"])
        P.op("act", sp_, R=["dtv"], W=["dtv"])
        if n == 0:
            dbg("dtv", dtv, ["dtv"])
        P.op("dve", lambda: V.tensor_tensor(out=av_, in0=dtv, in1=negA, op=ALU.mult), R=["dtv", "rowp"], W=["av"])
        bY, kY = pb()

        def acsm():
            T.matmul(bY[:, 0:8], lhsT=tri, rhs=av_, start=True, stop=True)
            return T.matmul(bY[:, 8:16], lhsT=ones, rhs=av_, start=True, stop=True)
        P.op("pe", acsm, R=["av", "cst"], W=[kY])
        P.op("act", lambda: S.copy(out=acs_tot, in_=bY[:, 0:16]), R=[kY], W=["acs_tot"])
        if n == 0:
            dbg("av", av_, ["av"])
            dbg("acs_tot", acs_tot, ["acs_tot"])

        def edf():
            V.tensor_copy(out=ed[:, 0:8], in_=acs_tot[:, 0:8])
            return V.tensor_tensor(out=ed[:, 8:16], in0=acs_tot[:, 8:16], in1=acs_tot[:, 0:8], op=ALU.subtract)
        P.op("dve", edf, R=["acs_tot"], W=["ed"])

        def exps():
            S.activation(out=eaed, in_=ed, func=AF.Exp)
            return S.activation(out=cdec, in_=acs_tot[:, 8:16], func=AF.Exp)
        P.op("act", exps, R=["ed", "acs_tot"], W=["eaed", "cdec"])
        if not full:
            P.op("pool", lambda: G.tensor_tensor(out=totacc, in0=totacc, in1=acs_tot[:, 8:16], op=ALU.add), R=["acs_tot", "totacc"], W=["totacc"])
        P.op("dve", lambda: V.tensor_tensor(out=dd, in0=dtv, in1=eaed[:, 8:16], op=ALU.mult), R=["dtv", "eaed"], W=["dd"])
        X3 = bX[:].rearrange("p (h d) -> p h d", h=8)
        P.op("dve", lambda: V.tensor_tensor(out=xdd_b, in0=_bc(dd.unsqueeze(2), [128, 8, 64]), in1=X3, op=ALU.mult), R=[kX, "dd"], W=["xdd_b"])
        if full:
            def xd():
                V.tensor_tensor(out=xdt_b, in0=_bc(dtv.unsqueeze(2), [128, 8, 64]), in1=X3, op=ALU.mult)
                return V.tensor_tensor(out=xskip, in0=X3, in1=_bc(dskip.unsqueeze(2), [128, 8, 64]), op=ALU.mult)
            P.op("dve", xd, R=[kX, "dtv", "rowp"], W=["xdt_b", "xskip"])
            P.op("pool", lambda: G.tensor_tensor(out=amask, in0=_bc(mgt.unsqueeze(1), [128, 8, 128]), in1=_bc(av_.unsqueeze(2), [128, 8, 128]), op=ALU.mult),
                 R=["av", "cst"], W=["amask"])
            bCB, kCB = pb()

            def cbm_():
                r = None
                for g in range(2):
                    pr = slice(g * 64, g * 64 + 64)
                    r = T.matmul(bCB[:, g * 128:(g + 1) * 128], lhsT=BCm[:, g, :], rhs=bc_b[:, 1, :], start=True, stop=True)
                return r
            P.op("pe", cbm_, R=["bc_b", "BCm"], W=[kCB])
            P.op("dve", lambda: V.tensor_tensor(out=cbm, in0=bCB[:, 0:256].rearrange("p (g l) -> p g l", g=2), in1=_bc(tri.unsqueeze(1), [128, 2, 128]), op=ALU.mult),
                 R=[kCB, "cst"], W=["cbm"])
            for g in range(2):
                bSg, kSg = pb()

                def segm(g=g, bSg=bSg):
                    r = None
                    for r_ in range(4):
                        r = T.matmul(bSg[:, r_ * 128:(r_ + 1) * 128], lhsT=amask[:, g * 4 + r_, :], rhs=tri, start=True, stop=True)
                    return r
                P.op("pe", segm, R=["amask", "cst"], W=[kSg])
                P.op("act", lambda g=g, bSg=bSg: S.activation(out=eseg[:, g * 4:g * 4 + 4, :], in_=bSg[:].rearrange("p (r l) -> p r l", r=4), func=AF.Exp),
                     R=[kSg], W=["eseg%d" % g])
                P.op("dve", lambda g=g: V.tensor_tensor(out=mT[:, g * 4:g * 4 + 4, :], in0=_bc(cbm[:, g, :].unsqueeze(1), [128, 4, 128]),
                                                        in1=eseg[:, g * 4:g * 4 + 4, :], op=ALU.mult),
                     R=["eseg%d" % g, "cbm"], W=["mT%d" % g])
            bYD, kYD = pb()

            def ydm():
                r = None
                for h in range(8):
                    r = T.matmul(bYD[:, h * 64:(h + 1) * 64], lhsT=mT[:, h, :], rhs=xdt_b[:, h, :], start=True, stop=True)
                return r
            P.op("pe", ydm, R=["mT0", "mT1", "xdt_b"], W=[kYD])
            bYO, kYO = pb()

            def yom():
                r = None
                for g in range(2):
                    pr = slice(g * 64, g * 64 + 64)
                    r = T.matmul(bYO[:, g * 256:(g + 1) * 256], lhsT=BCm[:, 2 + g, :], rhs=Sssd_b, start=True, stop=True)
                return r
            P.op("pe", yom, R=["BCm", "Sssd_b"], W=[kYO])
        if SUB[0] < 4:
            return
        bST, kST = pb()
        P.op("pe", lambda: T.matmul(bST[:, 0:512], lhsT=Bm_b, rhs=xdd_b[:].rearrange("p h d -> p (h d)"), start=True, stop=True),
             R=["Bm_b", "xdd_b"], W=[kST])

        def sssd():
            r = None
            for g in range(2):
                pr = slice(g * 64, g * 64 + 64)
                V.tensor_tensor(out=Sssd[pr, :].rearrange("p (r e) -> p r e", r=4), in0=Sssd[pr, :].rearrange("p (r e) -> p r e", r=4),
                                in1=_bc(cdec[pr, g * 4:g * 4 + 4].unsqueeze(2), [64, 4, 64]), op=ALU.mult)
                r = V.tensor_tensor(out=Sssd[pr, :], in0=Sssd[pr, :], in1=bST[pr, g * 256:(g + 1) * 256], op=ALU.add)
            return r
        P.op("dve", sssd, R=[kST, "Sssd", "cdec"], W=["Sssd"])
        if not full:
            return
        P.op("act", lambda: S.copy(out=Sssd_b, in_=Sssd), R=["Sssd"], W=["Sssd_b"])

        def ycomb():
            V.tensor_tensor(out=t1, in0=bYO[:].rearrange("p (h d) -> p h d", h=8), in1=_bc(eaed[:, 0:8].unsqueeze(2), [128, 8, 64]), op=ALU.mult)
            return V.tensor_tensor(out=t1, in0=t1, in1=bYD[:].rearrange("p (h d) -> p h d", h=8), op=ALU.add)
        P.op("dve", ycomb, R=[kYO, kYD, "eaed"], W=["t1"])
        t1f = t1.rearrange("p h d -> p (h d)")

        def yg():
            G.tensor_tensor(out=t1f, in0=t1f, in1=xskip.rearrange("p h d -> p (h d)"), op=ALU.add)
            G.tensor_tensor(out=t1f, in0=t1f, in1=szs, op=ALU.mult)
            return G.tensor_tensor(out=hsq, in0=t1f, in1=t1f, op=ALU.mult)
        P.op("pool", yg, R=["t1", "xskip", "szs"], W=["t1", "hsq"])

        def rms1():
            V.tensor_reduce(out=rr[:, 0:2], in_=hsq.rearrange("p (g e) -> p g e", g=2), axis=AX.X, op=ALU.add)
            return V.tensor_scalar(out=rr[:, 0:2], in0=rr[:, 0:2], scalar1=1.0 / 256, scalar2=EPS, op0=ALU.mult, op1=ALU.add)
        P.op("dve", rms1, R=["hsq"], W=["rr"])
        P.op("act", lambda: S.sqrt(out=rr[:, 0:2], in_=rr[:, 0:2]), R=["rr"], W=["rr"])

        def rms2():
            V.reciprocal(out=rr[:, 0:2], in_=rr[:, 0:2])
            V.tensor_tensor(out=hsq.rearrange("p (g e) -> p g e", g=2), in0=t1f.rearrange("p (g e) -> p g e", g=2),
                            in1=_bc(rr[:, 0:2].unsqueeze(2), [128, 2, 256]), op=ALU.mult)
            return V.tensor_tensor(out=mix_tok[:, 256:768], in0=hsq, in1=normw, op=ALU.mult)
        P.op("dve", rms2, R=["rr", "t1", "hsq", "rowp"], W=["hsq", "mix_ssd", "rr"])

        bL = [pb(), pb()]

        def lgm():
            r = None
            for h in range(4):
                kvh, gq = h // 2, h % 2
                pr = slice(kvh * 64, kvh * 64 + 64)
                bank = bL[h // 2][0]
                for part, buf in ((0, kT_pp[prv]), (1, kT_pp[cur])):
                    o = (h % 2) * 256 + part * 128
                    r = T.matmul(bank[:, o:o + 128], lhsT=qT_s[:, h, :], rhs=buf, start=True, stop=True)
            return r
        P.op("pe", lgm, R=["qT_s", "kT_pp0", "kT_pp1"], W=[bL[0][1], bL[1][1]])

        def sls():
            r = None
            for hb in range(2):
                r = V.scalar_tensor_tensor(out=sl[:, hb * 2:hb * 2 + 2, :], in0=bL[hb][0][:].rearrange("p (h j) -> p h j", h=2), scalar=0.125,
                                           in1=biasw[:, hb * 2:hb * 2 + 2, :], op0=ALU.mult, op1=ALU.add)
            if n == 0:
                r = V.tensor_scalar(out=sl[:, :, 0:128], in0=sl[:, :, 0:128], scalar1=halomask, scalar2=None, op0=ALU.add)
            V.tensor_reduce(out=ssw[:, 0:4], in_=sl, axis=AX.X, op=ALU.max)
            V.tensor_tensor(out=ssw[:, 0:4], in0=ssw[:, 0:4], in1=sinks, op=ALU.max)
            V.tensor_scalar(out=ssw[:, 4:8], in0=ssw[:, 0:4], scalar1=-1.0, scalar2=None, op0=ALU.mult)
            return V.tensor_tensor(out=ssw[:, 8:12], in0=sinks, in1=ssw[:, 4:8], op=ALU.add)
        P.op("dve", sls, R=[bL[0][1], bL[1][1], "biasw", "misc", "rowp"], W=["amask", "ssw"])

        def pex():
            r = None
            for h in range(4):
                r = S.activation(out=p_b[:, h, :], in_=sl[:, h, :], func=AF.Exp, bias=ssw[:, 4 + h:5 + h], scale=1.0)
            return S.activation(out=ssw[:, 12:16], in_=ssw[:, 8:12], func=AF.Exp)
        P.op("act", pex, R=["amask", "ssw"], W=["p_b", "ssw2"])

        def den():
            V.tensor_reduce(out=ssw[:, 16:20], in_=p_b, axis=AX.X, op=ALU.add)
            V.tensor_tensor(out=ssw[:, 16:20], in0=ssw[:, 16:20], in1=ssw[:, 12:16], op=ALU.add)
            return V.reciprocal(out=ssw[:, 20:24], in_=ssw[:, 16:20])
        P.op("dve", den, R=["p_b", "ssw2"], W=["ssw3"])
        bPT, kPT = pb()
        bPTb = bPT[:].bitcast(BF16)

        def ptr():
            r = None
            for h in range(4):
                for part in range(2):
                    j_ = h * 2 + part
                    r = T.transpose(bPTb[:, j_ * 128:(j_ + 1) * 128], p_b[:, h, part * 128:(part + 1) * 128], ident_b[:])
            return r
        P.op("pe", ptr, R=["p_b", "ident_b"], W=[kPT])
        P.op("act", lambda: S.copy(out=pT_b[:, 0:4, :], in_=bPTb[:, 0:512].rearrange("p (j i) -> p j i", j=4)), R=[kPT], W=["pT_b0"])
        P.op("dve", lambda: V.tensor_copy(out=pT_b[:, 4:8, :], in_=bPTb[:, 512:1024].rearrange("p (j i) -> p j i", j=4)), R=[kPT], W=["pT_b1"])
        bOS, kOS = pb()

        def osw():
            r = None
            for h in range(4):
                kvh = h // 2
                for part, buf in ((0, v_pp[prv]), (1, v_pp[cur])):
                    r = T.matmul(bOS[:, h * 64:(h + 1) * 64], lhsT=pT_b[:, h * 2 + part, :], rhs=buf[:, kvh * 64:(kvh + 1) * 64],
                                 start=(part == 0), stop=(part == 1))
            return r
        P.op("pe", osw, R=["pT_b0", "pT_b1", "v_pp0", "v_pp1"], W=[kOS])
        P.op("dve", lambda: V.tensor_tensor(out=mix_tok[:, 768:1024].rearrange("p (h d) -> p h d", h=4), in0=_bc(ssw[:, 20:24].unsqueeze(2), [128, 4, 64]),
                                            in1=bOS[:, 0:256].rearrange("p (h d) -> p h d", h=4), op=ALU.mult), R=[kOS, "ssw3"], W=["mix_swa"])

        bMT, kMT = pb()
        bMTb = bMT[:].bitcast(BF16)

        def mtr():
            r = None
            for kc in range(8):
                r = T.transpose(bMTb[:, kc * 128:(kc + 1) * 128], mix_tok[:, kc * 128:(kc + 1) * 128], ident_b[:])
            return r
        P.op("pe", mtr, R=["mix_ret", "mix_ssd", "mix_swa", "ident_b"], W=[kMT])
        P.op("act", lambda: S.copy(out=mixT, in_=bMTb[:, 0:1024].rearrange("p (k t) -> p k t", k=8)), R=[kMT], W=["mixT"])
        for half in range(2):
            bW, kW = pb()

            def wo(half=half, bW=bW):
                r = None
                for q in range(4):
                    fc = half * 4 + q
                    for kc in range(8):
                        r = T.matmul(bW[:, q * 128:(q + 1) * 128], lhsT=w_out[:, kc, fc * 128:(fc + 1) * 128], rhs=mixT[:, kc, :],
                                     start=(kc == 0), stop=(kc == 7))
                return r
            P.op("pe", wo, R=["w_out", "mixT"], W=[kW])

            def res(half=half, bW=bW):
                r = None
                for q in range(4):
                    fc = half * 4 + q
                    r = V.scalar_tensor_tensor(out=xT[:, fc, Tn], in0=bW[:, q * 128:(q + 1) * 128], scalar=g1a[:, fc:fc + 1], in1=xT[:, fc, Tn],
                                               op0=ALU.mult, op1=ALU.add)
                return r
            P.op("dve", res, R=[kW, "der", ("xT", n, half)], W=[("xT", n, half)])
        ln_inplace(128, xk, xT[:, :, Tn], GA1, BA1, 128)

    for n in range(-1, NCH):
        if STOP[0] >= 4 + (n + 1) and not ffn_only:
            chunk(n)

    if not full:
        sto = ar.alloc([128, STW])

        def pk():
            V.tensor_copy(out=sto[:, 0:128], in_=Sret.rearrange("p t e -> p (t e)"))
            V.tensor_copy(out=sto[:, 128:384], in_=Sssd)
            return V.tensor_copy(out=sto[:, 384:392], in_=totacc)
        P.op("dve", pk, R=["Sret", "Sssd", "totacc"], W=["sto"])
        P.dma(sp, st_out_d, sto, R=["sto"], is_output=True)
        return P.emit()

    P.barrier()
    ar = Arena(arena_t, AW)
    hT = ar.alloc([128, 8, NTOK], BF16)
    aT = [ar.alloc([128, 4, 1024], BF16) for _ in range(2)]
    wgu = [ar.alloc([128, 8, 1024], BF16) for _ in range(2)]
    wdb = [ar.alloc([128, 4, D], BF16) for _ in range(2)]
    sgt = [ar.alloc([128, 512]) for _ in range(2)]
    evt = [ar.alloc([128, 512]) for _ in range(2)]
    if moe:
        gbc = [ar.alloc([128, 1024]) for _ in range(2)]
        rw_sb = ar.alloc([128, 8, 8])
        lgT = ar.alloc([128, 512])
        gatesT = ar.alloc([128, NTOK])
        lg = ar.alloc([128, NCH, 8])
        gts = ar.alloc([128, NCH, 8])
        e1 = ar.alloc([128, NCH, 8])
        e2 = ar.alloc([128, NCH, 8])
        l2 = ar.alloc([128, NCH, 8])
        tk = ar.alloc([128, 6, NCH])
        h2f = [ar.alloc([128, 512]) for _ in range(2)]
        P.dma(sp, rw_sb, rw_d, W=["rw_sb"])

    if moe:
        bG, kG = pb()
    for tg in range(4):
        Tg = slice(tg * 512, (tg + 1) * 512)
        xkeys = [("xT", n, hf_) for n in range(tg * 4, tg * 4 + 4) for hf_ in range(2)]
        if moe:
            bR, kR = pb()
        for fc in range(8):
            P.op("act", lambda fc=fc, Tg=Tg: S.activation(out=hT[:, fc, Tg], in_=xT[:, fc, Tg], func=AF.Identity, bias=B2[:, fc:fc + 1], scale=A2[:, fc:fc + 1]),
                 R=xkeys + ["der"], W=[("hT", tg)])
            if moe:
                hb = h2f[fc % 2]
                hbk = "h2f%d" % (fc % 2)
                P.op("dve", lambda fc=fc, Tg=Tg, hb=hb: V.tensor_scalar(out=hb, in0=xT[:, fc, Tg], scalar1=A2[:, fc:fc + 1], scalar2=B2[:, fc:fc + 1], op0=ALU.mult, op1=ALU.add),
                     R=xkeys + ["der"], W=[hbk])
                P.op("pe", lambda fc=fc, hb=hb, bR=bR: T.matmul(bR[0:8, 0:512], lhsT=rw_sb[:, fc, :], rhs=hb, start=(fc == 0), stop=(fc == 7)),
                     R=[hbk, "rw_sb"], W=[kR])
        if moe:
            P.op("act", lambda bR=bR: S.activation(out=lgT[0:8, 0:512], in_=bR[0:8, 0:512], func=AF.Identity, bias=rbias[0:8, 0:1], scale=1.0),
                 R=[kR, "misc"], W=["lgT"])

            def ltr(tg=tg):
                r = None
                for q in range(4):
                    n = tg * 4 + q
                    r = T.transpose(bG[:, n * 8:(n + 1) * 8], lgT[0:8, q * 128:(q + 1) * 128], ident[0:8, 0:8])
                return r
            P.op("pe", ltr, R=["lgT", "cst"], W=[kG])
    if moe:

        def top2():
            V.tensor_copy(out=lg, in_=bG[:, 0:128].rearrange("p (n e) -> p n e", e=8))
            m1_, m2_, dlt, w1_, w2_ = tk[:, 0, :], tk[:, 1, :], tk[:, 2, :], tk[:, 3, :], tk[:, 4, :]
            V.tensor_reduce(out=m1_, in_=lg, axis=AX.X, op=ALU.max)
            V.tensor_tensor(out=e1, in0=lg, in1=_bc(m1_.unsqueeze(2), [128, NCH, 8]), op=ALU.is_equal)
            V.scalar_tensor_tensor(out=l2, in0=e1, scalar=-1e30, in1=lg, op0=ALU.mult, op1=ALU.add)
            V.tensor_reduce(out=m2_, in_=l2, axis=AX.X, op=ALU.max)
            V.tensor_tensor(out=e2, in0=l2, in1=_bc(m2_.unsqueeze(2), [128, NCH, 8]), op=ALU.is_equal)
            return V.tensor_tensor(out=dlt, in0=m2_, in1=m1_, op=ALU.subtract)
        P.op("dve", top2, R=[kG], W=["tk", "lg"])
        P.op("act", lambda: S.activation(out=tk[:, 2, :], in_=tk[:, 2, :], func=AF.Exp), R=["tk"], W=["tk"])

        def top2b():
            dlt, w1_, w2_ = tk[:, 2, :], tk[:, 3, :], tk[:, 4, :]
            V.tensor_scalar(out=w1_, in0=dlt, scalar1=1.0, scalar2=None, op0=ALU.add)
            V.reciprocal(out=w1_, in_=w1_)
            V.tensor_tensor(out=w2_, in0=dlt, in1=w1_, op=ALU.mult)
            V.tensor_tensor(out=e1, in0=e1, in1=_bc(w1_.unsqueeze(2), [128, NCH, 8]), op=ALU.mult)
            V.tensor_tensor(out=e2, in0=e2, in1=_bc(w2_.unsqueeze(2), [128, NCH, 8]), op=ALU.mult)
            return V.tensor_tensor(out=gts, in0=e1, in1=e2, op=ALU.add)
        P.op("dve", top2b, R=["tk", "lg"], W=["gts", "tk", "lg"])
        for tg in range(4):
            bG2, kG2 = pb()

            def gtr(tg=tg, bG2=bG2):
                r = None
                for q in range(4):
                    n = tg * 4 + q
                    r = T.transpose(bG2[0:8, q * 128:(q + 1) * 128], gts[:, n, :], ident)
                return r
            P.op("pe", gtr, R=["gts", "cst"], W=[kG2])
            P.op("act", lambda tg=tg, bG2=bG2: S.copy(out=gatesT[0:8, tg * 512:(tg + 1) * 512], in_=bG2[0:8, 0:512]), R=[kG2], W=["gatesT"])

    nexp = NEXP if moe else 1
    dff = D_FFE if moe else D_FF
    pieces = []
    o = 0
    while o < dff:
        w = min(512, dff - o)
        pieces.append((o, w))
        o += w
    pi = 0
    for e in range(nexp):
        if moe:
            for half in range(2):
                for q in range(2):
                    bg_, kg_ = pb()
                    P.op("pe", lambda e=e, half=half, q=q, bg_=bg_: T.matmul(bg_[:, 0:512], lhsT=sel8[0:8, e * 128:(e + 1) * 128],
                                                                              rhs=gatesT[0:8, half * 1024 + q * 512:half * 1024 + (q + 1) * 512], start=True, stop=True),
                         R=["gatesT", "cst"], W=[kg_])
                    P.op("act", lambda half=half, q=q, bg_=bg_: S.copy(out=gbc[half][:, q * 512:(q + 1) * 512], in_=bg_[:, 0:512]), R=[kg_], W=["gbc%d" % half])
        for (o, w) in pieces:
            nb = w // 128
            wb = pi % 2
            kwg, kwd = "wgu%d" % wb, "wd%d" % wb
            P.dma("pool", wgu[wb][:, :, 0:w], wg_d[e].rearrange("(c p) n -> p c n", p=128)[:, :, o:o + w], W=[kwg])
            P.dma("pool", wgu[wb][:, :, 512:512 + w], wu_d[e].rearrange("(c p) n -> p c n", p=128)[:, :, o:o + w], W=[kwg])
            P.dma("pool", wdb[wb][:, 0:nb, :], wd_d[e][o:o + w, :].rearrange("(c p) n -> p c n", p=128), W=[kwd])
            for half in range(2):
                for blk in range(nb):
                    for q in range(2):
                        tg = half * 2 + q
                        Tg = slice(tg * 512, (tg + 1) * 512)
                        bg_, kg_ = pb()
                        bu_, ku_ = pb()

                        def gu(blk=blk, Tg=Tg, bg_=bg_, bu_=bu_, wb=wb):
                            r = None
                            for kc in range(8):
                                T.matmul(bg_[:, 0:512], lhsT=wgu[wb][:, kc, blk * 128:(blk + 1) * 128], rhs=hT[:, kc, Tg], start=(kc == 0), stop=(kc == 7))
                            for kc in range(8):
                                r = T.matmul(bu_[:, 0:512], lhsT=wgu[wb][:, kc, 512 + blk * 128:512 + (blk + 1) * 128], rhs=hT[:, kc, Tg], start=(kc == 0), stop=(kc == 7))
                            return r
                        P.op("pe", gu, R=[kwg, ("hT", tg)], W=[kg_, ku_])
                        sb_ = sgt[(blk * 2 + q) % 2]
                        sk_ = "sgt%d" % ((blk * 2 + q) % 2)
                        P.op("act", lambda bg_=bg_, sb_=sb_: S.activation(out=sb_, in_=bg_[:, 0:512], func=AF.Silu), R=[kg_], W=[sk_])
                        P.op("dve", lambda half=half, blk=blk, q=q, bu_=bu_, sb_=sb_: V.tensor_tensor(out=aT[half][:, blk, q * 512:(q + 1) * 512], in0=bu_[:, 0:512], in1=sb_, op=ALU.mult),
                             R=[ku_, sk_], W=[("aT", half, blk, q)])
            for half in range(2):
                for fc in range(8):
                    for q in range(2):
                        tg = half * 2 + q
                        Tg = slice(tg * 512, (tg + 1) * 512)
                        bo_, ko_ = pb()

                        def dn(half=half, fc=fc, q=q, bo_=bo_, wb=wb, nb=nb):
                            r = None
                            for blk in range(nb):
                                r = T.matmul(bo_[:, 0:512], lhsT=wdb[wb][:, blk, fc * 128:(fc + 1) * 128], rhs=aT[half][:, blk, q * 512:(q + 1) * 512],
                                             start=(blk == 0), stop=(blk == nb - 1))
                            return r
                        P.op("pe", dn, R=[kwd] + [("aT", half, blk, q) for blk in range(nb)], W=[ko_])
                        eb = evt[(fc * 2 + q) % 2]
                        ek = "evt%d" % ((fc * 2 + q) % 2)
                        if moe:
                            P.op("dve", lambda fc=fc, half=half, q=q, bo_=bo_, eb=eb: V.scalar_tensor_tensor(out=eb, in0=bo_[:, 0:512], scalar=g1f[:, fc:fc + 1],
                                                                                                          in1=gbc[half][:, q * 512:(q + 1) * 512], op0=ALU.mult, op1=ALU.mult),
                                 R=[ko_, "der", "gbc%d" % half], W=[ek])
                        else:
                            P.op("dve", lambda fc=fc, bo_=bo_, eb=eb: V.tensor_scalar(out=eb, in0=bo_[:, 0:512], scalar1=g1f[:, fc:fc + 1], scalar2=None, op0=ALU.mult),
                                 R=[ko_, "der"], W=[ek])
                        xkeys = [("xT", n, fc // 4) for n in range(tg * 4, tg * 4 + 4)]
                        P.op("pool", lambda fc=fc, Tg=Tg, eb=eb: G.tensor_tensor(out=xT[:, fc, Tg], in0=xT[:, fc, Tg], in1=eb, op=ALU.add),
                             R=[ek] + xkeys, W=xkeys)
            pi += 1

    P.barrier()
    ar = Arena(arena_t, AW)
    sq = [ar.alloc([128, 512]) for _ in range(2)]
    lnst = ar.alloc([128, 4, 512])
    lnk = ["lnst"]
    for tg in range(4):
        Tg = slice(tg * 512, (tg + 1) * 512)
        xkeys = [("xT", n, hf_) for n in range(tg * 4, tg * 4 + 4) for hf_ in range(2)]
        ln_inplace(512, xkeys, xT[:, :, Tg], GA2, BA2, 512)

    xo = [ar.alloc([128, D]) for _ in range(2)]
    for n in range(NCH):
        buf = xo[n % 2]
        bk = "xo%d" % (n % 2)
        for half in range(2):
            bank, bkey = pb()

            def tr2(n=n, half=half, bank=bank):
                r = None
                for q in range(4):
                    fc = half * 4 + q
                    r = T.transpose(bank[:, q * 128:(q + 1) * 128], xT[:, fc, n * 128:(n + 1) * 128], ident)
                return r
            P.op("pe", tr2, R=[("xT", n, 0), ("xT", n, 1), "cst"], W=[bkey])
            P.op("act", lambda half=half, bank=bank, buf=buf: S.copy(out=buf[:, half * 512:(half + 1) * 512], in_=bank[:, 0:512]), R=[bkey], W=[bk + "_%d" % half])
        P.dma(sp, xo_d[n * 128:(n + 1) * 128, :], buf, R=[bk + "_0", bk + "_1"], is_output=True)
    return P.emit()


FSTOP = [99]
NR = 4
AONLY = [True]
NEXPRUN = [99]
MAINCH = [99]
MSUB = [99]
DECL_IN = set()


def build_fused():
    P = Prog()
    nc = P.nc
    V, S, G, T = EngProxy(P, "dve", nc.vector), EngProxy(P, "act", nc.scalar), EngProxy(P, "pool", nc.gpsimd), nc.tensor

    def din(name, shape, dt=F32):
        DECL_IN.add(name)
        return nc.dram_tensor(name, list(shape), dt, kind="ExternalInput").ap()

    def dout(name, shape, dt=F32):
        return nc.dram_tensor(name, list(shape), dt, kind="ExternalOutput").ap()

    def dbg(name, ap, keys):
        if name not in DBG:
            return
        shp = list(ap.shape)
        d_ = dout("dbg_" + name, shp, ap.dtype)
        P.dma("sp", d_, ap, R=list(keys), is_output=True)

    x_d = din("xin", [NTOK + 128, D])
    pos_d = din("pos", [128, NCH], I32)
    cst_d = din("cst", [128, CW])
    misc_all = din("misc", [DEPTH, 128, MW])
    rowp_all = din("rowp", [DEPTH, RW])
    relb_d = din("relb", [128])
    selw_d = din("selw", [128, 32])
    w_in_all = din("w_in", [DEPTH, D, IN_DIM])
    w_out_all = din("w_out", [DEPTH, D, D])
    wada_all = din("w_ada", [DEPTH, D, 6 * D])
    eoh_d = din("eoh", [128, 256 * 32])
    wg0_d = din("wg0", [1, D, D_FF])
    wu0_d = din("wu0", [1, D, D_FF])
    wd0_d = din("wd0", [1, D_FF, D])
    if FSTOP[0] >= 14:
        wg1_d = din("wg1", [NEXP, D, D_FFE])
        wu1_d = din("wu1", [NEXP, D, D_FFE])
        wd1_d = din("wd1", [NEXP, D_FFE, D])
        rw_d = din("rw", [128, 8, 8])
    else:
        wg1_d = wu1_d = wd1_d = rw_d = None
    xo_d = dout("xout", [NTOK, D])
    bounce_s = [nc.dram_tensor("bounce_s%d" % i, [128, STW], F32).ap() for i in range(DEPTH)]
    gath_s = [nc.dram_tensor("gath_s%d" % i, [NR * 128, STW], F32).ap() for i in range(DEPTH)]
    bounce_h = nc.dram_tensor("bounce_h", [128, D], F32).ap()
    gath_h = nc.dram_tensor("gath_h", [NR * 128, D], F32).ap()
    ALLC = [[0, 1, 2, 3], [4, 5, 6, 7]]

    xT = P.sbuf("xT", [128, 8, NTOK], F32)
    cst = P.sbuf("cst_sb", [128, CW], F32)
    misc = P.sbuf("misc_sb", [128, MW], F32)
    rowp = P.sbuf("rowp_sb", [128, RW], F32)
    ident_b = P.sbuf("ident_b", [128, 128], BF16)
    AW = 33700
    arena_t = P.sbuf("arena", [128, AW], F32)
    TAIL = AW - 2048
    cosT = arena_t[:, TAIL:TAIL + 512].rearrange("p (n f) -> p n f", f=32)
    sinT = arena_t[:, TAIL + 512:TAIL + 1024].rearrange("p (n f) -> p n f", f=32)
    biasw = arena_t[:, TAIL + 1024:TAIL + 2048].rearrange("p (h j) -> p h j", h=4)
    ps = [P.psum("ps%d" % i, [128, 512], F32) for i in range(8)]
    psk = ["ps%d" % i for i in range(8)]
    pctr = [0]

    def pb():
        i = pctr[0] % 8
        pctr[0] += 1
        return ps[i], psk[i]

    ident = cst[:, C_ID:C_ID + 128]
    tri = cst[:, C_TRI:C_TRI + 128]
    mgt = cst[:, C_MGT:C_MGT + 128]
    ones = cst[:, C_ONE:C_ONE + 128]
    dq = cst[:, C_DQ:C_DQ + 4]
    dk = cst[:, C_DK:C_DK + 4]
    gC = cst[:, C_GC:C_GC + 2]
    invf = cst[:, C_INV:C_INV + 32]
    madd = cst[:, C_MADD:C_MADD + 256]
    wret = cst[:, C_WRET:C_WRET + 6]
    m0 = cst[:, C_M0:C_M0 + 1]
    m1 = cst[:, C_M0 + 1:C_M0 + 2]
    sel8 = cst[:, C_SEL:C_SEL + 8 * 128]

    def mcol(o, n):
        return misc[:, o:o + n]
    A_in, B_in = mcol(M_AIN, 8), mcol(M_BIN, 8)
    g1a, g1f = mcol(M_G1A, 8), mcol(M_G1F, 8)
    convw = mcol(M_CW, 24)
    convb = mcol(M_CB, 6)
    halovalid = mcol(M_HV, 1)
    halomask = mcol(M_HM, 1)
    modT = mcol(M_MOD, 96)
    lncol = mcol(M_LN, 32)
    ccol = mcol(M_C, 8)
    badaT = mcol(M_BADA, 96)
    dcol = mcol(M_DER, 64)
    rbias = mcol(M_RB, 8)

    dtb = rowp[:, R_DTB:R_DTB + 8]
    alog = rowp[:, R_ALOG:R_ALOG + 8]
    dskip = rowp[:, R_DSK:R_DSK + 8]
    normw = rowp[:, R_NW:R_NW + 512]
    sinks = rowp[:, R_SINK:R_SINK + 4]

    selw = P.sbuf("selw_sb", [128, 32], F32)
    relbt = P.sbuf("relb_sb", [128, 128], F32)
    relb = relbt[:, 0:128]
    sp = "sp"
    P.dma(sp, cst[:], cst_d, W=["cst"])
    P.dma(sp, selw[:], selw_d, W=["selw"])
    P.dma(sp, relbt[:], relb_d.partition_broadcast(128), W=["relb"])
    P.op("dve", lambda: V.tensor_copy(out=ident_b[:], in_=ident), R=["cst"], W=["ident_b"])

    ar = Arena(arena_t, TAIL)
    posi = ar.alloc([128, NCH], I32)
    posf = ar.alloc([128, NCH])
    ang = ar.alloc([128, NCH, 32])
    ang2 = ar.alloc([128, NCH, 32])
    ti = ar.alloc([128, NCH, 32], I32)
    tf = ar.alloc([128, NCH, 32])
    P.dma(sp, posi, pos_d, W=["posi"])

    def rot_tables():
        V.tensor_copy(out=posf, in_=posi)
        V.tensor_tensor(out=ang, in0=_bc(posf.unsqueeze(2), [128, NCH, 32]), in1=_bc(invf.unsqueeze(1), [128, NCH, 32]), op=ALU.mult)
        V.tensor_scalar(out=ang, in0=ang, scalar1=float(1.0 / (2 * np.pi)), scalar2=None, op0=ALU.mult)
        V.tensor_scalar(out=ang2, in0=ang, scalar1=0.25, scalar2=None, op0=ALU.add)
        r = None
        for a in (ang, ang2):
            V.tensor_copy(out=ti, in_=a)
            V.tensor_copy(out=tf, in_=ti)
            V.tensor_tensor(out=a, in0=a, in1=tf, op=ALU.subtract)
            V.tensor_scalar(out=tf, in0=a, scalar1=0.5, scalar2=None, op0=ALU.is_gt)
            V.tensor_tensor(out=a, in0=a, in1=tf, op=ALU.subtract)
            V.tensor_scalar(out=tf, in0=a, scalar1=-0.5, scalar2=None, op0=ALU.is_lt)
            r = V.tensor_tensor(out=a, in0=a, in1=tf, op=ALU.add)
        return r
    P.op("dve", rot_tables, R=["posi", "cst"], W=["ang"])

    def rot_sin():
        S.activation(out=sinT, in_=ang, func=AF.Sin, scale=float(2 * np.pi))
        return S.activation(out=cosT, in_=ang2, func=AF.Sin, scale=float(2 * np.pi))
    P.op("act", rot_sin, R=["ang"], W=["rot"])

    if True:
        eoh = ar.alloc([128, 256, 32])
        etmp = ar.alloc([128, 256, 32])
        P.dma(sp, eoh.rearrange("p a b -> p (a b)"), eoh_d, W=["eoh"])
        rb3 = relb.rearrange("p (b h) -> p b h", h=4)
        for h in range(4):
            P.op("pool", lambda h=h: G.tensor_tensor(out=etmp, in0=eoh, in1=_bc(rb3[:, :, h].unsqueeze(1), [128, 256, 32]), op=ALU.mult),
                 R=["eoh", "relb"], W=["etmp"])

            def red(h=h):
                V.tensor_reduce(out=biasw[:, h, :], in_=etmp, axis=AX.X, op=ALU.add)
                return V.tensor_tensor(out=biasw[:, h, :], in0=biasw[:, h, :], in1=madd, op=ALU.add)
            P.op("dve", red, R=["etmp", "cst"], W=["biasw"])
    P.barrier()

    def layer(L):
        moe = (L % 2 == 1)
        last = (L == DEPTH - 1)
        w_in_d, w_out_d = w_in_all[L], w_out_all[L]
        wg_d, wu_d, wd_d = (wg1_d, wu1_d, wd1_d) if moe else (wg0_d, wu0_d, wd0_d)
        LIM = AW if moe else TAIL
        P.barrier()
        P.dma(sp, misc[:], misc_all[L], W=["misc", "der", "mod"])
        P.dma(sp, rowp[:], rowp_all[L].partition_broadcast(128), W=["rowp"])

        P.op("act", lambda: S.activation(out=alog, in_=alog, func=AF.Exp), R=["rowp"], W=["rowp"])
        P.op("dve", lambda: V.tensor_scalar(out=alog, in0=alog, scalar1=-1.0, scalar2=None, op0=ALU.mult), R=["rowp"], W=["rowp"])
        negA = alog
        dbg("rowp", rowp[:, 0:32], ["rowp"])
        dbg("modT", modT[:, 0:48], ["mod"])

        ar = Arena(arena_t, TAIL)
        wada_d = wada_all[L:L + 1]
        wada_sb = [ar.alloc([128, 8, 512]) for _ in range(2)]
        bank_mod, kmod = pb()
        nl = 1
        j = 0
        for li in range(nl):
            for cg in range(12):
                buf = wada_sb[j % 2]
                bk = "wada%d" % (j % 2)
                P.dma(sp, buf, wada_d[li].rearrange("(c p) n -> p c n", p=128)[:, :, cg * 512:(cg + 1) * 512], W=[bk])
                for cc in range(4):
                    col = li * 48 + cg * 4 + cc

                    def mm(buf=buf, cc=cc, col=col):
                        r = None
                        for kc in range(8):
                            r = T.matmul(bank_mod[:, col:col + 1], lhsT=buf[:, kc, cc * 128:(cc + 1) * 128],
                                         rhs=ccol[:, kc:kc + 1], start=(kc == 0), stop=(kc == 7))
                        return r
                    P.op("pe", mm, R=[bk, "misc"], W=[kmod])
                j += 1
        P.op("dve", lambda: V.tensor_tensor(out=modT[:, 0:48 * nl], in0=bank_mod[:, 0:48 * nl], in1=badaT[:, 0:48 * nl], op=ALU.add),
             R=[kmod, "misc"], W=["mod"])
        lg1, lb1, lg2, lb2 = lncol[:, 0:8], lncol[:, 8:16], lncol[:, 16:24], lncol[:, 24:32]
        GA1, BA1 = dcol[:, 0:8], dcol[:, 8:16]
        A2, B2 = dcol[:, 16:24], dcol[:, 24:32]
        GA2, BA2 = dcol[:, 32:40], dcol[:, 40:48]
        tmpc = dcol[:, 48:56]

        def der():
            V.tensor_scalar(out=A_in, in0=modT[:, 8:16], scalar1=1.0, scalar2=1.0 / ALPHA, op0=ALU.add, op1=ALU.mult)
            V.tensor_copy(out=B_in, in_=modT[:, 0:8])
            V.tensor_scalar(out=g1a, in0=modT[:, 16:24], scalar1=1.0, scalar2=None, op0=ALU.add)
            V.tensor_scalar(out=g1f, in0=modT[:, 40:48], scalar1=1.0, scalar2=None, op0=ALU.add)
            V.tensor_scalar(out=GA1, in0=lg1, scalar1=ALPHA, scalar2=None, op0=ALU.mult)
            V.tensor_scalar(out=BA1, in0=lb1, scalar1=ALPHA, scalar2=None, op0=ALU.mult)
            V.tensor_scalar(out=A2, in0=modT[:, 32:40], scalar1=1.0, scalar2=1.0 / ALPHA, op0=ALU.add, op1=ALU.mult)
            V.tensor_copy(out=B2, in_=modT[:, 24:32])
            sc = 1.0 if last else ALPHA
            V.tensor_scalar(out=GA2, in0=lg2, scalar1=sc, scalar2=None, op0=ALU.mult)
            return V.tensor_scalar(out=BA2, in0=lb2, scalar1=sc, scalar2=None, op0=ALU.mult)
        P.op("dve", der, R=["mod", "misc"], W=["der"])
        P.barrier()

        if L == 0:
            ar = Arena(arena_t, TAIL)
            hT_halo = ar.alloc([128, 8, 128], BF16)
            _mark = ar.off
            xtok = [ar.alloc([128, D]) for _ in range(2)]
            for n in range(-1, NCH):
                buf = xtok[n % 2]
                bk = "xtok%d" % (n % 2)
                P.dma(sp, buf, x_d[(n + 1) * 128:(n + 2) * 128, :], W=[bk])
                for half in range(2):
                    bank, bkey = pb()

                    def tr(buf=buf, half=half, bank=bank):
                        r = None
                        for q in range(4):
                            fc = half * 4 + q
                            r = T.transpose(bank[:, q * 128:(q + 1) * 128], buf[:, fc * 128:(fc + 1) * 128], ident)
                        return r
                    P.op("pe", tr, R=[bk, "cst"], W=[bkey])
                    if n >= 0:
                        P.op("act", lambda n=n, half=half, bank=bank: S.mul(out=xT[:, half * 4:half * 4 + 4, n * 128:(n + 1) * 128],
                                                                           in_=bank[:].rearrange("p (q t) -> p q t", q=4), mul=ALPHA),
                             R=[bkey], W=[("xT", n, half)])
                    else:
                        def hh(half=half, bank=bank):
                            r = None
                            for q in range(4):
                                fc = half * 4 + q
                                r = V.tensor_scalar(out=hT_halo[:, fc, :], in0=bank[:, q * 128:(q + 1) * 128],
                                                    scalar1=A_in[:, fc:fc + 1], scalar2=B_in[:, fc:fc + 1], op0=ALU.mult, op1=ALU.add)
                            return r
                        def hh2(half=half, bank=bank):
                            r = None
                            for q in range(4):
                                fc = half * 4 + q
                                V.tensor_scalar(out=tmpc[:, 0:1], in0=A_in[:, fc:fc + 1], scalar1=ALPHA, scalar2=None, op0=ALU.mult)
                                r = V.tensor_scalar(out=hT_halo[:, fc, :], in0=bank[:, q * 128:(q + 1) * 128],
                                                    scalar1=tmpc[:, 0:1], scalar2=B_in[:, fc:fc + 1], op0=ALU.mult, op1=ALU.add)
                            return r
                        P.op("dve", hh2, R=[bkey, "misc", "der"], W=["hT_halo", "der"])

        else:
            ar = Arena(arena_t, TAIL)
            hT_halo = ar.alloc([128, 8, 128], BF16)
            _mark = ar.off
            halo_g = ar.alloc([128, NR, D])
            hacc = ar.alloc([128, 8, 128])
            P.dma(sp, bounce_h.rearrange("p (c t) -> p c t", c=8), xT[:, :, NTOK - 128:NTOK], R=[("xT", NCH - 1, 0), ("xT", NCH - 1, 1)], W=["bounce_h"])
            P.coll("AllGather", gath_h, bounce_h, ALLC, R=["bounce_h"], W=["gath_h"])
            P.dma(sp, halo_g, gath_h.rearrange("(r p) w -> p r w", p=128), R=["gath_h"], W=["halo_g"])

            def hsel():
                hf_ = hacc.rearrange("p c t -> p (c t)")
                V.tensor_scalar(out=hf_, in0=halo_g[:, 0, :], scalar1=selw[:, 0:1], scalar2=None, op0=ALU.mult)
                for r_ in range(1, NR):
                    V.scalar_tensor_tensor(out=hf_, in0=halo_g[:, r_, :], scalar=selw[:, r_:r_ + 1], in1=hf_, op0=ALU.mult, op1=ALU.add)
                r = None
                for fc in range(8):
                    r = V.tensor_scalar(out=hT_halo[:, fc, :], in0=hacc[:, fc, :], scalar1=A_in[:, fc:fc + 1], scalar2=B_in[:, fc:fc + 1],
                                        op0=ALU.mult, op1=ALU.add)
                return r
            P.op("dve", hsel, R=["halo_g", "selw", "misc", "der"], W=["hT_halo", "hacc"])

        P.barrier()
        ar.off = _mark
        w_in = ar.alloc([128, 8, IN_DIM], BF16)
        wcols = "w_in_d"
        wi_src = w_in_d.rearrange("(c p) n -> p c n", p=128)
        for a, b in ((0, 1024), (1024, 2048), (2048, 2312), (2568, 2824)):
            P.dma("pool", w_in[:, :, a:b], wi_src[:, :, a:b], W=["w_in"])
        for slot, h in enumerate((0, 2, 1, 3)):
            P.dma("pool", w_in[:, :, 2312 + slot * 64:2312 + (slot + 1) * 64], wi_src[:, :, 2312 + h * 64:2312 + (h + 1) * 64], W=["w_in"])
        if True:
            w_out = ar.alloc([128, 8, D], BF16)
            P.dma("pool", w_out, w_out_d.rearrange("(c p) n -> p c n", p=128), W=["w_out"])

        Sret = ar.alloc([128, 2, 64])
        Sret_b = ar.alloc([128, 2, 64], BF16)
        Sssd = ar.alloc([128, 256])
        Sssd_b = ar.alloc([128, 256], BF16)
        totacc = ar.alloc([128, 8])
        def zinit():
            V.memset(Sret, 0.0)
            V.memset(Sssd, 0.0)
            return V.memset(totacc, 0.0)
        P.op("dve", zinit, W=["Sret", "Sssd", "totacc"])

        _off_tmp = ar.off
        hTc = [ar.alloc([128, 8, 128], BF16) for _ in range(2)]
        qk_sb = ar.alloc([128, 512])
        qr = ar.alloc([128, 4, 2, 32])
        kr = ar.alloc([128, 4, 2, 32])
        rt = [ar.alloc([128, 4, 32]) for _ in range(4)]
        q2b = ar.alloc([128, 256], BF16)
        k2b = ar.alloc([128, 256], BF16)
        v_b = ar.alloc([128, 256], BF16)
        sg = ar.alloc([128, 256])
        qkT = ar.alloc([128, 512], BF16)
        qm = ar.alloc([128, 4, 128], BF16)
        BCm = ar.alloc([128, 4, 128], BF16)
        sTm = ar.alloc([128, 512], BF16)
        osq = qr.rearrange("p h two f -> p h (two f)")
        onr = kr.rearrange("p h two f -> p h (two f)")
        gst = ar.alloc([128, 16])
        xr = ar.alloc([128, 6, 131])
        acc = ar.alloc([128, 6, 128])
        bc_b = ar.alloc([128, 2, 128], BF16)
        Bm_b = ar.alloc([128, 128], BF16)
        amask = ar.alloc([128, 8, 128])
        eseg = ar.alloc([128, 8, 128], BF16)
        mT = ar.alloc([128, 8, 128], BF16)
        cbm = ar.alloc([128, 2, 128])
        xdt_b = ar.alloc([128, 8, 64], BF16)
        xdd_b = ar.alloc([128, 8, 64], BF16)
        xskip = ar.alloc([128, 8, 64])
        t1 = ar.alloc([128, 8, 64])
        szs = ar.alloc([128, 512])
        hsq = ar.alloc([128, 512])
        sm = ar.alloc([128, 64])
        qT_s = ar.alloc([128, 4, 128], BF16)
        kT_pp = [ar.alloc([128, 128], BF16) for _ in range(2)]
        v_pp = [ar.alloc([128, 128], BF16) for _ in range(2)]
        sl = amask.rearrange("p h l -> p (h l)").rearrange("p (h j) -> p h j", h=4)
        p_b = ar.alloc([128, 4, 256], BF16)
        pT_b = ar.alloc([128, 8, 128], BF16)
        ssw = ar.alloc([128, 32])
        mix_tok = ar.alloc([128, D], BF16)
        mixT = ar.alloc([128, 8, 128], BF16)
        sq = [ar.alloc([128, 128]) for _ in range(2)]
        lnst = t1.rearrange("p h d -> p (h d)").rearrange("p (a b) -> p a b", a=4)
        lnk = ["t1"]
        mixer_arena_end = ar.off

        def zmask():
            V.memset(qm, 0.0)
            V.memset(BCm, 0.0)
            return V.memset(qT_s, 0.0)


        dtv, av_, acs_tot, ed, eaed, cdec, dd = (sm[:, 0:8], sm[:, 8:16], sm[:, 16:32], sm[:, 32:48], sm[:, 48:64], None, None)

        sm2 = ar.alloc([128, 32])
        cdec = sm2[:, 0:8]
        dd = sm2[:, 8:16]
        rr = sm2[:, 16:24]

        def ln_inplace(n_cols, xs_keyR, xview, GA, BA, nfree):
            bank, bkey = pb()
            bank2, bkey2 = (bank[:, nfree:2 * nfree], bkey) if nfree <= 256 else pb()
            if nfree > 256:
                bank2 = bank2[:, 0:nfree]
            for fc in range(8):
                sqb = sq[fc % 2]
                sk = "sq%d" % (fc % 2)
                P.op("pool", lambda fc=fc, sqb=sqb: G.tensor_tensor(out=sqb[:, 0:nfree], in0=xview[:, fc, :], in1=xview[:, fc, :], op=ALU.mult),
                     R=xs_keyR, W=[sk])

                def mm(fc=fc, sqb=sqb, bank=bank):
                    T.matmul(bank[:, 0:nfree], lhsT=ones, rhs=xview[:, fc, :], start=(fc == 0), stop=(fc == 7))
                    return T.matmul(bank2, lhsT=ones, rhs=sqb[:, 0:nfree], start=(fc == 0), stop=(fc == 7))
                P.op("pe", mm, R=xs_keyR + [sk, "cst"], W=[bkey, bkey2])
            mean, msq, var, rstd = lnst[:, 0, 0:nfree], lnst[:, 1, 0:nfree], lnst[:, 2, 0:nfree], lnst[:, 3, 0:nfree]

            def st(bank=bank):
                V.tensor_scalar(out=mean, in0=bank[:, 0:nfree], scalar1=1.0 / D, scalar2=None, op0=ALU.mult)
                V.tensor_tensor(out=msq, in0=mean, in1=mean, op=ALU.mult)
                V.scalar_tensor_tensor(out=var, in0=bank2, scalar=1.0 / D, in1=msq, op0=ALU.mult, op1=ALU.subtract)
                return V.tensor_scalar(out=var, in0=var, scalar1=EPS, scalar2=None, op0=ALU.add)
            P.op("dve", st, R=[bkey, bkey2], W=[lnk[0]])
            P.op("act", lambda: S.sqrt(out=rstd, in_=var), R=[lnk[0]], W=[lnk[0]])

            def nrm():
                V.reciprocal(out=rstd, in_=rstd)
                V.tensor_tensor(out=xview, in0=xview, in1=_bc(mean.unsqueeze(1), [128, 8, nfree]), op=ALU.subtract)
                return V.tensor_tensor(out=xview, in0=xview, in1=_bc(rstd.unsqueeze(1), [128, 8, nfree]), op=ALU.mult)
            P.op("dve", nrm, R=[lnk[0]] + xs_keyR, W=xs_keyR + [lnk[0]])

            def aff():
                G.tensor_tensor(out=xview, in0=xview, in1=_bc(GA.unsqueeze(2), [128, 8, nfree]), op=ALU.mult)
                return G.tensor_tensor(out=xview, in0=xview, in1=_bc(BA.unsqueeze(2), [128, 8, nfree]), op=ALU.add)
            P.op("pool", aff, R=xs_keyR + ["der", "misc"], W=xs_keyR)

        def chunk(n):
            halo = n < 0
            cur, prv = (n % 2), ((n + 1) % 2)
            if halo:
                hc = hT_halo
                hk = "hT_halo"
            else:
                hc = hTc[n % 2]
                hk = "hTc%d" % (n % 2)
                xk = [("xT", n, 0), ("xT", n, 1)]
                Tn = slice(n * 128, (n + 1) * 128)

                def mkh2():
                    r = None
                    for fc in range(8):
                        r = G.tensor_scalar(out=hc[:, fc, :], in0=xT[:, fc, Tn], scalar1=A_in[:, fc:fc + 1], scalar2=B_in[:, fc:fc + 1],
                                            op0=ALU.mult, op1=ALU.add)
                    return r
                P.op("pool", mkh2, R=xk + ["misc", "der"], W=[hk])

            def proj_tok(bank, c0, c1, o0=0):
                def f():
                    r = None
                    for kc in range(8):
                        r = T.matmul(bank[:, o0:o0 + (c1 - c0)], lhsT=hc[:, kc, :], rhs=w_in[:, kc, c0:c1], start=(kc == 0), stop=(kc == 7))
                    return r
                return f

            def proj_feat(bank, c0, o0):
                def f():
                    r = None
                    for kc in range(8):
                        r = T.matmul(bank[:, o0:o0 + 128], lhsT=w_in[:, kc, c0:c0 + 128], rhs=hc[:, kc, :], start=(kc == 0), stop=(kc == 7))
                    return r
                return f

            bD, kD = pb()
            bE, kE = pb()
            bF, kF = pb()
            if full:
                P.op("pe", proj_tok(bD, 2696, 2824, 8), R=[hk, "w_in"], W=[kD])
                P.op("pe", proj_feat(bD, 2568, 256), R=[hk, "w_in"], W=[kD])
            for c in range(4):
                P.op("pe", proj_feat(bE, 1536 + c * 128, c * 128), R=[hk, "w_in"], W=[kE])
            P.op("pe", proj_feat(bF, 2048, 0), R=[hk, "w_in"], W=[kF])
            P.op("pe", proj_feat(bF, 2176, 128), R=[hk, "w_in"], W=[kF])
            if halo:
                def tail():
                    V.tensor_scalar(out=xr[:, 0:4, 128:131], in0=bE[:].rearrange("p (c t) -> p c t", c=4)[:, :, 125:128],
                                    scalar1=halovalid, scalar2=None, op0=ALU.mult)
                    return V.tensor_scalar(out=xr[:, 4:6, 128:131], in0=bF[:, 0:256].rearrange("p (c t) -> p c t", c=2)[:, :, 125:128],
                                           scalar1=halovalid, scalar2=None, op0=ALU.mult)
                P.op("dve", tail, R=[kE, kF, "misc"], W=["xr"])
                if full:
                    def kv():
                        S.copy(out=kT_pp[cur], in_=bD[:, 256:384])
                        return S.copy(out=v_pp[cur], in_=bD[:, 8:136])
                    P.op("act", kv, R=[kD], W=["kT_pp%d" % cur, "v_pp%d" % cur])
                return
            P.op("pe", proj_tok(bD, 2304, 2312, 0), R=[hk, "w_in"], W=[kD])
            bA, kA = pb()
            bB, kB = pb()
            if full:
                P.op("pe", proj_tok(bA, 0, 512), R=[hk, "w_in"], W=[kA])
                P.op("pe", proj_tok(bB, 512, 1024), R=[hk, "w_in"], W=[kB])
                bC, kC = pb()
                P.op("pe", proj_tok(bC, 1024, 1536), R=[hk, "w_in"], W=[kC])
                P.op("pe", proj_feat(bF, 2312, 256), R=[hk, "w_in"], W=[kF])
                P.op("pe", proj_feat(bF, 2440, 384), R=[hk, "w_in"], W=[kF])
            else:
                P.op("pe", proj_tok(bA, 256, 512, 256), R=[hk, "w_in"], W=[kA])
                P.op("pe", proj_tok(bB, 512, 768, 0), R=[hk, "w_in"], W=[kB])

            P.op("dve", lambda: V.tensor_tensor(out=dtv, in0=bD[:, 0:8], in1=dtb, op=ALU.add), R=[kD, "rowp"], W=["dtv"])
            P.op("pool", lambda: G.tensor_copy(out=xr[:, :, 0:3], in_=xr[:, :, 128:131]), R=["xr"], W=["xr"])

            def xrcp():
                S.copy(out=xr[:, 0:4, 3:131], in_=bE[:].rearrange("p (c t) -> p c t", c=4))
                return S.copy(out=xr[:, 4:6, 3:131], in_=bF[:, 0:256].rearrange("p (c t) -> p c t", c=2))
            P.op("act", xrcp, R=[kE, kF], W=["xr"])
            P.op("act", lambda: S.copy(out=qk_sb[:, (0 if full else 256):512], in_=bA[:, (0 if full else 256):512]), R=[kA], W=["qk_sb"])
            P.op("act", lambda: S.copy(out=v_b, in_=bB[:, 0:256]), R=[kB], W=["v_b"])
            if full:
                def swc():
                    for h_ in range(4):
                        kvh_, gq_ = h_ // 2, h_ % 2
                        pr_ = slice(kvh_ * 64, kvh_ * 64 + 64)
                        S.copy(out=qT_s[pr_, h_, :], in_=bF[pr_, 256 + gq_ * 128:256 + (gq_ + 1) * 128])
                    S.copy(out=kT_pp[cur], in_=bD[:, 256:384])
                    return S.copy(out=v_pp[cur], in_=bD[:, 8:136])
                P.op("act", swc, R=[kF, kD], W=["qT_s", "kT_pp%d" % cur, "v_pp%d" % cur])
                P.op("act", lambda: S.activation(out=sg, in_=bB[:, 256:512], func=AF.Silu), R=[kB], W=["sg"])
                P.op("act", lambda: S.activation(out=szs, in_=bC[:], func=AF.Silu), R=[kC], W=["szs"])
            if SUB[0] < 1:
                return
            cosb = _bc(cosT[:, n, :].unsqueeze(1), [128, 4, 32])
            sinb = _bc(sinT[:, n, :].unsqueeze(1), [128, 4, 32])

            def rotary(E, src, dst, ta, tb):
                X = src.rearrange("p (h two f) -> p h two f", h=4, two=2)
                x1, x2 = X[:, :, 0, :], X[:, :, 1, :]
                E.tensor_tensor(out=ta, in0=x1, in1=cosb, op=ALU.mult)
                E.tensor_tensor(out=tb, in0=x2, in1=sinb, op=ALU.mult)
                E.tensor_tensor(out=dst[:, :, 0, :], in0=ta, in1=tb, op=ALU.subtract)
                E.tensor_tensor(out=ta, in0=x1, in1=sinb, op=ALU.mult)
                E.tensor_tensor(out=tb, in0=x2, in1=cosb, op=ALU.mult)
                return E.tensor_tensor(out=dst[:, :, 1, :], in0=ta, in1=tb, op=ALU.add)

            def krot():
                rotary(V, qk_sb[:, 256:512], kr, rt[2], rt[3])
                return V.tensor_tensor(out=k2b[:].rearrange("p (h d) -> p h d", h=4), in0=_bc(dk.unsqueeze(2), [128, 4, 64]),
                                       in1=kr.rearrange("p h two f -> p h (two f)"), op=ALU.mult)
            P.op("dve", krot, R=["qk_sb", "rot", "cst"], W=["kr", "k2b"])
            if full:
                def qrot():
                    rotary(V, qk_sb[:, 0:256], qr, rt[0], rt[1])
                    return V.tensor_tensor(out=q2b[:].rearrange("p (h d) -> p h d", h=4), in0=_bc(dq.unsqueeze(2), [128, 4, 64]),
                                           in1=qr.rearrange("p h two f -> p h (two f)"), op=ALU.mult)
                P.op("dve", qrot, R=["qk_sb", "rot", "cst"], W=["qr", "q2b"])
                if SUB[0] < 1.05:
                    return
                bT, kT_ = pb()
                bTb = bT[:].bitcast(BF16)

                def trqk():
                    r = None
                    for t in range(2):
                        T.transpose(bTb[:, t * 128:(t + 1) * 128], q2b[:, t * 128:(t + 1) * 128], ident_b[:])
                        r = T.transpose(bTb[:, 256 + t * 128:256 + (t + 1) * 128], k2b[:, t * 128:(t + 1) * 128], ident_b[:])
                    return r
                P.op("pe", trqk, R=["q2b", "k2b", "ident_b"], W=[kT_])
                def qkcp():
                    for h_ in range(4):
                        t_, hf2 = h_ // 2, h_ % 2
                        pr_ = slice(hf2 * 64, hf2 * 64 + 64)
                        S.copy(out=qm[pr_, h_, :], in_=bTb[pr_, t_ * 128:(t_ + 1) * 128])
                    return S.copy(out=qkT[:, 256:512], in_=bTb[:, 256:512])
                P.op("act", qkcp, R=[kT_], W=["qkT", "qm"])
                if SUB[0] < 1.1:
                    return
                bS, kS = pb()

                def scores():
                    r = None
                    import os
                    for h in [int(c_) for c_ in os.environ.get("SUBH", "0123")]:
                        t, hf_ = h // 2, h % 2
                        pr = slice(hf_ * 64, hf_ * 64 + 64)
                        r = T.matmul(bS[:, h * 128:(h + 1) * 128], lhsT=qkT[:, 256 + t * 128:256 + (t + 1) * 128],
                                     rhs=qm[:, h, :], start=True, stop=True)
                    return r
                P.op("pe", scores, R=["qkT", "qm"], W=[kS])
                if SUB[0] < 1.15:
                    return
                P.op("dve", lambda: V.tensor_tensor(out=sTm[:].rearrange("p (h i) -> p h i", h=4), in0=_bc(tri.unsqueeze(1), [128, 4, 128]),
                                                    in1=bS[:].rearrange("p (h i) -> p h i", h=4), op=ALU.mult), R=[kS, "cst"], W=["sTm"])
                if SUB[0] < 1.2:
                    return
                bO, kO = pb()

                def oret():
                    r = None
                    for h in range(4):
                        t, hf_ = h // 2, h % 2
                        pr = slice(hf_ * 64, hf_ * 64 + 64)
                        T.matmul(bO[:, h * 64:(h + 1) * 64], lhsT=sTm[:, h * 128:(h + 1) * 128], rhs=v_b[:, h * 64:(h + 1) * 64], start=True, stop=False)
                        r = T.matmul(bO[:, h * 64:(h + 1) * 64], lhsT=qm[:, h, :], rhs=Sret_b[:, t, :], start=False, stop=True)
                    return r
                P.op("pe", oret, R=["sTm", "v_b", "qm", "Sret_b"], W=[kO])
            if SUB[0] < 1.3:
                return
            bK, kK = pb()

            def kvm():
                r = None
                for t in range(2):
                    r = T.matmul(bK[:, t * 128:(t + 1) * 128], lhsT=k2b[:, t * 128:(t + 1) * 128], rhs=v_b[:, t * 128:(t + 1) * 128], start=True, stop=True)
                return r
            P.op("pe", kvm, R=["k2b", "v_b"], W=[kK])

            if SUB[0] < 1.6:
                return

            def supd():
                K4 = bK[:, 0:256].rearrange("p (t hf e) -> p t hf e", t=2, hf=2)
                V.scalar_tensor_tensor(out=Sret, in0=K4[:, :, 0, :], scalar=m0, in1=Sret, op0=ALU.mult, op1=ALU.add)
                V.scalar_tensor_tensor(out=Sret, in0=K4[:, :, 1, :], scalar=m1, in1=Sret, op0=ALU.mult, op1=ALU.add)
                V.tensor_tensor(out=Sret, in0=Sret, in1=_bc(gC.unsqueeze(2), [128, 2, 64]), op=ALU.mult)
                return V.tensor_copy(out=Sret_b, in_=Sret)
            P.op("dve", supd, R=[kK, "Sret", "cst"], W=["Sret", "Sret_b"])
            if full:
                P.op("act", lambda: S.activation(out=osq, in_=bO[:, 0:256].rearrange("p (h d) -> p h d", h=4), func=AF.Square), R=[kO], W=["qr"])

                def gn1():
                    V.tensor_reduce(out=gst[:, 0:4], in_=bO[:, 0:256].rearrange("p (h d) -> p h d", h=4), axis=AX.X, op=ALU.add)
                    V.tensor_reduce(out=gst[:, 4:8], in_=osq, axis=AX.X, op=ALU.add)
                    V.tensor_scalar(out=gst[:, 0:4], in0=gst[:, 0:4], scalar1=1.0 / 64, scalar2=None, op0=ALU.mult)
                    V.tensor_tensor(out=gst[:, 8:12], in0=gst[:, 0:4], in1=gst[:, 0:4], op=ALU.mult)
                    V.scalar_tensor_tensor(out=gst[:, 4:8], in0=gst[:, 4:8], scalar=1.0 / 64, in1=gst[:, 8:12], op0=ALU.mult, op1=ALU.subtract)
                    return V.tensor_scalar(out=gst[:, 4:8], in0=gst[:, 4:8], scalar1=EPS, scalar2=None, op0=ALU.add)
                P.op("dve", gn1, R=[kO, "qr"], W=["gst"])
                P.op("act", lambda: S.sqrt(out=gst[:, 4:8], in_=gst[:, 4:8]), R=["gst"], W=["gst"])

                def gn2():
                    V.reciprocal(out=gst[:, 4:8], in_=gst[:, 4:8])
                    V.tensor_tensor(out=onr, in0=bO[:, 0:256].rearrange("p (h d) -> p h d", h=4), in1=_bc(gst[:, 0:4].unsqueeze(2), [128, 4, 64]), op=ALU.subtract)
                    V.tensor_tensor(out=onr, in0=onr, in1=_bc(gst[:, 4:8].unsqueeze(2), [128, 4, 64]), op=ALU.mult)
                    return V.tensor_tensor(out=mix_tok[:, 0:256], in0=onr.rearrange("p h d -> p (h d)"), in1=sg, op=ALU.mult)
                P.op("dve", gn2, R=[kO, "gst", "sg"], W=["kr", "gst", "mix_ret"])

            if SUB[0] < 2:
                return

            def conv(E, cs):
                def f():
                    r = None
                    for c in cs:
                        E.tensor_scalar(out=acc[:, c, :], in0=xr[:, c, 0:128], scalar1=convw[:, c * 4:c * 4 + 1], scalar2=convb[:, c:c + 1],
                                        op0=ALU.mult, op1=ALU.add)
                        for w in range(1, 4):
                            r = E.scalar_tensor_tensor(out=acc[:, c, :], in0=xr[:, c, w:w + 128], scalar=convw[:, c * 4 + w:c * 4 + w + 1],
                                                       in1=acc[:, c, :], op0=ALU.mult, op1=ALU.add)
                    return r
                return f
            P.op("dve", conv(V, (0, 1, 4)), R=["xr", "misc"], W=["accA"])
            P.op("dve", conv(V, (2, 3, 5)), R=["xr", "misc"], W=["accB"])

            def sil():
                S.activation(out=acc[:, 0:4, :], in_=acc[:, 0:4, :], func=AF.Silu)
                return S.activation(out=bc_b, in_=acc[:, 4:6, :], func=AF.Silu)
            P.op("act", sil, R=["accA", "accB"], W=["accA", "accB", "bc_b"])
            if full:
                def bcm():
                    r = None
                    for g_ in range(2):
                        pr_ = slice(g_ * 64, g_ * 64 + 64)
                        G.tensor_copy(out=BCm[pr_, g_, :], in_=bc_b[pr_, 0, :])
                        r = G.tensor_copy(out=BCm[pr_, 2 + g_, :], in_=bc_b[pr_, 1, :])
                    return r
                P.op("pool", bcm, R=["bc_b"], W=["BCm"])
            bX, kX = pb()

            def trx():
                r = None
                for c in range(4):
                    r = T.transpose(bX[:, c * 128:(c + 1) * 128], acc[:, c, :], ident)
                return r
            P.op("pe", trx, R=["accA", "accB", "cst"], W=[kX])
            bBm, kBm = pb()
            bBmb = bBm[:].bitcast(BF16)
            P.op("pe", lambda: T.transpose(bBmb[:, 0:128], bc_b[:, 0, :], ident_b[:]), R=["bc_b", "ident_b"], W=[kBm])
            P.op("act", lambda: S.copy(out=Bm_b, in_=bBmb[:, 0:128]), R=[kBm], W=["Bm_b"])
            if SUB[0] < 3:
                return
            def sp_():
                S.activation(out=dtv, in_=dtv, func=AF.Exp)
                return S.activation(out=dtv, in_=dtv, func=AF.Ln, bias=1.0)
            if n == 0:
                dbg("dtv_pre", dtv, ["dtv"])
            P.op("act", sp_, R=["dtv"], W=["dtv"])
            if n == 0:
                dbg("dtv", dtv, ["dtv"])
            P.op("dve", lambda: V.tensor_tensor(out=av_, in0=dtv, in1=negA, op=ALU.mult), R=["dtv", "rowp"], W=["av"])
            bY, kY = pb()

            def acsm():
                T.matmul(bY[:, 0:8], lhsT=tri, rhs=av_, start=True, stop=True)
                return T.matmul(bY[:, 8:16], lhsT=ones, rhs=av_, start=True, stop=True)
            P.op("pe", acsm, R=["av", "cst"], W=[kY])
            P.op("act", lambda: S.copy(out=acs_tot, in_=bY[:, 0:16]), R=[kY], W=["acs_tot"])
            if n == 0:
                dbg("av", av_, ["av"])
                dbg("acs_tot", acs_tot, ["acs_tot"])

            def edf():
                V.tensor_copy(out=ed[:, 0:8], in_=acs_tot[:, 0:8])
                return V.tensor_tensor(out=ed[:, 8:16], in0=acs_tot[:, 8:16], in1=acs_tot[:, 0:8], op=ALU.subtract)
            P.op("dve", edf, R=["acs_tot"], W=["ed"])

            def exps():
                S.activation(out=eaed, in_=ed, func=AF.Exp)
                return S.activation(out=cdec, in_=acs_tot[:, 8:16], func=AF.Exp)
            P.op("act", exps, R=["ed", "acs_tot"], W=["eaed", "cdec"])
            if not full:
                P.op("pool", lambda: G.tensor_tensor(out=totacc, in0=totacc, in1=acs_tot[:, 8:16], op=ALU.add), R=["acs_tot", "totacc"], W=["totacc"])
            P.op("dve", lambda: V.tensor_tensor(out=dd, in0=dtv, in1=eaed[:, 8:16], op=ALU.mult), R=["dtv", "eaed"], W=["dd"])
            X3 = bX[:].rearrange("p (h d) -> p h d", h=8)
            P.op("dve", lambda: V.tensor_tensor(out=xdd_b, in0=_bc(dd.unsqueeze(2), [128, 8, 64]), in1=X3, op=ALU.mult), R=[kX, "dd"], W=["xdd_b"])
            if full:
                def xd():
                    V.tensor_tensor(out=xdt_b, in0=_bc(dtv.unsqueeze(2), [128, 8, 64]), in1=X3, op=ALU.mult)
                    return V.tensor_tensor(out=xskip, in0=X3, in1=_bc(dskip.unsqueeze(2), [128, 8, 64]), op=ALU.mult)
                P.op("dve", xd, R=[kX, "dtv", "rowp"], W=["xdt_b", "xskip"])
                P.op("pool", lambda: G.tensor_tensor(out=amask, in0=_bc(mgt.unsqueeze(1), [128, 8, 128]), in1=_bc(av_.unsqueeze(2), [128, 8, 128]), op=ALU.mult),
                     R=["av", "cst"], W=["amask"])
                bCB, kCB = pb()

                def cbm_():
                    r = None
                    for g in range(2):
                        pr = slice(g * 64, g * 64 + 64)
                        r = T.matmul(bCB[:, g * 128:(g + 1) * 128], lhsT=BCm[:, g, :], rhs=bc_b[:, 1, :], start=True, stop=True)
                    return r
                P.op("pe", cbm_, R=["bc_b", "BCm"], W=[kCB])
                P.op("dve", lambda: V.tensor_tensor(out=cbm, in0=bCB[:, 0:256].rearrange("p (g l) -> p g l", g=2), in1=_bc(tri.unsqueeze(1), [128, 2, 128]), op=ALU.mult),
                     R=[kCB, "cst"], W=["cbm"])
                for g in range(2):
                    bSg, kSg = pb()

                    def segm(g=g, bSg=bSg):
                        r = None
                        for r_ in range(4):
                            r = T.matmul(bSg[:, r_ * 128:(r_ + 1) * 128], lhsT=amask[:, g * 4 + r_, :], rhs=tri, start=True, stop=True)
                        return r
                    P.op("pe", segm, R=["amask", "cst"], W=[kSg])
                    P.op("act", lambda g=g, bSg=bSg: S.activation(out=eseg[:, g * 4:g * 4 + 4, :], in_=bSg[:].rearrange("p (r l) -> p r l", r=4), func=AF.Exp),
                         R=[kSg], W=["eseg%d" % g])
                    P.op("dve", lambda g=g: V.tensor_tensor(out=mT[:, g * 4:g * 4 + 4, :], in0=_bc(cbm[:, g, :].unsqueeze(1), [128, 4, 128]),
                                                            in1=eseg[:, g * 4:g * 4 + 4, :], op=ALU.mult),
                         R=["eseg%d" % g, "cbm"], W=["mT%d" % g])
                bYD, kYD = pb()

                def ydm():
                    r = None
                    for h in range(8):
                        r = T.matmul(bYD[:, h * 64:(h + 1) * 64], lhsT=mT[:, h, :], rhs=xdt_b[:, h, :], start=True, stop=True)
                    return r
                P.op("pe", ydm, R=["mT0", "mT1", "xdt_b"], W=[kYD])
                bYO, kYO = pb()

                def yom():
                    r = None
                    for g in range(2):
                        pr = slice(g * 64, g * 64 + 64)
                        r = T.matmul(bYO[:, g * 256:(g + 1) * 256], lhsT=BCm[:, 2 + g, :], rhs=Sssd_b, start=True, stop=True)
                    return r
                P.op("pe", yom, R=["BCm", "Sssd_b"], W=[kYO])
            if SUB[0] < 4:
                return
            bST, kST = pb()
            P.op("pe", lambda: T.matmul(bST[:, 0:512], lhsT=Bm_b, rhs=xdd_b[:].rearrange("p h d -> p (h d)"), start=True, stop=True),
                 R=["Bm_b", "xdd_b"], W=[kST])

            def sssd():
                r = None
                for g in range(2):
                    pr = slice(g * 64, g * 64 + 64)
                    V.tensor_tensor(out=Sssd[pr, :].rearrange("p (r e) -> p r e", r=4), in0=Sssd[pr, :].rearrange("p (r e) -> p r e", r=4),
                                    in1=_bc(cdec[pr, g * 4:g * 4 + 4].unsqueeze(2), [64, 4, 64]), op=ALU.mult)
                    r = V.tensor_tensor(out=Sssd[pr, :], in0=Sssd[pr, :], in1=bST[pr, g * 256:(g + 1) * 256], op=ALU.add)
                return r
            P.op("dve", sssd, R=[kST, "Sssd", "cdec"], W=["Sssd"])
            if not full:
                return
            P.op("act", lambda: S.copy(out=Sssd_b, in_=Sssd), R=["Sssd"], W=["Sssd_b"])

            def ycomb():
                V.tensor_tensor(out=t1, in0=bYO[:].rearrange("p (h d) -> p h d", h=8), in1=_bc(eaed[:, 0:8].unsqueeze(2), [128, 8, 64]), op=ALU.mult)
                return V.tensor_tensor(out=t1, in0=t1, in1=bYD[:].rearrange("p (h d) -> p h d", h=8), op=ALU.add)
            P.op("dve", ycomb, R=[kYO, kYD, "eaed"], W=["t1"])
            t1f = t1.rearrange("p h d -> p (h d)")

            def yg():
                G.tensor_tensor(out=t1f, in0=t1f, in1=xskip.rearrange("p h d -> p (h d)"), op=ALU.add)
                G.tensor_tensor(out=t1f, in0=t1f, in1=szs, op=ALU.mult)
                return G.tensor_tensor(out=hsq, in0=t1f, in1=t1f, op=ALU.mult)
            P.op("pool", yg, R=["t1", "xskip", "szs"], W=["t1", "hsq"])

            def rms1():
                V.tensor_reduce(out=rr[:, 0:2], in_=hsq.rearrange("p (g e) -> p g e", g=2), axis=AX.X, op=ALU.add)
                return V.tensor_scalar(out=rr[:, 0:2], in0=rr[:, 0:2], scalar1=1.0 / 256, scalar2=EPS, op0=ALU.mult, op1=ALU.add)
            P.op("dve", rms1, R=["hsq"], W=["rr"])
            P.op("act", lambda: S.sqrt(out=rr[:, 0:2], in_=rr[:, 0:2]), R=["rr"], W=["rr"])

            def rms2():
                V.reciprocal(out=rr[:, 0:2], in_=rr[:, 0:2])
                V.tensor_tensor(out=hsq.rearrange("p (g e) -> p g e", g=2), in0=t1f.rearrange("p (g e) -> p g e", g=2),
                                in1=_bc(rr[:, 0:2].unsqueeze(2), [128, 2, 256]), op=ALU.mult)
                return V.tensor_tensor(out=mix_tok[:, 256:768], in0=hsq, in1=normw, op=ALU.mult)
            P.op("dve", rms2, R=["rr", "t1", "hsq", "rowp"], W=["hsq", "mix_ssd", "rr"])

            bL = [pb(), pb()]

            def lgm():
                r = None
                for h in range(4):
                    kvh, gq = h // 2, h % 2
                    pr = slice(kvh * 64, kvh * 64 + 64)
                    bank = bL[h // 2][0]
                    for part, buf in ((0, kT_pp[prv]), (1, kT_pp[cur])):
                        o = (h % 2) * 256 + part * 128
                        r = T.matmul(bank[:, o:o + 128], lhsT=qT_s[:, h, :], rhs=buf, start=True, stop=True)
                return r
            P.op("pe", lgm, R=["qT_s", "kT_pp0", "kT_pp1"], W=[bL[0][1], bL[1][1]])

            def sls():
                r = None
                for hb in range(2):
                    r = V.scalar_tensor_tensor(out=sl[:, hb * 2:hb * 2 + 2, :], in0=bL[hb][0][:].rearrange("p (h j) -> p h j", h=2), scalar=0.125,
                                               in1=biasw[:, hb * 2:hb * 2 + 2, :], op0=ALU.mult, op1=ALU.add)
                if n == 0:
                    r = V.tensor_scalar(out=sl[:, :, 0:128], in0=sl[:, :, 0:128], scalar1=halomask, scalar2=None, op0=ALU.add)
                V.tensor_reduce(out=ssw[:, 0:4], in_=sl, axis=AX.X, op=ALU.max)
                V.tensor_tensor(out=ssw[:, 0:4], in0=ssw[:, 0:4], in1=sinks, op=ALU.max)
                V.tensor_scalar(out=ssw[:, 4:8], in0=ssw[:, 0:4], scalar1=-1.0, scalar2=None, op0=ALU.mult)
                return V.tensor_tensor(out=ssw[:, 8:12], in0=sinks, in1=ssw[:, 4:8], op=ALU.add)
            P.op("dve", sls, R=[bL[0][1], bL[1][1], "biasw", "misc", "rowp"], W=["amask", "ssw"])

            def pex():
                r = None
                for h in range(4):
                    r = S.activation(out=p_b[:, h, :], in_=sl[:, h, :], func=AF.Exp, bias=ssw[:, 4 + h:5 + h], scale=1.0)
                return S.activation(out=ssw[:, 12:16], in_=ssw[:, 8:12], func=AF.Exp)
            P.op("act", pex, R=["amask", "ssw"], W=["p_b", "ssw2"])

            def den():
                V.tensor_reduce(out=ssw[:, 16:20], in_=p_b, axis=AX.X, op=ALU.add)
                V.tensor_tensor(out=ssw[:, 16:20], in0=ssw[:, 16:20], in1=ssw[:, 12:16], op=ALU.add)
                return V.reciprocal(out=ssw[:, 20:24], in_=ssw[:, 16:20])
            P.op("dve", den, R=["p_b", "ssw2"], W=["ssw3"])
            bPT, kPT = pb()
            bPTb = bPT[:].bitcast(BF16)

            def ptr():
                r = None
                for h in range(4):
                    for part in range(2):
                        j_ = h * 2 + part
                        r = T.transpose(bPTb[:, j_ * 128:(j_ + 1) * 128], p_b[:, h, part * 128:(part + 1) * 128], ident_b[:])
                return r
            P.op("pe", ptr, R=["p_b", "ident_b"], W=[kPT])
            P.op("act", lambda: S.copy(out=pT_b[:, 0:4, :], in_=bPTb[:, 0:512].rearrange("p (j i) -> p j i", j=4)), R=[kPT], W=["pT_b0"])
            P.op("dve", lambda: V.tensor_copy(out=pT_b[:, 4:8, :], in_=bPTb[:, 512:1024].rearrange("p (j i) -> p j i", j=4)), R=[kPT], W=["pT_b1"])
            bOS, kOS = pb()

            def osw():
                r = None
                for h in range(4):
                    kvh = h // 2
                    for part, buf in ((0, v_pp[prv]), (1, v_pp[cur])):
                        r = T.matmul(bOS[:, h * 64:(h + 1) * 64], lhsT=pT_b[:, h * 2 + part, :], rhs=buf[:, kvh * 64:(kvh + 1) * 64],
                                     start=(part == 0), stop=(part == 1))
                return r
            P.op("pe", osw, R=["pT_b0", "pT_b1", "v_pp0", "v_pp1"], W=[kOS])
            P.op("dve", lambda: V.tensor_tensor(out=mix_tok[:, 768:1024].rearrange("p (h d) -> p h d", h=4), in0=_bc(ssw[:, 20:24].unsqueeze(2), [128, 4, 64]),
                                                in1=bOS[:, 0:256].rearrange("p (h d) -> p h d", h=4), op=ALU.mult), R=[kOS, "ssw3"], W=["mix_swa"])

            bMT, kMT = pb()
            bMTb = bMT[:].bitcast(BF16)

            def mtr():
                r = None
                for kc in range(8):
                    r = T.transpose(bMTb[:, kc * 128:(kc + 1) * 128], mix_tok[:, kc * 128:(kc + 1) * 128], ident_b[:])
                return r
            P.op("pe", mtr, R=["mix_ret", "mix_ssd", "mix_swa", "ident_b"], W=[kMT])
            P.op("act", lambda: S.copy(out=mixT, in_=bMTb[:, 0:1024].rearrange("p (k t) -> p k t", k=8)), R=[kMT], W=["mixT"])
            for half in range(2):
                bW, kW = pb()

                def wo(half=half, bW=bW):
                    r = None
                    for q in range(4):
                        fc = half * 4 + q
                        for kc in range(8):
                            r = T.matmul(bW[:, q * 128:(q + 1) * 128], lhsT=w_out[:, kc, fc * 128:(fc + 1) * 128], rhs=mixT[:, kc, :],
                                         start=(kc == 0), stop=(kc == 7))
                    return r
                P.op("pe", wo, R=["w_out", "mixT"], W=[kW])

                def res(half=half, bW=bW):
                    r = None
                    for q in range(4):
                        fc = half * 4 + q
                        r = V.scalar_tensor_tensor(out=xT[:, fc, Tn], in0=bW[:, q * 128:(q + 1) * 128], scalar=g1a[:, fc:fc + 1], in1=xT[:, fc, Tn],
                                                   op0=ALU.mult, op1=ALU.add)
                    return r
                P.op("dve", res, R=[kW, "der", ("xT", n, half)], W=[("xT", n, half)])
            ln_inplace(128, xk, xT[:, :, Tn], GA1, BA1, 128)

        if FSTOP[0] < L * 10 + 1:
            return
        full = False
        for n in range(-1, NCH):
            chunk(n)
        P.barrier()
        if FSTOP[0] < L * 10 + 2:
            return
        ex = Arena(arena_t, TAIL)
        ex.off = _off_tmp
        sto = ex.alloc([128, STW])
        g8 = ex.alloc([128, NR, STW])
        stin = ex.alloc([128, 3, STW])
        wss = ex.alloc([128, 3, 4])

        def pk():
            V.tensor_copy(out=sto[:, 0:128], in_=Sret.rearrange("p t e -> p (t e)"))
            V.tensor_copy(out=sto[:, 128:384], in_=Sssd)
            return V.tensor_copy(out=sto[:, 384:392], in_=totacc)
        P.op("dve", pk, R=["Sret", "Sssd", "totacc"], W=["sto"])
        P.dma(sp, bounce_s[L], sto, R=["sto"], W=["bounce_s%d" % L])
        P.coll("AllGather", gath_s[L], bounce_s[L], ALLC, R=["bounce_s%d" % L], W=["gath_s%d" % L])
        P.dma(sp, g8, gath_s[L].rearrange("(r p) w -> p r w", p=128), R=["gath_s%d" % L], W=["g8"])

        def ssel():
            r = None
            for s_ in range(3):
                V.tensor_scalar(out=stin[:, s_, :], in0=g8[:, 0, :], scalar1=selw[:, s_ * 8:s_ * 8 + 1], scalar2=None, op0=ALU.mult)
                for r_ in range(1, NR):
                    r = V.scalar_tensor_tensor(out=stin[:, s_, :], in0=g8[:, r_, :], scalar=selw[:, s_ * 8 + r_:s_ * 8 + r_ + 1], in1=stin[:, s_, :],
                                               op0=ALU.mult, op1=ALU.add)
            return r
        P.op("dve", ssel, R=["g8", "selw"], W=["stin"])


        def comb():
            V.tensor_tensor(out=Sret, in0=stin[:, 0, 0:128].rearrange("p (t e) -> p t e", t=2),
                            in1=_bc(wret[:, 0:2].unsqueeze(2), [128, 2, 64]), op=ALU.mult)
            for s_ in (1, 2):
                V.tensor_tensor(out=Sssd[:, 0:128].rearrange("p (t e) -> p t e", t=2), in0=stin[:, s_, 0:128].rearrange("p (t e) -> p t e", t=2),
                                in1=_bc(wret[:, 2 * s_:2 * s_ + 2].unsqueeze(2), [128, 2, 64]), op=ALU.mult)
                V.tensor_tensor(out=Sret, in0=Sret, in1=Sssd[:, 0:128].rearrange("p (t e) -> p t e", t=2), op=ALU.add)
            for g in range(2):
                pr = slice(g * 64, (g + 1) * 64)
                V.tensor_copy(out=wss[pr, 1, :], in_=stin[pr, 0, 384 + g * 4:384 + g * 4 + 4])
                V.tensor_tensor(out=wss[pr, 2, :], in0=stin[pr, 0, 384 + g * 4:384 + g * 4 + 4],
                                in1=stin[pr, 1, 384 + g * 4:384 + g * 4 + 4], op=ALU.add)
            return V.memset(wss[:, 0, :], 0.0)
        P.op("dve", comb, R=["stin", "cst"], W=["Sret", "Sssd", "wss"])
        P.op("act", lambda: S.activation(out=wss, in_=wss, func=AF.Exp), R=["wss"], W=["wss"])

        def comb2():
            V.tensor_tensor(out=Sssd.rearrange("p (r e) -> p r e", r=4), in0=stin[:, 0, 128:384].rearrange("p (r e) -> p r e", r=4),
                            in1=_bc(wss[:, 0, :].unsqueeze(2), [128, 4, 64]), op=ALU.mult)
            for s_ in (1, 2):
                V.tensor_tensor(out=stin[:, s_, 128:384].rearrange("p (r e) -> p r e", r=4), in0=stin[:, s_, 128:384].rearrange("p (r e) -> p r e", r=4),
                                in1=_bc(wss[:, s_, :].unsqueeze(2), [128, 4, 64]), op=ALU.mult)
                V.tensor_tensor(out=Sssd, in0=Sssd, in1=stin[:, s_, 128:384], op=ALU.add)
            V.tensor_copy(out=Sssd_b, in_=Sssd)
            return V.tensor_copy(out=Sret_b, in_=Sret)
        P.op("dve", comb2, R=["stin", "wss", "Sret", "Sssd"], W=["Sret", "Sssd", "stin"])
        P.barrier()
        if FSTOP[0] < L * 10 + 3:
            return
        P.op("dve", zmask, W=["qm", "BCm", "qT_s"])
        full = True
        SUB[0] = MSUB[0]
        for n in range(-1, min(NCH, MAINCH[0])):
            chunk(n)
        SUB[0] = 99
        if FSTOP[0] < L * 10 + 4:
            return

        P.barrier()
        ar = Arena(arena_t, LIM)
        hT = ar.alloc([128, 8, NTOK], BF16)
        aT = [ar.alloc([128, 4, 1024], BF16) for _ in range(2)]
        wgu = [ar.alloc([128, 8, 1024], BF16) for _ in range(2)]
        wdb = [ar.alloc([128, 4, D], BF16) for _ in range(2)]
        sgt = [ar.alloc([128, 512]) for _ in range(2)]
        evt = [ar.alloc([128, 512]) for _ in range(2)]
        if moe:
            gbc = [ar.alloc([128, 1024]) for _ in range(2)]
            rw_sb = ar.alloc([128, 8, 8])
            lgT = ar.alloc([128, 512])
            gatesT = ar.alloc([128, NTOK])
            lg = ar.alloc([128, NCH, 8])
            gts = ar.alloc([128, NCH, 8])
            e1 = ar.alloc([128, NCH, 8])
            e2 = ar.alloc([128, NCH, 8])
            l2 = ar.alloc([128, NCH, 8])
            tk = ar.alloc([128, 6, NCH])
            h2f = [ar.alloc([128, 512]) for _ in range(2)]
            P.dma(sp, rw_sb, rw_d, W=["rw_sb"])

        if moe:
            bG, kG = pb()
        for tg in range(4):
            Tg = slice(tg * 512, (tg + 1) * 512)
            xkeys = [("xT", n, hf_) for n in range(tg * 4, tg * 4 + 4) for hf_ in range(2)]
            if moe:
                bR, kR = pb()
            for fc in range(8):
                P.op("act", lambda fc=fc, Tg=Tg: S.activation(out=hT[:, fc, Tg], in_=xT[:, fc, Tg], func=AF.Identity, bias=B2[:, fc:fc + 1], scale=A2[:, fc:fc + 1]),
                     R=xkeys + ["der"], W=[("hT", tg)])
                if moe:
                    hb = h2f[fc % 2]
                    hbk = "h2f%d" % (fc % 2)
                    P.op("dve", lambda fc=fc, Tg=Tg, hb=hb: V.tensor_scalar(out=hb, in0=xT[:, fc, Tg], scalar1=A2[:, fc:fc + 1], scalar2=B2[:, fc:fc + 1], op0=ALU.mult, op1=ALU.add),
                         R=xkeys + ["der"], W=[hbk])
                    P.op("pe", lambda fc=fc, hb=hb, bR=bR: T.matmul(bR[0:8, 0:512], lhsT=rw_sb[:, fc, :], rhs=hb, start=(fc == 0), stop=(fc == 7)),
                         R=[hbk, "rw_sb"], W=[kR])
            if moe:
                P.op("act", lambda bR=bR: S.activation(out=lgT[0:8, 0:512], in_=bR[0:8, 0:512], func=AF.Identity, bias=rbias[0:8, 0:1], scale=1.0),
                     R=[kR, "misc"], W=["lgT"])

                def ltr(tg=tg):
                    r = None
                    for q in range(4):
                        n = tg * 4 + q
                        r = T.transpose(bG[:, n * 8:(n + 1) * 8], lgT[0:8, q * 128:(q + 1) * 128], ident[0:8, 0:8])
                    return r
                P.op("pe", ltr, R=["lgT", "cst"], W=[kG])
        if moe:

            def top2():
                V.tensor_copy(out=lg, in_=bG[:, 0:128].rearrange("p (n e) -> p n e", e=8))
                m1_, m2_, dlt, w1_, w2_ = tk[:, 0, :], tk[:, 1, :], tk[:, 2, :], tk[:, 3, :], tk[:, 4, :]
                V.tensor_reduce(out=m1_, in_=lg, axis=AX.X, op=ALU.max)
                V.tensor_tensor(out=e1, in0=lg, in1=_bc(m1_.unsqueeze(2), [128, NCH, 8]), op=ALU.is_equal)
                V.scalar_tensor_tensor(out=l2, in0=e1, scalar=-1e30, in1=lg, op0=ALU.mult, op1=ALU.add)
                V.tensor_reduce(out=m2_, in_=l2, axis=AX.X, op=ALU.max)
                V.tensor_tensor(out=e2, in0=l2, in1=_bc(m2_.unsqueeze(2), [128, NCH, 8]), op=ALU.is_equal)
                return V.tensor_tensor(out=dlt, in0=m2_, in1=m1_, op=ALU.subtract)
            P.op("dve", top2, R=[kG], W=["tk", "lg"])
            P.op("act", lambda: S.activation(out=tk[:, 2, :], in_=tk[:, 2, :], func=AF.Exp), R=["tk"], W=["tk"])

            def top2b():
                dlt, w1_, w2_ = tk[:, 2, :], tk[:, 3, :], tk[:, 4, :]
                V.tensor_scalar(out=w1_, in0=dlt, scalar1=1.0, scalar2=None, op0=ALU.add)
                V.reciprocal(out=w1_, in_=w1_)
                V.tensor_tensor(out=w2_, in0=dlt, in1=w1_, op=ALU.mult)
                V.tensor_tensor(out=e1, in0=e1, in1=_bc(w1_.unsqueeze(2), [128, NCH, 8]), op=ALU.mult)
                V.tensor_tensor(out=e2, in0=e2, in1=_bc(w2_.unsqueeze(2), [128, NCH, 8]), op=ALU.mult)
                return V.tensor_tensor(out=gts, in0=e1, in1=e2, op=ALU.add)
            P.op("dve", top2b, R=["tk", "lg"], W=["gts", "tk", "lg"])
            for tg in range(4):
                bG2, kG2 = pb()

                def gtr(tg=tg, bG2=bG2):
                    r = None
                    for q in range(4):
                        n = tg * 4 + q
                        r = T.transpose(bG2[0:8, q * 128:(q + 1) * 128], gts[:, n, :], ident)
                    return r
                P.op("pe", gtr, R=["gts", "cst"], W=[kG2])
                P.op("act", lambda tg=tg, bG2=bG2: S.copy(out=gatesT[0:8, tg * 512:(tg + 1) * 512], in_=bG2[0:8, 0:512]), R=[kG2], W=["gatesT"])

        nexp = NEXP if moe else 1
        dff = D_FFE if moe else D_FF
        pieces = []
        o = 0
        while o < dff:
            w = min(512, dff - o)
            pieces.append((o, w))
            o += w
        pi = 0
        for e in range(min(nexp, NEXPRUN[0])):
            if moe:
                for half in range(2):
                    for q in range(2):
                        bg_, kg_ = pb()
                        P.op("pe", lambda e=e, half=half, q=q, bg_=bg_: T.matmul(bg_[:, 0:512], lhsT=sel8[0:8, e * 128:(e + 1) * 128],
                                                                                  rhs=gatesT[0:8, half * 1024 + q * 512:half * 1024 + (q + 1) * 512], start=True, stop=True),
                             R=["gatesT", "cst"], W=[kg_])
                        P.op("act", lambda half=half, q=q, bg_=bg_: S.copy(out=gbc[half][:, q * 512:(q + 1) * 512], in_=bg_[:, 0:512]), R=[kg_], W=["gbc%d" % half])
            for (o, w) in pieces:
                nb = w // 128
                wb = pi % 2
                kwg, kwd = "wgu%d" % wb, "wd%d" % wb
                P.dma("pool", wgu[wb][:, :, 0:w], wg_d[e].rearrange("(c p) n -> p c n", p=128)[:, :, o:o + w], W=[kwg])
                P.dma("pool", wgu[wb][:, :, 512:512 + w], wu_d[e].rearrange("(c p) n -> p c n", p=128)[:, :, o:o + w], W=[kwg])
                P.dma("pool", wdb[wb][:, 0:nb, :], wd_d[e][o:o + w, :].rearrange("(c p) n -> p c n", p=128), W=[kwd])
                for half in range(2):
                    for blk in range(nb):
                        for q in range(2):
                            tg = half * 2 + q
                            Tg = slice(tg * 512, (tg + 1) * 512)
                            bg_, kg_ = pb()
                            bu_, ku_ = pb()

                            def gu(blk=blk, Tg=Tg, bg_=bg_, bu_=bu_, wb=wb):
                                r = None
                                for kc in range(8):
                                    T.matmul(bg_[:, 0:512], lhsT=wgu[wb][:, kc, blk * 128:(blk + 1) * 128], rhs=hT[:, kc, Tg], start=(kc == 0), stop=(kc == 7))
                                for kc in range(8):
                                    r = T.matmul(bu_[:, 0:512], lhsT=wgu[wb][:, kc, 512 + blk * 128:512 + (blk + 1) * 128], rhs=hT[:, kc, Tg], start=(kc == 0), stop=(kc == 7))
                                return r
                            P.op("pe", gu, R=[kwg, ("hT", tg)], W=[kg_, ku_])
                            sb_ = sgt[(blk * 2 + q) % 2]
                            sk_ = "sgt%d" % ((blk * 2 + q) % 2)
                            P.op("act", lambda bg_=bg_, sb_=sb_: S.activation(out=sb_, in_=bg_[:, 0:512], func=AF.Silu), R=[kg_], W=[sk_])
                            P.op("dve", lambda half=half, blk=blk, q=q, bu_=bu_, sb_=sb_: V.tensor_tensor(out=aT[half][:, blk, q * 512:(q + 1) * 512], in0=bu_[:, 0:512], in1=sb_, op=ALU.mult),
                                 R=[ku_, sk_], W=[("aT", half, blk, q)])
                for half in range(2):
                    for fc in range(8):
                        for q in range(2):
                            tg = half * 2 + q
                            Tg = slice(tg * 512, (tg + 1) * 512)
                            bo_, ko_ = pb()

                            def dn(half=half, fc=fc, q=q, bo_=bo_, wb=wb, nb=nb):
                                r = None
                                for blk in range(nb):
                                    r = T.matmul(bo_[:, 0:512], lhsT=wdb[wb][:, blk, fc * 128:(fc + 1) * 128], rhs=aT[half][:, blk, q * 512:(q + 1) * 512],
                                                 start=(blk == 0), stop=(blk == nb - 1))
                                return r
                            P.op("pe", dn, R=[kwd] + [("aT", half, blk, q) for blk in range(nb)], W=[ko_])
                            eb = evt[(fc * 2 + q) % 2]
                            ek = "evt%d" % ((fc * 2 + q) % 2)
                            if moe:
                                P.op("dve", lambda fc=fc, half=half, q=q, bo_=bo_, eb=eb: V.scalar_tensor_tensor(out=eb, in0=bo_[:, 0:512], scalar=g1f[:, fc:fc + 1],
                                                                                                              in1=gbc[half][:, q * 512:(q + 1) * 512], op0=ALU.mult, op1=ALU.mult),
                                     R=[ko_, "der", "gbc%d" % half], W=[ek])
                            else:
                                P.op("dve", lambda fc=fc, bo_=bo_, eb=eb: V.tensor_scalar(out=eb, in0=bo_[:, 0:512], scalar1=g1f[:, fc:fc + 1], scalar2=None, op0=ALU.mult),
                                     R=[ko_, "der"], W=[ek])
                            xkeys = [("xT", n, fc // 4) for n in range(tg * 4, tg * 4 + 4)]
                            P.op("pool", lambda fc=fc, Tg=Tg, eb=eb: G.tensor_tensor(out=xT[:, fc, Tg], in0=xT[:, fc, Tg], in1=eb, op=ALU.add),
                                 R=[ek] + xkeys, W=xkeys)
                pi += 1

        P.barrier()
        ar = Arena(arena_t, LIM)
        sq = [ar.alloc([128, 512]) for _ in range(2)]
        lnst = ar.alloc([128, 4, 512])
        lnk = ["lnst"]
        for tg in range(4):
            Tg = slice(tg * 512, (tg + 1) * 512)
            xkeys = [("xT", n, hf_) for n in range(tg * 4, tg * 4 + 4) for hf_ in range(2)]
            ln_inplace(512, xkeys, xT[:, :, Tg], GA2, BA2, 512)

    for L_ in range(DEPTH):
        layer(L_)
    ar = Arena(arena_t, TAIL)
    ar.off = 3072
    xo = [ar.alloc([128, D]) for _ in range(2)]
    for n in range(NCH):
        buf = xo[n % 2]
        bk = "xo%d" % (n % 2)
        for half in range(2):
            bank, bkey = pb()

            def tr2(n=n, half=half, bank=bank):
                r = None
                for q in range(4):
                    fc = half * 4 + q
                    r = T.transpose(bank[:, q * 128:(q + 1) * 128], xT[:, fc, n * 128:(n + 1) * 128], ident)
                return r
            P.op("pe", tr2, R=[("xT", n, 0), ("xT", n, 1), "cst"], W=[bkey])
            P.op("act", lambda half=half, bank=bank, buf=buf: S.copy(out=buf[:, half * 512:(half + 1) * 512], in_=bank[:, 0:512]), R=[bkey], W=[bk + "_%d" % half])
        P.dma(sp, xo_d[n * 128:(n + 1) * 128, :], buf, R=[bk + "_0", bk + "_1"], is_output=True)
    return P.emit()

C_ID, C_TRI, C_MGT, C_ONE = 0, 128, 256, 384
C_DQ, C_DK, C_GC, C_INV = 512, 516, 520, 522
C_MADD = 554
C_WRET = 810
C_M0 = 816
C_SEL = 818
CW = C_SEL + 8 * 128

M_AIN, M_BIN, M_G1A, M_G1F = 0, 8, 16, 24
M_CW, M_CB, M_HV, M_HM = 32, 56, 62, 63
M_MOD, M_LN, M_C, M_BADA, M_DER, M_RB = 64, 160, 192, 200, 296, 360
MW = 368

R_DTB, R_ALOG, R_DSK, R_NW, R_SINK, R_RB = 0, 8, 16, 24, 536, 540
RW = 668


def _t5_bucket(dist):
    exact = 16
    df = np.maximum(dist, 1).astype(np.float32)
    large = exact + (np.log(df / exact) / math.log(128 / exact) * (32 - exact)).astype(np.int32)
    large = np.minimum(large, 31)
    return np.where(dist < exact, dist, large)


def make_consts():
    c = np.zeros((128, CW), np.float32)
    i = np.arange(128)
    c[:, C_ID:C_ID + 128] = np.eye(128, dtype=np.float32)
    c[:, C_TRI:C_TRI + 128] = (i[:, None] <= i[None, :]).astype(np.float32)
    c[:, C_MGT:C_MGT + 128] = (i[:, None] > i[None, :]).astype(np.float32)
    c[:, C_ONE:C_ONE + 128] = 1.0
    lg = np.log(1.0 - 2.0 ** (-5.0 - np.arange(4, dtype=np.float64)))
    c[:, C_DQ:C_DQ + 4] = np.exp(lg[None, :] * (i[:, None] + 1.0))
    c[:, C_DK:C_DK + 4] = np.exp(-lg[None, :] * (i[:, None] + 1.0)) * (64 ** -0.5)
    for t in range(2):
        for hf in range(2):
            c[hf * 64:(hf + 1) * 64, C_GC + t] = np.exp(lg[2 * t + hf] * 128.0)
    c[:, C_INV:C_INV + 32] = np.exp(-math.log(10000.0) * np.arange(32, dtype=np.float32) / 32)[None, :]
    jj = np.arange(256)
    dist = i[:, None] + 128 - jj[None, :]
    valid = (dist >= 0) & (dist < 128)
    c[:, C_MADD:C_MADD + 256] = np.where(valid, 0.0, NEG)
    for s in range(3):
        for t in range(2):
            for hf in range(2):
                c[hf * 64:(hf + 1) * 64, C_WRET + s * 2 + t] = np.exp(lg[2 * t + hf] * 2048.0 * s)
    c[0:64, C_M0] = 1.0
    c[64:128, C_M0 + 1] = 1.0
    for e in range(8):
        c[e, C_SEL + e * 128:C_SEL + (e + 1) * 128] = 1.0
    bk = _t5_bucket(np.clip(dist, 0, 127))
    eoh = np.zeros((128, 256, 32), np.float32)
    ii, jj2 = np.meshgrid(i, jj, indexing="ij")
    eoh[ii, jj2, bk] = 1.0
    return c, eoh.reshape(128, 256 * 32)


def col(v):
    v = np.asarray(v, np.float32)
    return np.ascontiguousarray(v.reshape(-1, 128).T)


_PROG_CACHE = {}


def kernel(x, c, positions, rel_bias, w_ada, b_ada, w_in, w_out, conv_w, conv_b,
           dt_bias, a_log, d_skip, ssd_norm_w, sinks, ln_g, ln_b,
           ffn_w_gate, ffn_w_up, ffn_w_down, router_w, router_b,
           expert_w_gate, expert_w_up, expert_w_down):
    f = lambda a: np.ascontiguousarray(np.asarray(a, dtype=np.float32))
    x = f(x)
    cst, eoh = make_consts()
    positions = np.asarray(positions)
    shared = {
        "cst": cst, "eoh": eoh, "relb": f(rel_bias).reshape(-1),
        "w_in": f(w_in), "w_out": f(w_out), "w_ada": f(w_ada),
        "wg0": f(ffn_w_gate), "wu0": f(ffn_w_up), "wd0": f(ffn_w_down),
        "wg1": f(expert_w_gate[0]), "wu1": f(expert_w_up[0]), "wd1": f(expert_w_down[0]),
        "rw": np.ascontiguousarray(f(router_w[0]).reshape(8, 128, 8).transpose(1, 0, 2)),
    }
    rowp = np.zeros((DEPTH, RW), np.float32)
    for L in range(DEPTH):
        rowp[L, R_DTB:R_DTB + 8] = f(dt_bias[L])
        rowp[L, R_ALOG:R_ALOG + 8] = f(a_log[L])
        rowp[L, R_DSK:R_DSK + 8] = f(d_skip[L])
        rowp[L, R_NW:R_NW + 512] = f(ssd_norm_w[L])
        rowp[L, R_SINK:R_SINK + 4] = f(sinks[L])
    maps = []
    for core in range(8):
        b, sq_ = core // 4, core % 4
        t0 = sq_ * NTOK
        xin = np.zeros((NTOK + 128, D), np.float32)
        xin[128:] = x[b, t0:t0 + NTOK]
        if sq_ > 0:
            xin[:128] = x[b, t0 - 128:t0]
        misc = np.zeros((DEPTH, 128, MW), np.float32)
        for L in range(DEPTH):
            m = misc[L]
            m[:, M_CW:M_CW + 24] = np.ascontiguousarray(f(conv_w[L]).reshape(4, 6, 128).transpose(2, 1, 0)).reshape(128, 24)
            m[:, M_CB:M_CB + 6] = col(conv_b[L])
            m[:, M_HV] = 1.0 if sq_ > 0 else 0.0
            m[:, M_HM] = 0.0 if sq_ > 0 else NEG
            m[:, M_LN:M_LN + 8] = col(ln_g[L, 0])
            m[:, M_LN + 8:M_LN + 16] = col(ln_b[L, 0])
            m[:, M_LN + 16:M_LN + 24] = col(ln_g[L, 1])
            m[:, M_LN + 24:M_LN + 32] = col(ln_b[L, 1])
            m[:, M_C:M_C + 8] = col(c[b])
            m[:, M_BADA:M_BADA + 48] = col(b_ada[L])
            if L % 2 == 1:
                m[0:8, M_RB] = f(router_b[L // 2])
        selw = np.zeros((128, 32), np.float32)
        for s in range(3):
            if sq_ - 1 - s >= 0:
                selw[:, s * 8 + (sq_ - 1 - s)] = 1.0
        pos = np.ascontiguousarray(positions[b, t0:t0 + NTOK].astype(np.int32).reshape(NCH, 128).T)
        mm = dict(shared)
        mm.update({"xin": xin, "pos": pos, "misc": misc, "rowp": rowp, "selw": selw})
        maps.append(mm)
    FSTOP[0] = 99 if AONLY[0] else 13.5
    if "f" not in _PROG_CACHE:
        _PROG_CACHE["f"] = build_fused()
    mapsA = [{k_: v_ for k_, v_ in m_.items() if k_ in DECL_IN} for m_ in maps]
    res = run_bass_kernel_spmd(_PROG_CACHE["f"], mapsA, core_ids=list(range(8)))
    if AONLY[0]:
        out = np.zeros_like(x)
        for core in range(8):
            b, sq_ = core // 4, core % 4
            out[b, sq_ * NTOK:(sq_ + 1) * NTOK] = np.asarray(res.results[core]["xout"], np.float32)
        return out
    L = DEPTH - 1
    i = L // 2
    wada_l = f(w_ada[L:L + 1])
    w_in_l, w_out_l = f(w_in[L]), f(w_out[L])
    wg_l, wu_l, wd_l = f(expert_w_gate[i]), f(expert_w_up[i]), f(expert_w_down[i])
    rw_l = np.ascontiguousarray(f(router_w[i]).reshape(8, 128, 8).transpose(1, 0, 2))
    rowp1 = np.zeros((RW,), np.float32)
    rowp1[:] = rowp[L]
    rowp1[R_RB:R_RB + 128] = f(rel_bias).reshape(-1)
    st_in = np.zeros((3, 128, STW), np.float32)
    maps2 = []
    for core in range(8):
        xin = np.zeros((NTOK + 128, D), np.float32)
        xin[128:] = np.asarray(res.results[core]["xout"], np.float32)
        mA = maps[core]
        maps2.append({"xin": xin, "pos": mA["pos"], "cst": cst, "misc": np.ascontiguousarray(mA["misc"][L]), "rowp": rowp1,
                      "w_in": w_in_l, "w_ada": wada_l, "st_in": st_in, "eoh": eoh, "w_out": w_out_l,
                      "wg": wg_l, "wu": wu_l, "wd": wd_l, "rw": rw_l})
    if "b" not in _PROG_CACHE:
        _PROG_CACHE["b"] = build("ffn", L)
    res2 = run_bass_kernel_spmd(_PROG_CACHE["b"], maps2, core_ids=list(range(8)))
    out = np.zeros_like(x)
    for core in range(8):
        b, sq_ = core // 4, core % 4
        out[b, sq_ * NTOK:(sq_ + 1) * NTOK] = np.asarray(res2.results[core]["xout"], np.float32)
    return out
```

```python
import math
import contextlib
import numpy as np
import concourse.bass as bass
import concourse.mybir as mybir
from concourse.bass_utils import run_bass_kernel_spmd

F32 = mybir.dt.float32
BF16 = mybir.dt.bfloat16
I32 = mybir.dt.int32
ALU = mybir.AluOpType
AF = mybir.ActivationFunctionType
AX = mybir.AxisListType

D = 1024
DEPTH = 2
NTOK = 2048
NCH = 16
IN_DIM = 2824
D_FF = 2816
NEXP = 8
D_FFE = 3584
ALPHA = (2 * DEPTH) ** 0.25
EPS = 1e-5
NEG = -30000.0
STW = 392

ENGS = ("pe", "act", "dve", "pool", "sp")
ND = 12
SAME_ENGINE_SYNC = True


SEM_CAP = 3000
CHAIN_EPOCHS = 3


class Prog:
    def __init__(self):
        self.nc = bass.Bass("TRN2", target_bir_lowering=False)
        self.ops = {e: [] for e in ENGS}
        self.lastw = {}
        self.readers = {}
        self.seen = {e: {} for e in ENGS}
        self.dma_cnt = [0] * (ND + 1)
        self.dma_last_tok = [None] * (ND + 1)
        self.dma_next = 0
        self.out_tokens = []
        self.pending = {e: [] for e in ENGS}
        self.st = contextlib.ExitStack()
        self.chain = {e: {"last": None, "count": 0, "sems": None, "epoch": 0} for e in ("act", "dve", "pool")}

    def sbuf(self, name, shape, dtype):
        return self.st.enter_context(self.nc.sbuf_tensor(name, list(shape), dtype))

    def psum(self, name, shape, dtype):
        return self.st.enter_context(self.nc.psum_tensor(name, list(shape), dtype))

    def barrier(self):
        toks = []
        for e in ENGS:
            for i in range(len(self.ops[e]) - 1, -1, -1):
                if self.ops[e][i]["dma"] is None:
                    toks.append(("e", e, i))
                    break
        for t in self.dma_last_tok:
            if t is not None:
                toks.append(t)
        for e in ENGS:
            self.pending[e] = list(toks)

    def _need(self, eng, tok, waits):
        if tok is None:
            return
        if tok[0] == "e":
            _, src, seq = tok
            if src == eng and (not SAME_ENGINE_SYNC or eng == "pe"):
                return
            if self.seen[eng].get(src, -1) >= seq:
                return
            cur = waits.get(src)
            if cur is None or cur[2] < seq:
                waits[src] = tok
        else:
            _, idx, cnt = tok
            key = ("d", idx)
            if self.seen[eng].get(key, -1) >= cnt:
                return
            cur = waits.get(key)
            if cur is None or cur[2] < cnt:
                waits[key] = tok

    def _deps(self, eng, reads, writes):
        waits = {}
        for k in reads:
            self._need(eng, self.lastw.get(k), waits)
        for k in writes:
            self._need(eng, self.lastw.get(k), waits)
            for tok in self.readers.get(k, {}).values():
                self._need(eng, tok, waits)
        if self.pending[eng]:
            for tok in self.pending[eng]:
                self._need(eng, tok, waits)
            self.pending[eng] = []
        for key, tok in waits.items():
            self.seen[eng][key] = tok[2]
            if tok[0] == "e":
                self.ops[tok[1]][tok[2]]["sig"] = True
        return list(waits.values())

    def _commit(self, tok, reads, writes, rkey):
        for k in writes:
            self.lastw[k] = tok
            self.readers[k] = {}
        for k in reads:
            self.readers.setdefault(k, {})[rkey] = tok

    def op(self, eng, emit, R=(), W=()):
        waits = self._deps(eng, R, W)
        seq = len(self.ops[eng])
        self.ops[eng].append({"waits": waits, "emit": emit, "sig": False, "dma": None})
        self._commit(("e", eng, seq), R, W, eng)

    def dma(self, eng, out, in_, R=(), W=(), is_output=False, **kw):
        if "_coll" in kw:
            idx = ND
        else:
            idx = self.dma_next
            self.dma_next = (self.dma_next + 1) % ND
        waits = self._deps(eng, R, W)
        prev = self.dma_last_tok[idx]
        if prev is not None:
            w = {}
            self._need(eng, prev, w)
            for key, tok in w.items():
                self.seen[eng][key] = tok[2]
                waits.append(tok)
        self.dma_cnt[idx] += (1 if idx == ND else 16)
        tok = ("d", idx, self.dma_cnt[idx])
        self.dma_last_tok[idx] = tok
        self.ops[eng].append({"waits": waits, "emit": None, "sig": False,
                              "dma": (out, in_, idx, kw)})
        self._commit(tok, R, W, ("d", idx))
        if is_output:
            self.out_tokens.append(tok)
        return tok

    def coll(self, kind, out, in_, groups, R=(), W=()):
        import os
        if os.environ.get("NOCOLL"):
            return self.dma("sp", out[0:128, :], in_, R=R, W=W)
        return self.dma("pool", out, in_, R=R, W=W, _coll=(kind, groups))

    def emit(self):
        nc = self.nc
        fin = {}
        for tok in self.out_tokens:
            self._need("sp", tok, fin)
        fin_waits = list(fin.values())
        pref = {}
        for e in ENGS:
            c = 0
            arr = []
            for o in self.ops[e]:
                if o["sig"]:
                    c += 1
                arr.append(c)
            pref[e] = arr
        with self.st as st:
            esem = {e: [st.enter_context(nc.semaphore("s_%s%d" % (e, k_))) for k_ in range(max(1, -(-(pref[e][-1] if pref[e] else 0) // SEM_CAP)))]
                    for e in ENGS}
            dsem = [st.enter_context(nc.semaphore("d%d" % i)) for i in range(ND + 1)]
            for e in self.chain:
                self.chain[e]["sems"] = [st.enter_context(nc.semaphore("c_%s%d" % (e, k_))) for k_ in range(CHAIN_EPOCHS)]
            block = st.enter_context(nc.Block())

            def do_wait(E, tok):
                if tok[0] == "e":
                    c_ = pref[tok[1]][tok[2]]
                    E.wait_ge(esem[tok[1]][(c_ - 1) // SEM_CAP], (c_ - 1) % SEM_CAP + 1)
                else:
                    E.wait_ge(dsem[tok[1]], tok[2])

            def run(e, E):
                for oi, o in enumerate(self.ops[e]):
                    for tok in o["waits"]:
                        do_wait(E, tok)
                    if o["dma"] is not None:
                        out, in_, idx, kw = o["dma"]
                        if "_coll" in kw:
                            kind, groups = kw["_coll"]
                            nc.gpsimd.collective_compute(kind, ALU.bypass, replica_groups=groups,
                                                         ins=[in_], outs=[out]).then_inc(dsem[idx], 1)
                        else:
                            E.dma_start(out=out, in_=in_, **kw).then_inc(dsem[idx], 16)
                    else:
                        if e in self.chain:
                            self.chain[e]["last"] = None
                        ins = o["emit"]()
                        if o["sig"]:
                            c_ = pref[e][oi]
                            ins.then_inc(esem[e][(c_ - 1) // SEM_CAP], 1)
                if e == "sp":
                    for tok in fin_waits:
                        do_wait(E, tok)

            @block.tensor
            def _(E):
                run("pe", E)

            @block.scalar
            def _(E):
                run("act", E)

            @block.vector
            def _(E):
                run("dve", E)

            @block.gpsimd
            def _(E):
                run("pool", E)

            @block.sync
            def _(E):
                run("sp", E)
        return nc


class EngProxy:
    def __init__(self, prog, name, eng):
        self._p, self._n, self._e = prog, name, eng

    def __getattr__(self, attr):
        fn = getattr(self._e, attr)
        if attr in ("wait_ge", "dma_start"):
            return fn
        st = self._p.chain[self._n]

        def w(*a, **k):
            if st["last"] is not None:
                if st["count"] >= SEM_CAP:
                    st["epoch"] += 1
                    st["count"] = 0
                sem_ = st["sems"][st["epoch"]]
                st["last"].then_inc(sem_, 1)
                st["count"] += 1
                self._e.wait_ge(sem_, st["count"])
            ins = fn(*a, **k)
            st["last"] = ins
            return ins
        return w


class Arena:
    def __init__(self, t, nwords):
        self.t = t
        self.n = nwords
        self.off = 0

    def alloc(self, shape, dtype=F32):
        n = 1
        for s in shape[1:]:
            n *= s
        words = n if dtype in (F32, I32) else (n + 1) // 2
        assert self.off + words <= self.n, ("arena overflow", self.off, words, self.n)
        v = self.t[:, self.off:self.off + words]
        self.off += words
        if dtype == BF16:
            v = v.bitcast(BF16)[:, 0:n]
        elif dtype == I32:
            v = v.bitcast(I32)
        if len(shape) > 2:
            names = ["a%d" % i for i in range(len(shape) - 1)]
            pat = "p (" + " ".join(names) + ") -> p " + " ".join(names)
            v = v.rearrange(pat, **{nm: s for nm, s in zip(names, shape[1:])})
        return v


def _bc(ap, shape):
    return ap.to_broadcast(list(shape))


STOP = [99]
DBG = set()
DBG_OUT = {}
_P1ONLY = [False]
SUB = [99]


def build(stage, L):
    P = Prog()
    nc = P.nc
    V, S, G, T = EngProxy(P, "dve", nc.vector), EngProxy(P, "act", nc.scalar), EngProxy(P, "pool", nc.gpsimd), nc.tensor
    full = stage in ("main", "ffn")
    ffn_only = stage == "ffn"
    moe = (L % 2 == 1)
    last = (L == DEPTH - 1)

    def din(name, shape, dt=F32):
        return nc.dram_tensor(name, list(shape), dt, kind="ExternalInput").ap()

    def dout(name, shape, dt=F32):
        return nc.dram_tensor(name, list(shape), dt, kind="ExternalOutput").ap()

    def dbg(name, ap, keys):
        if name not in DBG:
            return
        shp = list(ap.shape)
        d_ = dout("dbg_" + name, shp, ap.dtype)
        P.dma("sp", d_, ap, R=list(keys), is_output=True)

    x_d = din("xin", [NTOK + 128, D])
    pos_d = din("pos", [128, NCH], I32)
    cst_d = din("cst", [128, CW])
    misc_d = din("misc", [128, MW])
    rowp_d = din("rowp", [RW])
    w_in_d = din("w_in", [D, IN_DIM])
    if full:
        w_out_d = din("w_out", [D, D])
        eoh_d = din("eoh", [128, 256 * 32])
        st_in_d = din("st_in", [3, 128, STW])
        if moe:
            wg_d = din("wg", [NEXP, D, D_FFE])
            wu_d = din("wu", [NEXP, D, D_FFE])
            wd_d = din("wd", [NEXP, D_FFE, D])
            rw_d = din("rw", [128, 8, 8])
        else:
            wg_d = din("wg", [1, D, D_FF])
            wu_d = din("wu", [1, D, D_FF])
            wd_d = din("wd", [1, D_FF, D])
        xo_d = dout("xout", [NTOK, D])
    else:
        st_out_d = dout("st_out", [128, STW])

    xT = P.sbuf("xT", [128, 8, NTOK], F32)
    cst = P.sbuf("cst_sb", [128, CW], F32)
    misc = P.sbuf("misc_sb", [128, MW], F32)
    rowp = P.sbuf("rowp_sb", [128, RW], F32)
    ident_b = P.sbuf("ident_b", [128, 128], BF16)
    AW = 33700
    arena_t = P.sbuf("arena", [128, AW], F32)
    TAIL = AW - 2048
    cosT = arena_t[:, TAIL:TAIL + 512].rearrange("p (n f) -> p n f", f=32)
    sinT = arena_t[:, TAIL + 512:TAIL + 1024].rearrange("p (n f) -> p n f", f=32)
    biasw = arena_t[:, TAIL + 1024:TAIL + 2048].rearrange("p (h j) -> p h j", h=4)
    ps = [P.psum("ps%d" % i, [128, 512], F32) for i in range(8)]
    psk = ["ps%d" % i for i in range(8)]
    pctr = [0]

    def pb():
        i = pctr[0] % 8
        pctr[0] += 1
        return ps[i], psk[i]

    ident = cst[:, C_ID:C_ID + 128]
    tri = cst[:, C_TRI:C_TRI + 128]
    mgt = cst[:, C_MGT:C_MGT + 128]
    ones = cst[:, C_ONE:C_ONE + 128]
    dq = cst[:, C_DQ:C_DQ + 4]
    dk = cst[:, C_DK:C_DK + 4]
    gC = cst[:, C_GC:C_GC + 2]
    invf = cst[:, C_INV:C_INV + 32]
    madd = cst[:, C_MADD:C_MADD + 256]
    wret = cst[:, C_WRET:C_WRET + 6]
    m0 = cst[:, C_M0:C_M0 + 1]
    m1 = cst[:, C_M0 + 1:C_M0 + 2]
    sel8 = cst[:, C_SEL:C_SEL + 8 * 128]

    def mcol(o, n):
        return misc[:, o:o + n]
    A_in, B_in = mcol(M_AIN, 8), mcol(M_BIN, 8)
    g1a, g1f = mcol(M_G1A, 8), mcol(M_G1F, 8)
    convw = mcol(M_CW, 24)
    convb = mcol(M_CB, 6)
    halovalid = mcol(M_HV, 1)
    halomask = mcol(M_HM, 1)
    modT = mcol(M_MOD, 96)
    lncol = mcol(M_LN, 32)
    ccol = mcol(M_C, 8)
    badaT = mcol(M_BADA, 96)
    dcol = mcol(M_DER, 64)
    rbias = mcol(M_RB, 8)

    dtb = rowp[:, R_DTB:R_DTB + 8]
    alog = rowp[:, R_ALOG:R_ALOG + 8]
    dskip = rowp[:, R_DSK:R_DSK + 8]
    normw = rowp[:, R_NW:R_NW + 512]
    sinks = rowp[:, R_SINK:R_SINK + 4]
    relb = rowp[:, R_RB:R_RB + 128]

    sp = "sp"
    P.dma(sp, cst[:], cst_d, W=["cst"])
    P.dma(sp, misc[:], misc_d, W=["misc"])
    P.dma(sp, rowp[:], rowp_d.partition_broadcast(128), W=["rowp"])
    P.op("dve", lambda: V.tensor_copy(out=ident_b[:], in_=ident), R=["cst"], W=["ident_b"])

    ar = Arena(arena_t, TAIL)
    wada_d = din("w_ada", [1, D, 6 * D])
    wada_sb = [ar.alloc([128, 8, 512]) for _ in range(2)]
    bank_mod, kmod = pb()
    nl = 1
    j = 0
    for li in range(nl):
        for cg in range(12):
            buf = wada_sb[j % 2]
            bk = "wada%d" % (j % 2)
            P.dma(sp, buf, wada_d[li].rearrange("(c p) n -> p c n", p=128)[:, :, cg * 512:(cg + 1) * 512], W=[bk])
            for cc in range(4):
                col = li * 48 + cg * 4 + cc

                def mm(buf=buf, cc=cc, col=col):
                    r = None
                    for kc in range(8):
                        r = T.matmul(bank_mod[:, col:col + 1], lhsT=buf[:, kc, cc * 128:(cc + 1) * 128],
                                     rhs=ccol[:, kc:kc + 1], start=(kc == 0), stop=(kc == 7))
                    return r
                P.op("pe", mm, R=[bk, "misc"], W=[kmod])
            j += 1
    P.op("dve", lambda: V.tensor_tensor(out=modT[:, 0:48 * nl], in0=bank_mod[:, 0:48 * nl], in1=badaT[:, 0:48 * nl], op=ALU.add),
         R=[kmod, "misc"], W=["mod"])
    lg1, lb1, lg2, lb2 = lncol[:, 0:8], lncol[:, 8:16], lncol[:, 16:24], lncol[:, 24:32]
    GA1, BA1 = dcol[:, 0:8], dcol[:, 8:16]
    A2, B2 = dcol[:, 16:24], dcol[:, 24:32]
    GA2, BA2 = dcol[:, 32:40], dcol[:, 40:48]
    tmpc = dcol[:, 48:56]

    def der():
        V.tensor_scalar(out=A_in, in0=modT[:, 8:16], scalar1=1.0, scalar2=1.0 / ALPHA, op0=ALU.add, op1=ALU.mult)
        V.tensor_copy(out=B_in, in_=modT[:, 0:8])
        V.tensor_scalar(out=g1a, in0=modT[:, 16:24], scalar1=1.0, scalar2=None, op0=ALU.add)
        V.tensor_scalar(out=g1f, in0=modT[:, 40:48], scalar1=1.0, scalar2=None, op0=ALU.add)
        V.tensor_scalar(out=GA1, in0=lg1, scalar1=ALPHA, scalar2=None, op0=ALU.mult)
        V.tensor_scalar(out=BA1, in0=lb1, scalar1=ALPHA, scalar2=None, op0=ALU.mult)
        V.tensor_scalar(out=A2, in0=modT[:, 32:40], scalar1=1.0, scalar2=1.0 / ALPHA, op0=ALU.add, op1=ALU.mult)
        V.tensor_copy(out=B2, in_=modT[:, 24:32])
        sc = 1.0
        V.tensor_scalar(out=GA2, in0=lg2, scalar1=sc, scalar2=None, op0=ALU.mult)
        return V.tensor_scalar(out=BA2, in0=lb2, scalar1=sc, scalar2=None, op0=ALU.mult)
    P.op("dve", der, R=["mod", "misc"], W=["der"])
    P.barrier()

    ar = Arena(arena_t, TAIL)
    posi = ar.alloc([128, NCH], I32)
    posf = ar.alloc([128, NCH])
    ang = ar.alloc([128, NCH, 32])
    ang2 = ar.alloc([128, NCH, 32])
    ti = ar.alloc([128, NCH, 32], I32)
    tf = ar.alloc([128, NCH, 32])
    P.dma(sp, posi, pos_d, W=["posi"])

    def rot_tables():
        V.tensor_copy(out=posf, in_=posi)
        V.tensor_tensor(out=ang, in0=_bc(posf.unsqueeze(2), [128, NCH, 32]), in1=_bc(invf.unsqueeze(1), [128, NCH, 32]), op=ALU.mult)
        V.tensor_scalar(out=ang, in0=ang, scalar1=float(1.0 / (2 * np.pi)), scalar2=None, op0=ALU.mult)
        V.tensor_scalar(out=ang2, in0=ang, scalar1=0.25, scalar2=None, op0=ALU.add)
        r = None
        for a in (ang, ang2):
            V.tensor_copy(out=ti, in_=a)
            V.tensor_copy(out=tf, in_=ti)
            V.tensor_tensor(out=a, in0=a, in1=tf, op=ALU.subtract)
            V.tensor_scalar(out=tf, in0=a, scalar1=0.5, scalar2=None, op0=ALU.is_gt)
            V.tensor_tensor(out=a, in0=a, in1=tf, op=ALU.subtract)
            V.tensor_scalar(out=tf, in0=a, scalar1=-0.5, scalar2=None, op0=ALU.is_lt)
            r = V.tensor_tensor(out=a, in0=a, in1=tf, op=ALU.add)
        return r
    P.op("dve", rot_tables, R=["posi", "cst"], W=["ang"])

    def rot_sin():
        S.activation(out=sinT, in_=ang, func=AF.Sin, scale=float(2 * np.pi))
        return S.activation(out=cosT, in_=ang2, func=AF.Sin, scale=float(2 * np.pi))
    P.op("act", rot_sin, R=["ang"], W=["rot"])

    P.op("act", lambda: S.activation(out=alog, in_=alog, func=AF.Exp), R=["rowp"], W=["rowp"])
    P.op("dve", lambda: V.tensor_scalar(out=alog, in0=alog, scalar1=-1.0, scalar2=None, op0=ALU.mult), R=["rowp"], W=["rowp"])
    negA = alog
    dbg("rowp", rowp[:, 0:32], ["rowp"])
    dbg("modT", modT[:, 0:48], ["mod"])

    if full:
        eoh = ar.alloc([128, 256, 32])
        etmp = ar.alloc([128, 256, 32])
        P.dma(sp, eoh.rearrange("p a b -> p (a b)"), eoh_d, W=["eoh"])
        rb3 = relb.rearrange("p (b h) -> p b h", h=4)
        for h in range(4):
            P.op("pool", lambda h=h: G.tensor_tensor(out=etmp, in0=eoh, in1=_bc(rb3[:, :, h].unsqueeze(1), [128, 256, 32]), op=ALU.mult),
                 R=["eoh", "rowp"], W=["etmp"])

            def red(h=h):
                V.tensor_reduce(out=biasw[:, h, :], in_=etmp, axis=AX.X, op=ALU.add)
                return V.tensor_tensor(out=biasw[:, h, :], in0=biasw[:, h, :], in1=madd, op=ALU.add)
            P.op("dve", red, R=["etmp", "cst"], W=["biasw"])
    P.barrier()

    ar = Arena(arena_t, TAIL)
    hT_halo = ar.alloc([128, 8, 128], BF16)
    _mark = ar.off
    xtok = [ar.alloc([128, D]) for _ in range(2)]
    for n in range(-1, NCH):
        buf = xtok[n % 2]
        bk = "xtok%d" % (n % 2)
        P.dma(sp, buf, x_d[(n + 1) * 128:(n + 2) * 128, :], W=[bk])
        for half in range(2):
            bank, bkey = pb()

            def tr(buf=buf, half=half, bank=bank):
                r = None
                for q in range(4):
                    fc = half * 4 + q
                    r = T.transpose(bank[:, q * 128:(q + 1) * 128], buf[:, fc * 128:(fc + 1) * 128], ident)
                return r
            P.op("pe", tr, R=[bk, "cst"], W=[bkey])
            if n >= 0:
                P.op("act", lambda n=n, half=half, bank=bank: S.mul(out=xT[:, half * 4:half * 4 + 4, n * 128:(n + 1) * 128],
                                                                   in_=bank[:].rearrange("p (q t) -> p q t", q=4), mul=(1.0 if ffn_only else ALPHA)),
                     R=[bkey], W=[("xT", n, half)])
            else:
                def hh(half=half, bank=bank):
                    r = None
                    for q in range(4):
                        fc = half * 4 + q
                        r = V.tensor_scalar(out=hT_halo[:, fc, :], in0=bank[:, q * 128:(q + 1) * 128],
                                            scalar1=A_in[:, fc:fc + 1], scalar2=B_in[:, fc:fc + 1], op0=ALU.mult, op1=ALU.add)
                    return r
                def hh2(half=half, bank=bank):
                    r = None
                    for q in range(4):
                        fc = half * 4 + q
                        V.tensor_scalar(out=tmpc[:, 0:1], in0=A_in[:, fc:fc + 1], scalar1=ALPHA, scalar2=None, op0=ALU.mult)
                        r = V.tensor_scalar(out=hT_halo[:, fc, :], in0=bank[:, q * 128:(q + 1) * 128],
                                            scalar1=tmpc[:, 0:1], scalar2=B_in[:, fc:fc + 1], op0=ALU.mult, op1=ALU.add)
                    return r
                P.op("dve", hh2, R=[bkey, "misc", "der"], W=["hT_halo", "der"])

    P.barrier()
    ar.off = _mark
    w_in = ar.alloc([128, 8, IN_DIM], BF16)
    wcols = "w_in_d"
    wi_src = w_in_d.rearrange("(c p) n -> p c n", p=128)
    for a, b in ((0, 1024), (1024, 2048), (2048, 2312), (2568, 2824)):
        P.dma("pool", w_in[:, :, a:b], wi_src[:, :, a:b], W=["w_in"])
    for slot, h in enumerate((0, 2, 1, 3)):
        P.dma("pool", w_in[:, :, 2312 + slot * 64:2312 + (slot + 1) * 64], wi_src[:, :, 2312 + h * 64:2312 + (h + 1) * 64], W=["w_in"])
    if full:
        w_out = ar.alloc([128, 8, D], BF16)
        P.dma("pool", w_out, w_out_d.rearrange("(c p) n -> p c n", p=128), W=["w_out"])

    Sret = ar.alloc([128, 2, 64])
    Sret_b = ar.alloc([128, 2, 64], BF16)
    Sssd = ar.alloc([128, 256])
    Sssd_b = ar.alloc([128, 256], BF16)
    totacc = ar.alloc([128, 8])
    if full:
        stin = ar.alloc([128, 3, STW])
        P.dma(sp, stin, st_in_d.rearrange("s p w -> p s w"), W=["stin"])
        wss = ar.alloc([128, 3, 4])

        def comb():
            V.tensor_tensor(out=Sret, in0=stin[:, 0, 0:128].rearrange("p (t e) -> p t e", t=2),
                            in1=_bc(wret[:, 0:2].unsqueeze(2), [128, 2, 64]), op=ALU.mult)
            for s_ in (1, 2):
                V.tensor_tensor(out=Sssd[:, 0:128].rearrange("p (t e) -> p t e", t=2), in0=stin[:, s_, 0:128].rearrange("p (t e) -> p t e", t=2),
                                in1=_bc(wret[:, 2 * s_:2 * s_ + 2].unsqueeze(2), [128, 2, 64]), op=ALU.mult)
                V.tensor_tensor(out=Sret, in0=Sret, in1=Sssd[:, 0:128].rearrange("p (t e) -> p t e", t=2), op=ALU.add)
            for g in range(2):
                pr = slice(g * 64, (g + 1) * 64)
                V.tensor_copy(out=wss[pr, 1, :], in_=stin[pr, 0, 384 + g * 4:384 + g * 4 + 4])
                V.tensor_tensor(out=wss[pr, 2, :], in0=stin[pr, 0, 384 + g * 4:384 + g * 4 + 4],
                                in1=stin[pr, 1, 384 + g * 4:384 + g * 4 + 4], op=ALU.add)
            return V.memset(wss[:, 0, :], 0.0)
        P.op("dve", comb, R=["stin", "cst"], W=["Sret", "Sssd", "wss"])
        P.op("act", lambda: S.activation(out=wss, in_=wss, func=AF.Exp), R=["wss"], W=["wss"])

        def comb2():
            V.tensor_tensor(out=Sssd.rearrange("p (r e) -> p r e", r=4), in0=stin[:, 0, 128:384].rearrange("p (r e) -> p r e", r=4),
                            in1=_bc(wss[:, 0, :].unsqueeze(2), [128, 4, 64]), op=ALU.mult)
            for s_ in (1, 2):
                V.tensor_tensor(out=stin[:, s_, 128:384].rearrange("p (r e) -> p r e", r=4), in0=stin[:, s_, 128:384].rearrange("p (r e) -> p r e", r=4),
                                in1=_bc(wss[:, s_, :].unsqueeze(2), [128, 4, 64]), op=ALU.mult)
                V.tensor_tensor(out=Sssd, in0=Sssd, in1=stin[:, s_, 128:384], op=ALU.add)
            V.tensor_copy(out=Sssd_b, in_=Sssd)
            return V.tensor_copy(out=Sret_b, in_=Sret)
        P.op("dve", comb2, R=["stin", "wss", "Sret", "Sssd"], W=["Sret", "Sssd", "stin"])
    else:
        def zinit():
            V.memset(Sret, 0.0)
            V.memset(Sssd, 0.0)
            return V.memset(totacc, 0.0)
        P.op("dve", zinit, W=["Sret", "Sssd", "totacc"])

    hTc = [ar.alloc([128, 8, 128], BF16) for _ in range(2)]
    qk_sb = ar.alloc([128, 512])
    qr = ar.alloc([128, 4, 2, 32])
    kr = ar.alloc([128, 4, 2, 32])
    rt = [ar.alloc([128, 4, 32]) for _ in range(4)]
    q2b = ar.alloc([128, 256], BF16)
    k2b = ar.alloc([128, 256], BF16)
    v_b = ar.alloc([128, 256], BF16)
    sg = ar.alloc([128, 256])
    qkT = ar.alloc([128, 512], BF16)
    qm = ar.alloc([128, 4, 128], BF16)
    BCm = ar.alloc([128, 4, 128], BF16)
    sTm = ar.alloc([128, 512], BF16)
    osq = qr.rearrange("p h two f -> p h (two f)")
    onr = kr.rearrange("p h two f -> p h (two f)")
    gst = ar.alloc([128, 16])
    xr = ar.alloc([128, 6, 131])
    acc = ar.alloc([128, 6, 128])
    bc_b = ar.alloc([128, 2, 128], BF16)
    Bm_b = ar.alloc([128, 128], BF16)
    amask = ar.alloc([128, 8, 128])
    eseg = ar.alloc([128, 8, 128], BF16)
    mT = ar.alloc([128, 8, 128], BF16)
    cbm = ar.alloc([128, 2, 128])
    xdt_b = ar.alloc([128, 8, 64], BF16)
    xdd_b = ar.alloc([128, 8, 64], BF16)
    xskip = ar.alloc([128, 8, 64])
    t1 = ar.alloc([128, 8, 64])
    szs = ar.alloc([128, 512])
    hsq = ar.alloc([128, 512])
    sm = ar.alloc([128, 64])
    qT_s = ar.alloc([128, 4, 128], BF16)
    kT_pp = [ar.alloc([128, 128], BF16) for _ in range(2)]
    v_pp = [ar.alloc([128, 128], BF16) for _ in range(2)]
    sl = amask.rearrange("p h l -> p (h l)").rearrange("p (h j) -> p h j", h=4)
    p_b = ar.alloc([128, 4, 256], BF16)
    pT_b = ar.alloc([128, 8, 128], BF16)
    ssw = ar.alloc([128, 32])
    mix_tok = ar.alloc([128, D], BF16)
    mixT = ar.alloc([128, 8, 128], BF16)
    sq = [ar.alloc([128, 128]) for _ in range(2)]
    lnst = t1.rearrange("p h d -> p (h d)").rearrange("p (a b) -> p a b", a=4)
    lnk = ["t1"]
    mixer_arena_end = ar.off

    def zmask():
        V.memset(qm, 0.0)
        V.memset(BCm, 0.0)
        return V.memset(qT_s, 0.0)
    P.op("dve", zmask, W=["qm", "BCm", "qT_s"])

    dtv, av_, acs_tot, ed, eaed, cdec, dd = (sm[:, 0:8], sm[:, 8:16], sm[:, 16:32], sm[:, 32:48], sm[:, 48:64], None, None)

    sm2 = ar.alloc([128, 32])
    cdec = sm2[:, 0:8]
    dd = sm2[:, 8:16]
    rr = sm2[:, 16:24]

    def ln_inplace(n_cols, xs_keyR, xview, GA, BA, nfree):
        bank, bkey = pb()
        bank2, bkey2 = (bank[:, nfree:2 * nfree], bkey) if nfree <= 256 else pb()
        if nfree > 256:
            bank2 = bank2[:, 0:nfree]
        for fc in range(8):
            sqb = sq[fc % 2]
            sk = "sq%d" % (fc % 2)
            P.op("pool", lambda fc=fc, sqb=sqb: G.tensor_tensor(out=sqb[:, 0:nfree], in0=xview[:, fc, :], in1=xview[:, fc, :], op=ALU.mult),
                 R=xs_keyR, W=[sk])

            def mm(fc=fc, sqb=sqb, bank=bank):
                T.matmul(bank[:, 0:nfree], lhsT=ones, rhs=xview[:, fc, :], start=(fc == 0), stop=(fc == 7))
                return T.matmul(bank2, lhsT=ones, rhs=sqb[:, 0:nfree], start=(fc == 0), stop=(fc == 7))
            P.op("pe", mm, R=xs_keyR + [sk, "cst"], W=[bkey, bkey2])
        mean, msq, var, rstd = lnst[:, 0, 0:nfree], lnst[:, 1, 0:nfree], lnst[:, 2, 0:nfree], lnst[:, 3, 0:nfree]

        def st(bank=bank):
            V.tensor_scalar(out=mean, in0=bank[:, 0:nfree], scalar1=1.0 / D, scalar2=None, op0=ALU.mult)
            V.tensor_tensor(out=msq, in0=mean, in1=mean, op=ALU.mult)
            V.scalar_tensor_tensor(out=var, in0=bank2, scalar=1.0 / D, in1=msq, op0=ALU.mult, op1=ALU.subtract)
            return V.tensor_scalar(out=var, in0=var, scalar1=EPS, scalar2=None, op0=ALU.add)
        P.op("dve", st, R=[bkey, bkey2], W=[lnk[0]])
        P.op("act", lambda: S.sqrt(out=rstd, in_=var), R=[lnk[0]], W=[lnk[0]])

        def nrm():
            V.reciprocal(out=rstd, in_=rstd)
            V.tensor_tensor(out=xview, in0=xview, in1=_bc(mean.unsqueeze(1), [128, 8, nfree]), op=ALU.subtract)
            return V.tensor_tensor(out=xview, in0=xview, in1=_bc(rstd.unsqueeze(1), [128, 8, nfree]), op=ALU.mult)
        P.op("dve", nrm, R=[lnk[0]] + xs_keyR, W=xs_keyR + [lnk[0]])

        def aff():
            G.tensor_tensor(out=xview, in0=xview, in1=_bc(GA.unsqueeze(2), [128, 8, nfree]), op=ALU.mult)
            return G.tensor_tensor(out=xview, in0=xview, in1=_bc(BA.unsqueeze(2), [128, 8, nfree]), op=ALU.add)
        P.op("pool", aff, R=xs_keyR + ["der", "misc"], W=xs_keyR)

    def chunk(n):
        halo = n < 0
        cur, prv = (n % 2), ((n + 1) % 2)
        if halo:
            hc = hT_halo
            hk = "hT_halo"
        else:
            hc = hTc[n % 2]
            hk = "hTc%d" % (n % 2)
            xk = [("xT", n, 0), ("xT", n, 1)]
            Tn = slice(n * 128, (n + 1) * 128)

            def mkh2():
                r = None
                for fc in range(8):
                    r = G.tensor_scalar(out=hc[:, fc, :], in0=xT[:, fc, Tn], scalar1=A_in[:, fc:fc + 1], scalar2=B_in[:, fc:fc + 1],
                                        op0=ALU.mult, op1=ALU.add)
                return r
            P.op("pool", mkh2, R=xk + ["misc", "der"], W=[hk])

        def proj_tok(bank, c0, c1, o0=0):
            def f():
                r = None
                for kc in range(8):
                    r = T.matmul(bank[:, o0:o0 + (c1 - c0)], lhsT=hc[:, kc, :], rhs=w_in[:, kc, c0:c1], start=(kc == 0), stop=(kc == 7))
                return r
            return f

        def proj_feat(bank, c0, o0):
            def f():
                r = None
                for kc in range(8):
                    r = T.matmul(bank[:, o0:o0 + 128], lhsT=w_in[:, kc, c0:c0 + 128], rhs=hc[:, kc, :], start=(kc == 0), stop=(kc == 7))
                return r
            return f

        bD, kD = pb()
        bE, kE = pb()
        bF, kF = pb()
        if full:
            P.op("pe", proj_tok(bD, 2696, 2824, 8), R=[hk, "w_in"], W=[kD])
            P.op("pe", proj_feat(bD, 2568, 256), R=[hk, "w_in"], W=[kD])
        for c in range(4):
            P.op("pe", proj_feat(bE, 1536 + c * 128, c * 128), R=[hk, "w_in"], W=[kE])
        P.op("pe", proj_feat(bF, 2048, 0), R=[hk, "w_in"], W=[kF])
        P.op("pe", proj_feat(bF, 2176, 128), R=[hk, "w_in"], W=[kF])
        if halo:
            def tail():
                V.tensor_scalar(out=xr[:, 0:4, 128:131], in0=bE[:].rearrange("p (c t) -> p c t", c=4)[:, :, 125:128],
                                scalar1=halovalid, scalar2=None, op0=ALU.mult)
                return V.tensor_scalar(out=xr[:, 4:6, 128:131], in0=bF[:, 0:256].rearrange("p (c t) -> p c t", c=2)[:, :, 125:128],
                                       scalar1=halovalid, scalar2=None, op0=ALU.mult)
            P.op("dve", tail, R=[kE, kF, "misc"], W=["xr"])
            if full:
                def kv():
                    S.copy(out=kT_pp[cur], in_=bD[:, 256:384])
                    return S.copy(out=v_pp[cur], in_=bD[:, 8:136])
                P.op("act", kv, R=[kD], W=["kT_pp%d" % cur, "v_pp%d" % cur])
            return
        P.op("pe", proj_tok(bD, 2304, 2312, 0), R=[hk, "w_in"], W=[kD])
        bA, kA = pb()
        bB, kB = pb()
        if full:
            P.op("pe", proj_tok(bA, 0, 512), R=[hk, "w_in"], W=[kA])
            P.op("pe", proj_tok(bB, 512, 1024), R=[hk, "w_in"], W=[kB])
            bC, kC = pb()
            P.op("pe", proj_tok(bC, 1024, 1536), R=[hk, "w_in"], W=[kC])
            P.op("pe", proj_feat(bF, 2312, 256), R=[hk, "w_in"], W=[kF])
            P.op("pe", proj_feat(bF, 2440, 384), R=[hk, "w_in"], W=[kF])
        else:
            P.op("pe", proj_tok(bA, 256, 512, 256), R=[hk, "w_in"], W=[kA])
            P.op("pe", proj_tok(bB, 512, 768, 0), R=[hk, "w_in"], W=[kB])

        P.op("dve", lambda: V.tensor_tensor(out=dtv, in0=bD[:, 0:8], in1=dtb, op=ALU.add), R=[kD, "rowp"], W=["dtv"])
        P.op("pool", lambda: G.tensor_copy(out=xr[:, :, 0:3], in_=xr[:, :, 128:131]), R=["xr"], W=["xr"])

        def xrcp():
            S.copy(out=xr[:, 0:4, 3:131], in_=bE[:].rearrange("p (c t) -> p c t", c=4))
            return S.copy(out=xr[:, 4:6, 3:131], in_=bF[:, 0:256].rearrange("p (c t) -> p c t", c=2))
        P.op("act", xrcp, R=[kE, kF], W=["xr"])
        P.op("act", lambda: S.copy(out=qk_sb[:, (0 if full else 256):512], in_=bA[:, (0 if full else 256):512]), R=[kA], W=["qk_sb"])
        P.op("act", lambda: S.copy(out=v_b, in_=bB[:, 0:256]), R=[kB], W=["v_b"])
        if full:
            def swc():
                for h_ in range(4):
                    kvh_, gq_ = h_ // 2, h_ % 2
                    pr_ = slice(kvh_ * 64, kvh_ * 64 + 64)
                    S.copy(out=qT_s[pr_, h_, :], in_=bF[pr_, 256 + gq_ * 128:256 + (gq_ + 1) * 128])
                S.copy(out=kT_pp[cur], in_=bD[:, 256:384])
                return S.copy(out=v_pp[cur], in_=bD[:, 8:136])
            P.op("act", swc, R=[kF, kD], W=["qT_s", "kT_pp%d" % cur, "v_pp%d" % cur])
            P.op("act", lambda: S.activation(out=sg, in_=bB[:, 256:512], func=AF.Silu), R=[kB], W=["sg"])
            P.op("act", lambda: S.activation(out=szs, in_=bC[:], func=AF.Silu), R=[kC], W=["szs"])
        if SUB[0] < 1:
            return
        cosb = _bc(cosT[:, n, :].unsqueeze(1), [128, 4, 32])
        sinb = _bc(sinT[:, n, :].unsqueeze(1), [128, 4, 32])

        def rotary(E, src, dst, ta, tb):
            X = src.rearrange("p (h two f) -> p h two f", h=4, two=2)
            x1, x2 = X[:, :, 0, :], X[:, :, 1, :]
            E.tensor_tensor(out=ta, in0=x1, in1=cosb, op=ALU.mult)
            E.tensor_tensor(out=tb, in0=x2, in1=sinb, op=ALU.mult)
            E.tensor_tensor(out=dst[:, :, 0, :], in0=ta, in1=tb, op=ALU.subtract)
            E.tensor_tensor(out=ta, in0=x1, in1=sinb, op=ALU.mult)
            E.tensor_tensor(out=tb, in0=x2, in1=cosb, op=ALU.mult)
            return E.tensor_tensor(out=dst[:, :, 1, :], in0=ta, in1=tb, op=ALU.add)

        def krot():
            rotary(V, qk_sb[:, 256:512], kr, rt[2], rt[3])
            return V.tensor_tensor(out=k2b[:].rearrange("p (h d) -> p h d", h=4), in0=_bc(dk.unsqueeze(2), [128, 4, 64]),
                                   in1=kr.rearrange("p h two f -> p h (two f)"), op=ALU.mult)
        P.op("dve", krot, R=["qk_sb", "rot", "cst"], W=["kr", "k2b"])
        if full:
            def qrot():
                rotary(V, qk_sb[:, 0:256], qr, rt[0], rt[1])
                return V.tensor_tensor(out=q2b[:].rearrange("p (h d) -> p h d", h=4), in0=_bc(dq.unsqueeze(2), [128, 4, 64]),
                                       in1=qr.rearrange("p h two f -> p h (two f)"), op=ALU.mult)
            P.op("dve", qrot, R=["qk_sb", "rot", "cst"], W=["qr", "q2b"])
            if SUB[0] < 1.05:
                return
            bT, kT_ = pb()
            bTb = bT[:].bitcast(BF16)

            def trqk():
                r = None
                for t in range(2):
                    T.transpose(bTb[:, t * 128:(t + 1) * 128], q2b[:, t * 128:(t + 1) * 128], ident_b[:])
                    r = T.transpose(bTb[:, 256 + t * 128:256 + (t + 1) * 128], k2b[:, t * 128:(t + 1) * 128], ident_b[:])
                return r
            P.op("pe", trqk, R=["q2b", "k2b", "ident_b"], W=[kT_])
            def qkcp():
                for h_ in range(4):
                    t_, hf2 = h_ // 2, h_ % 2
                    pr_ = slice(hf2 * 64, hf2 * 64 + 64)
                    S.copy(out=qm[pr_, h_, :], in_=bTb[pr_, t_ * 128:(t_ + 1) * 128])
                return S.copy(out=qkT[:, 256:512], in_=bTb[:, 256:512])
            P.op("act", qkcp, R=[kT_], W=["qkT", "qm"])
            if SUB[0] < 1.1:
                return
            bS, kS = pb()

            def scores():
                r = None
                import os
                for h in [int(c_) for c_ in os.environ.get("SUBH", "0123")]:
                    t, hf_ = h // 2, h % 2
                    pr = slice(hf_ * 64, hf_ * 64 + 64)
                    r = T.matmul(bS[:, h * 128:(h + 1) * 128], lhsT=qkT[:, 256 + t * 128:256 + (t + 1) * 128],
                                 rhs=qm[:, h, :], start=True, stop=True)
                return r
            P.op("pe", scores, R=["qkT", "qm"], W=[kS])
            if SUB[0] < 1.15:
                return
            P.op("dve", lambda: V.tensor_tensor(out=sTm[:].rearrange("p (h i) -> p h i", h=4), in0=_bc(tri.unsqueeze(1), [128, 4, 128]),
                                                in1=bS[:].rearrange("p (h i) -> p h i", h=4), op=ALU.mult), R=[kS, "cst"], W=["sTm"])
            if SUB[0] < 1.2:
                return
            bO, kO = pb()

            def oret():
                r = None
                for h in range(4):
                    t, hf_ = h // 2, h % 2
                    pr = slice(hf_ * 64, hf_ * 64 + 64)
                    T.matmul(bO[:, h * 64:(h + 1) * 64], lhsT=sTm[:, h * 128:(h + 1) * 128], rhs=v_b[:, h * 64:(h + 1) * 64], start=True, stop=False)
                    r = T.matmul(bO[:, h * 64:(h + 1) * 64], lhsT=qm[:, h, :], rhs=Sret_b[:, t, :], start=False, stop=True)
                return r
            P.op("pe", oret, R=["sTm", "v_b", "qm", "Sret_b"], W=[kO])
        if SUB[0] < 1.3:
            return
        bK, kK = pb()

        def kvm():
            r = None
            for t in range(2):
                r = T.matmul(bK[:, t * 128:(t + 1) * 128], lhsT=k2b[:, t * 128:(t + 1) * 128], rhs=v_b[:, t * 128:(t + 1) * 128], start=True, stop=True)
            return r
        P.op("pe", kvm, R=["k2b", "v_b"], W=[kK])

        if SUB[0] < 1.6:
            return

        def supd():
            K4 = bK[:, 0:256].rearrange("p (t hf e) -> p t hf e", t=2, hf=2)
            V.scalar_tensor_tensor(out=Sret, in0=K4[:, :, 0, :], scalar=m0, in1=Sret, op0=ALU.mult, op1=ALU.add)
            V.scalar_tensor_tensor(out=Sret, in0=K4[:, :, 1, :], scalar=m1, in1=Sret, op0=ALU.mult, op1=ALU.add)
            V.tensor_tensor(out=Sret, in0=Sret, in1=_bc(gC.unsqueeze(2), [128, 2, 64]), op=ALU.mult)
            return V.tensor_copy(out=Sret_b, in_=Sret)
        P.op("dve", supd, R=[kK, "Sret", "cst"], W=["Sret", "Sret_b"])
        if full:
            P.op("act", lambda: S.activation(out=osq, in_=bO[:, 0:256].rearrange("p (h d) -> p h d", h=4), func=AF.Square), R=[kO], W=["qr"])

            def gn1():
                V.tensor_reduce(out=gst[:, 0:4], in_=bO[:, 0:256].rearrange("p (h d) -> p h d", h=4), axis=AX.X, op=ALU.add)
                V.tensor_reduce(out=gst[:, 4:8], in_=osq, axis=AX.X, op=ALU.add)
                V.tensor_scalar(out=gst[:, 0:4], in0=gst[:, 0:4], scalar1=1.0 / 64, scalar2=None, op0=ALU.mult)
                V.tensor_tensor(out=gst[:, 8:12], in0=gst[:, 0:4], in1=gst[:, 0:4], op=ALU.mult)
                V.scalar_tensor_tensor(out=gst[:, 4:8], in0=gst[:, 4:8], scalar=1.0 / 64, in1=gst[:, 8:12], op0=ALU.mult, op1=ALU.subtract)
                return V.tensor_scalar(out=gst[:, 4:8], in0=gst[:, 4:8], scalar1=EPS, scalar2=None, op0=ALU.add)
            P.op("dve", gn1, R=[kO, "qr"], W=["gst"])
            P.op("act", lambda: S.sqrt(out=gst[:, 4:8], in_=gst[:, 4:8]), R=["gst"], W=["gst"])

            def gn2():
                V.reciprocal(out=gst[:, 4:8], in_=gst[:, 4:8])
                V.tensor_tensor(out=onr, in0=bO[:, 0:256].rearrange("p (h d) -> p h d", h=4), in1=_bc(gst[:, 0:4].unsqueeze(2), [128, 4, 64]), op=ALU.subtract)
                V.tensor_tensor(out=onr, in0=onr, in1=_bc(gst[:, 4:8].unsqueeze(2), [128, 4, 64]), op=ALU.mult)
                return V.tensor_tensor(out=mix_tok[:, 0:256], in0=onr.rearrange("p h d -> p (h d)"), in1=sg, op=ALU.mult)
            P.op("dve", gn2, R=[kO, "gst", "sg"], W=["kr", "gst", "mix_ret"])

        if SUB[0] < 2:
            return

        def conv(E, cs):
            def f():
                r = None
                for c in cs:
                    E.tensor_scalar(out=acc[:, c, :], in0=xr[:, c, 0:128], scalar1=convw[:, c * 4:c * 4 + 1], scalar2=convb[:, c:c + 1],
                                    op0=ALU.mult, op1=ALU.add)
                    for w in range(1, 4):
                        r = E.scalar_tensor_tensor(out=acc[:, c, :], in0=xr[:, c, w:w + 128], scalar=convw[:, c * 4 + w:c * 4 + w + 1],
                                                   in1=acc[:, c, :], op0=ALU.mult, op1=ALU.add)
                return r
            return f
        P.op("dve", conv(V, (0, 1, 4)), R=["xr", "misc"], W=["accA"])
        P.op("dve", conv(V, (2, 3, 5)), R=["xr", "misc"], W=["accB"])

        def sil():
            S.activation(out=acc[:, 0:4, :], in_=acc[:, 0:4, :], func=AF.Silu)
            return S.activation(out=bc_b, in_=acc[:, 4:6, :], func=AF.Silu)
        P.op("act", sil, R=["accA", "accB"], W=["accA", "accB", "bc_b"])
        if full:
            def bcm():
                r = None
                for g_ in range(2):
                    pr_ = slice(g_ * 64, g_ * 64 + 64)
                    G.tensor_copy(out=BCm[pr_, g_, :], in_=bc_b[pr_, 0, :])
                    r = G.tensor_copy(out=BCm[pr_, 2 + g_, :], in_=bc_b[pr_, 1, :])
                return r
            P.op("pool", bcm, R=["bc_b"], W=["BCm"])
        bX, kX = pb()

        def trx():
            r = None
            for c in range(4):
                r = T.transpose(bX[:, c * 128:(c + 1) * 128], acc[:, c, :], ident)
            return r
        P.op("pe", trx, R=["accA", "accB", "cst"], W=[kX])
        bBm, kBm = pb()
        bBmb = bBm[:].bitcast(BF16)
        P.op("pe", lambda: T.transpose(bBmb[:, 0:128], bc_b[:, 0, :], ident_b[:]), R=["bc_b", "ident_b"], W=[kBm])
        P.op("act", lambda: S.copy(out=Bm_b, in_=bBmb[:, 0:128]), R=[kBm], W=["Bm_b"])
        if SUB[0] < 3:
            return
        def sp_():
            S.activation(out=dtv, in_=dtv, func=AF.Exp)
            return S.activation(out=dtv, in_=dtv, func=AF.Ln, bias=1.0)
        if n == 0:
            dbg("dtv_pre", dtv, ["dtv"])
        P.op("act", sp_, R=["dtv"], W=["dtv"])
        if n == 0:
            dbg("dtv", dtv, ["dtv"])
        P.op("dve", lambda: V.tensor_tensor(out=av_, in0=dtv, in1=negA, op=ALU.mult), R=["dtv", "rowp"], W=["av"])
        bY, kY = pb()

        def acsm():
            T.matmul(bY[:, 0:8], lhsT=tri, rhs=av_, start=True, stop=True)
            return T.matmul(bY[:, 8:16], lhsT=ones, rhs=av_, start=True, stop=True)
        P.op("pe", acsm, R=["av", "cst"], W=[kY])
        P.op("act", lambda: S.copy(out=acs_tot, in_=bY[:, 0:16]), R=[kY], W=["acs_tot"])
        if n == 0:
            dbg("av", av_, ["av"])
            dbg("acs_tot", acs_tot, ["acs_tot"])

        def edf():
            V.tensor_copy(out=ed[:, 0:8], in_=acs_tot[:, 0:8])
            return V.tensor_tensor(out=ed[:, 8:16], in0=acs_tot[:, 8:16], in1=acs_tot[:, 0:8], op=ALU.subtract)
        P.op("dve", edf, R=["acs_tot"], W=["ed"])

        def exps():
            S.activation(out=eaed, in_=ed, func=AF.Exp)
            return S.activation(out=cdec, in_=acs_tot[:, 8:16], func=AF.Exp)
        P.op("act", exps, R=["ed", "acs_tot"], W=["eaed", "cdec"])
        if not full:
            P.op("pool", lambda: G.tensor_tensor(out=totacc, in0=totacc, in1=acs_tot[:, 8:16], op=ALU.add), R=["acs_tot", "totacc"], W=["totacc"])
        P.op("dve", lambda: V.tensor_tensor(out=dd, in0=dtv, in1=eaed[:, 8:16], op=ALU.mult), R=["dtv", "eaed"], W=["dd"])
        X3 = bX[:].rearrange("p (h d) -> p h d", h=8)
        P.op("dve", lambda: V.tensor_tensor(out=xdd_b, in0=_bc(dd.unsqueeze(2), [128, 8, 64]), in1=X3, op=ALU.mult), R=[kX, "dd"], W=["xdd_b"])
        if full:
            def xd():
                V.tensor_tensor(out=xdt_b, in0=_bc(dtv.unsqueeze(2), [128, 8, 64]), in1=X3, op=ALU.mult)
                return V.tensor_tensor(out=xskip, in0=X3, in1=_bc(dskip.unsqueeze(2), [128, 8, 64]), op=ALU.mult)
            P.op("dve", xd, R=[kX, "dtv", "rowp"], W=["xdt_b", "xskip"])
            P.op("pool", lambda: G.tensor_tensor(out=amask, in0=_bc(mgt.unsqueeze(1), [128, 8, 128]), in1=_bc(av_.unsqueeze(2), [128, 8, 128]), op=ALU.mult),
                 R=["av", "cst"], W=["amask"])
            bCB, kCB = pb()

            def cbm_():
                r = None
                for g in range(2):
                    pr = slice(g * 64, g * 64 + 64)
                    r = T.matmul(bCB[:, g * 128:(g + 1) * 128], lhsT=BCm[:, g, :], rhs=bc_b[:, 1, :], start=True, stop=True)
                return r
            P.op("pe", cbm_, R=["bc_b", "BCm"], W=[kCB])
            P.op("dve", lambda: V.tensor_tensor(out=cbm, in0=bCB[:, 0:256].rearrange("p (g l) -> p g l", g=2), in1=_bc(tri.unsqueeze(1), [128, 2, 128]), op=ALU.mult),
                 R=[kCB, "cst"], W=["cbm"])
            for g in range(2):
                bSg, kSg = pb()

                def segm(g=g, bSg=bSg):
                    r = None
                    for r_ in range(4):
                        r = T.matmul(bSg[:, r_ * 128:(r_ + 1) * 128], lhsT=amask[:, g * 4 + r_, :], rhs=tri, start=True, stop=True)
                    return r
                P.op("pe", segm, R=["amask", "cst"], W=[kSg])
                P.op("act", lambda g=g, bSg=bSg: S.activation(out=eseg[:, g * 4:g * 4 + 4, :], in_=bSg[:].rearrange("p (r l) -> p r l", r=4), func=AF.Exp),
                     R=[kSg], W=["eseg%d" % g])
                P.op("dve", lambda g=g: V.tensor_tensor(out=mT[:, g * 4:g * 4 + 4, :], in0=_bc(cbm[:, g, :].unsqueeze(1), [128, 4, 128]),
                                                        in1=eseg[:, g * 4:g * 4 + 4, :], op=ALU.mult),
                     R=["eseg%d" % g, "cbm"], W=["mT%d" % g])
            bYD, kYD = pb()

            def ydm():
                r = None
                for h in range(8):
                    r = T.matmul(bYD[:, h * 64:(h + 1) * 64], lhsT=mT[:, h, :], rhs=xdt_b[:, h, :], start=True, stop=True)
                return r
            P.op("pe", ydm, R=["mT0", "mT1", "xdt_b"], W=[kYD])
            bYO, kYO = pb()

            def yom():
                r = None
                for g in range(2):
                    pr = slice(g * 64, g * 64 + 64)
                    r = T.matmul(bYO[:, g * 256:(g + 1) * 256], lhsT=BCm[:, 2 + g, :], rhs=Sssd_b, start=True, stop=True)
                return r
            P.op("pe", yom, R=["BCm", "Sssd_b"], W=[kYO])
        if SUB[0] < 4:
            return
        bST, kST = pb()
        P.op("pe", lambda: T.matmul(bST[:, 0:512], lhsT=Bm_b, rhs=xdd_b[:].rearrange("p h d -> p (h d)"), start=True, stop=True),
             R=["Bm_b", "xdd_b"], W=[kST])

        def sssd():
            r = None
            for g in range(2):
                pr = slice(g * 64, g * 64 + 64)
                V.tensor_tensor(out=Sssd[pr, :].rearrange("p (r e) -> p r e", r=4), in0=Sssd[pr, :].rearrange("p (r e) -> p r e", r=4),
                                in1=_bc(cdec[pr, g * 4:g * 4 + 4].unsqueeze(2), [64, 4, 64]), op=ALU.mult)
                r = V.tensor_tensor(out=Sssd[pr, :], in0=Sssd[pr, :], in1=bST[pr, g * 256:(g + 1) * 256], op=ALU.add)
            return r
        P.op("dve", sssd, R=[kST, "Sssd", "cdec"], W=["Sssd"])
        if not full:
            return
        P.op("act", lambda: S.copy(out=Sssd_b, in_=Sssd), R=["Sssd"], W=["Sssd_b"])

        def ycomb():
            V.tensor_tensor(out=t1, in0=bYO[:].rearrange("p (h d) -> p h d", h=8), in1=_bc(eaed[:, 0:8].unsqueeze(2), [128, 8, 64]), op=ALU.mult)
            return V.tensor_tensor(out=t1, in0=t1, in1=bYD[:].rearrange("p (h d) -> p h d", h=8), op=ALU.add)
        P.op("dve", ycomb, R=[kYO, kYD, "eaed"], W=["t1"])
        t1f = t1.rearrange("p h d -> p (h d)")

        def yg():
            G.tensor_tensor(out=t1f, in0=t1f, in1=xskip.rearrange("p h d -> p (h d)"), op=ALU.add)
            G.tensor_tensor(out=t1f, in0=t1f, in1=szs, op=ALU.mult)
            return G.tensor_tensor(out=hsq, in0=t1f, in1=t1f, op=ALU.mult)
        P.op("pool", yg, R=["t1", "xskip", "szs"], W=["t1", "hsq"])

        def rms1():
            V.tensor_reduce(out=rr[:, 0:2], in_=hsq.rearrange("p (g e) -> p g e", g=2), axis=AX.X, op=ALU.add)
            return V.tensor_scalar(out=rr[:, 0:2], in0=rr[:, 0:2], scalar1=1.0 / 256, scalar2=EPS, op0=ALU.mult, op1=ALU.add)
        P.op("dve", rms1, R=["hsq"], W=["rr"])
        P.op("act", lambda: S.sqrt(out=rr[:, 0:2], in_=rr[:, 0:2]), R=["rr"], W=["rr"])

        def rms2():
            V.reciprocal(out=rr[:, 0:2], in_=rr[:, 0:2])
            V.tensor_tensor(out=hsq.rearrange("p (g e) -> p g e", g=2), in0=t1f.rearrange("p (g e) -> p g e", g=2),
                            in1=_bc(rr[:, 0:2].unsqueeze(2), [128, 2, 256]), op=ALU.mult)
            return V.tensor_tensor(out=mix_tok[:, 256:768], in0=hsq, in1=normw, op=ALU.mult)
        P.op("dve", rms2, R=["rr", "t1", "hsq", "rowp"], W=["hsq", "mix_ssd", "rr"])

        bL = [pb(), pb()]

        def lgm():
            r = None
            for h in range(4):
                kvh, gq = h // 2, h % 2
                pr = slice(kvh * 64, kvh * 64 + 64)
                bank = bL[h // 2][0]
                for part, buf in ((0, kT_pp[prv]), (1, kT_pp[cur])):
                    o = (h % 2) * 256 + part * 128
                    r = T.matmul(bank[:, o:o + 128], lhsT=qT_s[:, h, :], rhs=buf, start=True, stop=True)
            return r
        P.op("pe", lgm, R=["qT_s", "kT_pp0", "kT_pp1"], W=[bL[0][1], bL[1][1]])

        def sls():
            r = None
            for hb in range(2):
                r = V.scalar_tensor_tensor(out=sl[:, hb * 2:hb * 2 + 2, :], in0=bL[hb][0][:].rearrange("p (h j) -> p h j", h=2), scalar=0.125,
                                           in1=biasw[:, hb * 2:hb * 2 + 2, :], op0=ALU.mult, op1=ALU.add)
            if n == 0:
                r = V.tensor_scalar(out=sl[:, :, 0:128], in0=sl[:, :, 0:128], scalar1=halomask, scalar2=None, op0=ALU.add)
            V.tensor_reduce(out=ssw[:, 0:4], in_=sl, axis=AX.X, op=ALU.max)
            V.tensor_tensor(out=ssw[:, 0:4], in0=ssw[:, 0:4], in1=sinks, op=ALU.max)
            V.tensor_scalar(out=ssw[:, 4:8], in0=ssw[:, 0:4], scalar1=-1.0, scalar2=None, op0=ALU.mult)
            return V.tensor_tensor(out=ssw[:, 8:12], in0=sinks, in1=ssw[:, 4:8], op=ALU.add)
        P.op("dve", sls, R=[bL[0][1], bL[1][1], "biasw", "misc", "rowp"], W=["amask", "ssw"])

        def pex():
            r = None
            for h in range(4):
                r = S.activation(out=p_b[:, h, :], in_=sl[:, h, :], func=AF.Exp, bias=ssw[:, 4 + h:5 + h], scale=1.0)
            return S.activation(out=ssw[:, 12:16], in_=ssw[:, 8:12], func=AF.Exp)
        P.op("act", pex, R=["amask", "ssw"], W=["p_b", "ssw2"])

        def den():
            V.tensor_reduce(out=ssw[:, 16:20], in_=p_b, axis=AX.X, op=ALU.add)
            V.tensor_tensor(out=ssw[:, 16:20], in0=ssw[:, 16:20], in1=ssw[:, 12:16], op=ALU.add)
            return V.reciprocal(out=ssw[:, 20:24], in_=ssw[:, 16:20])
        P.op("dve", den, R=["p_b", "ssw2"], W=["ssw3"])
        bPT, kPT = pb()
        bPTb = bPT[:].bitcast(BF16)

        def ptr():
            r = None
            for h in range(4):
                for part in range(2):
                    j_ = h * 2 + part
                    r = T.transpose(bPTb[:, j_ * 128:(j_ + 1) * 128], p_b[:, h, part * 128:(part + 1) * 128], ident_b[:])
            return r
        P.op("pe", ptr, R=["p_b", "ident_b"], W=[kPT])
        P.op("act", lambda: S.copy(out=pT_b[:, 0:4, :], in_=bPTb[:, 0:512].rearrange("p (j i) -> p j i", j=4)), R=[kPT], W=["pT_b0"])
        P.op("dve", lambda: V.tensor_copy(out=pT_b[:, 4:8, :], in_=bPTb[:, 512:1024].rearrange("p (j i) -> p j i", j=4)), R=[kPT], W=["pT_b1"])
        bOS, kOS = pb()

        def osw():
            r = None
            for h in range(4):
                kvh = h // 2
                for part, buf in ((0, v_pp[prv]), (1, v_pp[cur])):
                    r = T.matmul(bOS[:, h * 64:(h + 1) * 64], lhsT=pT_b[:, h * 2 + part, :], rhs=buf[:, kvh * 64:(kvh + 1) * 64],
                                 start=(part == 0), stop=(part == 1))
            return r
        P.op("pe", osw, R=["pT_b0", "pT_b1", "v_pp0", "v_pp1"], W=[kOS])
        P.op("dve", lambda: V.tensor_tensor(out=mix_tok[:, 768:1024].rearrange("p (h d) -> p h d", h=4), in0=_bc(ssw[:, 20:24].unsqueeze(2), [128, 4, 64]),
                                            in1=bOS[:, 0:256].rearrange("p (h d) -> p h d", h=4), op=ALU.mult), R=[kOS, "ssw3"], W=["mix_swa"])

        bMT, kMT = pb()
        bMTb = bMT[:].bitcast(BF16)

        def mtr():
            r = None
            for kc in range(8):
                r = T.transpose(bMTb[:, kc * 128:(kc + 1) * 128], mix_tok[:, kc * 128:(kc + 1) * 128], ident_b[:])
            return r
        P.op("pe", mtr, R=["mix_ret", "mix_ssd", "mix_swa", "ident_b"], W=[kMT])
        P.op("act", lambda: S.copy(out=mixT, in_=bMTb[:, 0:1024].rearrange("p (k t) -> p k t", k=8)), R=[kMT], W=["mixT"])
        for half in range(2):
            bW, kW = pb()

            def wo(half=half, bW=bW):
                r = None
                for q in range(4):
                    fc = half * 4 + q
                    for kc in range(8):
                        r = T.matmul(bW[:, q * 128:(q + 1) * 128], lhsT=w_out[:, kc, fc * 128:(fc + 1) * 128], rhs=mixT[:, kc, :],
                                     start=(kc == 0), stop=(kc == 7))
                return r
            P.op("pe", wo, R=["w_out", "mixT"], W=[kW])

            def res(half=half, bW=bW):
                r = None
                for q in range(4):
                    fc = half * 4 + q
                    r = V.scalar_tensor_tensor(out=xT[:, fc, Tn], in0=bW[:, q * 128:(q + 1) * 128], scalar=g1a[:, fc:fc + 1], in1=xT[:, fc, Tn],
                                               op0=ALU.mult, op1=ALU.add)
                return r
            P.op("dve", res, R=[kW, "der", ("xT", n, half)], W=[("xT", n, half)])
        ln_inplace(128, xk, xT[:, :, Tn], GA1, BA1, 128)

    for n in range(-1, NCH):
        if STOP[0] >= 4 + (n + 1) and not ffn_only:
            chunk(n)

    if not full:
        sto = ar.alloc([128, STW])

        def pk():
            V.tensor_copy(out=sto[:, 0:128], in_=Sret.rearrange("p t e -> p (t e)"))
            V.tensor_copy(out=sto[:, 128:384], in_=Sssd)
            return V.tensor_copy(out=sto[:, 384:392], in_=totacc)
        P.op("dve", pk, R=["Sret", "Sssd", "totacc"], W=["sto"])
        P.dma(sp, st_out_d, sto, R=["sto"], is_output=True)
        return P.emit()

    P.barrier()
    ar = Arena(arena_t, AW)
    hT = ar.alloc([128, 8, NTOK], BF16)
    aT = [ar.alloc([128, 4, 1024], BF16) for _ in range(2)]
    wgu = [ar.alloc([128, 8, 1024], BF16) for _ in range(2)]
    wdb = [ar.alloc([128, 4, D], BF16) for _ in range(2)]
    sgt = [ar.alloc([128, 512]) for _ in range(2)]
    evt = [ar.alloc([128, 512]) for _ in range(2)]
    if moe:
        gbc = [ar.alloc([128, 1024]) for _ in range(2)]
        rw_sb = ar.alloc([128, 8, 8])
        lgT = ar.alloc([128, 512])
        gatesT = ar.alloc([128, NTOK])
        lg = ar.alloc([128, NCH, 8])
        gts = ar.alloc([128, NCH, 8])
        e1 = ar.alloc([128, NCH, 8])
        e2 = ar.alloc([128, NCH, 8])
        l2 = ar.alloc([128, NCH, 8])
        tk = ar.alloc([128, 6, NCH])
        h2f = [ar.alloc([128, 512]) for _ in range(2)]
        P.dma(sp, rw_sb, rw_d, W=["rw_sb"])

    if moe:
        bG, kG = pb()
    for tg in range(4):
        Tg = slice(tg * 512, (tg + 1) * 512)
        xkeys = [("xT", n, hf_) for n in range(tg * 4, tg * 4 + 4) for hf_ in range(2)]
        if moe:
            bR, kR = pb()
        for fc in range(8):
            P.op("act", lambda fc=fc, Tg=Tg: S.activation(out=hT[:, fc, Tg], in_=xT[:, fc, Tg], func=AF.Identity, bias=B2[:, fc:fc + 1], scale=A2[:, fc:fc + 1]),
                 R=xkeys + ["der"], W=[("hT", tg)])
            if moe:
                hb = h2f[fc % 2]
                hbk = "h2f%d" % (fc % 2)
                P.op("dve", lambda fc=fc, Tg=Tg, hb=hb: V.tensor_scalar(out=hb, in0=xT[:, fc, Tg], scalar1=A2[:, fc:fc + 1], scalar2=B2[:, fc:fc + 1], op0=ALU.mult, op1=ALU.add),
                     R=xkeys + ["der"], W=[hbk])
                P.op("pe", lambda fc=fc, hb=hb, bR=bR: T.matmul(bR[0:8, 0:512], lhsT=rw_sb[:, fc, :], rhs=hb, start=(fc == 0), stop=(fc == 7)),
                     R=[hbk, "rw_sb"], W=[kR])
        if moe:
            P.op("act", lambda bR=bR: S.activation(out=lgT[0:8, 0:512], in_=bR[0:8, 0:512], func=AF.Identity, bias=rbias[0:8, 0:1], scale=1.0),
                 R=[kR, "misc"], W=["lgT"])

            def ltr(tg=tg):
                r = None
                for q in range(4):
                    n = tg * 4 + q
                    r = T.transpose(bG[:, n * 8:(n + 1) * 8], lgT[0:8, q * 128:(q + 1) * 128], ident[0:8, 0:8])
                return r
            P.op("pe", ltr, R=["lgT", "cst"], W=[kG])
    if moe:

        def top2():
            V.tensor_copy(out=lg, in_=bG[:, 0:128].rearrange("p (n e) -> p n e", e=8))
            m1_, m2_, dlt, w1_, w2_ = tk[:, 0, :], tk[:, 1, :], tk[:, 2, :], tk[:, 3, :], tk[:, 4, :]
            V.tensor_reduce(out=m1_, in_=lg, axis=AX.X, op=ALU.max)
            V.tensor_tensor(out=e1, in0=lg, in1=_bc(m1_.unsqueeze(2), [128, NCH, 8]), op=ALU.is_equal)
            V.scalar_tensor_tensor(out=l2, in0=e1, scalar=-1e30, in1=lg, op0=ALU.mult, op1=ALU.add)
            V.tensor_reduce(out=m2_, in_=l2, axis=AX.X, op=ALU.max)
            V.tensor_tensor(out=e2, in0=l2, in1=_bc(m2_.unsqueeze(2), [128, NCH, 8]), op=ALU.is_equal)
            return V.tensor_tensor(out=dlt, in0=m2_, in1=m1_, op=ALU.subtract)
        P.op("dve", top2, R=[kG], W=["tk", "lg"])
        P.op("act", lambda: S.activation(out=tk[:, 2, :], in_=tk[:, 2, :], func=AF.Exp), R=["tk"], W=["tk"])

        def top2b():
            dlt, w1_, w2_ = tk[:, 2, :], tk[:, 3, :], tk[:, 4, :]
            V.tensor_scalar(out=w1_, in0=dlt, scalar1=1.0, scalar2=None, op0=ALU.add)
            V.reciprocal(out=w1_, in_=w1_)
            V.tensor_tensor(out=w2_, in0=dlt, in1=w1_, op=ALU.mult)
            V.tensor_tensor(out=e1, in0=e1, in1=_bc(w1_.unsqueeze(2), [128, NCH, 8]), op=ALU.mult)
            V.tensor_tensor(out=e2, in0=e2, in1=_bc(w2_.unsqueeze(2), [128, NCH, 8]), op=ALU.mult)
            return V.tensor_tensor(out=gts, in0=e1, in1=e2, op=ALU.add)
        P.op("dve", top2b, R=["tk", "lg"], W=["gts", "tk", "lg"])
        for tg in range(4):
            bG2, kG2 = pb()

            def gtr(tg=tg, bG2=bG2):
                r = None
                for q in range(4):
                    n = tg * 4 + q
                    r = T.transpose(bG2[0:8, q * 128:(q + 1) * 128], gts[:, n, :], ident)
                return r
            P.op("pe", gtr, R=["gts", "cst"], W=[kG2])
            P.op("act", lambda tg=tg, bG2=bG2: S.copy(out=gatesT[0:8, tg * 512:(tg + 1) * 512], in_=bG2[0:8, 0:512]), R=[kG2], W=["gatesT"])

    nexp = NEXP if moe else 1
    dff = D_FFE if moe else D_FF
    pieces = []
    o = 0
    while o < dff:
        w = min(512, dff - o)
        pieces.append((o, w))
        o += w
    pi = 0
    for e in range(nexp):
        if moe:
            for half in range(2):
                for q in range(2):
                    bg_, kg_ = pb()
                    P.op("pe", lambda e=e, half=half, q=q, bg_=bg_: T.matmul(bg_[:, 0:512], lhsT=sel8[0:8, e * 128:(e + 1) * 128],
                                                                              rhs=gatesT[0:8, half * 1024 + q * 512:half * 1024 + (q + 1) * 512], start=True, stop=True),
                         R=["gatesT", "cst"], W=[kg_])
                    P.op("act", lambda half=half, q=q, bg_=bg_: S.copy(out=gbc[half][:, q * 512:(q + 1) * 512], in_=bg_[:, 0:512]), R=[kg_], W=["gbc%d" % half])
        for (o, w) in pieces:
            nb = w // 128
            wb = pi % 2
            kwg, kwd = "wgu%d" % wb, "wd%d" % wb
            P.dma("pool", wgu[wb][:, :, 0:w], wg_d[e].rearrange("(c p) n -> p c n", p=128)[:, :, o:o + w], W=[kwg])
            P.dma("pool", wgu[wb][:, :, 512:512 + w], wu_d[e].rearrange("(c p) n -> p c n", p=128)[:, :, o:o + w], W=[kwg])
            P.dma("pool", wdb[wb][:, 0:nb, :], wd_d[e][o:o + w, :].rearrange("(c p) n -> p c n", p=128), W=[kwd])
            for half in range(2):
                for blk in range(nb):
                    for q in range(2):
                        tg = half * 2 + q
                        Tg = slice(tg * 512, (tg + 1) * 512)
                        bg_, kg_ = pb()
                        bu_, ku_ = pb()

                        def gu(blk=blk, Tg=Tg, bg_=bg_, bu_=bu_, wb=wb):
                            r = None
                            for kc in range(8):
                                T.matmul(bg_[:, 0:512], lhsT=wgu[wb][:, kc, blk * 128:(blk + 1) * 128], rhs=hT[:, kc, Tg], start=(kc == 0), stop=(kc == 7))
                            for kc in range(8):
                                r = T.matmul(bu_[:, 0:512], lhsT=wgu[wb][:, kc, 512 + blk * 128:512 + (blk + 1) * 128], rhs=hT[:, kc, Tg], start=(kc == 0), stop=(kc == 7))
                            return r
                        P.op("pe", gu, R=[kwg, ("hT", tg)], W=[kg_, ku_])
                        sb_ = sgt[(blk * 2 + q) % 2]
                        sk_ = "sgt%d" % ((blk * 2 + q) % 2)
                        P.op("act", lambda bg_=bg_, sb_=sb_: S.activation(out=sb_, in_=bg_[:, 0:512], func=AF.Silu), R=[kg_], W=[sk_])
                        P.op("dve", lambda half=half, blk=blk, q=q, bu_=bu_, sb_=sb_: V.tensor_tensor(out=aT[half][:, blk, q * 512:(q + 1) * 512], in0=bu_[:, 0:512], in1=sb_, op=ALU.mult),
                             R=[ku_, sk_], W=[("aT", half, blk, q)])
            for half in range(2):
                for fc in range(8):
                    for q in range(2):
                        tg = half * 2 + q
                        Tg = slice(tg * 512, (tg + 1) * 512)
                        bo_, ko_ = pb()

                        def dn(half=half, fc=fc, q=q, bo_=bo_, wb=wb, nb=nb):
                            r = None
                            for blk in range(nb):
                                r = T.matmul(bo_[:, 0:512], lhsT=wdb[wb][:, blk, fc * 128:(fc + 1) * 128], rhs=aT[half][:, blk, q * 512:(q + 1) * 512],
                                             start=(blk == 0), stop=(blk == nb - 1))
                            return r
                        P.op("pe", dn, R=[kwd] + [("aT", half, blk, q) for blk in range(nb)], W=[ko_])
                        eb = evt[(fc * 2 + q) % 2]
                        ek = "evt%d" % ((fc * 2 + q) % 2)
                        if moe:
                            P.op("dve", lambda fc=fc, half=half, q=q, bo_=bo_, eb=eb: V.scalar_tensor_tensor(out=eb, in0=bo_[:, 0:512], scalar=g1f[:, fc:fc + 1],
                                                                                                          in1=gbc[half][:, q * 512:(q + 1) * 512], op0=ALU.mult, op1=ALU.mult),
                                 R=[ko_, "der", "gbc%d" % half], W=[ek])
                        else:
                            P.op("dve", lambda fc=fc, bo_=bo_, eb=eb: V.tensor_scalar(out=eb, in0=bo_[:, 0:512], scalar1=g1f[:, fc:fc + 1], scalar2=None, op0=ALU.mult),
                                 R=[ko_, "der"], W=[ek])
                        xkeys = [("xT", n, fc // 4) for n in range(tg * 4, tg * 4 + 4)]
                        P.op("pool", lambda fc=fc, Tg=Tg, eb=eb: G.tensor_tensor(out=xT[:, fc, Tg], in0=xT[:, fc, Tg], in1=eb, op=ALU.add),
                             R=[ek] + xkeys, W=xkeys)
            pi += 1

    P.barrier()
    ar = Arena(arena_t, AW)
    sq = [ar.alloc([128, 512]) for _ in range(2)]
    lnst = ar.alloc([128, 4, 512])
    lnk = ["lnst"]
    for tg in range(4):
        Tg = slice(tg * 512, (tg + 1) * 512)
        xkeys = [("xT", n, hf_) for n in range(tg * 4, tg * 4 + 4) for hf_ in range(2)]
        ln_inplace(512, xkeys, xT[:, :, Tg], GA2, BA2, 512)

    xo = [ar.alloc([128, D]) for _ in range(2)]
    for n in range(NCH):
        buf = xo[n % 2]
        bk = "xo%d" % (n % 2)
        for half in range(2):
            bank, bkey = pb()

            def tr2(n=n, half=half, bank=bank):
                r = None
                for q in range(4):
                    fc = half * 4 + q
                    r = T.transpose(bank[:, q * 128:(q + 1) * 128], xT[:, fc, n * 128:(n + 1) * 128], ident)
                return r
            P.op("pe", tr2, R=[("xT", n, 0), ("xT", n, 1), "cst"], W=[bkey])
            P.op("act", lambda half=half, bank=bank, buf=buf: S.copy(out=buf[:, half * 512:(half + 1) * 512], in_=bank[:, 0:512]), R=[bkey], W=[bk + "_%d" % half])
        P.dma(sp, xo_d[n * 128:(n + 1) * 128, :], buf, R=[bk + "_0", bk + "_1"], is_output=True)
    return P.emit()


FSTOP = [99]
NR = 4
AONLY = [True]
NEXPRUN = [99]
MAINCH = [99]
MSUB = [99]
DECL_IN = set()


def build_fused():
    P = Prog()
    nc = P.nc
    V, S, G, T = EngProxy(P, "dve", nc.vector), EngProxy(P, "act", nc.scalar), EngProxy(P, "pool", nc.gpsimd), nc.tensor

    def din(name, shape, dt=F32):
        DECL_IN.add(name)
        return nc.dram_tensor(name, list(shape), dt, kind="ExternalInput").ap()

    def dout(name, shape, dt=F32):
        return nc.dram_tensor(name, list(shape), dt, kind="ExternalOutput").ap()

    def dbg(name, ap, keys):
        if name not in DBG:
            return
        shp = list(ap.shape)
        d_ = dout("dbg_" + name, shp, ap.dtype)
        P.dma("sp", d_, ap, R=list(keys), is_output=True)

    x_d = din("xin", [NTOK + 128, D])
    pos_d = din("pos", [128, NCH], I32)
    cst_d = din("cst", [128, CW])
    misc_all = din("misc", [DEPTH, 128, MW])
    rowp_all = din("rowp", [DEPTH, RW])
    relb_d = din("relb", [128])
    selw_d = din("selw", [128, 32])
    w_in_all = din("w_in", [DEPTH, D, IN_DIM])
    w_out_all = din("w_out", [DEPTH, D, D])
    wada_all = din("w_ada", [DEPTH, D, 6 * D])
    eoh_d = din("eoh", [128, 256 * 32])
    wg0_d = din("wg0", [1, D, D_FF])
    wu0_d = din("wu0", [1, D, D_FF])
    wd0_d = din("wd0", [1, D_FF, D])
    if FSTOP[0] >= 14:
        wg1_d = din("wg1", [NEXP, D, D_FFE])
        wu1_d = din("wu1", [NEXP, D, D_FFE])
        wd1_d = din("wd1", [NEXP, D_FFE, D])
        rw_d = din("rw", [128, 8, 8])
    else:
        wg1_d = wu1_d = wd1_d = rw_d = None
    xo_d = dout("xout", [NTOK, D])
    bounce_s = [nc.dram_tensor("bounce_s%d" % i, [128, STW], F32).ap() for i in range(DEPTH)]
    gath_s = [nc.dram_tensor("gath_s%d" % i, [NR * 128, STW], F32).ap() for i in range(DEPTH)]
    bounce_h = nc.dram_tensor("bounce_h", [128, D], F32).ap()
    gath_h = nc.dram_tensor("gath_h", [NR * 128, D], F32).ap()
    ALLC = [[0, 1, 2, 3], [4, 5, 6, 7]]

    xT = P.sbuf("xT", [128, 8, NTOK], F32)
    cst = P.sbuf("cst_sb", [128, CW], F32)
    misc = P.sbuf("misc_sb", [128, MW], F32)
    rowp = P.sbuf("rowp_sb", [128, RW], F32)
    ident_b = P.sbuf("ident_b", [128, 128], BF16)
    AW = 33700
    arena_t = P.sbuf("arena", [128, AW], F32)
    TAIL = AW - 2048
    cosT = arena_t[:, TAIL:TAIL + 512].rearrange("p (n f) -> p n f", f=32)
    sinT = arena_t[:, TAIL + 512:TAIL + 1024].rearrange("p (n f) -> p n f", f=32)
    biasw = arena_t[:, TAIL + 1024:TAIL + 2048].rearrange("p (h j) -> p h j", h=4)
    ps = [P.psum("ps%d" % i, [128, 512], F32) for i in range(8)]
    psk = ["ps%d" % i for i in range(8)]
    pctr = [0]

    def pb():
        i = pctr[0] % 8
        pctr[0] += 1
        return ps[i], psk[i]

    ident = cst[:, C_ID:C_ID + 128]
    tri = cst[:, C_TRI:C_TRI + 128]
    mgt = cst[:, C_MGT:C_MGT + 128]
    ones = cst[:, C_ONE:C_ONE + 128]
    dq = cst[:, C_DQ:C_DQ + 4]
    dk = cst[:, C_DK:C_DK + 4]
    gC = cst[:, C_GC:C_GC + 2]
    invf = cst[:, C_INV:C_INV + 32]
    madd = cst[:, C_MADD:C_MADD + 256]
    wret = cst[:, C_WRET:C_WRET + 6]
    m0 = cst[:, C_M0:C_M0 + 1]
    m1 = cst[:, C_M0 + 1:C_M0 + 2]
    sel8 = cst[:, C_SEL:C_SEL + 8 * 128]

    def mcol(o, n):
        return misc[:, o:o + n]
    A_in, B_in = mcol(M_AIN, 8), mcol(M_BIN, 8)
    g1a, g1f = mcol(M_G1A, 8), mcol(M_G1F, 8)
    convw = mcol(M_CW, 24)
    convb = mcol(M_CB, 6)
    halovalid = mcol(M_HV, 1)
    halomask = mcol(M_HM, 1)
    modT = mcol(M_MOD, 96)
    lncol = mcol(M_LN, 32)
    ccol = mcol(M_C, 8)
    badaT = mcol(M_BADA, 96)
    dcol = mcol(M_DER, 64)
    rbias = mcol(M_RB, 8)

    dtb = rowp[:, R_DTB:R_DTB + 8]
    alog = rowp[:, R_ALOG:R_ALOG + 8]
    dskip = rowp[:, R_DSK:R_DSK + 8]
    normw = rowp[:, R_NW:R_NW + 512]
    sinks = rowp[:, R_SINK:R_SINK + 4]

    selw = P.sbuf("selw_sb", [128, 32], F32)
    relbt = P.sbuf("relb_sb", [128, 128], F32)
    relb = relbt[:, 0:128]
    sp = "sp"
    P.dma(sp, cst[:], cst_d, W=["cst"])
    P.dma(sp, selw[:], selw_d, W=["selw"])
    P.dma(sp, relbt[:], relb_d.partition_broadcast(128), W=["relb"])
    P.op("dve", lambda: V.tensor_copy(out=ident_b[:], in_=ident), R=["cst"], W=["ident_b"])

    ar = Arena(arena_t, TAIL)
    posi = ar.alloc([128, NCH], I32)
    posf = ar.alloc([128, NCH])
    ang = ar.alloc([128, NCH, 32])
    ang2 = ar.alloc([128, NCH, 32])
    ti = ar.alloc([128, NCH, 32], I32)
    tf = ar.alloc([128, NCH, 32])
    P.dma(sp, posi, pos_d, W=["posi"])

    def rot_tables():
        V.tensor_copy(out=posf, in_=posi)
        V.tensor_tensor(out=ang, in0=_bc(posf.unsqueeze(2), [128, NCH, 32]), in1=_bc(invf.unsqueeze(1), [128, NCH, 32]), op=ALU.mult)
        V.tensor_scalar(out=ang, in0=ang, scalar1=float(1.0 / (2 * np.pi)), scalar2=None, op0=ALU.mult)
        V.tensor_scalar(out=ang2, in0=ang, scalar1=0.25, scalar2=None, op0=ALU.add)
        r = None
        for a in (ang, ang2):
            V.tensor_copy(out=ti, in_=a)
            V.tensor_copy(out=tf, in_=ti)
            V.tensor_tensor(out=a, in0=a, in1=tf, op=ALU.subtract)
            V.tensor_scalar(out=tf, in0=a, scalar1=0.5, scalar2=None, op0=ALU.is_gt)
            V.tensor_tensor(out=a, in0=a, in1=tf, op=ALU.subtract)
            V.tensor_scalar(out=tf, in0=a, scalar1=-0.5, scalar2=None, op0=ALU.is_lt)
            r = V.tensor_tensor(out=a, in0=a, in1=tf, op=ALU.add)
        return r
    P.op("dve", rot_tables, R=["posi", "cst"], W=["ang"])

    def rot_sin():
        S.activation(out=sinT, in_=ang, func=AF.Sin, scale=float(2 * np.pi))
        return S.activation(out=cosT, in_=ang2, func=AF.Sin, scale=float(2 * np.pi))
    P.op("act", rot_sin, R=["ang"], W=["rot"])

    if True:
        eoh = ar.alloc([128, 256, 32])
        etmp = ar.alloc([128, 256, 32])
        P.dma(sp, eoh.rearrange("p a b -> p (a b)"), eoh_d, W=["eoh"])
        rb3 = relb.rearrange("p (b h) -> p b h", h=4)
        for h in range(4):
            P.op("pool", lambda h=h: G.tensor_tensor(out=etmp, in0=eoh, in1=_bc(rb3[:, :, h].unsqueeze(1), [128, 256, 32]), op=ALU.mult),
                 R=["eoh", "relb"], W=["etmp"])

            def red(h=h):
                V.tensor_reduce(out=biasw[:, h, :], in_=etmp, axis=AX.X, op=ALU.add)
                return V.tensor_tensor(out=biasw[:, h, :], in0=biasw[:, h, :], in1=madd, op=ALU.add)
            P.op("dve", red, R=["etmp", "cst"], W=["biasw"])
    P.barrier()

    def layer(L):
        moe = (L % 2 == 1)
        last = (L == DEPTH - 1)
        w_in_d, w_out_d = w_in_all[L], w_out_all[L]
        wg_d, wu_d, wd_d = (wg1_d, wu1_d, wd1_d) if moe else (wg0_d, wu0_d, wd0_d)
        LIM = AW if moe else TAIL
        P.barrier()
        P.dma(sp, misc[:], misc_all[L], W=["misc", "der", "mod"])
        P.dma(sp, rowp[:], rowp_all[L].partition_broadcast(128), W=["rowp"])

        P.op("act", lambda: S.activation(out=alog, in_=alog, func=AF.Exp), R=["rowp"], W=["rowp"])
        P.op("dve", lambda: V.tensor_scalar(out=alog, in0=alog, scalar1=-1.0, scalar2=None, op0=ALU.mult), R=["rowp"], W=["rowp"])
        negA = alog
        dbg("rowp", rowp[:, 0:32], ["rowp"])
        dbg("modT", modT[:, 0:48], ["mod"])

        ar = Arena(arena_t, TAIL)
        wada_d = wada_all[L:L + 1]
        wada_sb = [ar.alloc([128, 8, 512]) for _ in range(2)]
        bank_mod, kmod = pb()
        nl = 1
        j = 0
        for li in range(nl):
            for cg in range(12):
                buf = wada_sb[j % 2]
                bk = "wada%d" % (j % 2)
                P.dma(sp, buf, wada_d[li].rearrange("(c p) n -> p c n", p=128)[:, :, cg * 512:(cg + 1) * 512], W=[bk])
                for cc in range(4):
                    col = li * 48 + cg * 4 + cc

                    def mm(buf=buf, cc=cc, col=col):
                        r = None
                        for kc in range(8):
                            r = T.matmul(bank_mod[:, col:col + 1], lhsT=buf[:, kc, cc * 128:(cc + 1) * 128],
                                         rhs=ccol[:, kc:kc + 1], start=(kc == 0), stop=(kc == 7))
                        return r
                    P.op("pe", mm, R=[bk, "misc"], W=[kmod])
                j += 1
        P.op("dve", lambda: V.tensor_tensor(out=modT[:, 0:48 * nl], in0=bank_mod[:, 0:48 * nl], in1=badaT[:, 0:48 * nl], op=ALU.add),
             R=[kmod, "misc"], W=["mod"])
        lg1, lb1, lg2, lb2 = lncol[:, 0:8], lncol[:, 8:16], lncol[:, 16:24], lncol[:, 24:32]
        GA1, BA1 = dcol[:, 0:8], dcol[:, 8:16]
        A2, B2 = dcol[:, 16:24], dcol[:, 24:32]
        GA2, BA2 = dcol[:, 32:40], dcol[:, 40:48]
        tmpc = dcol[:, 48:56]

        def der():
            V.tensor_scalar(out=A_in, in0=modT[:, 8:16], scalar1=1.0, scalar2=1.0 / ALPHA, op0=ALU.add, op1=ALU.mult)
            V.tensor_copy(out=B_in, in_=modT[:, 0:8])
            V.tensor_scalar(out=g1a, in0=modT[:, 16:24], scalar1=1.0, scalar2=None, op0=ALU.add)
            V.tensor_scalar(out=g1f, in0=modT[:, 40:48], scalar1=1.0, scalar2=None, op0=ALU.add)
            V.tensor_scalar(out=GA1, in0=lg1, scalar1=ALPHA, scalar2=None, op0=ALU.mult)
            V.tensor_scalar(out=BA1, in0=lb1, scalar1=ALPHA, scalar2=None, op0=ALU.mult)
            V.tensor_scalar(out=A2, in0=modT[:, 32:40], scalar1=1.0, scalar2=1.0 / ALPHA, op0=ALU.add, op1=ALU.mult)
            V.tensor_copy(out=B2, in_=modT[:, 24:32])
            sc = 1.0 if last else ALPHA
            V.tensor_scalar(out=GA2, in0=lg2, scalar1=sc, scalar2=None, op0=ALU.mult)
            return V.tensor_scalar(out=BA2, in0=lb2, scalar1=sc, scalar2=None, op0=ALU.mult)
        P.op("dve", der, R=["mod", "misc"], W=["der"])
        P.barrier()

        if L == 0:
            ar = Arena(arena_t, TAIL)
            hT_halo = ar.alloc([128, 8, 128], BF16)
            _mark = ar.off
            xtok = [ar.alloc([128, D]) for _ in range(2)]
            for n in range(-1, NCH):
                buf = xtok[n % 2]
                bk = "xtok%d" % (n % 2)
                P.dma(sp, buf, x_d[(n + 1) * 128:(n + 2) * 128, :], W=[bk])
                for half in range(2):
                    bank, bkey = pb()

                    def tr(buf=buf, half=half, bank=bank):
                        r = None
                        for q in range(4):
                            fc = half * 4 + q
                            r = T.transpose(bank[:, q * 128:(q + 1) * 128], buf[:, fc * 128:(fc + 1) * 128], ident)
                        return r
                    P.op("pe", tr, R=[bk, "cst"], W=[bkey])
                    if n >= 0:
                        P.op("act", lambda n=n, half=half, bank=bank: S.mul(out=xT[:, half * 4:half * 4 + 4, n * 128:(n + 1) * 128],
                                                                           in_=bank[:].rearrange("p (q t) -> p q t", q=4), mul=ALPHA),
                             R=[bkey], W=[("xT", n, half)])
                    else:
                        def hh(half=half, bank=bank):
                            r = None
                            for q in range(4):
                                fc = half * 4 + q
                                r = V.tensor_scalar(out=hT_halo[:, fc, :], in0=bank[:, q * 128:(q + 1) * 128],
                                                    scalar1=A_in[:, fc:fc + 1], scalar2=B_in[:, fc:fc + 1], op0=ALU.mult, op1=ALU.add)
                            return r
                        def hh2(half=half, bank=bank):
                            r = None
                            for q in range(4):
                                fc = half * 4 + q
                                V.tensor_scalar(out=tmpc[:, 0:1], in0=A_in[:, fc:fc + 1], scalar1=ALPHA, scalar2=None, op0=ALU.mult)
                                r = V.tensor_scalar(out=hT_halo[:, fc, :], in0=bank[:, q * 128:(q + 1) * 128],
                                                    scalar1=tmpc[:, 0:1], scalar2=B_in[:, fc:fc + 1], op0=ALU.mult, op1=ALU.add)
                            return r
                        P.op("dve", hh2, R=[bkey, "misc", "der"], W=["hT_halo", "der"])

        else:
            ar = Arena(arena_t, TAIL)
            hT_halo = ar.alloc([128, 8, 128], BF16)
            _mark = ar.off
            halo_g = ar.alloc([128, NR, D])
            hacc = ar.alloc([128, 8, 128])
            P.dma(sp, bounce_h.rearrange("p (c t) -> p c t", c=8), xT[:, :, NTOK - 128:NTOK], R=[("xT", NCH - 1, 0), ("xT", NCH - 1, 1)], W=["bounce_h"])
            P.coll("AllGather", gath_h, bounce_h, ALLC, R=["bounce_h"], W=["gath_h"])
            P.dma(sp, halo_g, gath_h.rearrange("(r p) w -> p r w", p=128), R=["gath_h"], W=["halo_g"])

            def hsel():
                hf_ = hacc.rearrange("p c t -> p (c t)")
                V.tensor_scalar(out=hf_, in0=halo_g[:, 0, :], scalar1=selw[:, 0:1], scalar2=None, op0=ALU.mult)
                for r_ in range(1, NR):
                    V.scalar_tensor_tensor(out=hf_, in0=halo_g[:, r_, :], scalar=selw[:, r_:r_ + 1], in1=hf_, op0=ALU.mult, op1=ALU.add)
                r = None
                for fc in range(8):
                    r = V.tensor_scalar(out=hT_halo[:, fc, :], in0=hacc[:, fc, :], scalar1=A_in[:, fc:fc + 1], scalar2=B_in[:, fc:fc + 1],
                                        op0=ALU.mult, op1=ALU.add)
                return r
            P.op("dve", hsel, R=["halo_g", "selw", "misc", "der"], W=["hT_halo", "hacc"])

        P.barrier()
        ar.off = _mark
        w_in = ar.alloc([128, 8, IN_DIM], BF16)
        wcols = "w_in_d"
        wi_src = w_in_d.rearrange("(c p) n -> p c n", p=128)
        for a, b in ((0, 1024), (1024, 2048), (2048, 2312), (2568, 2824)):
            P.dma("pool", w_in[:, :, a:b], wi_src[:, :, a:b], W=["w_in"])
        for slot, h in enumerate((0, 2, 1, 3)):
            P.dma("pool", w_in[:, :, 2312 + slot * 64:2312 + (slot + 1) * 64], wi_src[:, :, 2312 + h * 64:2312 + (h + 1) * 64], W=["w_in"])
        if True:
            w_out = ar.alloc([128, 8, D], BF16)
            P.dma("pool", w_out, w_out_d.rearrange("(c p) n -> p c n", p=128), W=["w_out"])

        Sret = ar.alloc([128, 2, 64])
        Sret_b = ar.alloc([128, 2, 64], BF16)
        Sssd = ar.alloc([128, 256])
        Sssd_b = ar.alloc([128, 256], BF16)
        totacc = ar.alloc([128, 8])
        def zinit():
            V.memset(Sret, 0.0)
            V.memset(Sssd, 0.0)
            return V.memset(totacc, 0.0)
        P.op("dve", zinit, W=["Sret", "Sssd", "totacc"])

        _off_tmp = ar.off
        hTc = [ar.alloc([128, 8, 128], BF16) for _ in range(2)]
        qk_sb = ar.alloc([128, 512])
        qr = ar.alloc([128, 4, 2, 32])
        kr = ar.alloc([128, 4, 2, 32])
        rt = [ar.alloc([128, 4, 32]) for _ in range(4)]
        q2b = ar.alloc([128, 256], BF16)
        k2b = ar.alloc([128, 256], BF16)
        v_b = ar.alloc([128, 256], BF16)
        sg = ar.alloc([128, 256])
        qkT = ar.alloc([128, 512], BF16)
        qm = ar.alloc([128, 4, 128], BF16)
        BCm = ar.alloc([128, 4, 128], BF16)
        sTm = ar.alloc([128, 512], BF16)
        osq = qr.rearrange("p h two f -> p h (two f)")
        onr = kr.rearrange("p h two f -> p h (two f)")
        gst = ar.alloc([128, 16])
        xr = ar.alloc([128, 6, 131])
        acc = ar.alloc([128, 6, 128])
        bc_b = ar.alloc([128, 2, 128], BF16)
        Bm_b = ar.alloc([128, 128], BF16)
        amask = ar.alloc([128, 8, 128])
        eseg = ar.alloc([128, 8, 128], BF16)
        mT = ar.alloc([128, 8, 128], BF16)
        cbm = ar.alloc([128, 2, 128])
        xdt_b = ar.alloc([128, 8, 64], BF16)
        xdd_b = ar.alloc([128, 8, 64], BF16)
        xskip = ar.alloc([128, 8, 64])
        t1 = ar.alloc([128, 8, 64])
        szs = ar.alloc([128, 512])
        hsq = ar.alloc([128, 512])
        sm = ar.alloc([128, 64])
        qT_s = ar.alloc([128, 4, 128], BF16)
        kT_pp = [ar.alloc([128, 128], BF16) for _ in range(2)]
        v_pp = [ar.alloc([128, 128], BF16) for _ in range(2)]
        sl = amask.rearrange("p h l -> p (h l)").rearrange("p (h j) -> p h j", h=4)
        p_b = ar.alloc([128, 4, 256], BF16)
        pT_b = ar.alloc([128, 8, 128], BF16)
        ssw = ar.alloc([128, 32])
        mix_tok = ar.alloc([128, D], BF16)
        mixT = ar.alloc([128, 8, 128], BF16)
        sq = [ar.alloc([128, 128]) for _ in range(2)]
        lnst = t1.rearrange("p h d -> p (h d)").rearrange("p (a b) -> p a b", a=4)
        lnk = ["t1"]
        mixer_arena_end = ar.off

        def zmask():
            V.memset(qm, 0.0)
            V.memset(BCm, 0.0)
            return V.memset(qT_s, 0.0)


        dtv, av_, acs_tot, ed, eaed, cdec, dd = (sm[:, 0:8], sm[:, 8:16], sm[:, 16:32], sm[:, 32:48], sm[:, 48:64], None, None)

        sm2 = ar.alloc([128, 32])
        cdec = sm2[:, 0:8]
        dd = sm2[:, 8:16]
        rr = sm2[:, 16:24]

        def ln_inplace(n_cols, xs_keyR, xview, GA, BA, nfree):
            bank, bkey = pb()
            bank2, bkey2 = (bank[:, nfree:2 * nfree], bkey) if nfree <= 256 else pb()
            if nfree > 256:
                bank2 = bank2[:, 0:nfree]
            for fc in range(8):
                sqb = sq[fc % 2]
                sk = "sq%d" % (fc % 2)
                P.op("pool", lambda fc=fc, sqb=sqb: G.tensor_tensor(out=sqb[:, 0:nfree], in0=xview[:, fc, :], in1=xview[:, fc, :], op=ALU.mult),
                     R=xs_keyR, W=[sk])

                def mm(fc=fc, sqb=sqb, bank=bank):
                    T.matmul(bank[:, 0:nfree], lhsT=ones, rhs=xview[:, fc, :], start=(fc == 0), stop=(fc == 7))
                    return T.matmul(bank2, lhsT=ones, rhs=sqb[:, 0:nfree], start=(fc == 0), stop=(fc == 7))
                P.op("pe", mm, R=xs_keyR + [sk, "cst"], W=[bkey, bkey2])
            mean, msq, var, rstd = lnst[:, 0, 0:nfree], lnst[:, 1, 0:nfree], lnst[:, 2, 0:nfree], lnst[:, 3, 0:nfree]

            def st(bank=bank):
                V.tensor_scalar(out=mean, in0=bank[:, 0:nfree], scalar1=1.0 / D, scalar2=None, op0=ALU.mult)
                V.tensor_tensor(out=msq, in0=mean, in1=mean, op=ALU.mult)
                V.scalar_tensor_tensor(out=var, in0=bank2, scalar=1.0 / D, in1=msq, op0=ALU.mult, op1=ALU.subtract)
                return V.tensor_scalar(out=var, in0=var, scalar1=EPS, scalar2=None, op0=ALU.add)
            P.op("dve", st, R=[bkey, bkey2], W=[lnk[0]])
            P.op("act", lambda: S.sqrt(out=rstd, in_=var), R=[lnk[0]], W=[lnk[0]])

            def nrm():
                V.reciprocal(out=rstd, in_=rstd)
                V.tensor_tensor(out=xview, in0=xview, in1=_bc(mean.unsqueeze(1), [128, 8, nfree]), op=ALU.subtract)
                return V.tensor_tensor(out=xview, in0=xview, in1=_bc(rstd.unsqueeze(1), [128, 8, nfree]), op=ALU.mult)
            P.op("dve", nrm, R=[lnk[0]] + xs_keyR, W=xs_keyR + [lnk[0]])

            def aff():
                G.tensor_tensor(out=xview, in0=xview, in1=_bc(GA.unsqueeze(2), [128, 8, nfree]), op=ALU.mult)
                return G.tensor_tensor(out=xview, in0=xview, in1=_bc(BA.unsqueeze(2), [128, 8, nfree]), op=ALU.add)
            P.op("pool", aff, R=xs_keyR + ["der", "misc"], W=xs_keyR)

        def chunk(n):
            halo = n < 0
            cur, prv = (n % 2), ((n + 1) % 2)
            if halo:
                hc = hT_halo
                hk = "hT_halo"
            else:
                hc = hTc[n % 2]
                hk = "hTc%d" % (n % 2)
                xk = [("xT", n, 0), ("xT", n, 1)]
                Tn = slice(n * 128, (n + 1) * 128)

                def mkh2():
                    r = None
                    for fc in range(8):
                        r = G.tensor_scalar(out=hc[:, fc, :], in0=xT[:, fc, Tn], scalar1=A_in[:, fc:fc + 1], scalar2=B_in[:, fc:fc + 1],
                                            op0=ALU.mult, op1=ALU.add)
                    return r
                P.op("pool", mkh2, R=xk + ["misc", "der"], W=[hk])

            def proj_tok(bank, c0, c1, o0=0):
                def f():
                    r = None
                    for kc in range(8):
                        r = T.matmul(bank[:, o0:o0 + (c1 - c0)], lhsT=hc[:, kc, :], rhs=w_in[:, kc, c0:c1], start=(kc == 0), stop=(kc == 7))
                    return r
                return f

            def proj_feat(bank, c0, o0):
                def f():
                    r = None
                    for kc in range(8):
                        r = T.matmul(bank[:, o0:o0 + 128], lhsT=w_in[:, kc, c0:c0 + 128], rhs=hc[:, kc, :], start=(kc == 0), stop=(kc == 7))
                    return r
                return f

            bD, kD = pb()
            bE, kE = pb()
            bF, kF = pb()
            if full:
                P.op("pe", proj_tok(bD, 2696, 2824, 8), R=[hk, "w_in"], W=[kD])
                P.op("pe", proj_feat(bD, 2568, 256), R=[hk, "w_in"], W=[kD])
            for c in range(4):
                P.op("pe", proj_feat(bE, 1536 + c * 128, c * 128), R=[hk, "w_in"], W=[kE])
            P.op("pe", proj_feat(bF, 2048, 0), R=[hk, "w_in"], W=[kF])
            P.op("pe", proj_feat(bF, 2176, 128), R=[hk, "w_in"], W=[kF])
            if halo:
                def tail():
                    V.tensor_scalar(out=xr[:, 0:4, 128:131], in0=bE[:].rearrange("p (c t) -> p c t", c=4)[:, :, 125:128],
                                    scalar1=halovalid, scalar2=None, op0=ALU.mult)
                    return V.tensor_scalar(out=xr[:, 4:6, 128:131], in0=bF[:, 0:256].rearrange("p (c t) -> p c t", c=2)[:, :, 125:128],
                                           scalar1=halovalid, scalar2=None, op0=ALU.mult)
                P.op("dve", tail, R=[kE, kF, "misc"], W=["xr"])
                if full:
                    def kv():
                        S.copy(out=kT_pp[cur], in_=bD[:, 256:384])
                        return S.copy(out=v_pp[cur], in_=bD[:, 8:136])
                    P.op("act", kv, R=[kD], W=["kT_pp%d" % cur, "v_pp%d" % cur])
                return
            P.op("pe", proj_tok(bD, 2304, 2312, 0), R=[hk, "w_in"], W=[kD])
            bA, kA = pb()
            bB, kB = pb()
            if full:
                P.op("pe", proj_tok(bA, 0, 512), R=[hk, "w_in"], W=[kA])
                P.op("pe", proj_tok(bB, 512, 1024), R=[hk, "w_in"], W=[kB])
                bC, kC = pb()
                P.op("pe", proj_tok(bC, 1024, 1536), R=[hk, "w_in"], W=[kC])
                P.op("pe", proj_feat(bF, 2312, 256), R=[hk, "w_in"], W=[kF])
                P.op("pe", proj_feat(bF, 2440, 384), R=[hk, "w_in"], W=[kF])
            else:
                P.op("pe", proj_tok(bA, 256, 512, 256), R=[hk, "w_in"], W=[kA])
                P.op("pe", proj_tok(bB, 512, 768, 0), R=[hk, "w_in"], W=[kB])

            P.op("dve", lambda: V.tensor_tensor(out=dtv, in0=bD[:, 0:8], in1=dtb, op=ALU.add), R=[kD, "rowp"], W=["dtv"])
            P.op("pool", lambda: G.tensor_copy(out=xr[:, :, 0:3], in_=xr[:, :, 128:131]), R=["xr"], W=["xr"])

            def xrcp():
                S.copy(out=xr[:, 0:4, 3:131], in_=bE[:].rearrange("p (c t) -> p c t", c=4))
                return S.copy(out=xr[:, 4:6, 3:131], in_=bF[:, 0:256].rearrange("p (c t) -> p c t", c=2))
            P.op("act", xrcp, R=[kE, kF], W=["xr"])
            P.op("act", lambda: S.copy(out=qk_sb[:, (0 if full else 256):512], in_=bA[:, (0 if full else 256):512]), R=[kA], W=["qk_sb"])
            P.op("act", lambda: S.copy(out=v_b, in_=bB[:, 0:256]), R=[kB], W=["v_b"])
            if full:
                def swc():
                    for h_ in range(4):
                        kvh_, gq_ = h_ // 2, h_ % 2
                        pr_ = slice(kvh_ * 64, kvh_ * 64 + 64)
                        S.copy(out=qT_s[pr_, h_, :], in_=bF[pr_, 256 + gq_ * 128:256 + (gq_ + 1) * 128])
                    S.copy(out=kT_pp[cur], in_=bD[:, 256:384])
                    return S.copy(out=v_pp[cur], in_=bD[:, 8:136])
                P.op("act", swc, R=[kF, kD], W=["qT_s", "kT_pp%d" % cur, "v_pp%d" % cur])
                P.op("act", lambda: S.activation(out=sg, in_=bB[:, 256:512], func=AF.Silu), R=[kB], W=["sg"])
                P.op("act", lambda: S.activation(out=szs, in_=bC[:], func=AF.Silu), R=[kC], W=["szs"])
            if SUB[0] < 1:
                return
            cosb = _bc(cosT[:, n, :].unsqueeze(1), [128, 4, 32])
            sinb = _bc(sinT[:, n, :].unsqueeze(1), [128, 4, 32])

            def rotary(E, src, dst, ta, tb):
                X = src.rearrange("p (h two f) -> p h two f", h=4, two=2)
                x1, x2 = X[:, :, 0, :], X[:, :, 1, :]
                E.tensor_tensor(out=ta, in0=x1, in1=cosb, op=ALU.mult)
                E.tensor_tensor(out=tb, in0=x2, in1=sinb, op=ALU.mult)
                E.tensor_tensor(out=dst[:, :, 0, :], in0=ta, in1=tb, op=ALU.subtract)
                E.tensor_tensor(out=ta, in0=x1, in1=sinb, op=ALU.mult)
                E.tensor_tensor(out=tb, in0=x2, in1=cosb, op=ALU.mult)
                return E.tensor_tensor(out=dst[:, :, 1, :], in0=ta, in1=tb, op=ALU.add)

            def krot():
                rotary(V, qk_sb[:, 256:512], kr, rt[2], rt[3])
                return V.tensor_tensor(out=k2b[:].rearrange("p (h d) -> p h d", h=4), in0=_bc(dk.unsqueeze(2), [128, 4, 64]),
                                       in1=kr.rearrange("p h two f -> p h (two f)"), op=ALU.mult)
            P.op("dve", krot, R=["qk_sb", "rot", "cst"], W=["kr", "k2b"])
            if full:
                def qrot():
                    rotary(V, qk_sb[:, 0:256], qr, rt[0], rt[1])
                    return V.tensor_tensor(out=q2b[:].rearrange("p (h d) -> p h d", h=4), in0=_bc(dq.unsqueeze(2), [128, 4, 64]),
                                           in1=qr.rearrange("p h two f -> p h (two f)"), op=ALU.mult)
                P.op("dve", qrot, R=["qk_sb", "rot", "cst"], W=["qr", "q2b"])
                if SUB[0] < 1.05:
                    return
                bT, kT_ = pb()
                bTb = bT[:].bitcast(BF16)

                def trqk():
                    r = None
                    for t in range(2):
                        T.transpose(bTb[:, t * 128:(t + 1) * 128], q2b[:, t * 128:(t + 1) * 128], ident_b[:])
                        r = T.transpose(bTb[:, 256 + t * 128:256 + (t + 1) * 128], k2b[:, t * 128:(t + 1) * 128], ident_b[:])
                    return r
                P.op("pe", trqk, R=["q2b", "k2b", "ident_b"], W=[kT_])
                def qkcp():
                    for h_ in range(4):
                        t_, hf2 = h_ // 2, h_ % 2
                        pr_ = slice(hf2 * 64, hf2 * 64 + 64)
                        S.copy(out=qm[pr_, h_, :], in_=bTb[pr_, t_ * 128:(t_ + 1) * 128])
                    return S.copy(out=qkT[:, 256:512], in_=bTb[:, 256:512])
                P.op("act", qkcp, R=[kT_], W=["qkT", "qm"])
                if SUB[0] < 1.1:
                    return
                bS, kS = pb()

                def scores():
                    r = None
                    import os
                    for h in [int(c_) for c_ in os.environ.get("SUBH", "0123")]:
                        t, hf_ = h // 2, h % 2
                        pr = slice(hf_ * 64, hf_ * 64 + 64)
                        r = T.matmul(bS[:, h * 128:(h + 1) * 128], lhsT=qkT[:, 256 + t * 128:256 + (t + 1) * 128],
                                     rhs=qm[:, h, :], start=True, stop=True)
                    return r
                P.op("pe", scores, R=["qkT", "qm"], W=[kS])
                if SUB[0] < 1.15:
                    return
                P.op("dve", lambda: V.tensor_tensor(out=sTm[:].rearrange("p (h i) -> p h i", h=4), in0=_bc(tri.unsqueeze(1), [128, 4, 128]),
                                                    in1=bS[:].rearrange("p (h i) -> p h i", h=4), op=ALU.mult), R=[kS, "cst"], W=["sTm"])
                if SUB[0] < 1.2:
                    return
                bO, kO = pb()

                def oret():
                    r = None
                    for h in range(4):
                        t, hf_ = h // 2, h % 2
                        pr = slice(hf_ * 64, hf_ * 64 + 64)
                        T.matmul(bO[:, h * 64:(h + 1) * 64], lhsT=sTm[:, h * 128:(h + 1) * 128], rhs=v_b[:, h * 64:(h + 1) * 64], start=True, stop=False)
                        r = T.matmul(bO[:, h * 64:(h + 1) * 64], lhsT=qm[:, h, :], rhs=Sret_b[:, t, :], start=False, stop=True)
                    return r
                P.op("pe", oret, R=["sTm", "v_b", "qm", "Sret_b"], W=[kO])
            if SUB[0] < 1.3:
                return
            bK, kK = pb()

            def kvm():
                r = None
                for t in range(2):
                    r = T.matmul(bK[:, t * 128:(t + 1) * 128], lhsT=k2b[:, t * 128:(t + 1) * 128], rhs=v_b[:, t * 128:(t + 1) * 128], start=True, stop=True)
                return r
            P.op("pe", kvm, R=["k2b", "v_b"], W=[kK])

            if SUB[0] < 1.6:
                return

            def supd():
                K4 = bK[:, 0:256].rearrange("p (t hf e) -> p t hf e", t=2, hf=2)
                V.scalar_tensor_tensor(out=Sret, in0=K4[:, :, 0, :], scalar=m0, in1=Sret, op0=ALU.mult, op1=ALU.add)
                V.scalar_tensor_tensor(out=Sret, in0=K4[:, :, 1, :], scalar=m1, in1=Sret, op0=ALU.mult, op1=ALU.add)
                V.tensor_tensor(out=Sret, in0=Sret, in1=_bc(gC.unsqueeze(2), [128, 2, 64]), op=ALU.mult)
                return V.tensor_copy(out=Sret_b, in_=Sret)
            P.op("dve", supd, R=[kK, "Sret", "cst"], W=["Sret", "Sret_b"])
            if full:
                P.op("act", lambda: S.activation(out=osq, in_=bO[:, 0:256].rearrange("p (h d) -> p h d", h=4), func=AF.Square), R=[kO], W=["qr"])

                def gn1():
                    V.tensor_reduce(out=gst[:, 0:4], in_=bO[:, 0:256].rearrange("p (h d) -> p h d", h=4), axis=AX.X, op=ALU.add)
                    V.tensor_reduce(out=gst[:, 4:8], in_=osq, axis=AX.X, op=ALU.add)
                    V.tensor_scalar(out=gst[:, 0:4], in0=gst[:, 0:4], scalar1=1.0 / 64, scalar2=None, op0=ALU.mult)
                    V.tensor_tensor(out=gst[:, 8:12], in0=gst[:, 0:4], in1=gst[:, 0:4], op=ALU.mult)
                    V.scalar_tensor_tensor(out=gst[:, 4:8], in0=gst[:, 4:8], scalar=1.0 / 64, in1=gst[:, 8:12], op0=ALU.mult, op1=ALU.subtract)
                    return V.tensor_scalar(out=gst[:, 4:8], in0=gst[:, 4:8], scalar1=EPS, scalar2=None, op0=ALU.add)
                P.op("dve", gn1, R=[kO, "qr"], W=["gst"])
                P.op("act", lambda: S.sqrt(out=gst[:, 4:8], in_=gst[:, 4:8]), R=["gst"], W=["gst"])

                def gn2():
                    V.reciprocal(out=gst[:, 4:8], in_=gst[:, 4:8])
                    V.tensor_tensor(out=onr, in0=bO[:, 0:256].rearrange("p (h d) -> p h d", h=4), in1=_bc(gst[:, 0:4].unsqueeze(2), [128, 4, 64]), op=ALU.subtract)
                    V.tensor_tensor(out=onr, in0=onr, in1=_bc(gst[:, 4:8].unsqueeze(2), [128, 4, 64]), op=ALU.mult)
                    return V.tensor_tensor(out=mix_tok[:, 0:256], in0=onr.rearrange("p h d -> p (h d)"), in1=sg, op=ALU.mult)
                P.op("dve", gn2, R=[kO, "gst", "sg"], W=["kr", "gst", "mix_ret"])

            if SUB[0] < 2:
                return

            def conv(E, cs):
                def f():
                    r = None
                    for c in cs:
                        E.tensor_scalar(out=acc[:, c, :], in0=xr[:, c, 0:128], scalar1=convw[:, c * 4:c * 4 + 1], scalar2=convb[:, c:c + 1],
                                        op0=ALU.mult, op1=ALU.add)
                        for w in range(1, 4):
                            r = E.scalar_tensor_tensor(out=acc[:, c, :], in0=xr[:, c, w:w + 128], scalar=convw[:, c * 4 + w:c * 4 + w + 1],
                                                       in1=acc[:, c, :], op0=ALU.mult, op1=ALU.add)
                    return r
                return f
            P.op("dve", conv(V, (0, 1, 4)), R=["xr", "misc"], W=["accA"])
            P.op("dve", conv(V, (2, 3, 5)), R=["xr", "misc"], W=["accB"])

            def sil():
                S.activation(out=acc[:, 0:4, :], in_=acc[:, 0:4, :], func=AF.Silu)
                return S.activation(out=bc_b, in_=acc[:, 4:6, :], func=AF.Silu)
            P.op("act", sil, R=["accA", "accB"], W=["accA", "accB", "bc_b"])
            if full:
                def bcm():
                    r = None
                    for g_ in range(2):
                        pr_ = slice(g_ * 64, g_ * 64 + 64)
                        G.tensor_copy(out=BCm[pr_, g_, :], in_=bc_b[pr_, 0, :])
                        r = G.tensor_copy(out=BCm[pr_, 2 + g_, :], in_=bc_b[pr_, 1, :])
                    return r
                P.op("pool", bcm, R=["bc_b"], W=["BCm"])
            bX, kX = pb()

            def trx():
                r = None
                for c in range(4):
                    r = T.transpose(bX[:, c * 128:(c + 1) * 128], acc[:, c, :], ident)
                return r
            P.op("pe", trx, R=["accA", "accB", "cst"], W=[kX])
            bBm, kBm = pb()
            bBmb = bBm[:].bitcast(BF16)
            P.op("pe", lambda: T.transpose(bBmb[:, 0:128], bc_b[:, 0, :], ident_b[:]), R=["bc_b", "ident_b"], W=[kBm])
            P.op("act", lambda: S.copy(out=Bm_b, in_=bBmb[:, 0:128]), R=[kBm], W=["Bm_b"])
            if SUB[0] < 3:
                return
            def sp_():
                S.activation(out=dtv, in_=dtv, func=AF.Exp)
                return S.activation(out=dtv, in_=dtv, func=AF.Ln, bias=1.0)
            if n == 0:
                dbg("dtv_pre", dtv, ["dtv"])
            P.op("act", sp_, R=["dtv"], W=["dtv"])
            if n == 0:
                dbg("dtv", dtv, ["dtv"])
            P.op("dve", lambda: V.tensor_tensor(out=av_, in0=dtv, in1=negA, op=ALU.mult), R=["dtv", "rowp"], W=["av"])
            bY, kY = pb()

            def acsm():
                T.matmul(bY[:, 0:8], lhsT=tri, rhs=av_, start=True, stop=True)
                return T.matmul(bY[:, 8:16], lhsT=ones, rhs=av_, start=True, stop=True)
            P.op("pe", acsm, R=["av", "cst"], W=[kY])
            P.op("act", lambda: S.copy(out=acs_tot, in_=bY[:, 0:16]), R=[kY], W=["acs_tot"])
            if n == 0:
                dbg("av", av_, ["av"])
                dbg("acs_tot", acs_tot, ["acs_tot"])

            def edf():
                V.tensor_copy(out=ed[:, 0:8], in_=acs_tot[:, 0:8])
                return V.tensor_tensor(out=ed[:, 8:16], in0=acs_tot[:, 8:16], in1=acs_tot[:, 0:8], op=ALU.subtract)
            P.op("dve", edf, R=["acs_tot"], W=["ed"])

            def exps():
                S.activation(out=eaed, in_=ed, func=AF.Exp)
                return S.activation(out=cdec, in_=acs_tot[:, 8:16], func=AF.Exp)
            P.op("act", exps, R=["ed", "acs_tot"], W=["eaed", "cdec"])
            if not full:
                P.op("pool", lambda: G.tensor_tensor(out=totacc, in0=totacc, in1=acs_tot[:, 8:16], op=ALU.add), R=["acs_tot", "totacc"], W=["totacc"])
            P.op("dve", lambda: V.tensor_tensor(out=dd, in0=dtv, in1=eaed[:, 8:16], op=ALU.mult), R=["dtv", "eaed"], W=["dd"])
            X3 = bX[:].rearrange("p (h d) -> p h d", h=8)
            P.op("dve", lambda: V.tensor_tensor(out=xdd_b, in0=_bc(dd.unsqueeze(2), [128, 8, 64]), in1=X3, op=ALU.mult), R=[kX, "dd"], W=["xdd_b"])
            if full:
                def xd():
                    V.tensor_tensor(out=xdt_b, in0=_bc(dtv.unsqueeze(2), [128, 8, 64]), in1=X3, op=ALU.mult)
                    return V.tensor_tensor(out=xskip, in0=X3, in1=_bc(dskip.unsqueeze(2), [128, 8, 64]), op=ALU.mult)
                P.op("dve", xd, R=[kX, "dtv", "rowp"], W=["xdt_b", "xskip"])
                P.op("pool", lambda: G.tensor_tensor(out=amask, in0=_bc(mgt.unsqueeze(1), [128, 8, 128]), in1=_bc(av_.unsqueeze(2), [128, 8, 128]), op=ALU.mult),
                     R=["av", "cst"], W=["amask"])
                bCB, kCB = pb()

                def cbm_():
                    r = None
                    for g in range(2):
                        pr = slice(g * 64, g * 64 + 64)
                        r = T.matmul(bCB[:, g * 128:(g + 1) * 128], lhsT=BCm[:, g, :], rhs=bc_b[:, 1, :], start=True, stop=True)
                    return r
                P.op("pe", cbm_, R=["bc_b", "BCm"], W=[kCB])
                P.op("dve", lambda: V.tensor_tensor(out=cbm, in0=bCB[:, 0:256].rearrange("p (g l) -> p g l", g=2), in1=_bc(tri.unsqueeze(1), [128, 2, 128]), op=ALU.mult),
                     R=[kCB, "cst"], W=["cbm"])
                for g in range(2):
                    bSg, kSg = pb()

                    def segm(g=g, bSg=bSg):
                        r = None
                        for r_ in range(4):
                            r = T.matmul(bSg[:, r_ * 128:(r_ + 1) * 128], lhsT=amask[:, g * 4 + r_, :], rhs=tri, start=True, stop=True)
                        return r
                    P.op("pe", segm, R=["amask", "cst"], W=[kSg])
                    P.op("act", lambda g=g, bSg=bSg: S.activation(out=eseg[:, g * 4:g * 4 + 4, :], in_=bSg[:].rearrange("p (r l) -> p r l", r=4), func=AF.Exp),
                         R=[kSg], W=["eseg%d" % g])
                    P.op("dve", lambda g=g: V.tensor_tensor(out=mT[:, g * 4:g * 4 + 4, :], in0=_bc(cbm[:, g, :].unsqueeze(1), [128, 4, 128]),
                                                            in1=eseg[:, g * 4:g * 4 + 4, :], op=ALU.mult),
                         R=["eseg%d" % g, "cbm"], W=["mT%d" % g])
                bYD, kYD = pb()

                def ydm():
                    r = None
                    for h in range(8):
                        r = T.matmul(bYD[:, h * 64:(h + 1) * 64], lhsT=mT[:, h, :], rhs=xdt_b[:, h, :], start=True, stop=True)
                    return r
                P.op("pe", ydm, R=["mT0", "mT1", "xdt_b"], W=[kYD])
                bYO, kYO = pb()

                def yom():
                    r = None
                    for g in range(2):
                        pr = slice(g * 64, g * 64 + 64)
                        r = T.matmul(bYO[:, g * 256:(g + 1) * 256], lhsT=BCm[:, 2 + g, :], rhs=Sssd_b, start=True, stop=True)
                    return r
                P.op("pe", yom, R=["BCm", "Sssd_b"], W=[kYO])
            if SUB[0] < 4:
                return
            bST, kST = pb()
            P.op("pe", lambda: T.matmul(bST[:, 0:512], lhsT=Bm_b, rhs=xdd_b[:].rearrange("p h d -> p (h d)"), start=True, stop=True),
                 R=["Bm_b", "xdd_b"], W=[kST])

            def sssd():
                r = None
                for g in range(2):
                    pr = slice(g * 64, g * 64 + 64)
                    V.tensor_tensor(out=Sssd[pr, :].rearrange("p (r e) -> p r e", r=4), in0=Sssd[pr, :].rearrange("p (r e) -> p r e", r=4),
                                    in1=_bc(cdec[pr, g * 4:g * 4 + 4].unsqueeze(2), [64, 4, 64]), op=ALU.mult)
                    r = V.tensor_tensor(out=Sssd[pr, :], in0=Sssd[pr, :], in1=bST[pr, g * 256:(g + 1) * 256], op=ALU.add)
                return r
            P.op("dve", sssd, R=[kST, "Sssd", "cdec"], W=["Sssd"])
            if not full:
                return
            P.op("act", lambda: S.copy(out=Sssd_b, in_=Sssd), R=["Sssd"], W=["Sssd_b"])

            def ycomb():
                V.tensor_tensor(out=t1, in0=bYO[:].rearrange("p (h d) -> p h d", h=8), in1=_bc(eaed[:, 0:8].unsqueeze(2), [128, 8, 64]), op=ALU.mult)
                return V.tensor_tensor(out=t1, in0=t1, in1=bYD[:].rearrange("p (h d) -> p h d", h=8), op=ALU.add)
            P.op("dve", ycomb, R=[kYO, kYD, "eaed"], W=["t1"])
            t1f = t1.rearrange("p h d -> p (h d)")

            def yg():
                G.tensor_tensor(out=t1f, in0=t1f, in1=xskip.rearrange("p h d -> p (h d)"), op=ALU.add)
                G.tensor_tensor(out=t1f, in0=t1f, in1=szs, op=ALU.mult)
                return G.tensor_tensor(out=hsq, in0=t1f, in1=t1f, op=ALU.mult)
            P.op("pool", yg, R=["t1", "xskip", "szs"], W=["t1", "hsq"])

            def rms1():
                V.tensor_reduce(out=rr[:, 0:2], in_=hsq.rearrange("p (g e) -> p g e", g=2), axis=AX.X, op=ALU.add)
                return V.tensor_scalar(out=rr[:, 0:2], in0=rr[:, 0:2], scalar1=1.0 / 256, scalar2=EPS, op0=ALU.mult, op1=ALU.add)
            P.op("dve", rms1, R=["hsq"], W=["rr"])
            P.op("act", lambda: S.sqrt(out=rr[:, 0:2], in_=rr[:, 0:2]), R=["rr"], W=["rr"])

            def rms2():
                V.reciprocal(out=rr[:, 0:2], in_=rr[:, 0:2])
                V.tensor_tensor(out=hsq.rearrange("p (g e) -> p g e", g=2), in0=t1f.rearrange("p (g e) -> p g e", g=2),
                                in1=_bc(rr[:, 0:2].unsqueeze(2), [128, 2, 256]), op=ALU.mult)
                return V.tensor_tensor(out=mix_tok[:, 256:768], in0=hsq, in1=normw, op=ALU.mult)
            P.op("dve", rms2, R=["rr", "t1", "hsq", "rowp"], W=["hsq", "mix_ssd", "rr"])

            bL = [pb(), pb()]

            def lgm():
                r = None
                for h in range(4):
                    kvh, gq = h // 2, h % 2
                    pr = slice(kvh * 64, kvh * 64 + 64)
                    bank = bL[h // 2][0]
                    for part, buf in ((0, kT_pp[prv]), (1, kT_pp[cur])):
                        o = (h % 2) * 256 + part * 128
                        r = T.matmul(bank[:, o:o + 128], lhsT=qT_s[:, h, :], rhs=buf, start=True, stop=True)
                return r
            P.op("pe", lgm, R=["qT_s", "kT_pp0", "kT_pp1"], W=[bL[0][1], bL[1][1]])

            def sls():
                r = None
                for hb in range(2):
                    r = V.scalar_tensor_tensor(out=sl[:, hb * 2:hb * 2 + 2, :], in0=bL[hb][0][:].rearrange("p (h j) -> p h j", h=2), scalar=0.125,
                                               in1=biasw[:, hb * 2:hb * 2 + 2, :], op0=ALU.mult, op1=ALU.add)
                if n == 0:
                    r = V.tensor_scalar(out=sl[:, :, 0:128], in0=sl[:, :, 0:128], scalar1=halomask, scalar2=None, op0=ALU.add)
                V.tensor_reduce(out=ssw[:, 0:4], in_=sl, axis=AX.X, op=ALU.max)
                V.tensor_tensor(out=ssw[:, 0:4], in0=ssw[:, 0:4], in1=sinks, op=ALU.max)
                V.tensor_scalar(out=ssw[:, 4:8], in0=ssw[:, 0:4], scalar1=-1.0, scalar2=None, op0=ALU.mult)
                return V.tensor_tensor(out=ssw[:, 8:12], in0=sinks, in1=ssw[:, 4:8], op=ALU.add)
            P.op("dve", sls, R=[bL[0][1], bL[1][1], "biasw", "misc", "rowp"], W=["amask", "ssw"])

            def pex():
                r = None
                for h in range(4):
                    r = S.activation(out=p_b[:, h, :], in_=sl[:, h, :], func=AF.Exp, bias=ssw[:, 4 + h:5 + h], scale=1.0)
                return S.activation(out=ssw[:, 12:16], in_=ssw[:, 8:12], func=AF.Exp)
            P.op("act", pex, R=["amask", "ssw"], W=["p_b", "ssw2"])

            def den():
                V.tensor_reduce(out=ssw[:, 16:20], in_=p_b, axis=AX.X, op=ALU.add)
                V.tensor_tensor(out=ssw[:, 16:20], in0=ssw[:, 16:20], in1=ssw[:, 12:16], op=ALU.add)
                return V.reciprocal(out=ssw[:, 20:24], in_=ssw[:, 16:20])
            P.op("dve", den, R=["p_b", "ssw2"], W=["ssw3"])
            bPT, kPT = pb()
            bPTb = bPT[:].bitcast(BF16)

            def ptr():
                r = None
                for h in range(4):
                    for part in range(2):
                        j_ = h * 2 + part
                        r = T.transpose(bPTb[:, j_ * 128:(j_ + 1) * 128], p_b[:, h, part * 128:(part + 1) * 128], ident_b[:])
                return r
            P.op("pe", ptr, R=["p_b", "ident_b"], W=[kPT])
            P.op("act", lambda: S.copy(out=pT_b[:, 0:4, :], in_=bPTb[:, 0:512].rearrange("p (j i) -> p j i", j=4)), R=[kPT], W=["pT_b0"])
            P.op("dve", lambda: V.tensor_copy(out=pT_b[:, 4:8, :], in_=bPTb[:, 512:1024].rearrange("p (j i) -> p j i", j=4)), R=[kPT], W=["pT_b1"])
            bOS, kOS = pb()

            def osw():
                r = None
                for h in range(4):
                    kvh = h // 2
                    for part, buf in ((0, v_pp[prv]), (1, v_pp[cur])):
                        r = T.matmul(bOS[:, h * 64:(h + 1) * 64], lhsT=pT_b[:, h * 2 + part, :], rhs=buf[:, kvh * 64:(kvh + 1) * 64],
                                     start=(part == 0), stop=(part == 1))
                return r
            P.op("pe", osw, R=["pT_b0", "pT_b1", "v_pp0", "v_pp1"], W=[kOS])
            P.op("dve", lambda: V.tensor_tensor(out=mix_tok[:, 768:1024].rearrange("p (h d) -> p h d", h=4), in0=_bc(ssw[:, 20:24].unsqueeze(2), [128, 4, 64]),
                                                in1=bOS[:, 0:256].rearrange("p (h d) -> p h d", h=4), op=ALU.mult), R=[kOS, "ssw3"], W=["mix_swa"])

            bMT, kMT = pb()
            bMTb = bMT[:].bitcast(BF16)

            def mtr():
                r = None
                for kc in range(8):
                    r = T.transpose(bMTb[:, kc * 128:(kc + 1) * 128], mix_tok[:, kc * 128:(kc + 1) * 128], ident_b[:])
                return r
            P.op("pe", mtr, R=["mix_ret", "mix_ssd", "mix_swa", "ident_b"], W=[kMT])
            P.op("act", lambda: S.copy(out=mixT, in_=bMTb[:, 0:1024].rearrange("p (k t) -> p k t", k=8)), R=[kMT], W=["mixT"])
            for half in range(2):
                bW, kW = pb()

                def wo(half=half, bW=bW):
                    r = None
                    for q in range(4):
                        fc = half * 4 + q
                        for kc in range(8):
                            r = T.matmul(bW[:, q * 128:(q + 1) * 128], lhsT=w_out[:, kc, fc * 128:(fc + 1) * 128], rhs=mixT[:, kc, :],
                                         start=(kc == 0), stop=(kc == 7))
                    return r
                P.op("pe", wo, R=["w_out", "mixT"], W=[kW])

                def res(half=half, bW=bW):
                    r = None
                    for q in range(4):
                        fc = half * 4 + q
                        r = V.scalar_tensor_tensor(out=xT[:, fc, Tn], in0=bW[:, q * 128:(q + 1) * 128], scalar=g1a[:, fc:fc + 1], in1=xT[:, fc, Tn],
                                                   op0=ALU.mult, op1=ALU.add)
                    return r
                P.op("dve", res, R=[kW, "der", ("xT", n, half)], W=[("xT", n, half)])
            ln_inplace(128, xk, xT[:, :, Tn], GA1, BA1, 128)

        if FSTOP[0] < L * 10 + 1:
            return
        full = False
        for n in range(-1, NCH):
            chunk(n)
        P.barrier()
        if FSTOP[0] < L * 10 + 2:
            return
        ex = Arena(arena_t, TAIL)
        ex.off = _off_tmp
        sto = ex.alloc([128, STW])
        g8 = ex.alloc([128, NR, STW])
        stin = ex.alloc([128, 3, STW])
        wss = ex.alloc([128, 3, 4])

        def pk():
            V.tensor_copy(out=sto[:, 0:128], in_=Sret.rearrange("p t e -> p (t e)"))
            V.tensor_copy(out=sto[:, 128:384], in_=Sssd)
            return V.tensor_copy(out=sto[:, 384:392], in_=totacc)
        P.op("dve", pk, R=["Sret", "Sssd", "totacc"], W=["sto"])
        P.dma(sp, bounce_s[L], sto, R=["sto"], W=["bounce_s%d" % L])
        P.coll("AllGather", gath_s[L], bounce_s[L], ALLC, R=["bounce_s%d" % L], W=["gath_s%d" % L])
        P.dma(sp, g8, gath_s[L].rearrange("(r p) w -> p r w", p=128), R=["gath_s%d" % L], W=["g8"])

        def ssel():
            r = None
            for s_ in range(3):
                V.tensor_scalar(out=stin[:, s_, :], in0=g8[:, 0, :], scalar1=selw[:, s_ * 8:s_ * 8 + 1], scalar2=None, op0=ALU.mult)
                for r_ in range(1, NR):
                    r = V.scalar_tensor_tensor(out=stin[:, s_, :], in0=g8[:, r_, :], scalar=selw[:, s_ * 8 + r_:s_ * 8 + r_ + 1], in1=stin[:, s_, :],
                                               op0=ALU.mult, op1=ALU.add)
            return r
        P.op("dve", ssel, R=["g8", "selw"], W=["stin"])


        def comb():
            V.tensor_tensor(out=Sret, in0=stin[:, 0, 0:128].rearrange("p (t e) -> p t e", t=2),
                            in1=_bc(wret[:, 0:2].unsqueeze(2), [128, 2, 64]), op=ALU.mult)
            for s_ in (1, 2):
                V.tensor_tensor(out=Sssd[:, 0:128].rearrange("p (t e) -> p t e", t=2), in0=stin[:, s_, 0:128].rearrange("p (t e) -> p t e", t=2),
                                in1=_bc(wret[:, 2 * s_:2 * s_ + 2].unsqueeze(2), [128, 2, 64]), op=ALU.mult)
                V.tensor_tensor(out=Sret, in0=Sret, in1=Sssd[:, 0:128].rearrange("p (t e) -> p t e", t=2), op=ALU.add)
            for g in range(2):
                pr = slice(g * 64, (g + 1) * 64)
                V.tensor_copy(out=wss[pr, 1, :], in_=stin[pr, 0, 384 + g * 4:384 + g * 4 + 4])
                V.tensor_tensor(out=wss[pr, 2, :], in0=stin[pr, 0, 384 + g * 4:384 + g * 4 + 4],
                                in1=stin[pr, 1, 384 + g * 4:384 + g * 4 + 4], op=ALU.add)
            return V.memset(wss[:, 0, :], 0.0)
        P.op("dve", comb, R=["stin", "cst"], W=["Sret", "Sssd", "wss"])
        P.op("act", lambda: S.activation(out=wss, in_=wss, func=AF.Exp), R=["wss"], W=["wss"])

        def comb2():
            V.tensor_tensor(out=Sssd.rearrange("p (r e) -> p r e", r=4), in0=stin[:, 0, 128:384].rearrange("p (r e) -> p r e", r=4),
                            in1=_bc(wss[:, 0, :].unsqueeze(2), [128, 4, 64]), op=ALU.mult)
            for s_ in (1, 2):
                V.tensor_tensor(out=stin[:, s_, 128:384].rearrange("p (r e) -> p r e", r=4), in0=stin[:, s_, 128:384].rearrange("p (r e) -> p r e", r=4),
                                in1=_bc(wss[:, s_, :].unsqueeze(2), [128, 4, 64]), op=ALU.mult)
                V.tensor_tensor(out=Sssd, in0=Sssd, in1=stin[:, s_, 128:384], op=ALU.add)
            V.tensor_copy(out=Sssd_b, in_=Sssd)
            return V.tensor_copy(out=Sret_b, in_=Sret)
        P.op("dve", comb2, R=["stin", "wss", "Sret", "Sssd"], W=["Sret", "Sssd", "stin"])
        P.barrier()
        if FSTOP[0] < L * 10 + 3:
            return
        P.op("dve", zmask, W=["qm", "BCm", "qT_s"])
        full = True
        SUB[0] = MSUB[0]
        for n in range(-1, min(NCH, MAINCH[0])):
            chunk(n)
        SUB[0] = 99
        if FSTOP[0] < L * 10 + 4:
            return

        P.barrier()
        ar = Arena(arena_t, LIM)
        hT = ar.alloc([128, 8, NTOK], BF16)
        aT = [ar.alloc([128, 4, 1024], BF16) for _ in range(2)]
        wgu = [ar.alloc([128, 8, 1024], BF16) for _ in range(2)]
        wdb = [ar.alloc([128, 4, D], BF16) for _ in range(2)]
        sgt = [ar.alloc([128, 512]) for _ in range(2)]
        evt = [ar.alloc([128, 512]) for _ in range(2)]
        if moe:
            gbc = [ar.alloc([128, 1024]) for _ in range(2)]
            rw_sb = ar.alloc([128, 8, 8])
            lgT = ar.alloc([128, 512])
            gatesT = ar.alloc([128, NTOK])
            lg = ar.alloc([128, NCH, 8])
            gts = ar.alloc([128, NCH, 8])
            e1 = ar.alloc([128, NCH, 8])
            e2 = ar.alloc([128, NCH, 8])
            l2 = ar.alloc([128, NCH, 8])
            tk = ar.alloc([128, 6, NCH])
            h2f = [ar.alloc([128, 512]) for _ in range(2)]
            P.dma(sp, rw_sb, rw_d, W=["rw_sb"])

        if moe:
            bG, kG = pb()
        for tg in range(4):
            Tg = slice(tg * 512, (tg + 1) * 512)
            xkeys = [("xT", n, hf_) for n in range(tg * 4, tg * 4 + 4) for hf_ in range(2)]
            if moe:
                bR, kR = pb()
            for fc in range(8):
                P.op("act", lambda fc=fc, Tg=Tg: S.activation(out=hT[:, fc, Tg], in_=xT[:, fc, Tg], func=AF.Identity, bias=B2[:, fc:fc + 1], scale=A2[:, fc:fc + 1]),
                     R=xkeys + ["der"], W=[("hT", tg)])
                if moe:
                    hb = h2f[fc % 2]
                    hbk = "h2f%d" % (fc % 2)
                    P.op("dve", lambda fc=fc, Tg=Tg, hb=hb: V.tensor_scalar(out=hb, in0=xT[:, fc, Tg], scalar1=A2[:, fc:fc + 1], scalar2=B2[:, fc:fc + 1], op0=ALU.mult, op1=ALU.add),
                         R=xkeys + ["der"], W=[hbk])
                    P.op("pe", lambda fc=fc, hb=hb, bR=bR: T.matmul(bR[0:8, 0:512], lhsT=rw_sb[:, fc, :], rhs=hb, start=(fc == 0), stop=(fc == 7)),
                         R=[hbk, "rw_sb"], W=[kR])
            if moe:
                P.op("act", lambda bR=bR: S.activation(out=lgT[0:8, 0:512], in_=bR[0:8, 0:512], func=AF.Identity, bias=rbias[0:8, 0:1], scale=1.0),
                     R=[kR, "misc"], W=["lgT"])

                def ltr(tg=tg):
                    r = None
                    for q in range(4):
                        n = tg * 4 + q
                        r = T.transpose(bG[:, n * 8:(n + 1) * 8], lgT[0:8, q * 128:(q + 1) * 128], ident[0:8, 0:8])
                    return r
                P.op("pe", ltr, R=["lgT", "cst"], W=[kG])
        if moe:

            def top2():
                V.tensor_copy(out=lg, in_=bG[:, 0:128].rearrange("p (n e) -> p n e", e=8))
                m1_, m2_, dlt, w1_, w2_ = tk[:, 0, :], tk[:, 1, :], tk[:, 2, :], tk[:, 3, :], tk[:, 4, :]
                V.tensor_reduce(out=m1_, in_=lg, axis=AX.X, op=ALU.max)
                V.tensor_tensor(out=e1, in0=lg, in1=_bc(m1_.unsqueeze(2), [128, NCH, 8]), op=ALU.is_equal)
                V.scalar_tensor_tensor(out=l2, in0=e1, scalar=-1e30, in1=lg, op0=ALU.mult, op1=ALU.add)
                V.tensor_reduce(out=m2_, in_=l2, axis=AX.X, op=ALU.max)
                V.tensor_tensor(out=e2, in0=l2, in1=_bc(m2_.unsqueeze(2), [128, NCH, 8]), op=ALU.is_equal)
                return V.tensor_tensor(out=dlt, in0=m2_, in1=m1_, op=ALU.subtract)
            P.op("dve", top2, R=[kG], W=["tk", "lg"])
            P.op("act", lambda: S.activation(out=tk[:, 2, :], in_=tk[:, 2, :], func=AF.Exp), R=["tk"], W=["tk"])

            def top2b():
                dlt, w1_, w2_ = tk[:, 2, :], tk[:, 3, :], tk[:, 4, :]
                V.tensor_scalar(out=w1_, in0=dlt, scalar1=1.0, scalar2=None, op0=ALU.add)
                V.reciprocal(out=w1_, in_=w1_)
                V.tensor_tensor(out=w2_, in0=dlt, in1=w1_, op=ALU.mult)
                V.tensor_tensor(out=e1, in0=e1, in1=_bc(w1_.unsqueeze(2), [128, NCH, 8]), op=ALU.mult)
                V.tensor_tensor(out=e2, in0=e2, in1=_bc(w2_.unsqueeze(2), [128, NCH, 8]), op=ALU.mult)
                return V.tensor_tensor(out=gts, in0=e1, in1=e2, op=ALU.add)
            P.op("dve", top2b, R=["tk", "lg"], W=["gts", "tk", "lg"])
            for tg in range(4):
                bG2, kG2 = pb()

                def gtr(tg=tg, bG2=bG2):
                    r = None
                    for q in range(4):
                        n = tg * 4 + q
                        r = T.transpose(bG2[0:8, q * 128:(q + 1) * 128], gts[:, n, :], ident)
                    return r
                P.op("pe", gtr, R=["gts", "cst"], W=[kG2])
                P.op("act", lambda tg=tg, bG2=bG2: S.copy(out=gatesT[0:8, tg * 512:(tg + 1) * 512], in_=bG2[0:8, 0:512]), R=[kG2], W=["gatesT"])

        nexp = NEXP if moe else 1
        dff = D_FFE if moe else D_FF
        pieces = []
        o = 0
        while o < dff:
            w = min(512, dff - o)
            pieces.append((o, w))
            o += w
        pi = 0
        items = [(e_, o_, w_) for e_ in range(min(nexp, NEXPRUN[0])) for (o_, w_) in pieces]

        def wload(idx):
            e_, o_, w_ = items[idx]
            wb_ = idx % 2
            P.dma("pool", wgu[wb_][:, :, 0:w_], wg_d[e_].rearrange("(c p) n -> p c n", p=128)[:, :, o_:o_ + w_], W=["wgu%d" % wb_])
            P.dma("pool", wgu[wb_][:, :, 512:512 + w_], wu_d[e_].rearrange("(c p) n -> p c n", p=128)[:, :, o_:o_ + w_], W=["wgu%d" % wb_])
            P.dma("pool", wdb[wb_][:, 0:w_ // 128, :], wd_d[e_][o_:o_ + w_, :].rearrange("(c p) n -> p c n", p=128), W=["wd%d" % wb_])
        if items:
            wload(0)
        for e in range(min(nexp, NEXPRUN[0])):
            if moe:
                for half in range(2):
                    for q in range(2):
                        bg_, kg_ = pb()
                        P.op("pe", lambda e=e, half=half, q=q, bg_=bg_: T.matmul(bg_[:, 0:512], lhsT=sel8[0:8, e * 128:(e + 1) * 128],
                                                                                  rhs=gatesT[0:8, half * 1024 + q * 512:half * 1024 + (q + 1) * 512], start=True, stop=True),
                             R=["gatesT", "cst"], W=[kg_])
                        P.op("act", lambda half=half, q=q, bg_=bg_: S.copy(out=gbc[half][:, q * 512:(q + 1) * 512], in_=bg_[:, 0:512]), R=[kg_], W=["gbc%d" % half])
            for (o, w) in pieces:
                nb = w // 128
                wb = pi % 2
                kwg, kwd = "wgu%d" % wb, "wd%d" % wb
                if pi + 1 < len(items):
                    wload(pi + 1)
                for half in range(2):
                    for blk in range(nb):
                        for q in range(2):
                            tg = half * 2 + q
                            Tg = slice(tg * 512, (tg + 1) * 512)
                            bg_, kg_ = pb()
                            bu_, ku_ = pb()

                            def gu(blk=blk, Tg=Tg, bg_=bg_, bu_=bu_, wb=wb):
                                r = None
                                for kc in range(8):
                                    T.matmul(bg_[:, 0:512], lhsT=wgu[wb][:, kc, blk * 128:(blk + 1) * 128], rhs=hT[:, kc, Tg], start=(kc == 0), stop=(kc == 7))
                                for kc in range(8):
                                    r = T.matmul(bu_[:, 0:512], lhsT=wgu[wb][:, kc, 512 + blk * 128:512 + (blk + 1) * 128], rhs=hT[:, kc, Tg], start=(kc == 0), stop=(kc == 7))
                                return r
                            P.op("pe", gu, R=[kwg, ("hT", tg)], W=[kg_, ku_])
                            sb_ = sgt[(blk * 2 + q) % 2]
                            sk_ = "sgt%d" % ((blk * 2 + q) % 2)
                            P.op("act", lambda bg_=bg_, sb_=sb_: S.activation(out=sb_, in_=bg_[:, 0:512], func=AF.Silu), R=[kg_], W=[sk_])
                            P.op("dve", lambda half=half, blk=blk, q=q, bu_=bu_, sb_=sb_: V.tensor_tensor(out=aT[half][:, blk, q * 512:(q + 1) * 512], in0=bu_[:, 0:512], in1=sb_, op=ALU.mult),
                                 R=[ku_, sk_], W=[("aT", half, blk, q)])
                for half in range(2):
                    for fc in range(8):
                        for q in range(2):
                            tg = half * 2 + q
                            Tg = slice(tg * 512, (tg + 1) * 512)
                            bo_, ko_ = pb()

                            def dn(half=half, fc=fc, q=q, bo_=bo_, wb=wb, nb=nb):
                                r = None
                                for blk in range(nb):
                                    r = T.matmul(bo_[:, 0:512], lhsT=wdb[wb][:, blk, fc * 128:(fc + 1) * 128], rhs=aT[half][:, blk, q * 512:(q + 1) * 512],
                                                 start=(blk == 0), stop=(blk == nb - 1))
                                return r
                            P.op("pe", dn, R=[kwd] + [("aT", half, blk, q) for blk in range(nb)], W=[ko_])
                            eb = evt[(fc * 2 + q) % 2]
                            ek = "evt%d" % ((fc * 2 + q) % 2)
                            if moe:
                                P.op("dve", lambda fc=fc, half=half, q=q, bo_=bo_, eb=eb: V.scalar_tensor_tensor(out=eb, in0=bo_[:, 0:512], scalar=g1f[:, fc:fc + 1],
                                                                                                              in1=gbc[half][:, q * 512:(q + 1) * 512], op0=ALU.mult, op1=ALU.mult),
                                     R=[ko_, "der", "gbc%d" % half], W=[ek])
                            else:
                                P.op("dve", lambda fc=fc, bo_=bo_, eb=eb: V.tensor_scalar(out=eb, in0=bo_[:, 0:512], scalar1=g1f[:, fc:fc + 1], scalar2=None, op0=ALU.mult),
                                     R=[ko_, "der"], W=[ek])
                            xkeys = [("xT", n, fc // 4) for n in range(tg * 4, tg * 4 + 4)]
                            P.op("pool", lambda fc=fc, Tg=Tg, eb=eb: G.tensor_tensor(out=xT[:, fc, Tg], in0=xT[:, fc, Tg], in1=eb, op=ALU.add),
                                 R=[ek] + xkeys, W=xkeys)
                pi += 1

        P.barrier()
        ar = Arena(arena_t, LIM)
        sq = [ar.alloc([128, 512]) for _ in range(2)]
        lnst = ar.alloc([128, 4, 512])
        lnk = ["lnst"]
        for tg in range(4):
            Tg = slice(tg * 512, (tg + 1) * 512)
            xkeys = [("xT", n, hf_) for n in range(tg * 4, tg * 4 + 4) for hf_ in range(2)]
            ln_inplace(512, xkeys, xT[:, :, Tg], GA2, BA2, 512)

    for L_ in range(DEPTH):
        layer(L_)
    ar = Arena(arena_t, TAIL)
    ar.off = 3072
    xo = [ar.alloc([128, D]) for _ in range(2)]
    for n in range(NCH):
        buf = xo[n % 2]
        bk = "xo%d" % (n % 2)
        for half in range(2):
            bank, bkey = pb()

            def tr2(n=n, half=half, bank=bank):
                r = None
                for q in range(4):
                    fc = half * 4 + q
                    r = T.transpose(bank[:, q * 128:(q + 1) * 128], xT[:, fc, n * 128:(n + 1) * 128], ident)
                return r
            P.op("pe", tr2, R=[("xT", n, 0), ("xT", n, 1), "cst"], W=[bkey])
            P.op("act", lambda half=half, bank=bank, buf=buf: S.copy(out=buf[:, half * 512:(half + 1) * 512], in_=bank[:, 0:512]), R=[bkey], W=[bk + "_%d" % half])
        P.dma(sp, xo_d[n * 128:(n + 1) * 128, :], buf, R=[bk + "_0", bk + "_1"], is_output=True)
    return P.emit()

C_ID, C_TRI, C_MGT, C_ONE = 0, 128, 256, 384
C_DQ, C_DK, C_GC, C_INV = 512, 516, 520, 522
C_MADD = 554
C_WRET = 810
C_M0 = 816
C_SEL = 818
CW = C_SEL + 8 * 128

M_AIN, M_BIN, M_G1A, M_G1F = 0, 8, 16, 24
M_CW, M_CB, M_HV, M_HM = 32, 56, 62, 63
M_MOD, M_LN, M_C, M_BADA, M_DER, M_RB = 64, 160, 192, 200, 296, 360
MW = 368

R_DTB, R_ALOG, R_DSK, R_NW, R_SINK, R_RB = 0, 8, 16, 24, 536, 540
RW = 668


def _t5_bucket(dist):
    exact = 16
    df = np.maximum(dist, 1).astype(np.float32)
    large = exact + (np.log(df / exact) / math.log(128 / exact) * (32 - exact)).astype(np.int32)
    large = np.minimum(large, 31)
    return np.where(dist < exact, dist, large)


def make_consts():
    c = np.zeros((128, CW), np.float32)
    i = np.arange(128)
    c[:, C_ID:C_ID + 128] = np.eye(128, dtype=np.float32)
    c[:, C_TRI:C_TRI + 128] = (i[:, None] <= i[None, :]).astype(np.float32)
    c[:, C_MGT:C_MGT + 128] = (i[:, None] > i[None, :]).astype(np.float32)
    c[:, C_ONE:C_ONE + 128] = 1.0
    lg = np.log(1.0 - 2.0 ** (-5.0 - np.arange(4, dtype=np.float64)))
    c[:, C_DQ:C_DQ + 4] = np.exp(lg[None, :] * (i[:, None] + 1.0))
    c[:, C_DK:C_DK + 4] = np.exp(-lg[None, :] * (i[:, None] + 1.0)) * (64 ** -0.5)
    for t in range(2):
        for hf in range(2):
            c[hf * 64:(hf + 1) * 64, C_GC + t] = np.exp(lg[2 * t + hf] * 128.0)
    c[:, C_INV:C_INV + 32] = np.exp(-math.log(10000.0) * np.arange(32, dtype=np.float32) / 32)[None, :]
    jj = np.arange(256)
    dist = i[:, None] + 128 - jj[None, :]
    valid = (dist >= 0) & (dist < 128)
    c[:, C_MADD:C_MADD + 256] = np.where(valid, 0.0, NEG)
    for s in range(3):
        for t in range(2):
            for hf in range(2):
                c[hf * 64:(hf + 1) * 64, C_WRET + s * 2 + t] = np.exp(lg[2 * t + hf] * 2048.0 * s)
    c[0:64, C_M0] = 1.0
    c[64:128, C_M0 + 1] = 1.0
    for e in range(8):
        c[e, C_SEL + e * 128:C_SEL + (e + 1) * 128] = 1.0
    bk = _t5_bucket(np.clip(dist, 0, 127))
    eoh = np.zeros((128, 256, 32), np.float32)
    ii, jj2 = np.meshgrid(i, jj, indexing="ij")
    eoh[ii, jj2, bk] = 1.0
    return c, eoh.reshape(128, 256 * 32)


def col(v):
    v = np.asarray(v, np.float32)
    return np.ascontiguousarray(v.reshape(-1, 128).T)


_PROG_CACHE = {}


def kernel(x, c, positions, rel_bias, w_ada, b_ada, w_in, w_out, conv_w, conv_b,
           dt_bias, a_log, d_skip, ssd_norm_w, sinks, ln_g, ln_b,
           ffn_w_gate, ffn_w_up, ffn_w_down, router_w, router_b,
           expert_w_gate, expert_w_up, expert_w_down):
    f = lambda a: np.ascontiguousarray(np.asarray(a, dtype=np.float32))
    x = f(x)
    cst, eoh = make_consts()
    positions = np.asarray(positions)
    shared = {
        "cst": cst, "eoh": eoh, "relb": f(rel_bias).reshape(-1),
        "w_in": f(w_in), "w_out": f(w_out), "w_ada": f(w_ada),
        "wg0": f(ffn_w_gate), "wu0": f(ffn_w_up), "wd0": f(ffn_w_down),
        "wg1": f(expert_w_gate[0]), "wu1": f(expert_w_up[0]), "wd1": f(expert_w_down[0]),
        "rw": np.ascontiguousarray(f(router_w[0]).reshape(8, 128, 8).transpose(1, 0, 2)),
    }
    rowp = np.zeros((DEPTH, RW), np.float32)
    for L in range(DEPTH):
        rowp[L, R_DTB:R_DTB + 8] = f(dt_bias[L])
        rowp[L, R_ALOG:R_ALOG + 8] = f(a_log[L])
        rowp[L, R_DSK:R_DSK + 8] = f(d_skip[L])
        rowp[L, R_NW:R_NW + 512] = f(ssd_norm_w[L])
        rowp[L, R_SINK:R_SINK + 4] = f(sinks[L])
    maps = []
    for core in range(8):
        b, sq_ = core // 4, core % 4
        t0 = sq_ * NTOK
        xin = np.zeros((NTOK + 128, D), np.float32)
        xin[128:] = x[b, t0:t0 + NTOK]
        if sq_ > 0:
            xin[:128] = x[b, t0 - 128:t0]
        misc = np.zeros((DEPTH, 128, MW), np.float32)
        for L in range(DEPTH):
            m = misc[L]
            m[:, M_CW:M_CW + 24] = np.ascontiguousarray(f(conv_w[L]).reshape(4, 6, 128).transpose(2, 1, 0)).reshape(128, 24)
            m[:, M_CB:M_CB + 6] = col(conv_b[L])
            m[:, M_HV] = 1.0 if sq_ > 0 else 0.0
            m[:, M_HM] = 0.0 if sq_ > 0 else NEG
            m[:, M_LN:M_LN + 8] = col(ln_g[L, 0])
            m[:, M_LN + 8:M_LN + 16] = col(ln_b[L, 0])
            m[:, M_LN + 16:M_LN + 24] = col(ln_g[L, 1])
            m[:, M_LN + 24:M_LN + 32] = col(ln_b[L, 1])
            m[:, M_C:M_C + 8] = col(c[b])
            m[:, M_BADA:M_BADA + 48] = col(b_ada[L])
            if L % 2 == 1:
                m[0:8, M_RB] = f(router_b[L // 2])
        selw = np.zeros((128, 32), np.float32)
        for s in range(3):
            if sq_ - 1 - s >= 0:
                selw[:, s * 8 + (sq_ - 1 - s)] = 1.0
        pos = np.ascontiguousarray(positions[b, t0:t0 + NTOK].astype(np.int32).reshape(NCH, 128).T)
        mm = dict(shared)
        mm.update({"xin": xin, "pos": pos, "misc": misc, "rowp": rowp, "selw": selw})
        maps.append(mm)
    FSTOP[0] = 99 if AONLY[0] else 13.5
    if "f" not in _PROG_CACHE:
        _PROG_CACHE["f"] = build_fused()
    mapsA = [{k_: v_ for k_, v_ in m_.items() if k_ in DECL_IN} for m_ in maps]
    res = run_bass_kernel_spmd(_PROG_CACHE["f"], mapsA, core_ids=list(range(8)))
    if AONLY[0]:
        out = np.zeros_like(x)
        for core in range(8):
            b, sq_ = core // 4, core % 4
            out[b, sq_ * NTOK:(sq_ + 1) * NTOK] = np.asarray(res.results[core]["xout"], np.float32)
        return out
    L = DEPTH - 1
    i = L // 2
    wada_l = f(w_ada[L:L + 1])
    w_in_l, w_out_l = f(w_in[L]), f(w_out[L])
    wg_l, wu_l, wd_l = f(expert_w_gate[i]), f(expert_w_up[i]), f(expert_w_down[i])
    rw_l = np.ascontiguousarray(f(router_w[i]).reshape(8, 128, 8).transpose(1, 0, 2))
    rowp1 = np.zeros((RW,), np.float32)
    rowp1[:] = rowp[L]
    rowp1[R_RB:R_RB + 128] = f(rel_bias).reshape(-1)
    st_in = np.zeros((3, 128, STW), np.float32)
    maps2 = []
    for core in range(8):
        xin = np.zeros((NTOK + 128, D), np.float32)
        xin[128:] = np.asarray(res.results[core]["xout"], np.float32)
        mA = maps[core]
        maps2.append({"xin": xin, "pos": mA["pos"], "cst": cst, "misc": np.ascontiguousarray(mA["misc"][L]), "rowp": rowp1,
                      "w_in": w_in_l, "w_ada": wada_l, "st_in": st_in, "eoh": eoh, "w_out": w_out_l,
                      "wg": wg_l, "wu": wu_l, "wd": wd_l, "rw": rw_l})
    if "b" not in _PROG_CACHE:
        _PROG_CACHE["b"] = build("ffn", L)
    res2 = run_bass_kernel_spmd(_PROG_CACHE["b"], maps2, core_ids=list(range(8)))
    out = np.zeros_like(x)
    for core in range(8):
        b, sq_ = core // 4, core % 4
        out[b, sq_ * NTOK:(sq_ + 1) * NTOK] = np.asarray(res2.results[core]["xout"], np.float32)
    return out
```
